# Optimizing a Trainium2 kernel written in Bass

```python
import math
import jax, jax.numpy as jnp
from jax import lax
import numpy as np

D_MODEL = 1024
BATCH = 8
SEQ = 4096
DEPTH = 4

HGRN_HEADS = 8
HGRN_DK = 128
HGRN_DV = D_MODEL // HGRN_HEADS
D_HGRN_K = HGRN_HEADS * HGRN_DK
D_HGRN_V = HGRN_HEADS * HGRN_DV
MLSTM_HEADS = 4
D_MLSTM = D_MODEL
MLSTM_DH = D_MLSTM // MLSTM_HEADS
MLSTM_CONV = 4
QKV_BLOCK = 4
N_QKV_BLOCKS = D_MLSTM // QKV_BLOCK
CHUNK = 64
IN_SIZES = (D_HGRN_K, D_HGRN_K, D_HGRN_V, D_HGRN_V, D_MLSTM, D_MLSTM, D_MODEL, D_MODEL)
D_IN = sum(IN_SIZES)
IN_SPLITS = tuple(int(s) for s in np.cumsum(IN_SIZES)[:-1])
D_FF = 2816
N_EXPERTS = 8
TOP_K = 2
D_FF_EXPERT = 3584
N_DENSE = (DEPTH + 1) // 2
N_MOE = DEPTH // 2
EPS = 1e-6
NEG = -1e30

kernel_name = 'hybrid_hgrn2_mlstm_moe_adaln'


def rmsnorm(x, g):
    xf = x.astype(jnp.float32)
    y = xf * lax.rsqrt(jnp.mean(xf * xf, axis=-1, keepdims=True) + EPS)
    return (y * g).astype(x.dtype)


def head_rmsnorm(o):
    return o * lax.rsqrt(jnp.mean(o * o, axis=-1, keepdims=True) + EPS)


def head_layernorm(o):
    mu = jnp.mean(o, axis=-1, keepdims=True)
    oc = o - mu
    return oc * lax.rsqrt(jnp.mean(oc * oc, axis=-1, keepdims=True) + EPS)


def to_chunks(t, heads):
    b, s, w = t.shape
    return t.reshape(b, s // CHUNK, CHUNK, heads, w // heads).transpose(1, 0, 3, 2, 4)


def gate_chunks(t):
    b, s, h = t.shape
    return t.reshape(b, s // CHUNK, CHUNK, h).transpose(1, 0, 3, 2)


def from_chunks(o):
    nc, b, h, l, d = o.shape
    return o.transpose(1, 0, 3, 2, 4).reshape(b, nc * l, h, d)


def hgrn_lower_bounds(lb_raw):
    p = jax.nn.softmax(lb_raw.astype(jnp.float32), axis=0)
    return jnp.cumsum(p, axis=0) - p[0:1]


def hgrn2_chunked(q, k, v, log_f):
    L = q.shape[-2]
    causal = jnp.tril(jnp.ones((L, L), dtype=bool))

    def step(state, inp):
        qc, kc, vc, lfc = inp
        b = jnp.cumsum(lfc, axis=-2)
        diff = b[..., :, None, :] - b[..., None, :, :]
        decay = jnp.exp(jnp.where(causal[:, :, None], diff, NEG))
        scores = jnp.einsum('bhtd,bhtsd,bhsd->bhts', qc, decay, kc)
        o = (jnp.einsum('bhts,bhsv->bhtv', scores, vc)
             + jnp.einsum('bhtd,bhdv->bhtv', qc * jnp.exp(b), state))
        b_last = b[..., -1:, :]
        state = (jnp.exp(b_last[..., 0, :])[..., None] * state
                 + jnp.einsum('bhsd,bhsv->bhdv', kc * jnp.exp(b_last - b), vc))
        return state, o

    nc, bsz, heads, l, dk = q.shape
    init = jnp.zeros((bsz, heads, dk, v.shape[-1]), jnp.float32)
    _, o = lax.scan(step, init, (q, k, v, log_f))
    return o


def hgrn2_branch(q, f_pre, i, g, lb, gnorm):
    f32 = jnp.float32
    bsz, s, _ = q.shape
    q = jax.nn.silu(q.astype(f32))
    fp = f_pre.astype(f32)
    lb = lb.astype(f32)
    f = lb + (1.0 - lb) * jax.nn.sigmoid(fp)
    log_f = jnp.log(f)
    k = (1.0 - lb) * jax.nn.sigmoid(-fp)
    o = hgrn2_chunked(to_chunks(q, HGRN_HEADS), to_chunks(k, HGRN_HEADS),
                      to_chunks(i.astype(f32), HGRN_HEADS), to_chunks(log_f, HGRN_HEADS))
    o = head_rmsnorm(from_chunks(o)).reshape(bsz, s, D_HGRN_V)
    return o * gnorm * jax.nn.silu(g.astype(f32))


def causal_conv(x, w, b):
    kw = w.shape[0]
    y = lax.conv_general_dilated(x, w[:, None, :], window_strides=(1,), padding=[(kw - 1, 0)],
                                 dimension_numbers=('NWC', 'WIO', 'NWC'),
                                 feature_group_count=x.shape[-1])
    return y + b


def blockdiag(x, w):
    bsz, s, _ = x.shape
    xb = x.reshape(bsz, s, N_QKV_BLOCKS, QKV_BLOCK)
    return jnp.einsum('bsni,nio->bsno', xb, w).reshape(bsz, s, D_MLSTM)


def mlstm_chunked(q, k, v, log_i, log_f):
    L = q.shape[-2]
    causal = jnp.tril(jnp.ones((L, L), dtype=bool))

    def step(carry, inp):
        C, n, m = carry
        qc, kc, vc, lic, lfc = inp
        b = jnp.cumsum(lfc, axis=-1)
        d = jnp.where(causal, b[..., :, None] - b[..., None, :] + lic[..., None, :], NEG)
        m_inter = b + m[..., None]
        m_t = jnp.maximum(m_inter, jnp.max(d, axis=-1))
        s = jnp.einsum('bhtd,bhsd->bhts', qc, kc) * jnp.exp(d - m_t[..., None])
        w_inter = jnp.exp(m_inter - m_t)
        num = (jnp.einsum('bhts,bhsv->bhtv', s, vc)
               + w_inter[..., None] * jnp.einsum('bhtd,bhdv->bhtv', qc, C))
        den = jnp.sum(s, axis=-1) + w_inter * jnp.einsum('bhtd,bhd->bht', qc, n)
        h = num / jnp.maximum(jnp.abs(den), jnp.exp(-m_t))[..., None]
        b_last = b[..., -1]
        g = b_last[..., None] - b + lic
        m_new = jnp.maximum(b_last + m, jnp.max(g, axis=-1))
        w_s = jnp.exp(g - m_new[..., None])
        decay = jnp.exp(b_last + m - m_new)
        C = decay[..., None, None] * C + jnp.einsum('bhs,bhsd,bhsv->bhdv', w_s, kc, vc)
        n = decay[..., None] * n + jnp.einsum('bhs,bhsd->bhd', w_s, kc)
        return (C, n, m_new), h

    nc, bsz, heads, l, dk = q.shape
    init = (jnp.zeros((bsz, heads, dk, v.shape[-1]), jnp.float32),
            jnp.zeros((bsz, heads, dk), jnp.float32),
            jnp.zeros((bsz, heads), jnp.float32))
    _, h = lax.scan(step, init, (q, k, v, log_i, log_f))
    return h


def mlstm_branch(xm, o_pre, conv_w, conv_b, wq, wk, wv, w_ig, b_ig, w_fg, b_fg, gnorm, skip):
    f32 = jnp.float32
    bsz, s, _ = xm.shape
    xm = xm.astype(f32)
    xc = jax.nn.silu(causal_conv(xm, conv_w.astype(f32), conv_b.astype(f32)))
    q = blockdiag(xc, wq)
    k = blockdiag(xc, wk)
    v = blockdiag(xm, wv)
    qkv = jnp.concatenate([q, k, v], axis=-1)
    log_i = (qkv @ w_ig + b_ig).astype(f32)
    log_f = jax.nn.log_sigmoid((qkv @ w_fg + b_fg).astype(f32))
    hc = mlstm_chunked(to_chunks(q, MLSTM_HEADS), to_chunks(k * (MLSTM_DH ** -0.5), MLSTM_HEADS),
                       to_chunks(v, MLSTM_HEADS), gate_chunks(log_i), gate_chunks(log_f))
    h = head_layernorm(from_chunks(hc)).reshape(bsz, s, D_MLSTM) * gnorm + skip * xc
    return jax.nn.sigmoid(o_pre.astype(f32)) * h


def hybrid_mixer(h, lb, w_in, hgrn_gnorm, conv_w, conv_b, wq, wk, wv, w_ig, b_ig, w_fg, b_fg,
                 mlstm_gnorm, skip, w_proj_a, w_proj_b, w_out):
    proj = h @ w_in
    q_a, f_a, i_a, g_a, xm_b, o_b, gate_a, gate_b = jnp.split(proj, IN_SPLITS, axis=-1)
    y_a = hgrn2_branch(q_a, f_a, i_a, g_a, lb, hgrn_gnorm) @ w_proj_a
    y_b = mlstm_branch(xm_b, o_b, conv_w, conv_b, wq, wk, wv, w_ig, b_ig, w_fg, b_fg,
                       mlstm_gnorm, skip) @ w_proj_b
    mixed = (jax.nn.sigmoid(gate_a.astype(jnp.float32)) * y_a
             + jax.nn.sigmoid(gate_b.astype(jnp.float32)) * y_b)
    return (mixed @ w_out).astype(h.dtype)


def swiglu(h, w1, w3, w2):
    return (jax.nn.silu(h @ w1) * (h @ w3)) @ w2


def moe_swiglu(h, router, w1, w3, w2):
    logits = (h @ router).astype(jnp.float32)
    top_v, top_i = lax.top_k(logits, TOP_K)
    top_w = jax.nn.softmax(top_v, axis=-1)
    gates = jnp.sum(jax.nn.one_hot(top_i, N_EXPERTS, dtype=jnp.float32) * top_w[..., None], axis=-2)
    out = jnp.zeros(h.shape, jnp.float32)
    for e in range(N_EXPERTS):
        out = out + gates[..., e:e + 1] * swiglu(h, w1[e], w3[e], w2[e])
    return out.astype(h.dtype)


def setup_inputs(seed: int = 0) -> dict:
    key = jax.random.key(seed)
    ks = iter(jax.random.split(key, 40))
    f32 = jnp.float32

    def nrm(shape, scale):
        return scale * jax.random.normal(next(ks), shape, f32)

    D = D_MODEL
    return {
        'x': nrm((BATCH, SEQ, D), 1.0),
        'c': nrm((BATCH, D), 1.0),
        'w_ada': nrm((DEPTH, D, 6 * D), 0.3 * D ** -0.5),
        'b_ada': nrm((DEPTH, 6 * D), 0.02),
        'g_pre_mix': 1.0 + nrm((DEPTH, D), 0.05),
        'g_post_mix': 1.0 + nrm((DEPTH, D), 0.05),
        'g_pre_ffn': 1.0 + nrm((DEPTH, D), 0.05),
        'g_post_ffn': 1.0 + nrm((DEPTH, D), 0.05),
        'w_in': nrm((DEPTH, D, D_IN), D ** -0.5),
        'hgrn_lb': 1.0 + nrm((DEPTH, D_HGRN_K), 0.5),
        'hgrn_gnorm': 1.0 + nrm((DEPTH, D_HGRN_V), 0.05),
        'mlstm_conv_w': nrm((DEPTH, MLSTM_CONV, D_MLSTM), MLSTM_CONV ** -0.5),
        'mlstm_conv_b': nrm((DEPTH, D_MLSTM), 0.02),
        'mlstm_wq': nrm((DEPTH, N_QKV_BLOCKS, QKV_BLOCK, QKV_BLOCK), QKV_BLOCK ** -0.5),
        'mlstm_wk': nrm((DEPTH, N_QKV_BLOCKS, QKV_BLOCK, QKV_BLOCK), QKV_BLOCK ** -0.5),
        'mlstm_wv': nrm((DEPTH, N_QKV_BLOCKS, QKV_BLOCK, QKV_BLOCK), QKV_BLOCK ** -0.5),
        'mlstm_w_ig': nrm((DEPTH, 3 * D_MLSTM, MLSTM_HEADS), 0.1 * (3 * D_MLSTM) ** -0.5),
        'mlstm_b_ig': nrm((DEPTH, MLSTM_HEADS), 0.1),
        'mlstm_w_fg': nrm((DEPTH, 3 * D_MLSTM, MLSTM_HEADS), 0.1 * (3 * D_MLSTM) ** -0.5),
        'mlstm_b_fg': jnp.linspace(3.0, 6.0, MLSTM_HEADS, dtype=f32)[None, :] + nrm((DEPTH, MLSTM_HEADS), 0.1),
        'mlstm_gnorm': 1.0 + nrm((DEPTH, D_MLSTM), 0.05),
        'mlstm_skip': 1.0 + nrm((DEPTH, D_MLSTM), 0.05),
        'w_proj_a': nrm((DEPTH, D_HGRN_V, D), D_HGRN_V ** -0.5),
        'w_proj_b': nrm((DEPTH, D_MLSTM, D), D_MLSTM ** -0.5),
        'w_out': nrm((DEPTH, D, D), D ** -0.5),
        'ffn_w1': nrm((N_DENSE, D, D_FF), D ** -0.5),
        'ffn_w3': nrm((N_DENSE, D, D_FF), D ** -0.5),
        'ffn_w2': nrm((N_DENSE, D_FF, D), D_FF ** -0.5),
        'moe_router': nrm((N_MOE, D, N_EXPERTS), D ** -0.5),
        'moe_w1': nrm((N_MOE, N_EXPERTS, D, D_FF_EXPERT), D ** -0.5),
        'moe_w3': nrm((N_MOE, N_EXPERTS, D, D_FF_EXPERT), D ** -0.5),
        'moe_w2': nrm((N_MOE, N_EXPERTS, D_FF_EXPERT, D), D_FF_EXPERT ** -0.5),
    }


def reference(x, c, w_ada, b_ada, g_pre_mix, g_post_mix, g_pre_ffn, g_post_ffn, w_in, hgrn_lb,
              hgrn_gnorm, mlstm_conv_w, mlstm_conv_b, mlstm_wq, mlstm_wk, mlstm_wv, mlstm_w_ig,
              mlstm_b_ig, mlstm_w_fg, mlstm_b_fg, mlstm_gnorm, mlstm_skip, w_proj_a, w_proj_b, w_out,
              ffn_w1, ffn_w3, ffn_w2, moe_router, moe_w1, moe_w3, moe_w2):
    lower_bounds = hgrn_lower_bounds(hgrn_lb)
    c_act = jax.nn.silu(c)
    for l in range(DEPTH):
        mod = c_act @ w_ada[l] + b_ada[l]
        sh_m, sc_m, gt_m, sh_f, sc_f, gt_f = [m[:, None, :] for m in jnp.split(mod, 6, axis=-1)]
        h = rmsnorm(x, g_pre_mix[l]) * (1.0 + sc_m) + sh_m
        y = hybrid_mixer(h, lower_bounds[l], w_in[l], hgrn_gnorm[l], mlstm_conv_w[l], mlstm_conv_b[l],
                         mlstm_wq[l], mlstm_wk[l], mlstm_wv[l], mlstm_w_ig[l], mlstm_b_ig[l],
                         mlstm_w_fg[l], mlstm_b_fg[l], mlstm_gnorm[l], mlstm_skip[l],
                         w_proj_a[l], w_proj_b[l], w_out[l])
        x = x + (gt_m * rmsnorm(y, g_post_mix[l])).astype(x.dtype)
        h = rmsnorm(x, g_pre_ffn[l]) * (1.0 + sc_f) + sh_f
        j = l // 2
        if l % 2 == 0:
            y = swiglu(h, ffn_w1[j], ffn_w3[j], ffn_w2[j]).astype(x.dtype)
        else:
            y = moe_swiglu(h, moe_router[j], moe_w1[j], moe_w3[j], moe_w2[j])
        x = x + (gt_f * rmsnorm(y, g_post_ffn[l])).astype(x.dtype)
    return x
```

```python
import contextlib
import types
import numpy as np
import concourse.bass as bass
import concourse.mybir as mybir
from concourse.bass_utils import run_bass_kernel_spmd

F32 = mybir.dt.float32
BF16 = mybir.dt.bfloat16
AF = mybir.ActivationFunctionType
ALU = mybir.AluOpType
AX = mybir.AxisListType

SEM_CAP = 30000
D = 1024
SEQ = 4096
DEPTH = 4
NCORES = 8
TT = 256
TF = 512
EPS = 1e-6
NV = 10
DEBUG = False


class Dep:
    __slots__ = ("name", "last_w", "readers")

    def __init__(self, name):
        self.name = name
        self.last_w = None
        self.readers = []


class Op:
    __slots__ = ("eng", "fn", "deps", "is_dma", "key", "needs_inc", "sem", "val")

    def __init__(self, eng, fn, is_dma=False, key=None):
        self.eng = eng
        self.fn = fn
        self.deps = []
        self.is_dma = is_dma
        self.key = key
        self.needs_inc = False
        self.sem = None
        self.val = 0


class Sched:
    ENGS = ("pe", "act", "dve", "pool", "sp")

    def __init__(self, nc):
        self.nc = nc
        self.ops = {e: [] for e in self.ENGS}
        self.all_ops = []
        self.deps = {}
        self.stack = contextlib.ExitStack()
        self.fence = []
        self.passed = {e: True for e in self.ENGS}

    def sb(self, name, shape, dtype=F32):
        return self.stack.enter_context(self.nc.sbuf_tensor("sb_" + name, list(shape), dtype))

    def ps(self, name, shape, dtype=F32):
        return self.stack.enter_context(self.nc.psum_tensor("ps_" + name, list(shape), dtype))

    def _D(self, x):
        d = self.deps.get(x)
        if d is None:
            d = self.deps[x] = Dep(x)
        return d

    def barrier(self):
        self.fence = [self.ops[e][-1] for e in self.ENGS if self.ops[e]]
        self.passed = {e: False for e in self.ENGS}

    limit = None

    def _record(self, op, reads, writes):
        if self.limit is not None and len(self.all_ops) >= self.limit:
            return op
        rr, ww = [], []
        for r in reads:
            if len(r) >= 2 and r[0] == "P" and r[1].isdigit():
                ww.append(r[:2])
            else:
                rr.append(r)
        for w in writes:
            if len(w) >= 2 and w[0] == "P" and w[1].isdigit():
                ww.append(w[:2])
            else:
                ww.append(w)
        reads, writes = rr, list(dict.fromkeys(ww))
        deps = []
        if not self.passed[op.eng]:
            deps.extend(self.fence)
            self.passed[op.eng] = True
        for r in reads:
            r = self._D(r)
            if r.last_w is not None:
                deps.append(r.last_w)
        for w in writes:
            w = self._D(w)
            if w.last_w is not None:
                deps.append(w.last_w)
            deps.extend(w.readers)
        seen = set()
        for d in deps:
            if d is op or id(d) in seen:
                continue
            seen.add(id(d))
            op.deps.append(d)
        for r in reads:
            self._D(r).readers.append(op)
        for w in writes:
            w = self._D(w)
            w.last_w = op
            w.readers = []
        self.ops[op.eng].append(op)
        self.all_ops.append(op)
        return op

    @staticmethod
    def _freeze(fn):
        if fn.__closure__ is None:
            return fn
        cells = []
        for c in fn.__closure__:
            try:
                cells.append(types.CellType(c.cell_contents))
            except ValueError:
                cells.append(c)
        return types.FunctionType(fn.__code__, fn.__globals__, fn.__name__, fn.__defaults__, tuple(cells))

    def op(self, eng, fn, reads=(), writes=()):
        return self._record(Op(eng, self._freeze(fn)), reads, writes)

    def dma(self, eng, out, in_, reads=(), writes=(), key=None, **kw):
        if key is None:
            key = writes[0] if writes else reads[0]
        fn = lambda e: e.dma_start(out=out, in_=in_, **kw)
        return self._record(Op(eng, fn, is_dma=True, key=key), reads, writes)

    def emit(self, final_waits=()):
        nc = self.nc
        for op in self.all_ops:
            for d in op.deps:
                if d.eng == "pe" and op.eng == "pe" and not d.is_dma:
                    continue
                d.needs_inc = True
        for op in final_waits:
            op.needs_inc = True
        print("ops per engine", {e: len(self.ops[e]) for e in self.ENGS})
        sems = {}

        def get_sem(name):
            s = sems.get(name)
            if s is None:
                s = sems[name] = self.stack.enter_context(nc.semaphore(name))
            return s

        cnt = {}
        ccnt = {e: 0 for e in self.ENGS}
        for op in self.all_ops:
            if op.is_dma:
                k = cnt.get(op.key, 0) + 1
                cnt[op.key] = k
                per = SEM_CAP // 16
                ep, v = divmod(k - 1, per)
                op.sem = get_sem("d_%s_%d" % (op.key, ep))
                op.val = (v + 1) * 16
            elif op.needs_inc:
                c = ccnt[op.eng]
                ep, v = divmod(c, SEM_CAP)
                op.sem = get_sem("e_%s_%d" % (op.eng, ep))
                op.val = v + 1
                ccnt[op.eng] = c + 1
        engmap = {"pe": "tensor", "act": "scalar", "dve": "vector", "pool": "gpsimd", "sp": "sync"}

        def run(engname, eng):
            known = {}
            for op in self.ops[engname]:
                for d in op.deps:
                    if d.eng == "pe" and engname == "pe" and not d.is_dma:
                        continue
                    sid = d.sem.name
                    if known.get(sid, 0) >= d.val:
                        continue
                    eng.wait_ge(d.sem, d.val)
                    known[sid] = d.val
                ins = op.fn(eng)
                if op.is_dma:
                    ins.then_inc(op.sem, 16)
                elif op.needs_inc:
                    ins.then_inc(op.sem, 1)
            if engname == "sp":
                for op in final_waits:
                    eng.wait_ge(op.sem, op.val)

        with nc.Block() as block:
            for engname in self.ENGS:
                getattr(block, engmap[engname])(lambda eng, _n=engname: run(_n, eng))

    def close(self):
        self.stack.close()


class Arena:
    def __init__(self, S, name, words):
        self.t = S.sb(name, [128, words], F32)
        self.words = words
        self.off = 0

    def reset(self):
        self.off = 0

    def alloc(self, shape, dtype=F32):
        n = int(np.prod(shape[1:]))
        w = n if dtype == F32 else (n + 1) // 2
        assert self.off + w <= self.words, ("arena overflow", self.off, w, self.words)
        v = self.t[0:shape[0], self.off:self.off + w]
        self.off += w
        if dtype != F32:
            v = v.bitcast(dtype)[:, 0:n]
        if len(shape) == 3:
            v = v.rearrange("p (a b) -> p a b", b=shape[2])
        elif len(shape) == 4:
            v = v.rearrange("p (a b c) -> p a b c", b=shape[2], c=shape[3])
        elif len(shape) == 5:
            v = v.rearrange("p (a b c d) -> p a b c d", b=shape[2], c=shape[3], d=shape[4])
        return v


def bc(ap, shape):
    return ap.to_broadcast(list(shape))


def build_program(n_layers=DEPTH, do_ffn=True, n_mix_tiles=SEQ // TT, n_ffn_tiles=SEQ // TF):
    nc = bass.Bass("TRN2", target_bir_lowering=False)
    di = lambda name, shape, dt=F32: nc.dram_tensor(name, list(shape), dt, kind="ExternalInput").ap()
    x_in = di("x", [SEQ, D])
    ccol_d = di("ccol", [128, 8])
    vecs_d = di("vecs", [128, DEPTH * NV * 8 + DEPTH * 8])
    w_ada_d = di("w_ada", [DEPTH, D, 6 * D])
    b_ada_d = di("b_ada", [DEPTH, 6 * D])
    gpm_d = di("g_post_mix", [DEPTH, D])
    gpf_d = di("g_post_ffn", [DEPTH, D])
    w_in_d = di("w_in", [DEPTH, D, 8 * D])
    bd_d = di("bd", [DEPTH, 128, 3 * 8 * 128])
    wgate_d = di("wgate", [DEPTH, 128, 24 * 8])
    bgate_d = di("bgate", [DEPTH, 8])
    wpa_d = di("w_proj_a", [DEPTH, D, D])
    wpb_d = di("w_proj_b", [DEPTH, D, D])
    wo_d = di("w_out", [DEPTH, D, D])
    if not do_ffn:
        di = lambda name, shape, dt=F32: nc.dram_tensor(name, [1, 1], dt, kind="ExternalInput").ap()
    f1_d = di("ffn_w1", [2, D, 2816])
    f3_d = di("ffn_w3", [2, D, 2816])
    f2_d = di("ffn_w2", [2, 2816, D])
    rt_d = di("router", [2, 128, 64])
    m1_d = di("moe_w1", [2, 8, D, 3584])
    m3_d = di("moe_w3", [2, 8, D, 3584])
    m2_d = di("moe_w2", [2, 8, 3584, D])
    out_d = nc.dram_tensor("out", [SEQ, D], F32, kind="ExternalOutput").ap()
    sc = lambda name, shape: nc.dram_tensor(name, list(shape), BF16).ap()
    win_s = sc("win_s", [DEPTH, 32, 128, 8, 256])
    wpa_s = sc("wpa_s", [DEPTH, 4, 128, 8, 256])
    wpb_s = sc("wpb_s", [DEPTH, 4, 128, 8, 256])
    wo_s = sc("wo_s", [DEPTH, 4, 128, 8, 256])
    f13_s = sc("f13_s", [2, 11, 128, 2, 8, 256])
    f2_s = sc("f2_s", [2, 2, 128, 22, 512])
    m13_s = sc("m13_s", [2, 8, 14, 128, 2, 8, 256])
    m2_s = sc("m2_s", [2, 8, 2, 128, 28, 512])

    S = Sched(nc)
    op = S.op
    idf = S.sb("idf", [128, 128], F32)
    idb = S.sb("idb", [128, 128], BF16)
    mask2 = S.sb("mask2", [128, 128], F32)
    maskm = S.sb("maskm", [128, 128], F32)
    mask01 = S.sb("mask01", [128, TT], F32)
    negm = S.sb("negm", [4, 128], F32)
    m01r = S.sb("m01r", [4, 128], F32)
    ones4 = S.sb("ones4", [4, 128], F32)
    sel4 = S.sb("sel4", [4, 4, 128], F32)
    id4 = S.sb("id4", [4, 4], F32)
    onesb = S.sb("onesb", [1, 128], BF16)
    vecs = S.sb("vecs", [128, DEPTH * NV * 8 + DEPTH * 8], F32)
    lbc = S.sb("lbc", [128, DEPTH, 8], F32)
    oml = S.sb("oml", [128, DEPTH, 8], F32)
    noml = S.sb("noml", [128, DEPTH, 8], F32)
    cbc = S.sb("cbc", [128, 8, 128], BF16)
    ccol = S.sb("ccol", [128, 8], F32)
    cact = S.sb("cact", [128, 8], F32)
    hsc_m = S.sb("hsc_m", [128, 8], F32)
    hbi_m = S.sb("hbi_m", [128, 8], F32)
    hsc_f = S.sb("hsc_f", [128, 8], F32)
    hbi_f = S.sb("hbi_f", [128, 8], F32)
    ggm = S.sb("ggm", [128, D], F32)
    ggf = S.sb("ggf", [128, D], F32)
    bdt = S.sb("bdt", [128, 3, 8, 128], BF16)
    wgt = S.sb("wgt", [128, 24, 8], BF16)
    bgt = S.sb("bgt", [128, 8], F32)
    rtt = S.sb("rtt", [128, 8, 8], F32)
    hst = S.sb("hst", [128, 8, 128], F32)
    hstb = S.sb("hstb", [128, 2, 2, 128], BF16)
    Cst = S.sb("Cst", [128, 4, 2, 257], F32)
    Cbf = S.sb("Cbf", [128, 4, 2, 257], BF16)
    mprev = S.sb("mprev", [4, 1], F32)
    PB = [S.ps("pb%d" % i, [128, 512], F32) for i in range(8)]
    P7b = PB[7][:, :].bitcast(BF16)
    P7N = ["P7a", "P7b", "P7c", "P7d"]

    def p7(q, n):
        nq = (n + 255) // 256
        return P7b[:, q * 256:q * 256 + n], P7N[q:q + nq]
    ARW = 42100
    AR = Arena(S, "arena", ARW)

    def vcol(l, v):
        o = (l * NV + v) * 8
        return vecs[:, o:o + 8]

    V_GPRE_M, V_GPRE_F, V_HGN, V_CW0, V_CB, V_MGN, V_SKIP = 0, 1, 2, 3, 7, 8, 9

    op("pool", lambda e: e.memset(idf[:], 0.0), writes=["idf"])
    op("pool", lambda e: e.affine_select(out=idf[:], in_=idf[:], pattern=[[-1, 128]], compare_op=ALU.not_equal,
                                         fill=1.0, base=0, channel_multiplier=1), reads=["idf"], writes=["idf"])
    op("dve", lambda e: e.tensor_copy(out=idb[:], in_=idf[:]), reads=["idf"], writes=["idb"])
    op("pool", lambda e: e.memset(mask2[:], 1.0), writes=["mask2"])
    op("pool", lambda e: e.affine_select(out=mask2[:], in_=mask2[:], pattern=[[1, 128]], compare_op=ALU.is_ge,
                                         fill=0.0, base=0, channel_multiplier=-1), reads=["mask2"], writes=["mask2"])
    op("pool", lambda e: e.memset(mask2[0:64, 64:128], 0.0), reads=["mask2"], writes=["mask2"])
    op("pool", lambda e: e.tensor_scalar_mul(out=maskm[:], in0=mask2[:], scalar1=1.0 / 16.0), reads=["mask2"], writes=["maskm"])
    op("pool", lambda e: e.memset(mask01[:], 1.0), writes=["mask01"])
    op("pool", lambda e: e.memset(mask01[:].rearrange("p (c j) -> p c j", j=64)[:, :, 0:1], 0.0), reads=["mask01"], writes=["mask01"])
    op("pool", lambda e: e.memset(negm[:], 0.0), writes=["negm"])
    op("pool", lambda e: e.memset(negm[:].rearrange("p (c j) -> p c j", j=64)[:, :, 0:1], -1e30), reads=["negm"], writes=["negm"])
    op("pool", lambda e: e.memset(m01r[:], 1.0), writes=["m01r"])
    op("pool", lambda e: e.memset(m01r[:].rearrange("p (c j) -> p c j", j=64)[:, :, 0:1], 0.0), reads=["m01r"], writes=["m01r"])
    op("pool", lambda e: e.memset(ones4[:], 1.0), writes=["ones4"])
    op("pool", lambda e: e.tensor_copy(out=id4[:], in_=idf[0:4, 0:4]), reads=["idf"], writes=["id4"])
    op("pool", lambda e: e.tensor_copy(out=sel4[:], in_=bc(idf[0:4, 0:4].unsqueeze(2), [4, 4, 128])), reads=["idf"], writes=["sel4"])
    op("pool", lambda e: e.memset(onesb[:], 1.0), writes=["onesb"])
    S.dma("sp", vecs[:], vecs_d, writes=["vecs"])
    S.dma("sp", ccol[:], ccol_d, writes=["ccol"])
    op("act", lambda e: e.activation(out=cact[:], in_=ccol[:], func=AF.Silu), reads=["ccol"], writes=["cact"])
    op("dve", lambda e: e.tensor_copy(out=cbc[:], in_=bc(cact[:].unsqueeze(2), [128, 8, 128])), reads=["cact"], writes=["cbc"])
    lbraw = vecs[:, DEPTH * NV * 8:DEPTH * NV * 8 + DEPTH * 8].rearrange("p (l c) -> p l c", c=8)
    lbe = S.sb("lbe", [128, DEPTH, 8], F32)
    lbs = S.sb("lbs", [128, 8], F32)
    op("act", lambda e: e.activation(out=lbe[:], in_=lbraw, func=AF.Exp), reads=["vecs"], writes=["lbe"])
    op("dve", lambda e: e.tensor_add(out=lbs[:], in0=lbe[:, 0, :], in1=lbe[:, 1, :]), reads=["lbe"], writes=["lbs"])
    op("dve", lambda e: e.tensor_add(out=lbs[:], in0=lbs[:], in1=lbe[:, 2, :]), reads=["lbe", "lbs"], writes=["lbs"])
    op("dve", lambda e: e.tensor_add(out=lbs[:], in0=lbs[:], in1=lbe[:, 3, :]), reads=["lbe", "lbs"], writes=["lbs"])
    op("dve", lambda e: e.reciprocal(out=lbs[:], in_=lbs[:]), reads=["lbs"], writes=["lbs"])
    op("dve", lambda e: e.tensor_mul(out=lbe[:], in0=lbe[:], in1=bc(lbs[:].unsqueeze(1), [128, DEPTH, 8])), reads=["lbe", "lbs"], writes=["lbe"])
    op("dve", lambda e: e.memset(lbc[:, 0, :], 0.0), writes=["lbc"])
    for l in range(1, DEPTH):
        op("dve", lambda e, l=l: e.tensor_add(out=lbc[:, l, :], in0=lbc[:, l - 1, :], in1=lbe[:, l, :]), reads=["lbe", "lbc"], writes=["lbc"])
    op("dve", lambda e: e.tensor_scalar(out=oml[:], in0=lbc[:], scalar1=-1.0, scalar2=1.0, op0=ALU.mult, op1=ALU.add), reads=["lbc"], writes=["oml"])
    op("dve", lambda e: e.tensor_scalar_mul(out=noml[:], in0=oml[:], scalar1=-1.0), reads=["oml"], writes=["noml"])

    def conv_kxn(dst, src, ngroups, depname):
        v = src.rearrange("(kc p) (g c) -> g p kc c", p=128, c=256)
        for g in range(ngroups):
            S.dma("pool", dst[g], v[g], writes=[depname])

    def conv_layer(l):
        conv_kxn(win_s[l], w_in_d[l], 32, "win%d" % l)
        conv_kxn(wpa_s[l], wpa_d[l], 4, "wpa%d" % l)
        conv_kxn(wpb_s[l], wpb_d[l], 4, "wpb%d" % l)
        conv_kxn(wo_s[l], wo_d[l], 4, "wo%d" % l)
        if not do_ffn:
            return
        j = l // 2
        if l % 2 == 0:
            v1 = f1_d[j].rearrange("(kc p) (g c) -> g p kc c", p=128, c=256)
            v3 = f3_d[j].rearrange("(kc p) (g c) -> g p kc c", p=128, c=256)
            for g in range(11):
                S.dma("pool", f13_s[j, g, :, 0], v1[g], writes=["f13_%d" % l])
                S.dma("pool", f13_s[j, g, :, 1], v3[g], writes=["f13_%d" % l])
            v2 = f2_d[j].rearrange("(fc p) (h c) -> h p fc c", p=128, c=512)
            for h in range(2):
                S.dma("pool", f2_s[j, h], v2[h], writes=["f2_%d" % l])
        else:
            for ex in range(8):
                v1 = m1_d[j, ex].rearrange("(kc p) (g c) -> g p kc c", p=128, c=256)
                v3 = m3_d[j, ex].rearrange("(kc p) (g c) -> g p kc c", p=128, c=256)
                for g in range(14):
                    S.dma("pool", m13_s[j, ex, g, :, 0], v1[g], writes=["m13_%d_%d" % (l, ex)])
                    S.dma("pool", m13_s[j, ex, g, :, 1], v3[g], writes=["m13_%d_%d" % (l, ex)])
                v2 = m2_d[j, ex].rearrange("(fc p) (h c) -> h p fc c", p=128, c=512)
                for h in range(2):
                    S.dma("pool", m2_s[j, ex, h], v2[h], writes=["m2_%d_%d" % (l, ex)])

    for l in range(n_layers):
        conv_layer(l)

    def rstd_from_ss(rs, ss, n, dep_ss, dep_rs):
        op("dve", lambda e: e.tensor_scalar(out=rs, in0=ss, scalar1=1.0 / n, scalar2=EPS, op0=ALU.mult, op1=ALU.add), reads=[dep_ss], writes=[dep_rs])
        op("act", lambda e: e.activation(out=rs, in_=rs, func=AF.Ln), reads=[dep_rs], writes=[dep_rs])
        op("act", lambda e: e.activation(out=rs, in_=rs, func=AF.Exp, scale=-0.5), reads=[dep_rs], writes=[dep_rs])

    xkeys = {}
    last_x_dma = {}

    def xsrc(l, first):
        return x_in if (l == 0 and first) else out_d

    def layer_prologue(l):
        AR.reset()
        S.barrier()
        wad = [AR.alloc([128, 8, 512], BF16) for _ in range(2)]
        bad = AR.alloc([1, 6 * D], BF16)
        gpb = [AR.alloc([128, D], F32) for _ in range(2)]
        tmpd = AR.alloc([128, 4, 128], F32)
        S.dma("pool", bad, b_ada_d[l:l + 1, :], writes=["bad"])
        S.dma("sp", gpb[0], gpm_d[l].partition_broadcast(128), writes=["gpb0"])
        S.dma("sp", gpb[1], gpf_d[l].partition_broadcast(128), writes=["gpb1"])
        S.dma("pool", bdt[:].rearrange("p a b c -> p (a b c)"), bd_d[l], writes=["bdt"])
        S.dma("pool", wgt[:].rearrange("p a b -> p (a b)"), wgate_d[l], writes=["wgt"])
        S.dma("sp", bgt[:], bgate_d[l].partition_broadcast(128), writes=["bgt"])
        if l % 2 == 1:
            S.dma("sp", rtt[:].rearrange("p a b -> p (a b)"), rt_d[l // 2], writes=["rtt"])
        wv = w_ada_d[l].rearrange("(kc p) (g c) -> g p kc c", p=128, c=512)
        for g in range(12):
            w = wad[g % 2]
            wn = "wad%d" % (g % 2)
            S.dma("pool", w, wv[g], writes=[wn], key="pl3_%d" % (g % 2))
            pb = PB[g % 2]
            pn = ["P%da" % (g % 2), "P%db" % (g % 2)]
            for kc in range(8):
                op("pe", lambda e, w=w, kc=kc, pb=pb: e.matmul(pb[:, :], lhsT=cbc[:, kc, :], rhs=w[:, kc, :], start=(kc == 0), stop=False),
                   reads=[wn, "cbc"], writes=pn)
            op("pe", lambda e, g=g, pb=pb: e.matmul(pb[:, :], lhsT=onesb[0:1, :], rhs=bad[0:1, g * 512:(g + 1) * 512], start=False, stop=True),
               reads=["bad", "onesb"], writes=pn)
            which = g // 2
            half = g % 2
            if which in (2, 5):
                dst = ggm if which == 2 else ggf
                dn = "ggm" if which == 2 else "ggf"
                gp = gpb[0] if which == 2 else gpb[1]
                gn = "gpb0" if which == 2 else "gpb1"
                op("dve", lambda e, pb=pb, dst=dst, gp=gp, half=half: e.tensor_mul(out=dst[:, half * 512:(half + 1) * 512], in0=pb[:, :], in1=gp[:, half * 512:(half + 1) * 512]),
                   reads=pn + [gn], writes=[dn])
            else:
                dst = {0: hbi_m, 1: hsc_m, 3: hbi_f, 4: hsc_f}[which]
                dn = {0: "hbi_m", 1: "hsc_m", 3: "hbi_f", 4: "hsc_f"}[which]
                op("dve", lambda e, pb=pb: e.tensor_mul(out=tmpd, in0=pb[:, :].rearrange("p (c j) -> p c j", j=128), in1=bc(idf[:].unsqueeze(1), [128, 4, 128])),
                   reads=pn + ["idf"], writes=["tmpd"])
                op("dve", lambda e, dst=dst, half=half: e.tensor_reduce(out=dst[:, half * 4:(half + 1) * 4], in_=tmpd, axis=AX.X, op=ALU.add),
                   reads=["tmpd"], writes=[dn])
        for (hs, hn, vi) in ((hsc_m, "hsc_m", V_GPRE_M), (hsc_f, "hsc_f", V_GPRE_F)):
            op("dve", lambda e, hs=hs, vi=vi: e.scalar_tensor_tensor(out=hs[:], in0=hs[:], scalar=1.0, in1=vcol(l, vi), op0=ALU.add, op1=ALU.mult),
               reads=[hn, "vecs"], writes=[hn])
        op("pool", lambda e: e.memset(hst[:], 0.0), writes=["hst"])
        op("pool", lambda e: e.memset(Cst[:], 0.0), writes=["Cst"])
        op("pool", lambda e: e.memset(Cbf[:], 0.0), writes=["Cbf"])
        op("pool", lambda e: e.memset(mprev[:], 0.0), writes=["mprev"])

    def load_norm_transpose(l, first, t0, nsub, hsc, hbi, hscn, hbin, xt, xn, hT, junk, ss, rs, hTf=None, xnn="xn", hTfn="hTf", junkn="junk"):
        src = xsrc(l, first)
        op("pool", lambda e: e.memset(ss, 0.0), writes=["ss"])
        for s in range(nsub):
            blk = (t0 + s * 128) // TT
            S.dma("sp", xt[:, s, :], src[t0 + s * 128:t0 + (s + 1) * 128, :], reads=["xrow%d" % blk], writes=["xt%d" % s], key="xl%d" % s)
            op("act", lambda e, s=s: e.activation(out=junk, in_=xt[:, s, :], func=AF.Square, accum_out=ss[:, s:s + 1]),
               reads=["xt%d" % s], writes=[junkn, "ss"])
        rstd_from_ss(rs, ss, D, "ss", "rs")
        dt = F32 if hTf is not None else BF16
        for s in range(nsub):
            op("dve", lambda e, s=s: e.tensor_scalar(out=xn[:, s, :], in0=xt[:, s, :], scalar1=rs[:, s:s + 1], scalar2=None, op0=ALU.mult),
               reads=["xt%d" % s, "rs"], writes=[xnn])
        idt = idf if hTf is not None else idb
        n = nsub * 128
        for dc in range(8):
            if hTf is not None:
                pt = PB[6 + dc % 2][:, 0:n]
                pn = ["P6a", "P6b"] if dc % 2 == 0 else P7N
            else:
                pt, pn = p7((dc % 2) * 2, n)
            for s in range(nsub):
                op("pe", lambda e, s=s, dc=dc, pt=pt: e.transpose(out=pt[:, s * 128:(s + 1) * 128], in_=xn[:, s, dc * 128:(dc + 1) * 128], identity=idt[:]),
                   reads=[xnn, "idb", "idf"], writes=pn)
            tgt = hTf if hTf is not None else hT
            tgn = hTfn if hTf is not None else "hT"
            op("act", lambda e, dc=dc, pt=pt, tgt=tgt: e.activation(out=tgt[:, dc, :], in_=pt, func=AF.Identity, scale=hsc[:, dc:dc + 1], bias=hbi[:, dc:dc + 1]),
               reads=pn + [hscn, hbin], writes=[tgn])
        if hTf is not None:
            op("dve", lambda e: e.tensor_copy(out=hT, in_=hTf), reads=[hTfn], writes=["hT"])

    def residual_update(l, t0, s, ypsum_list, ypn, xt, ysb, tmpx, junk, ssy, rsy, gg, ggn, yacc=None, ysbn="ysb", tmpxn="tmpx", junkn="junk"):
        if yacc is None:
            for (pa, c0, ncol) in ypsum_list:
                op("act", lambda e, pa=pa, c0=c0, ncol=ncol: e.copy(out=ysb[:, c0:c0 + ncol], in_=pa), reads=ypn, writes=[ysbn])
            ysrc, ysn = ysb, ysbn
        else:
            ysrc, ysn = yacc, "yacc"
        op("pool", lambda e: e.memset(ssy[:, 0:1], 0.0), writes=["ssy"])
        op("act", lambda e: e.activation(out=junk, in_=ysrc, func=AF.Square, accum_out=ssy[:, 0:1]), reads=[ysn], writes=[junkn, "ssy"])
        rstd_from_ss(rsy[:, 0:1], ssy[:, 0:1], D, "ssy", "rsy")
        op("dve", lambda e: e.scalar_tensor_tensor(out=tmpx, in0=ysrc, scalar=rsy[:, 0:1], in1=gg[:], op0=ALU.mult, op1=ALU.mult),
           reads=[ysn, "rsy", ggn], writes=[tmpxn])
        op("pool", lambda e: e.tensor_add(out=xt[:, s, :], in0=xt[:, s, :], in1=tmpx), reads=[tmpxn, "xt%d" % s], writes=["xt%d" % s])
        blk = (t0 + s * 128) // TT
        d = S.dma("sp", out_d[t0 + s * 128:t0 + (s + 1) * 128, :], xt[:, s, :], reads=["xt%d" % s], writes=["xrow%d" % blk], key="xs%d" % s)
        last_x_dma["xs%d" % s] = d

    def mixer(l):
        AR.reset()
        S.barrier()
        A = AR.alloc
        xt = A([128, 2, D]); xn = A([128, 2, D], BF16); hT = A([128, 8, TT], BF16)
        ss = A([128, 2]); rs = A([128, 2]); ssy = A([128, 2]); rsy = A([128, 2])
        NWG = 3
        wg = [A([128, 8, 256], BF16) for _ in range(NWG)]
        qs = A([128, 8, TT], BF16)
        T1 = A([128, 8, TT]); T2 = A([128, 8, TT]); T3 = A([128, 8, TT]); T4 = A([128, 8, TT])
        QEO = A([128, 8, 2, 2, 128], BF16)
        KT = A([128, 8, TT], BF16)
        Ktok = A([128, 8, 2, 128], BF16)
        vtok = A([128, 2, D], BF16)
        sga = A([128, 8, TT], BF16)
        yaT = A([128, 8, TT], BF16)
        E1 = A([128, 8, 4]); E2 = A([128, 8, 4]); E3 = A([128, 8, 4]); dE = A([128, 8, 4])
        tKV = A([128, 2, 128])
        STb = A([128, 2, 128], BF16)
        ssq = A([128, 8]); rsq = A([128, 8])
        onb = A([128, D], BF16)
        xm = A([128, 8, 3 + TT])
        xcT = A([128, 8, TT], BF16); xmT = A([128, 8, TT], BF16)
        qT = A([128, 8, TT], BF16); kT = A([128, 8, TT], BF16); vT = A([128, 8, TT], BF16)
        ktok = A([128, 2, D], BF16)
        vext = A([128, 2, 4, 257], BF16)
        sob = A([128, 8, TT], BF16); sgA = A([128, 8, TT], BF16); sgB = A([128, 8, TT], BF16)
        DT = A([128, 2, 128]); DTm = A([128, 2, 128])
        kw = A([128, 2, 256], BF16)
        tnum = A([128, 257])
        hnb = A([128, D], BF16)
        junk = hnb
        ybT = A([128, 8, TT], BF16)
        mixT = A([128, 8, TT], BF16)
        gpre = A([128, 8]); gli = A([128, 4]); glf = A([128, 4])
        rows = A([4, 8, 128])
        gcol = A([128, 2, 16])
        decb = A([128, 2, 2, 4])
        dexp = A([4, 4, 2])
        sm = A([128, 32])
        ctmp = A([128, TT])
        acc = T1; o_sb = T2.rearrange("p a b -> p (a b)")[:, 0:D]; sqb = T3.rearrange("p a b -> p (a b)")[:, 0:D]
        tot = T4.rearrange("p a b -> p (a b)")[:, 0:4 * 257].rearrange("p (a b) -> p a b", b=257)
        sqm = T3.rearrange("p a b -> p (a b)")[:, 0:D].rearrange("p (a b) -> p a b", b=256)
        ysb = T1.rearrange("p a b -> p (a b)")[:, 0:D]
        tmpx = T2.rearrange("p a b -> p (a b)")[:, 0:D]
        m1 = T3.rearrange("p a b -> p (a b)")[:, 0:2 * TT].rearrange("p (a b) -> p a b", b=TT)
        m2 = T4.rearrange("p a b -> p (a b)")[:, 0:2 * TT].rearrange("p (a b) -> p a b", b=TT)

        print("mixer arena words", AR.off)
        op("pool", lambda e: e.memset(xm[:, :, 0:3], 0.0), writes=["xm"])
        op("pool", lambda e: e.memset(QEO, 0.0), writes=["QEO"])
        op("pool", lambda e: e.memset(vext[:, :, :, 256:257], 1.0), writes=["vext"])

        pp_names = ["P0a", "P0b", "P1a", "P1b"]

        def pp_view(i):
            return PB[i // 2][:, (i % 2) * 256:(i % 2) * 256 + 256]

        state = {"pp": 0, "wg": 0}

        def next_pp():
            i = state["pp"] % 4
            state["pp"] += 1
            return pp_view(i), [pp_names[i]]

        def load_wg(src_ap, depname):
            i = state["wg"] % NWG
            state["wg"] += 1
            S.dma("sp", wg[i], src_ap, reads=[depname], writes=["wg%d" % i], key="wg%d" % i)
            return wg[i], "wg%d" % i

        for ti in range(n_mix_tiles):
            t0 = ti * TT
            load_norm_transpose(l, True, t0, 2, hsc_m, hbi_m, "hsc_m", "hbi_m", xt, xn, hT, junk, ss, rs, junkn="hnb")
            def fm_chunk(w, wn, half, rhs_t, rhs_n, evac):
                pv, pn = next_pp()
                for kc in range(8):
                    op("pe", lambda e, kc=kc, pv=pv: e.matmul(pv, lhsT=w[:, kc, half * 128:(half + 1) * 128], rhs=rhs_t[:, kc, :], start=(kc == 0), stop=(kc == 7)),
                       reads=[wn, rhs_n], writes=pn)
                evac(pv, pn)

            for grp in (0, 1, 2, 3, 12, 13, 14, 15, 4, 5, 6, 7, 20, 21, 22, 23, 24, 25, 26, 27, 28, 29, 30, 31, 16, 17, 18, 19, 8, 9, 10, 11):
                w, wn = load_wg(win_s[l, grp], "win%d" % l)
                if 8 <= grp < 12:
                    for s in range(2):
                        pv, pn = next_pp()
                        for kc in range(8):
                            op("pe", lambda e, kc=kc, pv=pv, s=s, w=w: e.matmul(pv, lhsT=hT[:, kc, s * 128:(s + 1) * 128], rhs=w[:, kc, :], start=(kc == 0), stop=(kc == 7)),
                               reads=[wn, "hT"], writes=pn)
                        c0 = (grp - 8) * 256
                        op("dve", lambda e, pv=pv, s=s, c0=c0: e.tensor_copy(out=vtok[:, s, c0:c0 + 256], in_=pv), reads=pn, writes=["vtok"])
                    continue
                for half in range(2):
                    m = grp * 2 + half
                    kind, hd = m // 8, m % 8
                    if kind == 0:
                        ev = lambda pv, pn, hd=hd: op("act", lambda e: e.activation(out=qs[:, hd, :], in_=pv, func=AF.Silu), reads=pn, writes=["qs"])
                    elif kind == 1:
                        ev = lambda pv, pn, hd=hd: op("act", lambda e: e.activation(out=T1[:, hd, :], in_=pv, func=AF.Sigmoid), reads=pn, writes=["T1"])
                    elif kind == 3:
                        ev = lambda pv, pn, hd=hd: op("act", lambda e: e.activation(out=sga[:, hd, :], in_=pv, func=AF.Silu), reads=pn, writes=["sga"])
                    elif kind == 4:
                        ev = lambda pv, pn, hd=hd: op("dve", lambda e: e.tensor_copy(out=xm[:, hd, 3:3 + TT], in_=pv), reads=pn, writes=["xm"])
                    elif kind == 5:
                        ev = lambda pv, pn, hd=hd: op("act", lambda e: e.activation(out=sob[:, hd, :], in_=pv, func=AF.Sigmoid), reads=pn, writes=["sob"])
                    elif kind == 6:
                        ev = lambda pv, pn, hd=hd: op("act", lambda e: e.activation(out=sgA[:, hd, :], in_=pv, func=AF.Sigmoid), reads=pn, writes=["sgA"])
                    else:
                        ev = lambda pv, pn, hd=hd: op("act", lambda e: e.activation(out=sgB[:, hd, :], in_=pv, func=AF.Sigmoid), reads=pn, writes=["sgB"])
                    fm_chunk(w, wn, half, hT, "hT", ev)

            for hd in range(8):
                op("dve", lambda e, hd=hd: e.tensor_scalar(out=T4[:, hd, :], in0=T1[:, hd, :], scalar1=noml[:, l, hd:hd + 1], scalar2=oml[:, l, hd:hd + 1], op0=ALU.mult, op1=ALU.add),
                   reads=["T1", "noml", "oml"], writes=["T4"])
            for hd in range(8):
                op("act", lambda e, hd=hd: e.activation(out=T2[:, hd, :], in_=T1[:, hd, :], func=AF.Ln, scale=oml[:, l, hd:hd + 1], bias=lbc[:, l, hd:hd + 1]),
                   reads=["T1", "oml", "lbc"], writes=["T2"])
            for hd in range(8):
                op("dve", lambda e, hd=hd: e.tensor_tensor_scan(out=T3[:, hd, :], data0=mask01[:], data1=T2[:, hd, :], initial=0.0, op0=ALU.mult, op1=ALU.add),
                   reads=["T2", "mask01"], writes=["T3"])
            b4 = T3.rearrange("p h (c j) -> p h c j", j=64)
            op("dve", lambda e: e.tensor_sub(out=dE, in0=b4[:, :, :, 63], in1=b4[:, :, :, 31]), reads=["T3"], writes=["dE"])
            op("act", lambda e: e.activation(out=E1, in_=b4[:, :, :, 63], func=AF.Exp), reads=["T3"], writes=["E1"])
            op("act", lambda e: e.activation(out=E2, in_=dE, func=AF.Exp), reads=["dE"], writes=["E2"])
            op("act", lambda e: e.activation(out=E3, in_=b4[:, :, :, 31], func=AF.Exp), reads=["T3"], writes=["E3"])
            op("dve", lambda e: e.tensor_sub(out=T2.rearrange("p h (c j) -> p h c j", j=64), in0=b4, in1=bc(b4[:, :, :, 31:32], [128, 8, 4, 64])),
               reads=["T3", "T2"], writes=["T2"])
            op("act", lambda e: e.activation(out=T1, in_=T2, func=AF.Exp), reads=["T2", "T1"], writes=["T1"])
            op("act", lambda e: e.activation(out=T3, in_=T2, func=AF.Exp, scale=-1.0), reads=["T2", "E1", "E3", "dE"], writes=["T3"])
            for hd in range(8):
                qo = QEO[:, hd].rearrange("p a b c -> p (a b c)")
                for pr in range(2):
                    for eo in range(2):
                        c = pr * 2 + eo
                        o = pr * 256 + eo * 192
                        op("dve", lambda e, hd=hd, c=c, o=o, qo=qo: e.tensor_mul(out=qo[:, o:o + 64], in0=qs[:, hd, c * 64:(c + 1) * 64], in1=T1[:, hd, c * 64:(c + 1) * 64]),
                           reads=["qs", "T1"], writes=["QEO"])
            op("dve", lambda e: e.tensor_mul(out=KT, in0=T4, in1=T3), reads=["T4", "T3"], writes=["KT"])
            for hd in range(8):
                pt, pn = p7(2 + hd % 2, 256)
                for pr in range(2):
                    op("pe", lambda e, hd=hd, pr=pr, pt=pt: e.transpose(out=pt[:, pr * 128:(pr + 1) * 128], in_=KT[:, hd, pr * 128:(pr + 1) * 128], identity=idb[:]),
                       reads=["KT", "idb"], writes=pn)
                op("act", lambda e, hd=hd, pt=pt: e.copy(out=Ktok[:, hd].rearrange("p a b -> p (a b)"), in_=pt), reads=pn, writes=["Ktok"])
            for dc in range(8):
                op("pool", lambda e, dc=dc: e.tensor_scalar(out=acc[:, dc, :], in0=xm[:, dc, 3:3 + TT], scalar1=vcol(l, V_CW0 + 3)[:, dc:dc + 1], scalar2=vcol(l, V_CB)[:, dc:dc + 1], op0=ALU.mult, op1=ALU.add),
                   reads=["xm", "vecs", "T1"], writes=["T1"])
                for j in range(3):
                    op("pool", lambda e, dc=dc, j=j: e.tensor_scalar(out=ctmp, in0=xm[:, dc, j:j + TT], scalar1=vcol(l, V_CW0 + j)[:, dc:dc + 1], scalar2=None, op0=ALU.mult),
                       reads=["xm", "vecs"], writes=["ctmp"])
                    op("pool", lambda e, dc=dc, j=j: e.tensor_add(out=acc[:, dc, :], in0=acc[:, dc, :], in1=ctmp),
                       reads=["ctmp", "T1"], writes=["T1"])
            op("act", lambda e: e.activation(out=xcT, in_=acc, func=AF.Silu), reads=["T1"], writes=["xcT"])
            op("pool", lambda e: e.tensor_copy(out=xmT, in_=xm[:, :, 3:3 + TT]), reads=["xm"], writes=["xmT"])
            op("pool", lambda e: e.tensor_copy(out=xm[:, :, 0:3], in_=xm[:, :, TT:TT + 3]), reads=["xm"], writes=["xm"])
            for (mi, srcT, srcn, dstT, dstn) in ((0, xcT, "xcT", qT, "qT"), (1, xcT, "xcT", kT, "kT"), (2, xmT, "xmT", vT, "vT")):
                for dc in range(8):
                    pv, pn = next_pp()
                    op("pe", lambda e, mi=mi, dc=dc, pv=pv, srcT=srcT: e.matmul(pv, lhsT=bdt[:, mi, dc, :], rhs=srcT[:, dc, :], start=True, stop=True),
                       reads=["bdt", srcn], writes=pn)
                    eng = "act" if dc % 2 == 0 else "dve"
                    if eng == "act":
                        op("act", lambda e, dc=dc, pv=pv, dstT=dstT: e.copy(out=dstT[:, dc, :], in_=pv), reads=pn, writes=[dstn])
                    else:
                        op("dve", lambda e, dc=dc, pv=pv, dstT=dstT: e.tensor_copy(out=dstT[:, dc, :], in_=pv), reads=pn, writes=[dstn])
            for pr in range(2):
                for (mi, srcT, srcn) in ((1, xcT, "xcT"), (2, xmT, "xmT")):
                    for hb in range(2):
                        pbk = PB[2]
                        pn = ["P2a", "P2b", "P2c", "P2d"]
                        for j in range(4):
                            dc = hb * 4 + j
                            op("pe", lambda e, mi=mi, dc=dc, j=j, srcT=srcT, pr=pr: e.matmul(pbk[:, j * 128:(j + 1) * 128], lhsT=srcT[:, dc, pr * 128:(pr + 1) * 128], rhs=bdt[:, mi, dc, :], start=True, stop=True),
                               reads=["bdt", srcn], writes=pn)
                        if mi == 1:
                            op("act", lambda e, pr=pr, hb=hb: e.copy(out=ktok[:, pr, hb * 512:(hb + 1) * 512], in_=pbk[:, :]), reads=pn, writes=["ktok"])
                        else:
                            op("dve", lambda e, pr=pr, hb=hb: e.tensor_copy(out=vext[:, pr, hb * 2:hb * 2 + 2, 0:256], in_=pbk[:, :].rearrange("p (a b) -> p a b", b=256)), reads=pn, writes=["vext"])
            for pr in range(2):
                pg = PB[5][:, 256:264]
                pgn = ["P5c"]
                i = 0
                for (srcT, srcn) in ((qT, "qT"), (kT, "kT"), (vT, "vT")):
                    for dc in range(8):
                        op("pe", lambda e, srcT=srcT, dc=dc, i=i, pr=pr: e.matmul(pg, lhsT=srcT[:, dc, pr * 128:(pr + 1) * 128], rhs=wgt[:, i, :], start=(i == 0), stop=(i == 23)),
                           reads=[srcn, "wgt"], writes=pgn)
                        i += 1
                op("dve", lambda e: e.tensor_add(out=gpre, in0=pg, in1=bgt[:]), reads=pgn + ["bgt"], writes=["gpre"])
                op("act", lambda e: e.activation(out=glf, in_=gpre[:, 4:8], func=AF.Exp, scale=-1.0), reads=["gpre"], writes=["glf"])
                op("act", lambda e: e.activation(out=glf, in_=glf, func=AF.Ln, bias=1.0), reads=["glf"], writes=["glf"])
                op("dve", lambda e: e.tensor_scalar_mul(out=glf, in0=glf, scalar1=-1.0), reads=["glf"], writes=["glf"])
                op("dve", lambda e: e.tensor_copy(out=gli, in_=gpre[:, 0:4]), reads=["gpre"], writes=["gli"])
                prw = PB[5][0:4, 384:512]
                prn = ["P5d"]
                op("pe", lambda e: e.matmul(prw, lhsT=gli, rhs=idf[:], start=True, stop=True), reads=["gli", "idf"], writes=prn)
                op("dve", lambda e: e.tensor_copy(out=rows[:, 0, :], in_=prw), reads=prn, writes=["rows"])
                op("pe", lambda e: e.matmul(prw, lhsT=glf, rhs=idf[:], start=True, stop=True), reads=["glf", "idf"], writes=prn)
                op("dve", lambda e: e.tensor_copy(out=rows[:, 1, :], in_=prw), reads=prn, writes=["rows"])
                op("dve", lambda e: e.tensor_tensor_scan(out=rows[:, 2, :], data0=m01r[:], data1=rows[:, 1, :], initial=0.0, op0=ALU.mult, op1=ALU.add), reads=["rows", "m01r"], writes=["rows"])
                op("dve", lambda e: e.tensor_sub(out=rows[:, 3, :], in0=rows[:, 0, :], in1=rows[:, 2, :]), reads=["rows"], writes=["rows"])
                op("dve", lambda e: e.tensor_tensor_scan(out=rows[:, 4, :], data0=negm[:], data1=rows[:, 3, :], initial=-1e30, op0=ALU.add, op1=ALU.max), reads=["rows", "negm"], writes=["rows"])
                for eo in range(2):
                    cs = slice(eo * 64, (eo + 1) * 64)
                    op("dve", lambda e, cs=cs: e.tensor_scalar(out=rows[:, 5, cs], in0=rows[:, 4, cs], scalar1=mprev[:, 0:1], scalar2=None, op0=ALU.max), reads=["rows", "mprev"], writes=["rows"])
                    op("dve", lambda e, cs=cs: e.tensor_scalar(out=rows[:, 6, cs], in0=rows[:, 5, cs], scalar1=mprev[:, 0:1], scalar2=-1.0, op0=ALU.subtract, op1=ALU.mult), reads=["rows", "mprev"], writes=["rows"])
                    last = eo * 64 + 63
                    op("dve", lambda e, cs=cs, last=last: e.tensor_scalar(out=rows[:, 7, cs], in0=rows[:, 3, cs], scalar1=rows[:, 5, last:last + 1], scalar2=None, op0=ALU.subtract), reads=["rows"], writes=["rows"])
                    op("dve", lambda e, last=last: e.tensor_add(out=mprev[:, 0:1], in0=rows[:, 2, last:last + 1], in1=rows[:, 5, last:last + 1]), reads=["rows", "mprev"], writes=["mprev"])
                op("dve", lambda e: e.scalar_tensor_tensor(out=rows[:, 1, :], in0=rows[:, 2, :], scalar=-1.0, in1=rows[:, 5, :], op0=ALU.mult, op1=ALU.subtract), reads=["rows"], writes=["rows"])
                op("act", lambda e: e.activation(out=rows[:, 6, :], in_=rows[:, 6, :], func=AF.Exp), reads=["rows"], writes=["rows"])
                op("act", lambda e: e.activation(out=rows[:, 1, :], in_=rows[:, 1, :], func=AF.Exp), reads=["rows"], writes=["rows"])
                op("act", lambda e: e.activation(out=rows[:, 7, :], in_=rows[:, 7, :], func=AF.Exp, bias=float(-np.log(16.0))), reads=["rows"], writes=["rows"])
                pcl = PB[5][:, 264:280]
                for qi, ri in enumerate((3, 6, 1, 7)):
                    op("pe", lambda e, qi=qi, ri=ri: e.matmul(pcl[:, qi * 4:(qi + 1) * 4], lhsT=rows[:, ri, :], rhs=id4[:], start=True, stop=True), reads=["rows", "id4"], writes=pgn)
                op("dve", lambda e, pr=pr: e.tensor_copy(out=gcol[:, pr, :], in_=pcl), reads=pgn, writes=["gcol"])
                wl = rows[:, 6, :].rearrange("p (c j) -> p c j", j=64)[:, :, 63]
                op("dve", lambda e, wl=wl: e.tensor_mul(out=dexp, in0=bc(wl.unsqueeze(1), [4, 4, 2]), in1=bc(id4[:].unsqueeze(2), [4, 4, 2])), reads=["rows", "id4"], writes=["dexp"])
                pdc = PB[5][:, 280:288]
                op("pe", lambda e: e.matmul(pdc, lhsT=ones4[:], rhs=dexp.rearrange("p a b -> p (a b)"), start=True, stop=True), reads=["dexp", "ones4"], writes=pgn)
                op("dve", lambda e, pr=pr: e.tensor_copy(out=decb[:, pr].rearrange("p e h -> p h e"), in_=pdc.rearrange("p (h e) -> p h e", e=2)), reads=pgn, writes=["decb"])
                for hd in range(8):
                    psc = PB[2][:, (hd % 2) * 128:(hd % 2) * 128 + 128]
                    pscn = ["P2a" if hd % 2 == 0 else "P2b"]
                    op("pe", lambda e, hd=hd, psc=psc: e.matmul(psc, lhsT=KT[:, hd, pr * 128:(pr + 1) * 128], rhs=QEO[:, hd, pr, 0, :], start=True, stop=False), reads=["KT", "QEO"], writes=pscn)
                    op("pe", lambda e, hd=hd, psc=psc: e.matmul(psc, lhsT=KT[:, hd, pr * 128:(pr + 1) * 128], rhs=QEO[:, hd, pr, 1, :], start=False, stop=True), reads=["KT", "QEO"], writes=pscn)
                    stn = "STb%d" % (hd % 2)
                    op("dve", lambda e, hd=hd, psc=psc: e.tensor_scalar(out=DT[:, hd % 2, :], in0=psc, scalar1=1e30, scalar2=-1e30, op0=ALU.min, op1=ALU.max), reads=pscn, writes=["DT%d" % (hd % 2)])
                    op("dve", lambda e, hd=hd: e.tensor_mul(out=STb[:, hd % 2, :], in0=DT[:, hd % 2, :], in1=mask2[:]), reads=["DT%d" % (hd % 2), "mask2"], writes=[stn])
                    po = PB[3 + hd // 4][:, (hd % 4) * 128:(hd % 4) * 128 + 128]
                    pon = ["P3" if hd < 4 else "P4"]
                    hn_ = "hst%d" % hd
                    hbn = "hstb%d" % (hd % 2)
                    for eo in range(2):
                        c = pr * 2 + eo
                        op("pool", lambda e, hd=hd, eo=eo, c=c: e.tensor_scalar(out=hstb[:, hd % 2, eo, :], in0=hst[:, hd, :], scalar1=E3[:, hd, c:c + 1], scalar2=None, op0=ALU.mult),
                           reads=[hn_, "E3"], writes=[hbn + "_%d" % eo])
                        pkv = PB[2][:, 256 + eo * 128:256 + eo * 128 + 128]
                        pkvn = ["P2c" if eo == 0 else "P2d"]
                        op("pe", lambda e, hd=hd, eo=eo, pkv=pkv: e.matmul(pkv, lhsT=Ktok[eo * 64:(eo + 1) * 64, hd, pr, :], rhs=vtok[eo * 64:(eo + 1) * 64, pr, hd * 128:(hd + 1) * 128], start=True, stop=True),
                           reads=["Ktok", "vtok"], writes=pkvn)
                        op("act", lambda e, hd=hd, eo=eo, c=c, pkv=pkv: e.activation(out=tKV[:, eo, :], in_=pkv, func=AF.Copy, scale=E2[:, hd, c:c + 1]), reads=pkvn + ["E2"], writes=["tKV%d" % eo])
                        op("dve", lambda e, hd=hd, eo=eo, c=c: e.scalar_tensor_tensor(out=hst[:, hd, :], in0=hst[:, hd, :], scalar=E1[:, hd, c:c + 1], in1=tKV[:, eo, :], op0=ALU.mult, op1=ALU.add),
                           reads=["tKV%d" % eo, "E1", hn_], writes=[hn_])
                    op("pe", lambda e, hd=hd, po=po: e.matmul(po, lhsT=STb[:, hd % 2, :], rhs=vtok[:, pr, hd * 128:(hd + 1) * 128], start=True, stop=False), reads=[stn, "vtok"], writes=pon)
                    op("pe", lambda e, hd=hd, po=po: e.matmul(po, lhsT=QEO[:, hd, pr, 0, :], rhs=hstb[:, hd % 2, 0, :], start=False, stop=False), reads=["QEO", hbn + "_0"], writes=pon)
                    op("pe", lambda e, hd=hd, po=po: e.matmul(po, lhsT=QEO[:, hd, pr, 1, :], rhs=hstb[:, hd % 2, 1, :], start=False, stop=True), reads=["QEO", hbn + "_1"], writes=pon)
                op("act", lambda e: e.copy(out=o_sb[:, 0:512], in_=PB[3][:, :]), reads=["P3"], writes=["T2"])
                op("act", lambda e: e.copy(out=o_sb[:, 512:1024], in_=PB[4][:, :]), reads=["P4"], writes=["T2"])
                op("pool", lambda e: e.tensor_mul(out=sqb, in0=o_sb, in1=o_sb), reads=["T2"], writes=["T3"])
                op("dve", lambda e: e.tensor_reduce(out=ssq, in_=sqb.rearrange("p (h v) -> p h v", v=128), axis=AX.X, op=ALU.add), reads=["T3"], writes=["ssq"])
                rstd_from_ss(rsq, ssq, 128, "ssq", "rsq")
                op("dve", lambda e: e.tensor_mul(out=onb.rearrange("p (h v) -> p h v", v=128), in0=o_sb.rearrange("p (h v) -> p h v", v=128), in1=bc(rsq.unsqueeze(2), [128, 8, 128])),
                   reads=["T2", "rsq"], writes=["onb"])
                for hd in range(8):
                    pt, pn = p7((hd % 2) * 2, 128)
                    op("pe", lambda e, hd=hd, pt=pt: e.transpose(out=pt, in_=onb[:, hd * 128:(hd + 1) * 128], identity=idb[:]), reads=["onb", "idb"], writes=pn)
                    op("dve", lambda e, hd=hd, pt=pt: e.scalar_tensor_tensor(out=yaT[:, hd, pr * 128:(pr + 1) * 128], in0=pt, scalar=vcol(l, V_HGN)[:, hd:hd + 1], in1=sga[:, hd, pr * 128:(pr + 1) * 128], op0=ALU.mult, op1=ALU.mult),
                       reads=pn + ["vecs", "sga"], writes=["yaT"])
                for h in range(4):
                    psc = PB[5][:, 0:128]
                    pM = PB[5][:, 128:256]
                    op("pe", lambda e, h=h: e.matmul(psc, lhsT=kT[:, 2 * h, pr * 128:(pr + 1) * 128], rhs=qT[:, 2 * h, pr * 128:(pr + 1) * 128], start=True, stop=False), reads=["kT", "qT"], writes=["P5a"])
                    op("pe", lambda e, h=h: e.matmul(psc, lhsT=kT[:, 2 * h + 1, pr * 128:(pr + 1) * 128], rhs=qT[:, 2 * h + 1, pr * 128:(pr + 1) * 128], start=False, stop=True), reads=["kT", "qT"], writes=["P5a"])
                    op("pe", lambda e, h=h: e.matmul(pM, lhsT=sel4[:, h, :], rhs=rows[:, 5, :], start=True, stop=True), reads=["sel4", "rows"], writes=["P5b"])
                    op("act", lambda e, h=h: e.activation(out=DT[:, h % 2, :], in_=pM, func=AF.Exp, scale=-1.0, bias=gcol[:, pr, h:h + 1]), reads=["P5b", "gcol"], writes=["DT%d" % (h % 2)])
                    op("pool", lambda e, h=h: e.tensor_mul(out=DTm[:, h % 2, :], in0=DT[:, h % 2, :], in1=maskm[:]), reads=["DT%d" % (h % 2), "maskm"], writes=["DTm%d" % (h % 2)])
                    stn = "STb%d" % (h % 2)
                    op("dve", lambda e, h=h: e.tensor_mul(out=STb[:, h % 2, :], in0=psc, in1=DTm[:, h % 2, :]), reads=["P5a", "DTm%d" % (h % 2)], writes=[stn])
                    pnum = PB[6][:, 0:257]
                    op("pe", lambda e, h=h: e.matmul(pnum, lhsT=STb[:, h % 2, :], rhs=vext[:, pr, h, :], start=True, stop=True), reads=[stn, "vext"], writes=["P6a", "P6b"])
                    op("act", lambda e: e.copy(out=tnum, in_=pnum), reads=["P6a", "P6b"], writes=["tnum"])
                    op("dve", lambda e, h=h: e.tensor_scalar(out=kw[:, h % 2, :], in0=ktok[:, pr, h * 256:(h + 1) * 256], scalar1=gcol[:, pr, 12 + h:13 + h], scalar2=None, op0=ALU.mult),
                       reads=["ktok", "gcol"], writes=["kw%d" % (h % 2)])
                    cn = "C%d" % h
                    cbn = "Cb%d" % h
                    for eo in range(2):
                        pint = PB[eo][:, 0:257]
                        pintn = ["P%da" % eo, "P%db" % eo]
                        rsl = slice(eo * 64, (eo + 1) * 64)
                        for j in range(2):
                            op("pe", lambda e, h=h, j=j, pint=pint: e.matmul(pint, lhsT=qT[:, 2 * h + j, pr * 128:(pr + 1) * 128], rhs=Cbf[:, h, j, :], start=(j == 0), stop=(j == 1)), reads=["qT", cbn], writes=pintn)
                        op("dve", lambda e, h=h, rsl=rsl, pint=pint: e.scalar_tensor_tensor(out=tot[rsl, h, :], in0=pint[rsl, :], scalar=gcol[rsl, pr, 4 + h:5 + h], in1=tnum[rsl, :], op0=ALU.mult, op1=ALU.add),
                           reads=pintn + ["gcol", "tnum"], writes=["T4"])
                        for j in range(2):
                            pC = PB[3 + j][:, 0:257]
                            pCn = ["P3" if j == 0 else "P4"]
                            op("pe", lambda e, h=h, j=j, rsl=rsl, pC=pC: e.matmul(pC, lhsT=kw[rsl, h % 2, j * 128:(j + 1) * 128], rhs=vext[rsl, pr, h, :], start=True, stop=True), reads=["kw%d" % (h % 2), "vext"], writes=pCn)
                            op("dve", lambda e, h=h, j=j, eo=eo, pC=pC: e.scalar_tensor_tensor(out=Cst[:, h, j, :], in0=Cst[:, h, j, :], scalar=decb[:, pr, eo, h:h + 1], in1=pC, op0=ALU.mult, op1=ALU.add),
                               reads=pCn + ["decb", cn], writes=[cn])
                            op("act", lambda e, h=h, j=j: e.copy(out=Cbf[:, h, j, :], in_=Cst[:, h, j, :]), reads=[cn], writes=[cbn])
                den = tot[:, :, 256]
                op("dve", lambda e: e.tensor_scalar_mul(out=sm[:, 16:20], in0=den, scalar1=-1.0), reads=["T4"], writes=["sm"])
                op("dve", lambda e: e.tensor_max(out=sm[:, 4:8], in0=sm[:, 16:20], in1=den), reads=["T4", "sm"], writes=["sm"])
                op("dve", lambda e: e.tensor_max(out=sm[:, 4:8], in0=sm[:, 4:8], in1=gcol[:, pr, 8:12]), reads=["sm", "gcol"], writes=["sm"])
                op("dve", lambda e: e.reciprocal(out=sm[:, 4:8], in_=sm[:, 4:8]), reads=["sm"], writes=["sm"])
                op("dve", lambda e: e.tensor_reduce(out=sm[:, 8:12], in_=tot[:, :, 0:256], axis=AX.X, op=ALU.add), reads=["T4"], writes=["sm"])
                op("pool", lambda e: e.tensor_mul(out=sqm, in0=tot[:, :, 0:256], in1=tot[:, :, 0:256]), reads=["T4"], writes=["T3"])
                op("dve", lambda e: e.tensor_reduce(out=sm[:, 12:16], in_=sqm, axis=AX.X, op=ALU.add), reads=["T3"], writes=["sm"])
                op("dve", lambda e: e.tensor_scalar_mul(out=sm[:, 8:12], in0=sm[:, 8:12], scalar1=1.0 / 256), reads=["sm"], writes=["sm"])
                op("dve", lambda e: e.tensor_mul(out=sm[:, 16:20], in0=sm[:, 8:12], in1=sm[:, 8:12]), reads=["sm"], writes=["sm"])
                op("dve", lambda e: e.scalar_tensor_tensor(out=sm[:, 12:16], in0=sm[:, 12:16], scalar=1.0 / 256, in1=sm[:, 16:20], op0=ALU.mult, op1=ALU.subtract), reads=["sm"], writes=["sm"])
                op("dve", lambda e: e.tensor_mul(out=sm[:, 16:20], in0=sm[:, 4:8], in1=sm[:, 4:8]), reads=["sm"], writes=["sm"])
                op("dve", lambda e: e.tensor_mul(out=sm[:, 12:16], in0=sm[:, 12:16], in1=sm[:, 16:20]), reads=["sm"], writes=["sm"])
                op("dve", lambda e: e.tensor_scalar(out=sm[:, 12:16], in0=sm[:, 12:16], scalar1=0.0, scalar2=EPS, op0=ALU.max, op1=ALU.add), reads=["sm"], writes=["sm"])
                op("act", lambda e: e.activation(out=sm[:, 12:16], in_=sm[:, 12:16], func=AF.Ln), reads=["sm"], writes=["sm"])
                op("act", lambda e: e.activation(out=sm[:, 12:16], in_=sm[:, 12:16], func=AF.Exp, scale=-0.5), reads=["sm"], writes=["sm"])
                op("dve", lambda e: e.tensor_mul(out=sm[:, 20:24], in0=sm[:, 4:8], in1=sm[:, 12:16]), reads=["sm"], writes=["sm"])
                op("dve", lambda e: e.scalar_tensor_tensor(out=sm[:, 24:28], in0=sm[:, 8:12], scalar=-1.0, in1=sm[:, 20:24], op0=ALU.mult, op1=ALU.mult), reads=["sm"], writes=["sm"])
                for h in range(4):
                    op("dve", lambda e, h=h: e.tensor_scalar(out=hnb[:, h * 256:(h + 1) * 256], in0=tot[:, h, 0:256], scalar1=sm[:, 20 + h:21 + h], scalar2=sm[:, 24 + h:25 + h], op0=ALU.mult, op1=ALU.add),
                       reads=["T4", "sm"], writes=["hnb"])
                for dc in range(8):
                    pt, pn = p7((dc % 2) * 2 + 1, 128)
                    op("pe", lambda e, dc=dc, pt=pt: e.transpose(out=pt, in_=hnb[:, dc * 128:(dc + 1) * 128], identity=idb[:]), reads=["hnb", "idb"], writes=pn)
                    cs = slice(pr * 128, (pr + 1) * 128)
                    op("pool", lambda e, dc=dc, cs=cs: e.tensor_scalar(out=ybT[:, dc, cs], in0=xcT[:, dc, cs], scalar1=vcol(l, V_SKIP)[:, dc:dc + 1], scalar2=None, op0=ALU.mult),
                       reads=["vecs", "xcT"], writes=["ybT"])
                    op("dve", lambda e, dc=dc, pt=pt, cs=cs: e.scalar_tensor_tensor(out=ybT[:, dc, cs], in0=pt, scalar=vcol(l, V_MGN)[:, dc:dc + 1], in1=ybT[:, dc, cs], op0=ALU.mult, op1=ALU.add),
                       reads=pn + ["vecs", "ybT"], writes=["ybT"])
                    op("pool", lambda e, dc=dc, cs=cs: e.tensor_mul(out=ybT[:, dc, cs], in0=ybT[:, dc, cs], in1=sob[:, dc, cs]), reads=["ybT", "sob"], writes=["ybT"])
            for g in range(4):
                wa, wan = load_wg(wpa_s[l, g], "wpa%d" % l)
                wb, wbn = load_wg(wpb_s[l, g], "wpb%d" % l)
                for half in range(2):
                    m = g * 2 + half
                    i = m % 2
                    fm_chunk(wa, wan, half, yaT, "yaT", lambda pv, pn, m=m, i=i: op("dve", lambda e: e.tensor_mul(out=m1[:, i, :], in0=pv, in1=sgA[:, m, :]), reads=pn + ["sgA"], writes=["T3"]))
                    fm_chunk(wb, wbn, half, ybT, "ybT", lambda pv, pn, m=m, i=i: op("dve", lambda e: e.tensor_mul(out=m2[:, i, :], in0=pv, in1=sgB[:, m, :]), reads=pn + ["sgB"], writes=["T4"]))
                    op("pool", lambda e, m=m, i=i: e.tensor_add(out=mixT[:, m, :], in0=m1[:, i, :], in1=m2[:, i, :]), reads=["T3", "T4"], writes=["mixT"])
            for s in range(2):
                ybanks = (PB[3], PB[4]) if s == 0 else (PB[5], PB[6])
                ybn = ["P3", "P4"] if s == 0 else ["P5a", "P5b", "P5c", "P5d", "P6a", "P6b"]
                for g in range(4):
                    w, wn = load_wg(wo_s[l, g], "wo%d" % l)
                    yv = ybanks[g // 2][:, (g % 2) * 256:(g % 2) * 256 + 256]
                    for kc in range(8):
                        op("pe", lambda e, kc=kc, yv=yv, w=w, s=s: e.matmul(yv, lhsT=mixT[:, kc, s * 128:(s + 1) * 128], rhs=w[:, kc, :], start=(kc == 0), stop=(kc == 7)),
                           reads=["mixT", wn], writes=ybn)
                residual_update(l, t0, s, [(ybanks[0][:, :], 0, 512), (ybanks[1][:, :], 512, 512)], ybn, xt, ysb, tmpx, junk, ssy, rsy, ggm, "ggm", ysbn="T1", tmpxn="T2", junkn="hnb")
            if DEBUG and l == 0 and ti == 0:
                for (nm, tl, dn) in (("hT", hT, "hT"), ("yaT", yaT, "yaT"), ("ybT", ybT, "ybT"), ("mixT", mixT, "mixT"), ("qT", qT, "qT"), ("kT", kT, "kT"), ("vT", vT, "vT"), ("xcT", xcT, "xcT"), ("KT", KT, "KT"), ("sga", sga, "sga")):
                    dd = nc.dram_tensor("dbg_" + nm, [128, 8, TT], BF16, kind="ExternalOutput").ap()
                    last_x_dma["dbg_" + nm] = S.dma("sp", dd, tl, reads=[dn], key="dbg_" + nm)
                dd = nc.dram_tensor("dbg_gcol", [128, 2, 16], F32, kind="ExternalOutput").ap()
                last_x_dma["dbg_gcol"] = S.dma("sp", dd, gcol, reads=["gcol"], key="dbg_gcol")
                dd = nc.dram_tensor("dbg_tot", [128, 4, 257], F32, kind="ExternalOutput").ap()
                last_x_dma["dbg_tot"] = S.dma("sp", dd, tot, reads=["T4"], key="dbg_tot")

    def ffn(l):
        AR.reset()
        S.barrier()
        A = AR.alloc
        moe = (l % 2 == 1)
        j = l // 2
        NG = 14 if moe else 11
        NFC = 2 * NG
        NE = 8 if moe else 1
        xt = A([128, 4, D])
        hT = A([128, 8, TF], BF16)
        junk = A([128, D], BF16); ss = A([128, 4]); rs = A([128, 4]); ssy = A([128, 2]); rsy = A([128, 2])
        aT = A([128, NFC, TF], BF16)
        w2t = [A([128, NFC, 512], BF16) for _ in range(2)]
        NW13 = 2
        w13 = [A([128, 2, 8, 256], BF16) for _ in range(NW13)]
        if moe:
            xn = w2t[0].rearrange("p a b -> p (a b)")[:, 0:8192].bitcast(F32).rearrange("p (a b) -> p a b", b=D)
            hTf = w2t[1].rearrange("p a b -> p (a b)")[:, 0:8192].bitcast(F32).rearrange("p (a b) -> p a b", b=TF)
            xnn, hTfn = "w2_0", "w2_1"
        else:
            xn = A([128, 4, D], BF16)
            hTf = None
            xnn, hTfn = "xn", "hTf"
        yacc = A([128, 4, D])
        gt = A([128, TF])
        tmpx = A([128, D])
        lg = A([128, 4, 8]); gates = A([128, 4, 8]); gm = A([128, 4, 8])
        mx1 = A([128, 4]); mx2 = A([128, 4]); den = A([128, 4])
        print("ffn arena words", AR.off)
        cnt = {"w13": 0, "w2": 0, "g": 0, "y": 0}
        for ti in range(n_ffn_tiles):
            t0 = ti * TF
            load_norm_transpose(l, False, t0, 4, hsc_f, hbi_f, "hsc_f", "hbi_f", xt, xn, hT, junk, ss, rs, hTf=hTf, xnn=xnn, hTfn=hTfn)
            if moe:
                for s in range(4):
                    pl = PB[5][:, 256 + s * 8:256 + s * 8 + 8]
                    for kc in range(8):
                        op("pe", lambda e, s=s, kc=kc, pl=pl: e.matmul(pl, lhsT=hTf[:, kc, s * 128:(s + 1) * 128], rhs=rtt[:, kc, :], start=(kc == 0), stop=(kc == 7)), reads=[hTfn, "rtt"], writes=["P5c"])
                op("dve", lambda e: e.tensor_copy(out=lg, in_=PB[5][:, 256:288].rearrange("p (s e) -> p s e", e=8)), reads=["P5c"], writes=["lg"])
                op("dve", lambda e: e.tensor_reduce(out=mx1, in_=lg, axis=AX.X, op=ALU.max), reads=["lg"], writes=["mx1"])
                op("dve", lambda e: e.tensor_tensor(out=gm, in0=lg, in1=bc(mx1.unsqueeze(2), [128, 4, 8]), op=ALU.is_equal), reads=["lg", "mx1"], writes=["gm"])
                op("dve", lambda e: e.scalar_tensor_tensor(out=gm, in0=gm, scalar=-1e30, in1=lg, op0=ALU.mult, op1=ALU.add), reads=["gm", "lg"], writes=["gm"])
                op("dve", lambda e: e.tensor_reduce(out=mx2, in_=gm, axis=AX.X, op=ALU.max), reads=["gm"], writes=["mx2"])
                op("dve", lambda e: e.tensor_tensor(out=gm, in0=lg, in1=bc(mx2.unsqueeze(2), [128, 4, 8]), op=ALU.is_ge), reads=["lg", "mx2", "gm"], writes=["gm"])
                op("dve", lambda e: e.tensor_sub(out=lg, in0=lg, in1=bc(mx1.unsqueeze(2), [128, 4, 8])), reads=["lg", "mx1"], writes=["lg"])
                op("act", lambda e: e.activation(out=lg, in_=lg, func=AF.Exp), reads=["lg"], writes=["lg"])
                op("dve", lambda e: e.tensor_sub(out=den, in0=mx2, in1=mx1), reads=["mx1", "mx2"], writes=["den"])
                op("act", lambda e: e.activation(out=den, in_=den, func=AF.Exp), reads=["den"], writes=["den"])
                op("dve", lambda e: e.tensor_scalar_add(out=den, in0=den, scalar1=1.0), reads=["den"], writes=["den"])
                op("dve", lambda e: e.reciprocal(out=den, in_=den), reads=["den"], writes=["den"])
                op("dve", lambda e: e.tensor_mul(out=gates, in0=lg, in1=gm), reads=["lg", "gm"], writes=["gates"])
                op("dve", lambda e: e.tensor_mul(out=gates, in0=gates, in1=bc(den.unsqueeze(2), [128, 4, 8])), reads=["gates", "den"], writes=["gates"])
            for ex in range(NE):
                for g in range(NG):
                    i = cnt["w13"] % NW13
                    cnt["w13"] += 1
                    wn = "w13_%d" % i
                    if moe:
                        S.dma("sp", w13[i], m13_s[j, ex, g], reads=["m13_%d_%d" % (l, ex)], writes=[wn], key=wn)
                    else:
                        S.dma("sp", w13[i], f13_s[j, g], reads=["f13_%d" % l], writes=[wn], key=wn)
                    for half in range(2):
                        fc = g * 2 + half
                        gi = cnt["g"] % 2
                        cnt["g"] += 1
                        pg_, pu_ = PB[gi], PB[2 + gi]
                        pgn = ["P%da" % gi, "P%db" % gi]
                        pun = ["P2a", "P2b", "P2c", "P2d"] if gi == 0 else ["P3"]
                        for kc in range(8):
                            op("pe", lambda e, kc=kc, half=half, w=w13[i], pg_=pg_: e.matmul(pg_[:, :], lhsT=w[:, 0, kc, half * 128:(half + 1) * 128], rhs=hT[:, kc, :], start=(kc == 0), stop=(kc == 7)), reads=[wn, "hT"], writes=pgn)
                        for kc in range(8):
                            op("pe", lambda e, kc=kc, half=half, w=w13[i], pu_=pu_: e.matmul(pu_[:, :], lhsT=w[:, 1, kc, half * 128:(half + 1) * 128], rhs=hT[:, kc, :], start=(kc == 0), stop=(kc == 7)), reads=[wn, "hT"], writes=pun)
                        op("act", lambda e, pg_=pg_: e.activation(out=gt, in_=pg_[:, :], func=AF.Silu), reads=pgn, writes=["gt"])
                        op("dve", lambda e, fc=fc, pu_=pu_: e.tensor_mul(out=aT[:, fc, :], in0=pu_[:, :], in1=gt), reads=pun + ["gt"], writes=["aT"])
                for half in range(2):
                    wi = cnt["w2"] % 2
                    cnt["w2"] += 1
                    w2n = "w2_%d" % wi
                    if moe:
                        S.dma("sp", w2t[wi], m2_s[j, ex, half], reads=["m2_%d_%d" % (l, ex)], writes=[w2n], key=w2n)
                    else:
                        S.dma("sp", w2t[wi], f2_s[j, half], reads=["f2_%d" % l], writes=[w2n], key=w2n)
                    for s in range(4):
                        yi = cnt["y"] % 2
                        cnt["y"] += 1
                        py = PB[4 + yi]
                        pyn = ["P4"] if yi == 0 else ["P5a", "P5b", "P5c", "P5d"]
                        for fc in range(NFC):
                            op("pe", lambda e, fc=fc, s=s, py=py, w=w2t[wi]: e.matmul(py[:, :], lhsT=aT[:, fc, s * 128:(s + 1) * 128], rhs=w[:, fc, :], start=(fc == 0), stop=(fc == NFC - 1)), reads=["aT", w2n], writes=pyn)
                        ya = yacc[:, s, half * 512:(half + 1) * 512]
                        if not moe:
                            op("act", lambda e, py=py, ya=ya: e.copy(out=ya, in_=py[:, :]), reads=pyn, writes=["yacc"])
                        elif ex == 0:
                            op("dve", lambda e, py=py, ya=ya, s=s, ex=ex: e.tensor_scalar(out=ya, in0=py[:, :], scalar1=gates[:, s, ex:ex + 1], scalar2=None, op0=ALU.mult), reads=pyn + ["gates"], writes=["yacc"])
                        else:
                            op("dve", lambda e, py=py, ya=ya, s=s, ex=ex: e.scalar_tensor_tensor(out=ya, in0=py[:, :], scalar=gates[:, s, ex:ex + 1], in1=ya, op0=ALU.mult, op1=ALU.add), reads=pyn + ["gates", "yacc"], writes=["yacc"])
            for s in range(4):
                residual_update(l, t0, s, None, None, xt, None, tmpx, junk, ssy, rsy, ggf, "ggf", yacc=yacc[:, s, :])

    for l in range(n_layers):
        layer_prologue(l)
        mixer(l)
        if do_ffn:
            ffn(l)
    if S.limit is not None:
        S.emit(final_waits=[S.ops[e][-1] for e in S.ENGS if S.ops[e]])
    else:
        S.emit(final_waits=list(last_x_dma.values()))
    S.close()
    return nc


def prep_inputs(inputs):
    f = lambda a: np.ascontiguousarray(np.asarray(a, dtype=np.float32))
    col = lambda v: f(v).reshape(8, 128).T
    L = DEPTH
    cols = []
    for l in range(L):
        vs = [inputs["g_pre_mix"][l], inputs["g_pre_ffn"][l], inputs["hgrn_gnorm"][l],
              inputs["mlstm_conv_w"][l][0], inputs["mlstm_conv_w"][l][1], inputs["mlstm_conv_w"][l][2], inputs["mlstm_conv_w"][l][3],
              inputs["mlstm_conv_b"][l], inputs["mlstm_gnorm"][l], inputs["mlstm_skip"][l]]
        for v in vs:
            cols.append(col(v))
    for l in range(L):
        cols.append(col(inputs["hgrn_lb"][l]))
    vecs = f(np.concatenate(cols, axis=1))
    bd = np.zeros((L, 128, 3, 8, 128), np.float32)
    for mi, nm in enumerate(("mlstm_wq", "mlstm_wk", "mlstm_wv")):
        w = f(inputs[nm]).reshape(L, 8, 32, 4, 4)
        for n in range(32):
            bd[:, 4 * n:4 * n + 4, mi, :, 4 * n:4 * n + 4] = w[:, :, n].transpose(0, 2, 1, 3)
    bd = f(bd.reshape(L, 128, 3 * 8 * 128))
    wg = np.concatenate([f(inputs["mlstm_w_ig"]), f(inputs["mlstm_w_fg"])], axis=2)
    wg = f(wg.reshape(L, 24, 128, 8).transpose(0, 2, 1, 3).reshape(L, 128, 192))
    bg = f(np.concatenate([f(inputs["mlstm_b_ig"]), f(inputs["mlstm_b_fg"])], axis=1))
    rt = f(f(inputs["moe_router"]).reshape(2, 8, 128, 8).transpose(0, 2, 1, 3).reshape(2, 128, 64))
    shared = {
        "vecs": vecs, "w_ada": f(inputs["w_ada"]), "b_ada": f(inputs["b_ada"]),
        "g_post_mix": f(inputs["g_post_mix"]), "g_post_ffn": f(inputs["g_post_ffn"]),
        "w_in": f(inputs["w_in"]), "bd": bd, "wgate": wg, "bgate": bg,
        "w_proj_a": f(inputs["w_proj_a"]), "w_proj_b": f(inputs["w_proj_b"]), "w_out": f(inputs["w_out"]),
        "ffn_w1": f(inputs["ffn_w1"]), "ffn_w3": f(inputs["ffn_w3"]), "ffn_w2": f(inputs["ffn_w2"]),
        "router": rt, "moe_w1": f(inputs["moe_w1"]), "moe_w3": f(inputs["moe_w3"]), "moe_w2": f(inputs["moe_w2"]),
    }
    x = f(inputs["x"])
    c = f(inputs["c"])
    maps = []
    for b in range(x.shape[0]):
        m = dict(shared)
        m["x"] = x[b]
        m["ccol"] = f(c[b].reshape(8, 128).T)
        maps.append(m)
    return maps


_NC_CACHE = {}


def kernel(**inputs):
    maps = prep_inputs(inputs)
    if "nc" not in _NC_CACHE:
        _NC_CACHE["nc"] = build_program()
    nc = _NC_CACHE["nc"]
    res = run_bass_kernel_spmd(nc, maps, core_ids=list(range(NCORES)))
    return np.stack([np.asarray(r["out"], dtype=np.float32) for r in res.results], axis=0)
```

```python
import contextlib
import types
import numpy as np
import concourse.bass as bass
import concourse.mybir as mybir
from concourse.bass_utils import run_bass_kernel_spmd

F32 = mybir.dt.float32
BF16 = mybir.dt.bfloat16
AF = mybir.ActivationFunctionType
ALU = mybir.AluOpType
AX = mybir.AxisListType

SEM_CAP = 30000
D = 1024
SEQ = 4096
DEPTH = 4
NCORES = 8
TT = 256
TF = 512
EPS = 1e-6
NV = 10
DEBUG = False


class Dep:
    __slots__ = ("name", "last_w", "readers")

    def __init__(self, name):
        self.name = name
        self.last_w = None
        self.readers = []


class Op:
    __slots__ = ("eng", "fn", "deps", "is_dma", "key", "needs_inc", "sem", "val")

    def __init__(self, eng, fn, is_dma=False, key=None):
        self.eng = eng
        self.fn = fn
        self.deps = []
        self.is_dma = is_dma
        self.key = key
        self.needs_inc = False
        self.sem = None
        self.val = 0


class Sched:
    ENGS = ("pe", "act", "dve", "pool", "sp")

    def __init__(self, nc):
        self.nc = nc
        self.ops = {e: [] for e in self.ENGS}
        self.all_ops = []
        self.deps = {}
        self.stack = contextlib.ExitStack()
        self.fence = []
        self.passed = {e: True for e in self.ENGS}

    def sb(self, name, shape, dtype=F32):
        return self.stack.enter_context(self.nc.sbuf_tensor("sb_" + name, list(shape), dtype))

    def ps(self, name, shape, dtype=F32):
        return self.stack.enter_context(self.nc.psum_tensor("ps_" + name, list(shape), dtype))

    def _D(self, x):
        d = self.deps.get(x)
        if d is None:
            d = self.deps[x] = Dep(x)
        return d

    def barrier(self):
        self.fence = [self.ops[e][-1] for e in self.ENGS if self.ops[e]]
        self.passed = {e: False for e in self.ENGS}

    limit = None

    def _record(self, op, reads, writes):
        if self.limit is not None and len(self.all_ops) >= self.limit:
            return op
        rr, ww = [], []
        for r in reads:
            if len(r) >= 2 and r[0] == "P" and r[1].isdigit():
                ww.append(r[:2])
            else:
                rr.append(r)
        for w in writes:
            if len(w) >= 2 and w[0] == "P" and w[1].isdigit():
                ww.append(w[:2])
            else:
                ww.append(w)
        reads, writes = rr, list(dict.fromkeys(ww))
        deps = []
        if not self.passed[op.eng]:
            deps.extend(self.fence)
            self.passed[op.eng] = True
        for r in reads:
            r = self._D(r)
            if r.last_w is not None:
                deps.append(r.last_w)
        for w in writes:
            w = self._D(w)
            if w.last_w is not None:
                deps.append(w.last_w)
            deps.extend(w.readers)
        seen = set()
        for d in deps:
            if d is op or id(d) in seen:
                continue
            seen.add(id(d))
            op.deps.append(d)
        for r in reads:
            self._D(r).readers.append(op)
        for w in writes:
            w = self._D(w)
            w.last_w = op
            w.readers = []
        self.ops[op.eng].append(op)
        self.all_ops.append(op)
        return op

    @staticmethod
    def _freeze(fn):
        if fn.__closure__ is None:
            return fn
        cells = []
        for c in fn.__closure__:
            try:
                cells.append(types.CellType(c.cell_contents))
            except ValueError:
                cells.append(c)
        return types.FunctionType(fn.__code__, fn.__globals__, fn.__name__, fn.__defaults__, tuple(cells))

    def op(self, eng, fn, reads=(), writes=()):
        return self._record(Op(eng, self._freeze(fn)), reads, writes)

    def dma(self, eng, out, in_, reads=(), writes=(), key=None, **kw):
        if key is None:
            key = writes[0] if writes else reads[0]
        fn = lambda e: e.dma_start(out=out, in_=in_, **kw)
        return self._record(Op(eng, fn, is_dma=True, key=key), reads, writes)

    def emit(self, final_waits=()):
        nc = self.nc
        for op in self.all_ops:
            for d in op.deps:
                if d.eng == "pe" and op.eng == "pe" and not d.is_dma:
                    continue
                d.needs_inc = True
        for op in final_waits:
            op.needs_inc = True
        print("ops per engine", {e: len(self.ops[e]) for e in self.ENGS})
        sems = {}

        def get_sem(name):
            s = sems.get(name)
            if s is None:
                s = sems[name] = self.stack.enter_context(nc.semaphore(name))
            return s

        cnt = {}
        ccnt = {e: 0 for e in self.ENGS}
        for op in self.all_ops:
            if op.is_dma:
                k = cnt.get(op.key, 0) + 1
                cnt[op.key] = k
                per = SEM_CAP // 16
                ep, v = divmod(k - 1, per)
                op.sem = get_sem("d_%s_%d" % (op.key, ep))
                op.val = (v + 1) * 16
            elif op.needs_inc:
                c = ccnt[op.eng]
                ep, v = divmod(c, SEM_CAP)
                op.sem = get_sem("e_%s_%d" % (op.eng, ep))
                op.val = v + 1
                ccnt[op.eng] = c + 1
        engmap = {"pe": "tensor", "act": "scalar", "dve": "vector", "pool": "gpsimd", "sp": "sync"}

        def run(engname, eng):
            known = {}
            for op in self.ops[engname]:
                for d in op.deps:
                    if d.eng == "pe" and engname == "pe" and not d.is_dma:
                        continue
                    sid = d.sem.name
                    if known.get(sid, 0) >= d.val:
                        continue
                    eng.wait_ge(d.sem, d.val)
                    known[sid] = d.val
                ins = op.fn(eng)
                if op.is_dma:
                    ins.then_inc(op.sem, 16)
                elif op.needs_inc:
                    ins.then_inc(op.sem, 1)
            if engname == "sp":
                for op in final_waits:
                    eng.wait_ge(op.sem, op.val)

        with nc.Block() as block:
            for engname in self.ENGS:
                getattr(block, engmap[engname])(lambda eng, _n=engname: run(_n, eng))

    def close(self):
        self.stack.close()


class Arena:
    def __init__(self, S, name, words):
        self.t = S.sb(name, [128, words], F32)
        self.words = words
        self.off = 0

    def reset(self):
        self.off = 0

    def alloc(self, shape, dtype=F32):
        n = int(np.prod(shape[1:]))
        w = n if dtype == F32 else (n + 1) // 2
        assert self.off + w <= self.words, ("arena overflow", self.off, w, self.words)
        v = self.t[0:shape[0], self.off:self.off + w]
        self.off += w
        if dtype != F32:
            v = v.bitcast(dtype)[:, 0:n]
        if len(shape) == 3:
            v = v.rearrange("p (a b) -> p a b", b=shape[2])
        elif len(shape) == 4:
            v = v.rearrange("p (a b c) -> p a b c", b=shape[2], c=shape[3])
        elif len(shape) == 5:
            v = v.rearrange("p (a b c d) -> p a b c d", b=shape[2], c=shape[3], d=shape[4])
        return v


def bc(ap, shape):
    return ap.to_broadcast(list(shape))


def build_program(n_layers=DEPTH, do_ffn=True, n_mix_tiles=SEQ // TT, n_ffn_tiles=SEQ // TF):
    nc = bass.Bass("TRN2", target_bir_lowering=False)
    di = lambda name, shape, dt=F32: nc.dram_tensor(name, list(shape), dt, kind="ExternalInput").ap()
    x_in = di("x", [SEQ, D])
    ccol_d = di("ccol", [128, 8])
    vecs_d = di("vecs", [128, DEPTH * NV * 8 + DEPTH * 8])
    w_ada_d = di("w_ada", [DEPTH, D, 6 * D])
    b_ada_d = di("b_ada", [DEPTH, 6 * D])
    gpm_d = di("g_post_mix", [DEPTH, D])
    gpf_d = di("g_post_ffn", [DEPTH, D])
    w_in_d = di("w_in", [DEPTH, D, 8 * D])
    bd_d = di("bd", [DEPTH, 128, 3 * 8 * 128])
    wgate_d = di("wgate", [DEPTH, 128, 24 * 8])
    bgate_d = di("bgate", [DEPTH, 8])
    wpa_d = di("w_proj_a", [DEPTH, D, D])
    wpb_d = di("w_proj_b", [DEPTH, D, D])
    wo_d = di("w_out", [DEPTH, D, D])
    if not do_ffn:
        di = lambda name, shape, dt=F32: nc.dram_tensor(name, [1, 1], dt, kind="ExternalInput").ap()
    f1_d = di("ffn_w1", [2, D, 2816])
    f3_d = di("ffn_w3", [2, D, 2816])
    f2_d = di("ffn_w2", [2, 2816, D])
    rt_d = di("router", [2, 128, 64])
    m1_d = di("moe_w1", [2, 8, D, 3584])
    m3_d = di("moe_w3", [2, 8, D, 3584])
    m2_d = di("moe_w2", [2, 8, 3584, D])
    out_d = nc.dram_tensor("out", [SEQ, D], F32, kind="ExternalOutput").ap()
    sc = lambda name, shape: nc.dram_tensor(name, list(shape), BF16).ap()
    win_s = sc("win_s", [DEPTH, 32, 128, 8, 256])
    wpa_s = sc("wpa_s", [DEPTH, 4, 128, 8, 256])
    wpb_s = sc("wpb_s", [DEPTH, 4, 128, 8, 256])
    wo_s = sc("wo_s", [DEPTH, 4, 128, 8, 256])
    f13_s = sc("f13_s", [2, 11, 128, 2, 8, 256])
    f2_s = sc("f2_s", [2, 2, 128, 22, 512])
    m13_s = sc("m13_s", [2, 8, 14, 128, 2, 8, 256])
    m2_s = sc("m2_s", [2, 8, 2, 128, 28, 512])

    S = Sched(nc)
    op = S.op
    idf = S.sb("idf", [128, 128], F32)
    idb = S.sb("idb", [128, 128], BF16)
    mask2 = S.sb("mask2", [128, 128], F32)
    maskm = S.sb("maskm", [128, 128], F32)
    mask01 = S.sb("mask01", [128, TT], F32)
    negm = S.sb("negm", [4, 128], F32)
    m01r = S.sb("m01r", [4, 128], F32)
    ones4 = S.sb("ones4", [4, 128], F32)
    sel4 = S.sb("sel4", [4, 4, 128], F32)
    id4 = S.sb("id4", [4, 4], F32)
    onesb = S.sb("onesb", [1, 128], BF16)
    vecs = S.sb("vecs", [128, DEPTH * NV * 8 + DEPTH * 8], F32)
    lbc = S.sb("lbc", [128, DEPTH, 8], F32)
    oml = S.sb("oml", [128, DEPTH, 8], F32)
    noml = S.sb("noml", [128, DEPTH, 8], F32)
    cbc = S.sb("cbc", [128, 8, 128], BF16)
    ccol = S.sb("ccol", [128, 8], F32)
    cact = S.sb("cact", [128, 8], F32)
    hsc_m = S.sb("hsc_m", [128, 8], F32)
    hbi_m = S.sb("hbi_m", [128, 8], F32)
    hsc_f = S.sb("hsc_f", [128, 8], F32)
    hbi_f = S.sb("hbi_f", [128, 8], F32)
    ggm = S.sb("ggm", [128, D], F32)
    ggf = S.sb("ggf", [128, D], F32)
    bdt = S.sb("bdt", [128, 3, 8, 128], BF16)
    wgt = S.sb("wgt", [128, 24, 8], BF16)
    bgt = S.sb("bgt", [128, 8], F32)
    rtt = S.sb("rtt", [128, 8, 8], F32)
    hst = S.sb("hst", [128, 8, 128], F32)
    hstb = S.sb("hstb", [128, 2, 2, 128], BF16)
    Cst = S.sb("Cst", [128, 4, 2, 257], F32)
    Cbf = S.sb("Cbf", [128, 4, 2, 257], BF16)
    mprev = S.sb("mprev", [4, 1], F32)
    PB = [S.ps("pb%d" % i, [128, 512], F32) for i in range(8)]
    P7b = PB[7][:, :].bitcast(BF16)
    P7N = ["P7a", "P7b", "P7c", "P7d"]

    def p7(q, n):
        nq = (n + 255) // 256
        return P7b[:, q * 256:q * 256 + n], P7N[q:q + nq]
    ARW = 42100
    AR = Arena(S, "arena", ARW)

    def vcol(l, v):
        o = (l * NV + v) * 8
        return vecs[:, o:o + 8]

    V_GPRE_M, V_GPRE_F, V_HGN, V_CW0, V_CB, V_MGN, V_SKIP = 0, 1, 2, 3, 7, 8, 9

    op("pool", lambda e: e.memset(idf[:], 0.0), writes=["idf"])
    op("pool", lambda e: e.affine_select(out=idf[:], in_=idf[:], pattern=[[-1, 128]], compare_op=ALU.not_equal,
                                         fill=1.0, base=0, channel_multiplier=1), reads=["idf"], writes=["idf"])
    op("dve", lambda e: e.tensor_copy(out=idb[:], in_=idf[:]), reads=["idf"], writes=["idb"])
    op("pool", lambda e: e.memset(mask2[:], 1.0), writes=["mask2"])
    op("pool", lambda e: e.affine_select(out=mask2[:], in_=mask2[:], pattern=[[1, 128]], compare_op=ALU.is_ge,
                                         fill=0.0, base=0, channel_multiplier=-1), reads=["mask2"], writes=["mask2"])
    op("pool", lambda e: e.memset(mask2[0:64, 64:128], 0.0), reads=["mask2"], writes=["mask2"])
    op("pool", lambda e: e.tensor_scalar_mul(out=maskm[:], in0=mask2[:], scalar1=1.0 / 16.0), reads=["mask2"], writes=["maskm"])
    op("pool", lambda e: e.memset(mask01[:], 1.0), writes=["mask01"])
    op("pool", lambda e: e.memset(mask01[:].rearrange("p (c j) -> p c j", j=64)[:, :, 0:1], 0.0), reads=["mask01"], writes=["mask01"])
    op("pool", lambda e: e.memset(negm[:], 0.0), writes=["negm"])
    op("pool", lambda e: e.memset(negm[:].rearrange("p (c j) -> p c j", j=64)[:, :, 0:1], -1e30), reads=["negm"], writes=["negm"])
    op("pool", lambda e: e.memset(m01r[:], 1.0), writes=["m01r"])
    op("pool", lambda e: e.memset(m01r[:].rearrange("p (c j) -> p c j", j=64)[:, :, 0:1], 0.0), reads=["m01r"], writes=["m01r"])
    op("pool", lambda e: e.memset(ones4[:], 1.0), writes=["ones4"])
    op("pool", lambda e: e.tensor_copy(out=id4[:], in_=idf[0:4, 0:4]), reads=["idf"], writes=["id4"])
    op("pool", lambda e: e.tensor_copy(out=sel4[:], in_=bc(idf[0:4, 0:4].unsqueeze(2), [4, 4, 128])), reads=["idf"], writes=["sel4"])
    op("pool", lambda e: e.memset(onesb[:], 1.0), writes=["onesb"])
    S.dma("sp", vecs[:], vecs_d, writes=["vecs"])
    S.dma("sp", ccol[:], ccol_d, writes=["ccol"])
    op("act", lambda e: e.activation(out=cact[:], in_=ccol[:], func=AF.Silu), reads=["ccol"], writes=["cact"])
    op("dve", lambda e: e.tensor_copy(out=cbc[:], in_=bc(cact[:].unsqueeze(2), [128, 8, 128])), reads=["cact"], writes=["cbc"])
    lbraw = vecs[:, DEPTH * NV * 8:DEPTH * NV * 8 + DEPTH * 8].rearrange("p (l c) -> p l c", c=8)
    lbe = S.sb("lbe", [128, DEPTH, 8], F32)
    lbs = S.sb("lbs", [128, 8], F32)
    op("act", lambda e: e.activation(out=lbe[:], in_=lbraw, func=AF.Exp), reads=["vecs"], writes=["lbe"])
    op("dve", lambda e: e.tensor_add(out=lbs[:], in0=lbe[:, 0, :], in1=lbe[:, 1, :]), reads=["lbe"], writes=["lbs"])
    op("dve", lambda e: e.tensor_add(out=lbs[:], in0=lbs[:], in1=lbe[:, 2, :]), reads=["lbe", "lbs"], writes=["lbs"])
    op("dve", lambda e: e.tensor_add(out=lbs[:], in0=lbs[:], in1=lbe[:, 3, :]), reads=["lbe", "lbs"], writes=["lbs"])
    op("dve", lambda e: e.reciprocal(out=lbs[:], in_=lbs[:]), reads=["lbs"], writes=["lbs"])
    op("dve", lambda e: e.tensor_mul(out=lbe[:], in0=lbe[:], in1=bc(lbs[:].unsqueeze(1), [128, DEPTH, 8])), reads=["lbe", "lbs"], writes=["lbe"])
    op("dve", lambda e: e.memset(lbc[:, 0, :], 0.0), writes=["lbc"])
    for l in range(1, DEPTH):
        op("dve", lambda e, l=l: e.tensor_add(out=lbc[:, l, :], in0=lbc[:, l - 1, :], in1=lbe[:, l, :]), reads=["lbe", "lbc"], writes=["lbc"])
    op("dve", lambda e: e.tensor_scalar(out=oml[:], in0=lbc[:], scalar1=-1.0, scalar2=1.0, op0=ALU.mult, op1=ALU.add), reads=["lbc"], writes=["oml"])
    op("dve", lambda e: e.tensor_scalar_mul(out=noml[:], in0=oml[:], scalar1=-1.0), reads=["oml"], writes=["noml"])

    def conv_kxn(dst, src, ngroups, depname):
        v = src.rearrange("(kc p) (g c) -> g p kc c", p=128, c=256)
        for g in range(ngroups):
            S.dma("pool", dst[g], v[g], writes=[depname])

    def conv_layer(l):
        conv_kxn(win_s[l], w_in_d[l], 32, "win%d" % l)
        conv_kxn(wpa_s[l], wpa_d[l], 4, "wpa%d" % l)
        conv_kxn(wpb_s[l], wpb_d[l], 4, "wpb%d" % l)
        conv_kxn(wo_s[l], wo_d[l], 4, "wo%d" % l)
        if not do_ffn:
            return
        j = l // 2
        if l % 2 == 0:
            v1 = f1_d[j].rearrange("(kc p) (g c) -> g p kc c", p=128, c=256)
            v3 = f3_d[j].rearrange("(kc p) (g c) -> g p kc c", p=128, c=256)
            for g in range(11):
                S.dma("pool", f13_s[j, g, :, 0], v1[g], writes=["f13_%d" % l])
                S.dma("pool", f13_s[j, g, :, 1], v3[g], writes=["f13_%d" % l])
            v2 = f2_d[j].rearrange("(fc p) (h c) -> h p fc c", p=128, c=512)
            for h in range(2):
                S.dma("pool", f2_s[j, h], v2[h], writes=["f2_%d" % l])
        else:
            for ex in range(8):
                v1 = m1_d[j, ex].rearrange("(kc p) (g c) -> g p kc c", p=128, c=256)
                v3 = m3_d[j, ex].rearrange("(kc p) (g c) -> g p kc c", p=128, c=256)
                for g in range(14):
                    S.dma("pool", m13_s[j, ex, g, :, 0], v1[g], writes=["m13_%d_%d" % (l, ex)])
                    S.dma("pool", m13_s[j, ex, g, :, 1], v3[g], writes=["m13_%d_%d" % (l, ex)])
                v2 = m2_d[j, ex].rearrange("(fc p) (h c) -> h p fc c", p=128, c=512)
                for h in range(2):
                    S.dma("pool", m2_s[j, ex, h], v2[h], writes=["m2_%d_%d" % (l, ex)])

    for l in range(n_layers):
        conv_layer(l)

    def rstd_from_ss(rs, ss, n, dep_ss, dep_rs):
        op("dve", lambda e: e.tensor_scalar(out=rs, in0=ss, scalar1=1.0 / n, scalar2=EPS, op0=ALU.mult, op1=ALU.add), reads=[dep_ss], writes=[dep_rs])
        op("act", lambda e: e.activation(out=rs, in_=rs, func=AF.Ln), reads=[dep_rs], writes=[dep_rs])
        op("act", lambda e: e.activation(out=rs, in_=rs, func=AF.Exp, scale=-0.5), reads=[dep_rs], writes=[dep_rs])

    xkeys = {}
    last_x_dma = {}

    def xsrc(l, first):
        return x_in if (l == 0 and first) else out_d

    def layer_prologue(l):
        AR.reset()
        S.barrier()
        wad = [AR.alloc([128, 8, 512], BF16) for _ in range(2)]
        bad = AR.alloc([1, 6 * D], BF16)
        gpb = [AR.alloc([128, D], F32) for _ in range(2)]
        tmpd = AR.alloc([128, 4, 128], F32)
        S.dma("pool", bad, b_ada_d[l:l + 1, :], writes=["bad"])
        S.dma("sp", gpb[0], gpm_d[l].partition_broadcast(128), writes=["gpb0"])
        S.dma("sp", gpb[1], gpf_d[l].partition_broadcast(128), writes=["gpb1"])
        S.dma("pool", bdt[:].rearrange("p a b c -> p (a b c)"), bd_d[l], writes=["bdt"])
        S.dma("pool", wgt[:].rearrange("p a b -> p (a b)"), wgate_d[l], writes=["wgt"])
        S.dma("sp", bgt[:], bgate_d[l].partition_broadcast(128), writes=["bgt"])
        if l % 2 == 1:
            S.dma("sp", rtt[:].rearrange("p a b -> p (a b)"), rt_d[l // 2], writes=["rtt"])
        wv = w_ada_d[l].rearrange("(kc p) (g c) -> g p kc c", p=128, c=512)
        for g in range(12):
            w = wad[g % 2]
            wn = "wad%d" % (g % 2)
            S.dma("pool", w, wv[g], writes=[wn], key="pl3_%d" % (g % 2))
            pb = PB[g % 2]
            pn = ["P%da" % (g % 2), "P%db" % (g % 2)]
            for kc in range(8):
                op("pe", lambda e, w=w, kc=kc, pb=pb: e.matmul(pb[:, :], lhsT=cbc[:, kc, :], rhs=w[:, kc, :], start=(kc == 0), stop=False),
                   reads=[wn, "cbc"], writes=pn)
            op("pe", lambda e, g=g, pb=pb: e.matmul(pb[:, :], lhsT=onesb[0:1, :], rhs=bad[0:1, g * 512:(g + 1) * 512], start=False, stop=True),
               reads=["bad", "onesb"], writes=pn)
            which = g // 2
            half = g % 2
            if which in (2, 5):
                dst = ggm if which == 2 else ggf
                dn = "ggm" if which == 2 else "ggf"
                gp = gpb[0] if which == 2 else gpb[1]
                gn = "gpb0" if which == 2 else "gpb1"
                op("dve", lambda e, pb=pb, dst=dst, gp=gp, half=half: e.tensor_mul(out=dst[:, half * 512:(half + 1) * 512], in0=pb[:, :], in1=gp[:, half * 512:(half + 1) * 512]),
                   reads=pn + [gn], writes=[dn])
            else:
                dst = {0: hbi_m, 1: hsc_m, 3: hbi_f, 4: hsc_f}[which]
                dn = {0: "hbi_m", 1: "hsc_m", 3: "hbi_f", 4: "hsc_f"}[which]
                op("dve", lambda e, pb=pb: e.tensor_mul(out=tmpd, in0=pb[:, :].rearrange("p (c j) -> p c j", j=128), in1=bc(idf[:].unsqueeze(1), [128, 4, 128])),
                   reads=pn + ["idf"], writes=["tmpd"])
                op("dve", lambda e, dst=dst, half=half: e.tensor_reduce(out=dst[:, half * 4:(half + 1) * 4], in_=tmpd, axis=AX.X, op=ALU.add),
                   reads=["tmpd"], writes=[dn])
        for (hs, hn, vi) in ((hsc_m, "hsc_m", V_GPRE_M), (hsc_f, "hsc_f", V_GPRE_F)):
            op("dve", lambda e, hs=hs, vi=vi: e.scalar_tensor_tensor(out=hs[:], in0=hs[:], scalar=1.0, in1=vcol(l, vi), op0=ALU.add, op1=ALU.mult),
               reads=[hn, "vecs"], writes=[hn])
        op("pool", lambda e: e.memset(hst[:], 0.0), writes=["hst"])
        op("pool", lambda e: e.memset(Cst[:], 0.0), writes=["Cst"])
        op("pool", lambda e: e.memset(Cbf[:], 0.0), writes=["Cbf"])
        op("pool", lambda e: e.memset(mprev[:], 0.0), writes=["mprev"])

    def load_norm_transpose(l, first, t0, nsub, hsc, hbi, hscn, hbin, xt, xn, hT, junk, ss, rs, hTf=None, xnn="xn", hTfn="hTf", junkn="junk"):
        src = xsrc(l, first)
        op("dve", lambda e: e.memset(ss, 0.0), writes=["ss"])
        for s in range(nsub):
            blk = (t0 + s * 128) // TT
            S.dma("sp", xt[:, s, :], src[t0 + s * 128:t0 + (s + 1) * 128, :], reads=["xrow%d" % blk], writes=["xt%d" % s], key="xl%d" % s)
            op("act", lambda e, s=s: e.activation(out=junk, in_=xt[:, s, :], func=AF.Square, accum_out=ss[:, s:s + 1]),
               reads=["xt%d" % s], writes=[junkn, "ss"])
        rstd_from_ss(rs, ss, D, "ss", "rs")
        dt = F32 if hTf is not None else BF16
        for s in range(nsub):
            op("dve", lambda e, s=s: e.tensor_scalar(out=xn[:, s, :], in0=xt[:, s, :], scalar1=rs[:, s:s + 1], scalar2=None, op0=ALU.mult),
               reads=["xt%d" % s, "rs"], writes=[xnn])
        idt = idf if hTf is not None else idb
        n = nsub * 128
        for dc in range(8):
            if hTf is not None:
                pt = PB[6 + dc % 2][:, 0:n]
                pn = ["P6a", "P6b"] if dc % 2 == 0 else P7N
            else:
                pt, pn = p7((dc % 2) * 2, n)
            for s in range(nsub):
                op("pe", lambda e, s=s, dc=dc, pt=pt: e.transpose(out=pt[:, s * 128:(s + 1) * 128], in_=xn[:, s, dc * 128:(dc + 1) * 128], identity=idt[:]),
                   reads=[xnn, "idb", "idf"], writes=pn)
            tgt = hTf if hTf is not None else hT
            tgn = hTfn if hTf is not None else "hT"
            op("act", lambda e, dc=dc, pt=pt, tgt=tgt: e.activation(out=tgt[:, dc, :], in_=pt, func=AF.Identity, scale=hsc[:, dc:dc + 1], bias=hbi[:, dc:dc + 1]),
               reads=pn + [hscn, hbin], writes=[tgn])
        if hTf is not None:
            op("dve", lambda e: e.tensor_copy(out=hT, in_=hTf), reads=[hTfn], writes=["hT"])

    def residual_update(l, t0, s, ypsum_list, ypn, xt, ysb, tmpx, junk, ssy, rsy, gg, ggn, yacc=None, ysbn="ysb", tmpxn="tmpx", junkn="junk"):
        if yacc is None:
            for (pa, c0, ncol) in ypsum_list:
                op("act", lambda e, pa=pa, c0=c0, ncol=ncol: e.copy(out=ysb[:, c0:c0 + ncol], in_=pa), reads=ypn, writes=[ysbn])
            ysrc, ysn = ysb, ysbn
        else:
            ysrc, ysn = yacc, "yacc"
        op("dve", lambda e: e.memset(ssy[:, 0:1], 0.0), writes=["ssy"])
        op("act", lambda e: e.activation(out=junk, in_=ysrc, func=AF.Square, accum_out=ssy[:, 0:1]), reads=[ysn], writes=[junkn, "ssy"])
        rstd_from_ss(rsy[:, 0:1], ssy[:, 0:1], D, "ssy", "rsy")
        op("dve", lambda e: e.scalar_tensor_tensor(out=tmpx, in0=ysrc, scalar=rsy[:, 0:1], in1=gg[:], op0=ALU.mult, op1=ALU.mult),
           reads=[ysn, "rsy", ggn], writes=[tmpxn])
        op("dve", lambda e: e.tensor_add(out=xt[:, s, :], in0=xt[:, s, :], in1=tmpx), reads=[tmpxn, "xt%d" % s], writes=["xt%d" % s])
        blk = (t0 + s * 128) // TT
        d = S.dma("sp", out_d[t0 + s * 128:t0 + (s + 1) * 128, :], xt[:, s, :], reads=["xt%d" % s], writes=["xrow%d" % blk], key="xs%d" % s)
        last_x_dma["xs%d" % s] = d

    def mixer(l):
        AR.reset()
        S.barrier()
        A = AR.alloc
        xt = A([128, 2, D]); xn = A([128, 2, D], BF16); hT = A([128, 8, TT], BF16)
        ss = A([128, 2]); rs = A([128, 2]); ssy = A([128, 2]); rsy = A([128, 2])
        NWG = 3
        wg = [A([128, 8, 256], BF16) for _ in range(NWG)]
        qs = A([128, 8, TT], BF16)
        T1 = A([128, 8, TT]); T2 = A([128, 8, TT]); T3 = A([128, 8, TT]); T4 = A([128, 8, TT])
        QEO = A([128, 8, 2, 2, 128], BF16)
        KT = A([128, 8, TT], BF16)
        Ktok = A([128, 8, 2, 128], BF16)
        vtok = A([128, 2, D], BF16)
        sga = A([128, 8, TT], BF16)
        yaT = A([128, 8, TT], BF16)
        E1 = A([128, 8, 4]); E2 = A([128, 8, 4]); E3 = A([128, 8, 4]); dE = A([128, 8, 4])
        tKV = A([128, 2, 128])
        STb = A([128, 2, 128], BF16)
        ssq = A([128, 8]); rsq = A([128, 8])
        onb = A([128, D], BF16)
        xm = A([128, 8, 3 + TT])
        xcT = A([128, 8, TT], BF16); xmT = A([128, 8, TT], BF16)
        qT = A([128, 8, TT], BF16); kT = A([128, 8, TT], BF16); vT = A([128, 8, TT], BF16)
        ktok = A([128, 2, D], BF16)
        vext = A([128, 2, 4, 257], BF16)
        sob = A([128, 8, TT], BF16); sgA = A([128, 8, TT], BF16); sgB = A([128, 8, TT], BF16)
        DT = A([128, 2, 128]); DTm = A([128, 2, 128])
        kw = A([128, 2, 256], BF16)
        tnum = A([128, 257])
        hnb = A([128, D], BF16)
        junk = hnb
        ybT = A([128, 8, TT], BF16)
        mixT = A([128, 8, TT], BF16)
        gpre = A([128, 8]); gli = A([128, 4]); glf = A([128, 4])
        rows = A([4, 8, 128])
        gcol = A([128, 2, 16])
        decb = A([128, 2, 2, 4])
        dexp = A([4, 4, 2])
        sm = A([128, 32])
        ctmp = A([128, TT])
        acc = T1; o_sb = T2.rearrange("p a b -> p (a b)")[:, 0:D]; sqb = T3.rearrange("p a b -> p (a b)")[:, 0:D]
        tot = T4.rearrange("p a b -> p (a b)")[:, 0:4 * 257].rearrange("p (a b) -> p a b", b=257)
        sqm = T3.rearrange("p a b -> p (a b)")[:, 0:D].rearrange("p (a b) -> p a b", b=256)
        ysb = T1.rearrange("p a b -> p (a b)")[:, 0:D]
        tmpx = T2.rearrange("p a b -> p (a b)")[:, 0:D]
        m1 = T3.rearrange("p a b -> p (a b)")[:, 0:2 * TT].rearrange("p (a b) -> p a b", b=TT)
        m2 = T4.rearrange("p a b -> p (a b)")[:, 0:2 * TT].rearrange("p (a b) -> p a b", b=TT)

        print("mixer arena words", AR.off)
        op("pool", lambda e: e.memset(xm[:, :, 0:3], 0.0), writes=["xm"])
        op("pool", lambda e: e.memset(QEO, 0.0), writes=["QEO"])
        op("pool", lambda e: e.memset(vext[:, :, :, 256:257], 1.0), writes=["vext"])

        NPP = 6
        pp_names = ["P%d" % i for i in range(NPP)]

        def pp_view(i):
            return PB[i][:, 0:256]

        state = {"pp": 0, "wg": 0}

        def next_pp():
            i = state["pp"] % NPP
            state["pp"] += 1
            return pp_view(i), [pp_names[i]]

        def load_wg(src_ap, depname):
            i = state["wg"] % NWG
            state["wg"] += 1
            S.dma("sp", wg[i], src_ap, reads=[depname], writes=["wg%d" % i], key="wg%d" % i)
            return wg[i], "wg%d" % i

        for ti in range(n_mix_tiles):
            t0 = ti * TT
            load_norm_transpose(l, True, t0, 2, hsc_m, hbi_m, "hsc_m", "hbi_m", xt, xn, hT, junk, ss, rs, junkn="hnb")
            def fm_chunk(w, wn, half, rhs_t, rhs_n, evac):
                pv, pn = next_pp()
                for kc in range(8):
                    op("pe", lambda e, kc=kc, pv=pv: e.matmul(pv, lhsT=w[:, kc, half * 128:(half + 1) * 128], rhs=rhs_t[:, kc, :], start=(kc == 0), stop=(kc == 7)),
                       reads=[wn, rhs_n], writes=pn)
                evac(pv, pn)

            for grp in (0, 1, 2, 3, 12, 13, 14, 15, 4, 5, 6, 7, 20, 21, 22, 23, 24, 25, 26, 27, 28, 29, 30, 31, 16, 17, 18, 19, 8, 9, 10, 11):
                w, wn = load_wg(win_s[l, grp], "win%d" % l)
                if 8 <= grp < 12:
                    for s in range(2):
                        pv, pn = next_pp()
                        for kc in range(8):
                            op("pe", lambda e, kc=kc, pv=pv, s=s, w=w: e.matmul(pv, lhsT=hT[:, kc, s * 128:(s + 1) * 128], rhs=w[:, kc, :], start=(kc == 0), stop=(kc == 7)),
                               reads=[wn, "hT"], writes=pn)
                        c0 = (grp - 8) * 256
                        op("dve", lambda e, pv=pv, s=s, c0=c0: e.tensor_copy(out=vtok[:, s, c0:c0 + 256], in_=pv), reads=pn, writes=["vtok"])
                    continue
                for half in range(2):
                    m = grp * 2 + half
                    kind, hd = m // 8, m % 8
                    if kind == 0:
                        ev = lambda pv, pn, hd=hd: op("act", lambda e: e.activation(out=qs[:, hd, :], in_=pv, func=AF.Silu), reads=pn, writes=["qs"])
                    elif kind == 1:
                        ev = lambda pv, pn, hd=hd: op("act", lambda e: e.activation(out=T1[:, hd, :], in_=pv, func=AF.Sigmoid), reads=pn, writes=["T1"])
                    elif kind == 3:
                        ev = lambda pv, pn, hd=hd: op("act", lambda e: e.activation(out=sga[:, hd, :], in_=pv, func=AF.Silu), reads=pn, writes=["sga"])
                    elif kind == 4:
                        ev = lambda pv, pn, hd=hd: op("dve", lambda e: e.tensor_copy(out=xm[:, hd, 3:3 + TT], in_=pv), reads=pn, writes=["xm"])
                    elif kind == 5:
                        ev = lambda pv, pn, hd=hd: op("act", lambda e: e.activation(out=sob[:, hd, :], in_=pv, func=AF.Sigmoid), reads=pn, writes=["sob"])
                    elif kind == 6:
                        ev = lambda pv, pn, hd=hd: op("act", lambda e: e.activation(out=sgA[:, hd, :], in_=pv, func=AF.Sigmoid), reads=pn, writes=["sgA"])
                    else:
                        ev = lambda pv, pn, hd=hd: op("act", lambda e: e.activation(out=sgB[:, hd, :], in_=pv, func=AF.Sigmoid), reads=pn, writes=["sgB"])
                    fm_chunk(w, wn, half, hT, "hT", ev)

            for hd in range(8):
                op("dve", lambda e, hd=hd: e.tensor_scalar(out=T4[:, hd, :], in0=T1[:, hd, :], scalar1=noml[:, l, hd:hd + 1], scalar2=oml[:, l, hd:hd + 1], op0=ALU.mult, op1=ALU.add),
                   reads=["T1", "noml", "oml"], writes=["T4"])
            for hd in range(8):
                op("act", lambda e, hd=hd: e.activation(out=T2[:, hd, :], in_=T1[:, hd, :], func=AF.Ln, scale=oml[:, l, hd:hd + 1], bias=lbc[:, l, hd:hd + 1]),
                   reads=["T1", "oml", "lbc"], writes=["T2"])
            for hd in range(8):
                op("dve", lambda e, hd=hd: e.tensor_tensor_scan(out=T3[:, hd, :], data0=mask01[:], data1=T2[:, hd, :], initial=0.0, op0=ALU.mult, op1=ALU.add),
                   reads=["T2", "mask01"], writes=["T3"])
            b4 = T3.rearrange("p h (c j) -> p h c j", j=64)
            op("dve", lambda e: e.tensor_sub(out=dE, in0=b4[:, :, :, 63], in1=b4[:, :, :, 31]), reads=["T3"], writes=["dE"])
            op("act", lambda e: e.activation(out=E1, in_=b4[:, :, :, 63], func=AF.Exp), reads=["T3"], writes=["E1"])
            op("act", lambda e: e.activation(out=E2, in_=dE, func=AF.Exp), reads=["dE"], writes=["E2"])
            op("act", lambda e: e.activation(out=E3, in_=b4[:, :, :, 31], func=AF.Exp), reads=["T3"], writes=["E3"])
            op("dve", lambda e: e.tensor_sub(out=T2.rearrange("p h (c j) -> p h c j", j=64), in0=b4, in1=bc(b4[:, :, :, 31:32], [128, 8, 4, 64])),
               reads=["T3", "T2"], writes=["T2"])
            op("act", lambda e: e.activation(out=T1, in_=T2, func=AF.Exp), reads=["T2", "T1"], writes=["T1"])
            op("act", lambda e: e.activation(out=T3, in_=T2, func=AF.Exp, scale=-1.0), reads=["T2", "E1", "E3", "dE"], writes=["T3"])
            for hd in range(8):
                qo = QEO[:, hd].rearrange("p a b c -> p (a b c)")
                for pr in range(2):
                    for eo in range(2):
                        c = pr * 2 + eo
                        o = pr * 256 + eo * 192
                        op("dve", lambda e, hd=hd, c=c, o=o, qo=qo: e.tensor_mul(out=qo[:, o:o + 64], in0=qs[:, hd, c * 64:(c + 1) * 64], in1=T1[:, hd, c * 64:(c + 1) * 64]),
                           reads=["qs", "T1"], writes=["QEO"])
            op("dve", lambda e: e.tensor_mul(out=KT, in0=T4, in1=T3), reads=["T4", "T3"], writes=["KT"])
            for hd in range(8):
                pt, pn = p7(2 + hd % 2, 256)
                for pr in range(2):
                    op("pe", lambda e, hd=hd, pr=pr, pt=pt: e.transpose(out=pt[:, pr * 128:(pr + 1) * 128], in_=KT[:, hd, pr * 128:(pr + 1) * 128], identity=idb[:]),
                       reads=["KT", "idb"], writes=pn)
                op("act", lambda e, hd=hd, pt=pt: e.copy(out=Ktok[:, hd].rearrange("p a b -> p (a b)"), in_=pt), reads=pn, writes=["Ktok"])
            for dc in range(8):
                op("dve", lambda e, dc=dc: e.tensor_scalar(out=acc[:, dc, :], in0=xm[:, dc, 3:3 + TT], scalar1=vcol(l, V_CW0 + 3)[:, dc:dc + 1], scalar2=vcol(l, V_CB)[:, dc:dc + 1], op0=ALU.mult, op1=ALU.add),
                   reads=["xm", "vecs", "T1"], writes=["T1"])
                for j in range(3):
                    op("dve", lambda e, dc=dc, j=j: e.scalar_tensor_tensor(out=acc[:, dc, :], in0=xm[:, dc, j:j + TT], scalar=vcol(l, V_CW0 + j)[:, dc:dc + 1], in1=acc[:, dc, :], op0=ALU.mult, op1=ALU.add),
                       reads=["xm", "vecs", "T1"], writes=["T1"])
            op("act", lambda e: e.activation(out=xcT, in_=acc, func=AF.Silu), reads=["T1"], writes=["xcT"])
            op("act", lambda e: e.copy(out=xmT, in_=xm[:, :, 3:3 + TT]), reads=["xm"], writes=["xmT"])
            op("pool", lambda e: e.tensor_copy(out=xm[:, :, 0:3], in_=xm[:, :, TT:TT + 3]), reads=["xm"], writes=["xm"])
            for (mi, srcT, srcn, dstT, dstn) in ((0, xcT, "xcT", qT, "qT"), (1, xcT, "xcT", kT, "kT"), (2, xmT, "xmT", vT, "vT")):
                for dc in range(8):
                    pv, pn = next_pp()
                    op("pe", lambda e, mi=mi, dc=dc, pv=pv, srcT=srcT: e.matmul(pv, lhsT=bdt[:, mi, dc, :], rhs=srcT[:, dc, :], start=True, stop=True),
                       reads=["bdt", srcn], writes=pn)
                    eng = "act" if dc % 2 == 0 else "dve"
                    if eng == "act":
                        op("act", lambda e, dc=dc, pv=pv, dstT=dstT: e.copy(out=dstT[:, dc, :], in_=pv), reads=pn, writes=[dstn])
                    else:
                        op("dve", lambda e, dc=dc, pv=pv, dstT=dstT: e.tensor_copy(out=dstT[:, dc, :], in_=pv), reads=pn, writes=[dstn])
            for pr in range(2):
                for (mi, srcT, srcn) in ((1, xcT, "xcT"), (2, xmT, "xmT")):
                    for hb in range(2):
                        pbk = PB[2]
                        pn = ["P2a", "P2b", "P2c", "P2d"]
                        for j in range(4):
                            dc = hb * 4 + j
                            op("pe", lambda e, mi=mi, dc=dc, j=j, srcT=srcT, pr=pr: e.matmul(pbk[:, j * 128:(j + 1) * 128], lhsT=srcT[:, dc, pr * 128:(pr + 1) * 128], rhs=bdt[:, mi, dc, :], start=True, stop=True),
                               reads=["bdt", srcn], writes=pn)
                        if mi == 1:
                            op("act", lambda e, pr=pr, hb=hb: e.copy(out=ktok[:, pr, hb * 512:(hb + 1) * 512], in_=pbk[:, :]), reads=pn, writes=["ktok"])
                        else:
                            op("dve", lambda e, pr=pr, hb=hb: e.tensor_copy(out=vext[:, pr, hb * 2:hb * 2 + 2, 0:256], in_=pbk[:, :].rearrange("p (a b) -> p a b", b=256)), reads=pn, writes=["vext"])
            for pr in range(2):
                pg = PB[5][:, 256:264]
                pgn = ["P5c"]
                i = 0
                for (srcT, srcn) in ((qT, "qT"), (kT, "kT"), (vT, "vT")):
                    for dc in range(8):
                        op("pe", lambda e, srcT=srcT, dc=dc, i=i, pr=pr: e.matmul(pg, lhsT=srcT[:, dc, pr * 128:(pr + 1) * 128], rhs=wgt[:, i, :], start=(i == 0), stop=(i == 23)),
                           reads=[srcn, "wgt"], writes=pgn)
                        i += 1
                op("dve", lambda e: e.tensor_add(out=gpre, in0=pg, in1=bgt[:]), reads=pgn + ["bgt"], writes=["gpre"])
                op("act", lambda e: e.activation(out=glf, in_=gpre[:, 4:8], func=AF.Exp, scale=-1.0), reads=["gpre"], writes=["glf"])
                op("act", lambda e: e.activation(out=glf, in_=glf, func=AF.Ln, bias=1.0), reads=["glf"], writes=["glf"])
                op("dve", lambda e: e.tensor_scalar_mul(out=glf, in0=glf, scalar1=-1.0), reads=["glf"], writes=["glf"])
                op("dve", lambda e: e.tensor_copy(out=gli, in_=gpre[:, 0:4]), reads=["gpre"], writes=["gli"])
                prw = PB[5][0:4, 384:512]
                prn = ["P5d"]
                op("pe", lambda e: e.matmul(prw, lhsT=gli, rhs=idf[:], start=True, stop=True), reads=["gli", "idf"], writes=prn)
                op("dve", lambda e: e.tensor_copy(out=rows[:, 0, :], in_=prw), reads=prn, writes=["rows"])
                op("pe", lambda e: e.matmul(prw, lhsT=glf, rhs=idf[:], start=True, stop=True), reads=["glf", "idf"], writes=prn)
                op("dve", lambda e: e.tensor_copy(out=rows[:, 1, :], in_=prw), reads=prn, writes=["rows"])
                op("dve", lambda e: e.tensor_tensor_scan(out=rows[:, 2, :], data0=m01r[:], data1=rows[:, 1, :], initial=0.0, op0=ALU.mult, op1=ALU.add), reads=["rows", "m01r"], writes=["rows"])
                op("dve", lambda e: e.tensor_sub(out=rows[:, 3, :], in0=rows[:, 0, :], in1=rows[:, 2, :]), reads=["rows"], writes=["rows"])
                op("dve", lambda e: e.tensor_tensor_scan(out=rows[:, 4, :], data0=negm[:], data1=rows[:, 3, :], initial=-1e30, op0=ALU.add, op1=ALU.max), reads=["rows", "negm"], writes=["rows"])
                for eo in range(2):
                    cs = slice(eo * 64, (eo + 1) * 64)
                    op("dve", lambda e, cs=cs: e.tensor_scalar(out=rows[:, 5, cs], in0=rows[:, 4, cs], scalar1=mprev[:, 0:1], scalar2=None, op0=ALU.max), reads=["rows", "mprev"], writes=["rows"])
                    op("dve", lambda e, cs=cs: e.tensor_scalar(out=rows[:, 6, cs], in0=rows[:, 5, cs], scalar1=mprev[:, 0:1], scalar2=-1.0, op0=ALU.subtract, op1=ALU.mult), reads=["rows", "mprev"], writes=["rows"])
                    last = eo * 64 + 63
                    op("dve", lambda e, cs=cs, last=last: e.tensor_scalar(out=rows[:, 7, cs], in0=rows[:, 3, cs], scalar1=rows[:, 5, last:last + 1], scalar2=None, op0=ALU.subtract), reads=["rows"], writes=["rows"])
                    op("dve", lambda e, last=last: e.tensor_add(out=mprev[:, 0:1], in0=rows[:, 2, last:last + 1], in1=rows[:, 5, last:last + 1]), reads=["rows", "mprev"], writes=["mprev"])
                op("dve", lambda e: e.scalar_tensor_tensor(out=rows[:, 1, :], in0=rows[:, 2, :], scalar=-1.0, in1=rows[:, 5, :], op0=ALU.mult, op1=ALU.subtract), reads=["rows"], writes=["rows"])
                op("act", lambda e: e.activation(out=rows[:, 6, :], in_=rows[:, 6, :], func=AF.Exp), reads=["rows"], writes=["rows"])
                op("act", lambda e: e.activation(out=rows[:, 1, :], in_=rows[:, 1, :], func=AF.Exp), reads=["rows"], writes=["rows"])
                op("act", lambda e: e.activation(out=rows[:, 7, :], in_=rows[:, 7, :], func=AF.Exp, bias=float(-np.log(16.0))), reads=["rows"], writes=["rows"])
                pcl = PB[5][:, 264:280]
                for qi, ri in enumerate((3, 6, 1, 7)):
                    op("pe", lambda e, qi=qi, ri=ri: e.matmul(pcl[:, qi * 4:(qi + 1) * 4], lhsT=rows[:, ri, :], rhs=id4[:], start=True, stop=True), reads=["rows", "id4"], writes=pgn)
                op("dve", lambda e, pr=pr: e.tensor_copy(out=gcol[:, pr, :], in_=pcl), reads=pgn, writes=["gcol"])
                wl = rows[:, 6, :].rearrange("p (c j) -> p c j", j=64)[:, :, 63]
                op("dve", lambda e, wl=wl: e.tensor_mul(out=dexp, in0=bc(wl.unsqueeze(1), [4, 4, 2]), in1=bc(id4[:].unsqueeze(2), [4, 4, 2])), reads=["rows", "id4"], writes=["dexp"])
                pdc = PB[5][:, 280:288]
                op("pe", lambda e: e.matmul(pdc, lhsT=ones4[:], rhs=dexp.rearrange("p a b -> p (a b)"), start=True, stop=True), reads=["dexp", "ones4"], writes=pgn)
                op("dve", lambda e, pr=pr: e.tensor_copy(out=decb[:, pr].rearrange("p e h -> p h e"), in_=pdc.rearrange("p (h e) -> p h e", e=2)), reads=pgn, writes=["decb"])
                for hd in range(8):
                    psc = PB[2][:, (hd % 2) * 128:(hd % 2) * 128 + 128]
                    pscn = ["P2a" if hd % 2 == 0 else "P2b"]
                    op("pe", lambda e, hd=hd, psc=psc: e.matmul(psc, lhsT=KT[:, hd, pr * 128:(pr + 1) * 128], rhs=QEO[:, hd, pr, 0, :], start=True, stop=False), reads=["KT", "QEO"], writes=pscn)
                    op("pe", lambda e, hd=hd, psc=psc: e.matmul(psc, lhsT=KT[:, hd, pr * 128:(pr + 1) * 128], rhs=QEO[:, hd, pr, 1, :], start=False, stop=True), reads=["KT", "QEO"], writes=pscn)
                    stn = "STb%d" % (hd % 2)
                    op("dve", lambda e, hd=hd, psc=psc: e.tensor_scalar(out=DT[:, hd % 2, :], in0=psc, scalar1=1e30, scalar2=-1e30, op0=ALU.min, op1=ALU.max), reads=pscn, writes=["DT%d" % (hd % 2)])
                    op("dve", lambda e, hd=hd: e.tensor_mul(out=STb[:, hd % 2, :], in0=DT[:, hd % 2, :], in1=mask2[:]), reads=["DT%d" % (hd % 2), "mask2"], writes=[stn])
                    po = PB[3 + hd // 4][:, (hd % 4) * 128:(hd % 4) * 128 + 128]
                    pon = ["P3" if hd < 4 else "P4"]
                    hn_ = "hst%d" % hd
                    hbn = "hstb%d" % (hd % 2)
                    for eo in range(2):
                        c = pr * 2 + eo
                        op("act", lambda e, hd=hd, eo=eo, c=c: e.activation(out=hstb[:, hd % 2, eo, :], in_=hst[:, hd, :], func=AF.Copy, scale=E3[:, hd, c:c + 1]),
                           reads=[hn_, "E3"], writes=[hbn + "_%d" % eo])
                        pkv = PB[2][:, 256 + eo * 128:256 + eo * 128 + 128]
                        pkvn = ["P2c" if eo == 0 else "P2d"]
                        op("pe", lambda e, hd=hd, eo=eo, pkv=pkv: e.matmul(pkv, lhsT=Ktok[eo * 64:(eo + 1) * 64, hd, pr, :], rhs=vtok[eo * 64:(eo + 1) * 64, pr, hd * 128:(hd + 1) * 128], start=True, stop=True),
                           reads=["Ktok", "vtok"], writes=pkvn)
                        op("act", lambda e, hd=hd, eo=eo, c=c, pkv=pkv: e.activation(out=tKV[:, eo, :], in_=pkv, func=AF.Copy, scale=E2[:, hd, c:c + 1]), reads=pkvn + ["E2"], writes=["tKV%d" % eo])
                        op("dve", lambda e, hd=hd, eo=eo, c=c: e.scalar_tensor_tensor(out=hst[:, hd, :], in0=hst[:, hd, :], scalar=E1[:, hd, c:c + 1], in1=tKV[:, eo, :], op0=ALU.mult, op1=ALU.add),
                           reads=["tKV%d" % eo, "E1", hn_], writes=[hn_])
                    op("pe", lambda e, hd=hd, po=po: e.matmul(po, lhsT=STb[:, hd % 2, :], rhs=vtok[:, pr, hd * 128:(hd + 1) * 128], start=True, stop=False), reads=[stn, "vtok"], writes=pon)
                    op("pe", lambda e, hd=hd, po=po: e.matmul(po, lhsT=QEO[:, hd, pr, 0, :], rhs=hstb[:, hd % 2, 0, :], start=False, stop=False), reads=["QEO", hbn + "_0"], writes=pon)
                    op("pe", lambda e, hd=hd, po=po: e.matmul(po, lhsT=QEO[:, hd, pr, 1, :], rhs=hstb[:, hd % 2, 1, :], start=False, stop=True), reads=["QEO", hbn + "_1"], writes=pon)
                op("act", lambda e: e.copy(out=o_sb[:, 0:512], in_=PB[3][:, :]), reads=["P3"], writes=["T2"])
                op("act", lambda e: e.copy(out=o_sb[:, 512:1024], in_=PB[4][:, :]), reads=["P4"], writes=["T2"])
                op("act", lambda e: e.activation(out=sqb, in_=o_sb, func=AF.Square), reads=["T2"], writes=["T3"])
                op("dve", lambda e: e.tensor_reduce(out=ssq, in_=sqb.rearrange("p (h v) -> p h v", v=128), axis=AX.X, op=ALU.add), reads=["T3"], writes=["ssq"])
                rstd_from_ss(rsq, ssq, 128, "ssq", "rsq")
                op("dve", lambda e: e.tensor_mul(out=onb.rearrange("p (h v) -> p h v", v=128), in0=o_sb.rearrange("p (h v) -> p h v", v=128), in1=bc(rsq.unsqueeze(2), [128, 8, 128])),
                   reads=["T2", "rsq"], writes=["onb"])
                for hd in range(8):
                    pt, pn = p7((hd % 2) * 2, 128)
                    op("pe", lambda e, hd=hd, pt=pt: e.transpose(out=pt, in_=onb[:, hd * 128:(hd + 1) * 128], identity=idb[:]), reads=["onb", "idb"], writes=pn)
                    op("dve", lambda e, hd=hd, pt=pt: e.scalar_tensor_tensor(out=yaT[:, hd, pr * 128:(pr + 1) * 128], in0=pt, scalar=vcol(l, V_HGN)[:, hd:hd + 1], in1=sga[:, hd, pr * 128:(pr + 1) * 128], op0=ALU.mult, op1=ALU.mult),
                       reads=pn + ["vecs", "sga"], writes=["yaT"])
                for h in range(4):
                    psc = PB[5][:, 0:128]
                    pM = PB[5][:, 128:256]
                    op("pe", lambda e, h=h: e.matmul(psc, lhsT=kT[:, 2 * h, pr * 128:(pr + 1) * 128], rhs=qT[:, 2 * h, pr * 128:(pr + 1) * 128], start=True, stop=False), reads=["kT", "qT"], writes=["P5a"])
                    op("pe", lambda e, h=h: e.matmul(psc, lhsT=kT[:, 2 * h + 1, pr * 128:(pr + 1) * 128], rhs=qT[:, 2 * h + 1, pr * 128:(pr + 1) * 128], start=False, stop=True), reads=["kT", "qT"], writes=["P5a"])
                    op("pe", lambda e, h=h: e.matmul(pM, lhsT=sel4[:, h, :], rhs=rows[:, 5, :], start=True, stop=True), reads=["sel4", "rows"], writes=["P5b"])
                    op("act", lambda e, h=h: e.activation(out=DT[:, h % 2, :], in_=pM, func=AF.Exp, scale=-1.0, bias=gcol[:, pr, h:h + 1]), reads=["P5b", "gcol"], writes=["DT%d" % (h % 2)])
                    op("dve", lambda e, h=h: e.tensor_mul(out=DTm[:, h % 2, :], in0=DT[:, h % 2, :], in1=maskm[:]), reads=["DT%d" % (h % 2), "maskm"], writes=["DTm%d" % (h % 2)])
                    stn = "STb%d" % (h % 2)
                    op("dve", lambda e, h=h: e.tensor_mul(out=STb[:, h % 2, :], in0=psc, in1=DTm[:, h % 2, :]), reads=["P5a", "DTm%d" % (h % 2)], writes=[stn])
                    pnum = PB[6][:, 0:257]
                    op("pe", lambda e, h=h: e.matmul(pnum, lhsT=STb[:, h % 2, :], rhs=vext[:, pr, h, :], start=True, stop=True), reads=[stn, "vext"], writes=["P6a", "P6b"])
                    op("act", lambda e: e.copy(out=tnum, in_=pnum), reads=["P6a", "P6b"], writes=["tnum"])
                    op("dve", lambda e, h=h: e.tensor_scalar(out=kw[:, h % 2, :], in0=ktok[:, pr, h * 256:(h + 1) * 256], scalar1=gcol[:, pr, 12 + h:13 + h], scalar2=None, op0=ALU.mult),
                       reads=["ktok", "gcol"], writes=["kw%d" % (h % 2)])
                    cn = "C%d" % h
                    cbn = "Cb%d" % h
                    for eo in range(2):
                        pint = PB[eo][:, 0:257]
                        pintn = ["P%da" % eo, "P%db" % eo]
                        rsl = slice(eo * 64, (eo + 1) * 64)
                        for j in range(2):
                            op("pe", lambda e, h=h, j=j, pint=pint: e.matmul(pint, lhsT=qT[:, 2 * h + j, pr * 128:(pr + 1) * 128], rhs=Cbf[:, h, j, :], start=(j == 0), stop=(j == 1)), reads=["qT", cbn], writes=pintn)
                        op("dve", lambda e, h=h, rsl=rsl, pint=pint: e.scalar_tensor_tensor(out=tot[rsl, h, :], in0=pint[rsl, :], scalar=gcol[rsl, pr, 4 + h:5 + h], in1=tnum[rsl, :], op0=ALU.mult, op1=ALU.add),
                           reads=pintn + ["gcol", "tnum"], writes=["T4"])
                        for j in range(2):
                            pC = PB[3 + j][:, 0:257]
                            pCn = ["P3" if j == 0 else "P4"]
                            op("pe", lambda e, h=h, j=j, rsl=rsl, pC=pC: e.matmul(pC, lhsT=kw[rsl, h % 2, j * 128:(j + 1) * 128], rhs=vext[rsl, pr, h, :], start=True, stop=True), reads=["kw%d" % (h % 2), "vext"], writes=pCn)
                            op("dve", lambda e, h=h, j=j, eo=eo, pC=pC: e.scalar_tensor_tensor(out=Cst[:, h, j, :], in0=Cst[:, h, j, :], scalar=decb[:, pr, eo, h:h + 1], in1=pC, op0=ALU.mult, op1=ALU.add),
                               reads=pCn + ["decb", cn], writes=[cn])
                            op("act", lambda e, h=h, j=j: e.copy(out=Cbf[:, h, j, :], in_=Cst[:, h, j, :]), reads=[cn], writes=[cbn])
                den = tot[:, :, 256]
                op("dve", lambda e: e.tensor_scalar_mul(out=sm[:, 16:20], in0=den, scalar1=-1.0), reads=["T4"], writes=["sm"])
                op("dve", lambda e: e.tensor_max(out=sm[:, 4:8], in0=sm[:, 16:20], in1=den), reads=["T4", "sm"], writes=["sm"])
                op("dve", lambda e: e.tensor_max(out=sm[:, 4:8], in0=sm[:, 4:8], in1=gcol[:, pr, 8:12]), reads=["sm", "gcol"], writes=["sm"])
                op("dve", lambda e: e.reciprocal(out=sm[:, 4:8], in_=sm[:, 4:8]), reads=["sm"], writes=["sm"])
                op("dve", lambda e: e.tensor_reduce(out=sm[:, 8:12], in_=tot[:, :, 0:256], axis=AX.X, op=ALU.add), reads=["T4"], writes=["sm"])
                op("act", lambda e: e.activation(out=sqm, in_=tot[:, :, 0:256], func=AF.Square), reads=["T4"], writes=["T3"])
                op("dve", lambda e: e.tensor_reduce(out=sm[:, 12:16], in_=sqm, axis=AX.X, op=ALU.add), reads=["T3"], writes=["sm"])
                op("dve", lambda e: e.tensor_scalar_mul(out=sm[:, 8:12], in0=sm[:, 8:12], scalar1=1.0 / 256), reads=["sm"], writes=["sm"])
                op("dve", lambda e: e.tensor_mul(out=sm[:, 16:20], in0=sm[:, 8:12], in1=sm[:, 8:12]), reads=["sm"], writes=["sm"])
                op("dve", lambda e: e.scalar_tensor_tensor(out=sm[:, 12:16], in0=sm[:, 12:16], scalar=1.0 / 256, in1=sm[:, 16:20], op0=ALU.mult, op1=ALU.subtract), reads=["sm"], writes=["sm"])
                op("dve", lambda e: e.tensor_mul(out=sm[:, 16:20], in0=sm[:, 4:8], in1=sm[:, 4:8]), reads=["sm"], writes=["sm"])
                op("dve", lambda e: e.tensor_mul(out=sm[:, 12:16], in0=sm[:, 12:16], in1=sm[:, 16:20]), reads=["sm"], writes=["sm"])
                op("dve", lambda e: e.tensor_scalar(out=sm[:, 12:16], in0=sm[:, 12:16], scalar1=0.0, scalar2=EPS, op0=ALU.max, op1=ALU.add), reads=["sm"], writes=["sm"])
                op("act", lambda e: e.activation(out=sm[:, 12:16], in_=sm[:, 12:16], func=AF.Ln), reads=["sm"], writes=["sm"])
                op("act", lambda e: e.activation(out=sm[:, 12:16], in_=sm[:, 12:16], func=AF.Exp, scale=-0.5), reads=["sm"], writes=["sm"])
                op("dve", lambda e: e.tensor_mul(out=sm[:, 20:24], in0=sm[:, 4:8], in1=sm[:, 12:16]), reads=["sm"], writes=["sm"])
                op("dve", lambda e: e.scalar_tensor_tensor(out=sm[:, 24:28], in0=sm[:, 8:12], scalar=-1.0, in1=sm[:, 20:24], op0=ALU.mult, op1=ALU.mult), reads=["sm"], writes=["sm"])
                for h in range(4):
                    op("dve", lambda e, h=h: e.tensor_scalar(out=hnb[:, h * 256:(h + 1) * 256], in0=tot[:, h, 0:256], scalar1=sm[:, 20 + h:21 + h], scalar2=sm[:, 24 + h:25 + h], op0=ALU.mult, op1=ALU.add),
                       reads=["T4", "sm"], writes=["hnb"])
                for dc in range(8):
                    pt, pn = p7((dc % 2) * 2 + 1, 128)
                    op("pe", lambda e, dc=dc, pt=pt: e.transpose(out=pt, in_=hnb[:, dc * 128:(dc + 1) * 128], identity=idb[:]), reads=["hnb", "idb"], writes=pn)
                    cs = slice(pr * 128, (pr + 1) * 128)
                    op("act", lambda e, dc=dc, cs=cs: e.activation(out=ybT[:, dc, cs], in_=xcT[:, dc, cs], func=AF.Copy, scale=vcol(l, V_SKIP)[:, dc:dc + 1]),
                       reads=["vecs", "xcT"], writes=["ybT"])
                    op("dve", lambda e, dc=dc, pt=pt, cs=cs: e.scalar_tensor_tensor(out=ybT[:, dc, cs], in0=pt, scalar=vcol(l, V_MGN)[:, dc:dc + 1], in1=ybT[:, dc, cs], op0=ALU.mult, op1=ALU.add),
                       reads=pn + ["vecs", "ybT"], writes=["ybT"])
                    op("dve", lambda e, dc=dc, cs=cs: e.tensor_mul(out=ybT[:, dc, cs], in0=ybT[:, dc, cs], in1=sob[:, dc, cs]), reads=["ybT", "sob"], writes=["ybT"])
            for g in range(4):
                wa, wan = load_wg(wpa_s[l, g], "wpa%d" % l)
                wb, wbn = load_wg(wpb_s[l, g], "wpb%d" % l)
                for half in range(2):
                    m = g * 2 + half
                    i = m % 2
                    fm_chunk(wa, wan, half, yaT, "yaT", lambda pv, pn, m=m, i=i: op("dve", lambda e: e.tensor_mul(out=m1[:, i, :], in0=pv, in1=sgA[:, m, :]), reads=pn + ["sgA"], writes=["T3"]))
                    fm_chunk(wb, wbn, half, ybT, "ybT", lambda pv, pn, m=m, i=i: op("dve", lambda e: e.tensor_mul(out=m2[:, i, :], in0=pv, in1=sgB[:, m, :]), reads=pn + ["sgB"], writes=["T4"]))
                    op("dve", lambda e, m=m, i=i: e.tensor_add(out=mixT[:, m, :], in0=m1[:, i, :], in1=m2[:, i, :]), reads=["T3", "T4"], writes=["mixT"])
            for s in range(2):
                ybanks = (PB[3], PB[4]) if s == 0 else (PB[5], PB[6])
                ybn = ["P3", "P4"] if s == 0 else ["P5a", "P5b", "P5c", "P5d", "P6a", "P6b"]
                for g in range(4):
                    w, wn = load_wg(wo_s[l, g], "wo%d" % l)
                    yv = ybanks[g // 2][:, (g % 2) * 256:(g % 2) * 256 + 256]
                    for kc in range(8):
                        op("pe", lambda e, kc=kc, yv=yv, w=w, s=s: e.matmul(yv, lhsT=mixT[:, kc, s * 128:(s + 1) * 128], rhs=w[:, kc, :], start=(kc == 0), stop=(kc == 7)),
                           reads=["mixT", wn], writes=ybn)
                residual_update(l, t0, s, [(ybanks[0][:, :], 0, 512), (ybanks[1][:, :], 512, 512)], ybn, xt, ysb, tmpx, junk, ssy, rsy, ggm, "ggm", ysbn="T1", tmpxn="T2", junkn="hnb")
            if DEBUG and l == 0 and ti == 0:
                for (nm, tl, dn) in (("hT", hT, "hT"), ("yaT", yaT, "yaT"), ("ybT", ybT, "ybT"), ("mixT", mixT, "mixT"), ("qT", qT, "qT"), ("kT", kT, "kT"), ("vT", vT, "vT"), ("xcT", xcT, "xcT"), ("KT", KT, "KT"), ("sga", sga, "sga")):
                    dd = nc.dram_tensor("dbg_" + nm, [128, 8, TT], BF16, kind="ExternalOutput").ap()
                    last_x_dma["dbg_" + nm] = S.dma("sp", dd, tl, reads=[dn], key="dbg_" + nm)
                dd = nc.dram_tensor("dbg_gcol", [128, 2, 16], F32, kind="ExternalOutput").ap()
                last_x_dma["dbg_gcol"] = S.dma("sp", dd, gcol, reads=["gcol"], key="dbg_gcol")
                dd = nc.dram_tensor("dbg_tot", [128, 4, 257], F32, kind="ExternalOutput").ap()
                last_x_dma["dbg_tot"] = S.dma("sp", dd, tot, reads=["T4"], key="dbg_tot")

    def ffn(l):
        AR.reset()
        S.barrier()
        A = AR.alloc
        moe = (l % 2 == 1)
        j = l // 2
        NG = 14 if moe else 11
        NFC = 2 * NG
        NE = 8 if moe else 1
        xt = A([128, 4, D])
        hT = A([128, 8, TF], BF16)
        junk = A([128, D], BF16); ss = A([128, 4]); rs = A([128, 4]); ssy = A([128, 2]); rsy = A([128, 2])
        aT = A([128, NFC, TF], BF16)
        w2t = [A([128, NFC, 512], BF16) for _ in range(2)]
        NW13 = 2
        w13 = [A([128, 2, 8, 256], BF16) for _ in range(NW13)]
        if moe:
            xn = w2t[0].rearrange("p a b -> p (a b)")[:, 0:8192].bitcast(F32).rearrange("p (a b) -> p a b", b=D)
            hTf = w2t[1].rearrange("p a b -> p (a b)")[:, 0:8192].bitcast(F32).rearrange("p (a b) -> p a b", b=TF)
            xnn, hTfn = "w2_0", "w2_1"
        else:
            xn = A([128, 4, D], BF16)
            hTf = None
            xnn, hTfn = "xn", "hTf"
        yacc = A([128, 4, D])
        gt = A([128, TF])
        tmpx = A([128, D])
        lg = A([128, 4, 8]); gates = A([128, 4, 8]); gm = A([128, 4, 8])
        mx1 = A([128, 4]); mx2 = A([128, 4]); den = A([128, 4])
        print("ffn arena words", AR.off)
        cnt = {"w13": 0, "w2": 0, "g": 0, "y": 0}
        for ti in range(n_ffn_tiles):
            t0 = ti * TF
            load_norm_transpose(l, False, t0, 4, hsc_f, hbi_f, "hsc_f", "hbi_f", xt, xn, hT, junk, ss, rs, hTf=hTf, xnn=xnn, hTfn=hTfn)
            if moe:
                for s in range(4):
                    pl = PB[5][:, 256 + s * 8:256 + s * 8 + 8]
                    for kc in range(8):
                        op("pe", lambda e, s=s, kc=kc, pl=pl: e.matmul(pl, lhsT=hTf[:, kc, s * 128:(s + 1) * 128], rhs=rtt[:, kc, :], start=(kc == 0), stop=(kc == 7)), reads=[hTfn, "rtt"], writes=["P5c"])
                op("dve", lambda e: e.tensor_copy(out=lg, in_=PB[5][:, 256:288].rearrange("p (s e) -> p s e", e=8)), reads=["P5c"], writes=["lg"])
                op("dve", lambda e: e.tensor_reduce(out=mx1, in_=lg, axis=AX.X, op=ALU.max), reads=["lg"], writes=["mx1"])
                op("dve", lambda e: e.tensor_tensor(out=gm, in0=lg, in1=bc(mx1.unsqueeze(2), [128, 4, 8]), op=ALU.is_equal), reads=["lg", "mx1"], writes=["gm"])
                op("dve", lambda e: e.scalar_tensor_tensor(out=gm, in0=gm, scalar=-1e30, in1=lg, op0=ALU.mult, op1=ALU.add), reads=["gm", "lg"], writes=["gm"])
                op("dve", lambda e: e.tensor_reduce(out=mx2, in_=gm, axis=AX.X, op=ALU.max), reads=["gm"], writes=["mx2"])
                op("dve", lambda e: e.tensor_tensor(out=gm, in0=lg, in1=bc(mx2.unsqueeze(2), [128, 4, 8]), op=ALU.is_ge), reads=["lg", "mx2", "gm"], writes=["gm"])
                op("dve", lambda e: e.tensor_sub(out=lg, in0=lg, in1=bc(mx1.unsqueeze(2), [128, 4, 8])), reads=["lg", "mx1"], writes=["lg"])
                op("act", lambda e: e.activation(out=lg, in_=lg, func=AF.Exp), reads=["lg"], writes=["lg"])
                op("dve", lambda e: e.tensor_sub(out=den, in0=mx2, in1=mx1), reads=["mx1", "mx2"], writes=["den"])
                op("act", lambda e: e.activation(out=den, in_=den, func=AF.Exp), reads=["den"], writes=["den"])
                op("dve", lambda e: e.tensor_scalar_add(out=den, in0=den, scalar1=1.0), reads=["den"], writes=["den"])
                op("dve", lambda e: e.reciprocal(out=den, in_=den), reads=["den"], writes=["den"])
                op("dve", lambda e: e.tensor_mul(out=gates, in0=lg, in1=gm), reads=["lg", "gm"], writes=["gates"])
                op("dve", lambda e: e.tensor_mul(out=gates, in0=gates, in1=bc(den.unsqueeze(2), [128, 4, 8])), reads=["gates", "den"], writes=["gates"])
            for ex in range(NE):
                for g in range(NG):
                    i = cnt["w13"] % NW13
                    cnt["w13"] += 1
                    wn = "w13_%d" % i
                    if moe:
                        S.dma("sp", w13[i], m13_s[j, ex, g], reads=["m13_%d_%d" % (l, ex)], writes=[wn], key=wn)
                    else:
                        S.dma("sp", w13[i], f13_s[j, g], reads=["f13_%d" % l], writes=[wn], key=wn)
                    for half in range(2):
                        fc = g * 2 + half
                        gi = cnt["g"] % 2
                        cnt["g"] += 1
                        pg_, pu_ = PB[gi], PB[2 + gi]
                        pgn = ["P%da" % gi, "P%db" % gi]
                        pun = ["P2a", "P2b", "P2c", "P2d"] if gi == 0 else ["P3"]
                        for kc in range(8):
                            op("pe", lambda e, kc=kc, half=half, w=w13[i], pg_=pg_: e.matmul(pg_[:, :], lhsT=w[:, 0, kc, half * 128:(half + 1) * 128], rhs=hT[:, kc, :], start=(kc == 0), stop=(kc == 7)), reads=[wn, "hT"], writes=pgn)
                        for kc in range(8):
                            op("pe", lambda e, kc=kc, half=half, w=w13[i], pu_=pu_: e.matmul(pu_[:, :], lhsT=w[:, 1, kc, half * 128:(half + 1) * 128], rhs=hT[:, kc, :], start=(kc == 0), stop=(kc == 7)), reads=[wn, "hT"], writes=pun)
                        op("act", lambda e, pg_=pg_: e.activation(out=gt, in_=pg_[:, :], func=AF.Silu), reads=pgn, writes=["gt"])
                        op("dve", lambda e, fc=fc, pu_=pu_: e.tensor_mul(out=aT[:, fc, :], in0=pu_[:, :], in1=gt), reads=pun + ["gt"], writes=["aT"])
                for half in range(2):
                    wi = cnt["w2"] % 2
                    cnt["w2"] += 1
                    w2n = "w2_%d" % wi
                    if moe:
                        S.dma("sp", w2t[wi], m2_s[j, ex, half], reads=["m2_%d_%d" % (l, ex)], writes=[w2n], key=w2n)
                    else:
                        S.dma("sp", w2t[wi], f2_s[j, half], reads=["f2_%d" % l], writes=[w2n], key=w2n)
                    for s in range(4):
                        yi = cnt["y"] % 2
                        cnt["y"] += 1
                        py = PB[4 + yi]
                        pyn = ["P4"] if yi == 0 else ["P5a", "P5b", "P5c", "P5d"]
                        for fc in range(NFC):
                            op("pe", lambda e, fc=fc, s=s, py=py, w=w2t[wi]: e.matmul(py[:, :], lhsT=aT[:, fc, s * 128:(s + 1) * 128], rhs=w[:, fc, :], start=(fc == 0), stop=(fc == NFC - 1)), reads=["aT", w2n], writes=pyn)
                        ya = yacc[:, s, half * 512:(half + 1) * 512]
                        if not moe:
                            op("act", lambda e, py=py, ya=ya: e.copy(out=ya, in_=py[:, :]), reads=pyn, writes=["yacc"])
                        elif ex == 0:
                            op("dve", lambda e, py=py, ya=ya, s=s, ex=ex: e.tensor_scalar(out=ya, in0=py[:, :], scalar1=gates[:, s, ex:ex + 1], scalar2=None, op0=ALU.mult), reads=pyn + ["gates"], writes=["yacc"])
                        else:
                            op("dve", lambda e, py=py, ya=ya, s=s, ex=ex: e.scalar_tensor_tensor(out=ya, in0=py[:, :], scalar=gates[:, s, ex:ex + 1], in1=ya, op0=ALU.mult, op1=ALU.add), reads=pyn + ["gates", "yacc"], writes=["yacc"])
            for s in range(4):
                residual_update(l, t0, s, None, None, xt, None, tmpx, junk, ssy, rsy, ggf, "ggf", yacc=yacc[:, s, :])

    for l in range(n_layers):
        layer_prologue(l)
        mixer(l)
        if do_ffn:
            ffn(l)
    if S.limit is not None:
        S.emit(final_waits=[S.ops[e][-1] for e in S.ENGS if S.ops[e]])
    else:
        S.emit(final_waits=list(last_x_dma.values()))
    S.close()
    return nc


def prep_inputs(inputs):
    f = lambda a: np.ascontiguousarray(np.asarray(a, dtype=np.float32))
    col = lambda v: f(v).reshape(8, 128).T
    L = DEPTH
    cols = []
    for l in range(L):
        vs = [inputs["g_pre_mix"][l], inputs["g_pre_ffn"][l], inputs["hgrn_gnorm"][l],
              inputs["mlstm_conv_w"][l][0], inputs["mlstm_conv_w"][l][1], inputs["mlstm_conv_w"][l][2], inputs["mlstm_conv_w"][l][3],
              inputs["mlstm_conv_b"][l], inputs["mlstm_gnorm"][l], inputs["mlstm_skip"][l]]
        for v in vs:
            cols.append(col(v))
    for l in range(L):
        cols.append(col(inputs["hgrn_lb"][l]))
    vecs = f(np.concatenate(cols, axis=1))
    bd = np.zeros((L, 128, 3, 8, 128), np.float32)
    for mi, nm in enumerate(("mlstm_wq", "mlstm_wk", "mlstm_wv")):
        w = f(inputs[nm]).reshape(L, 8, 32, 4, 4)
        for n in range(32):
            bd[:, 4 * n:4 * n + 4, mi, :, 4 * n:4 * n + 4] = w[:, :, n].transpose(0, 2, 1, 3)
    bd = f(bd.reshape(L, 128, 3 * 8 * 128))
    wg = np.concatenate([f(inputs["mlstm_w_ig"]), f(inputs["mlstm_w_fg"])], axis=2)
    wg = f(wg.reshape(L, 24, 128, 8).transpose(0, 2, 1, 3).reshape(L, 128, 192))
    bg = f(np.concatenate([f(inputs["mlstm_b_ig"]), f(inputs["mlstm_b_fg"])], axis=1))
    rt = f(f(inputs["moe_router"]).reshape(2, 8, 128, 8).transpose(0, 2, 1, 3).reshape(2, 128, 64))
    shared = {
        "vecs": vecs, "w_ada": f(inputs["w_ada"]), "b_ada": f(inputs["b_ada"]),
        "g_post_mix": f(inputs["g_post_mix"]), "g_post_ffn": f(inputs["g_post_ffn"]),
        "w_in": f(inputs["w_in"]), "bd": bd, "wgate": wg, "bgate": bg,
        "w_proj_a": f(inputs["w_proj_a"]), "w_proj_b": f(inputs["w_proj_b"]), "w_out": f(inputs["w_out"]),
        "ffn_w1": f(inputs["ffn_w1"]), "ffn_w3": f(inputs["ffn_w3"]), "ffn_w2": f(inputs["ffn_w2"]),
        "router": rt, "moe_w1": f(inputs["moe_w1"]), "moe_w3": f(inputs["moe_w3"]), "moe_w2": f(inputs["moe_w2"]),
    }
    x = f(inputs["x"])
    c = f(inputs["c"])
    maps = []
    for b in range(x.shape[0]):
        m = dict(shared)
        m["x"] = x[b]
        m["ccol"] = f(c[b].reshape(8, 128).T)
        maps.append(m)
    return maps


_NC_CACHE = {}


def kernel(**inputs):
    maps = prep_inputs(inputs)
    if "nc" not in _NC_CACHE:
        _NC_CACHE["nc"] = build_program()
    nc = _NC_CACHE["nc"]
    res = run_bass_kernel_spmd(nc, maps, core_ids=list(range(NCORES)))
    return np.stack([np.asarray(r["out"], dtype=np.float32) for r in res.results], axis=0)
```

```python
import contextlib
import types
import numpy as np
import concourse.bass as bass
import concourse.mybir as mybir
from concourse.bass_utils import run_bass_kernel_spmd

F32 = mybir.dt.float32
BF16 = mybir.dt.bfloat16
AF = mybir.ActivationFunctionType
ALU = mybir.AluOpType
AX = mybir.AxisListType

SEM_CAP = 30000
D = 1024
SEQ = 4096
DEPTH = 4
NCORES = 8
TT = 256
TF = 512
EPS = 1e-6
NV = 10
DEBUG = False


class Dep:
    __slots__ = ("name", "last_w", "readers")

    def __init__(self, name):
        self.name = name
        self.last_w = None
        self.readers = []


class Op:
    __slots__ = ("eng", "fn", "deps", "is_dma", "key", "needs_inc", "sem", "val")

    def __init__(self, eng, fn, is_dma=False, key=None):
        self.eng = eng
        self.fn = fn
        self.deps = []
        self.is_dma = is_dma
        self.key = key
        self.needs_inc = False
        self.sem = None
        self.val = 0


class Sched:
    ENGS = ("pe", "act", "dve", "pool", "sp")

    def __init__(self, nc):
        self.nc = nc
        self.ops = {e: [] for e in self.ENGS}
        self.all_ops = []
        self.deps = {}
        self.stack = contextlib.ExitStack()
        self.fence = []
        self.passed = {e: True for e in self.ENGS}

    def sb(self, name, shape, dtype=F32):
        return self.stack.enter_context(self.nc.sbuf_tensor("sb_" + name, list(shape), dtype))

    def ps(self, name, shape, dtype=F32):
        return self.stack.enter_context(self.nc.psum_tensor("ps_" + name, list(shape), dtype))

    def _D(self, x):
        d = self.deps.get(x)
        if d is None:
            d = self.deps[x] = Dep(x)
        return d

    def barrier(self):
        self.fence = [self.ops[e][-1] for e in self.ENGS if self.ops[e]]
        self.passed = {e: False for e in self.ENGS}

    limit = None

    def _record(self, op, reads, writes):
        if self.limit is not None and len(self.all_ops) >= self.limit:
            return op
        rr, ww = [], []
        for r in reads:
            if len(r) >= 2 and r[0] == "P" and r[1].isdigit():
                ww.append(r[:2])
            else:
                rr.append(r)
        for w in writes:
            if len(w) >= 2 and w[0] == "P" and w[1].isdigit():
                ww.append(w[:2])
            else:
                ww.append(w)
        reads, writes = rr, list(dict.fromkeys(ww))
        deps = []
        if not self.passed[op.eng]:
            deps.extend(self.fence)
            self.passed[op.eng] = True
        for r in reads:
            r = self._D(r)
            if r.last_w is not None:
                deps.append(r.last_w)
        for w in writes:
            w = self._D(w)
            if w.last_w is not None:
                deps.append(w.last_w)
            deps.extend(w.readers)
        seen = set()
        for d in deps:
            if d is op or id(d) in seen:
                continue
            seen.add(id(d))
            op.deps.append(d)
        for r in reads:
            self._D(r).readers.append(op)
        for w in writes:
            w = self._D(w)
            w.last_w = op
            w.readers = []
        self.ops[op.eng].append(op)
        self.all_ops.append(op)
        return op

    @staticmethod
    def _freeze(fn):
        if fn.__closure__ is None:
            return fn
        cells = []
        for c in fn.__closure__:
            try:
                cells.append(types.CellType(c.cell_contents))
            except ValueError:
                cells.append(c)
        return types.FunctionType(fn.__code__, fn.__globals__, fn.__name__, fn.__defaults__, tuple(cells))

    def op(self, eng, fn, reads=(), writes=()):
        return self._record(Op(eng, self._freeze(fn)), reads, writes)

    def dma(self, eng, out, in_, reads=(), writes=(), key=None, **kw):
        if key is None:
            key = writes[0] if writes else reads[0]
        fn = lambda e: e.dma_start(out=out, in_=in_, **kw)
        return self._record(Op(eng, fn, is_dma=True, key=key), reads, writes)

    def emit(self, final_waits=()):
        nc = self.nc
        for op in self.all_ops:
            for d in op.deps:
                if d.eng == "pe" and op.eng == "pe" and not d.is_dma:
                    continue
                d.needs_inc = True
        for op in final_waits:
            op.needs_inc = True
        print("ops per engine", {e: len(self.ops[e]) for e in self.ENGS})
        sems = {}

        def get_sem(name):
            s = sems.get(name)
            if s is None:
                s = sems[name] = self.stack.enter_context(nc.semaphore(name))
            return s

        cnt = {}
        ccnt = {e: 0 for e in self.ENGS}
        for op in self.all_ops:
            if op.is_dma:
                k = cnt.get(op.key, 0) + 1
                cnt[op.key] = k
                per = SEM_CAP // 16
                ep, v = divmod(k - 1, per)
                op.sem = get_sem("d_%s_%d" % (op.key, ep))
                op.val = (v + 1) * 16
            elif op.needs_inc:
                c = ccnt[op.eng]
                ep, v = divmod(c, SEM_CAP)
                op.sem = get_sem("e_%s_%d" % (op.eng, ep))
                op.val = v + 1
                ccnt[op.eng] = c + 1
        engmap = {"pe": "tensor", "act": "scalar", "dve": "vector", "pool": "gpsimd", "sp": "sync"}

        def run(engname, eng):
            known = {}
            for op in self.ops[engname]:
                for d in op.deps:
                    if d.eng == "pe" and engname == "pe" and not d.is_dma:
                        continue
                    sid = d.sem.name
                    if known.get(sid, 0) >= d.val:
                        continue
                    eng.wait_ge(d.sem, d.val)
                    known[sid] = d.val
                ins = op.fn(eng)
                if op.is_dma:
                    ins.then_inc(op.sem, 16)
                elif op.needs_inc:
                    ins.then_inc(op.sem, 1)
            if engname == "sp":
                for op in final_waits:
                    eng.wait_ge(op.sem, op.val)

        with nc.Block() as block:
            for engname in self.ENGS:
                getattr(block, engmap[engname])(lambda eng, _n=engname: run(_n, eng))

    def close(self):
        self.stack.close()


class Arena:
    def __init__(self, S, name, words):
        self.t = S.sb(name, [128, words], F32)
        self.words = words
        self.off = 0

    def reset(self):
        self.off = 0

    def alloc(self, shape, dtype=F32):
        n = int(np.prod(shape[1:]))
        w = n if dtype == F32 else (n + 1) // 2
        assert self.off + w <= self.words, ("arena overflow", self.off, w, self.words)
        v = self.t[0:shape[0], self.off:self.off + w]
        self.off += w
        if dtype != F32:
            v = v.bitcast(dtype)[:, 0:n]
        if len(shape) == 3:
            v = v.rearrange("p (a b) -> p a b", b=shape[2])
        elif len(shape) == 4:
            v = v.rearrange("p (a b c) -> p a b c", b=shape[2], c=shape[3])
        elif len(shape) == 5:
            v = v.rearrange("p (a b c d) -> p a b c d", b=shape[2], c=shape[3], d=shape[4])
        return v


def bc(ap, shape):
    return ap.to_broadcast(list(shape))


def build_program(n_layers=DEPTH, do_ffn=True, n_mix_tiles=SEQ // TT, n_ffn_tiles=SEQ // TF):
    nc = bass.Bass("TRN2", target_bir_lowering=False)
    di = lambda name, shape, dt=F32: nc.dram_tensor(name, list(shape), dt, kind="ExternalInput").ap()
    x_in = di("x", [SEQ, D])
    ccol_d = di("ccol", [128, 8])
    vecs_d = di("vecs", [128, DEPTH * NV * 8 + DEPTH * 8])
    w_ada_d = di("w_ada", [DEPTH, D, 6 * D])
    b_ada_d = di("b_ada", [DEPTH, 6 * D])
    gpm_d = di("g_post_mix", [DEPTH, D])
    gpf_d = di("g_post_ffn", [DEPTH, D])
    w_in_d = di("w_in", [DEPTH, D, 8 * D])
    bd_d = di("bd", [DEPTH, 128, 3 * 8 * 128])
    wgate_d = di("wgate", [DEPTH, 128, 24 * 8])
    bgate_d = di("bgate", [DEPTH, 8])
    wpa_d = di("w_proj_a", [DEPTH, D, D])
    wpb_d = di("w_proj_b", [DEPTH, D, D])
    wo_d = di("w_out", [DEPTH, D, D])
    if not do_ffn:
        di = lambda name, shape, dt=F32: nc.dram_tensor(name, [1, 1], dt, kind="ExternalInput").ap()
    f1_d = di("ffn_w1", [2, D, 2816])
    f3_d = di("ffn_w3", [2, D, 2816])
    f2_d = di("ffn_w2", [2, 2816, D])
    rt_d = di("router", [2, 128, 64])
    m1_d = di("moe_w1", [2, 8, D, 3584])
    m3_d = di("moe_w3", [2, 8, D, 3584])
    m2_d = di("moe_w2", [2, 8, 3584, D])
    out_d = nc.dram_tensor("out", [SEQ, D], F32, kind="ExternalOutput").ap()
    sc = lambda name, shape: nc.dram_tensor(name, list(shape), BF16).ap()
    win_s = sc("win_s", [DEPTH, 32, 128, 8, 256])
    wpa_s = sc("wpa_s", [DEPTH, 4, 128, 8, 256])
    wpb_s = sc("wpb_s", [DEPTH, 4, 128, 8, 256])
    wo_s = sc("wo_s", [DEPTH, 4, 128, 8, 256])
    f13_s = sc("f13_s", [2, 11, 128, 2, 8, 256])
    f2_s = sc("f2_s", [2, 2, 128, 22, 512])
    m13_s = sc("m13_s", [2, 8, 14, 128, 2, 8, 256])
    m2_s = sc("m2_s", [2, 8, 2, 128, 28, 512])

    S = Sched(nc)
    op = S.op
    idf = S.sb("idf", [128, 128], F32)
    idb = S.sb("idb", [128, 128], BF16)
    mask2 = S.sb("mask2", [128, 128], F32)
    maskm = S.sb("maskm", [128, 128], F32)
    mask01 = S.sb("mask01", [128, TT], F32)
    negm = S.sb("negm", [4, 128], F32)
    m01r = S.sb("m01r", [4, 128], F32)
    ones4 = S.sb("ones4", [4, 128], F32)
    sel4 = S.sb("sel4", [4, 4, 128], F32)
    id4 = S.sb("id4", [4, 4], F32)
    onesb = S.sb("onesb", [1, 128], BF16)
    vecs = S.sb("vecs", [128, DEPTH * NV * 8 + DEPTH * 8], F32)
    lbc = S.sb("lbc", [128, DEPTH, 8], F32)
    oml = S.sb("oml", [128, DEPTH, 8], F32)
    noml = S.sb("noml", [128, DEPTH, 8], F32)
    cbc = S.sb("cbc", [128, 8, 128], BF16)
    ccol = S.sb("ccol", [128, 8], F32)
    cact = S.sb("cact", [128, 8], F32)
    hsc_m = S.sb("hsc_m", [128, 8], F32)
    hbi_m = S.sb("hbi_m", [128, 8], F32)
    hsc_f = S.sb("hsc_f", [128, 8], F32)
    hbi_f = S.sb("hbi_f", [128, 8], F32)
    ggm = S.sb("ggm", [128, D], F32)
    ggf = S.sb("ggf", [128, D], F32)
    bdt = S.sb("bdt", [128, 3, 8, 128], BF16)
    wgt = S.sb("wgt", [128, 24, 8], BF16)
    bgt = S.sb("bgt", [128, 8], F32)
    rtt = S.sb("rtt", [128, 8, 8], F32)
    hst = S.sb("hst", [128, 8, 128], F32)
    hstb = S.sb("hstb", [128, 2, 2, 128], BF16)
    Cst = S.sb("Cst", [128, 4, 2, 257], F32)
    Cbf = S.sb("Cbf", [128, 4, 2, 257], BF16)
    mprev = S.sb("mprev", [4, 1], F32)
    PB = [S.ps("pb%d" % i, [128, 512], F32) for i in range(8)]
    P7b = PB[7][:, :].bitcast(BF16)
    P7N = ["P7a", "P7b", "P7c", "P7d"]

    def p7(q, n):
        nq = (n + 255) // 256
        return P7b[:, q * 256:q * 256 + n], P7N[q:q + nq]
    ARW = 42100
    AR = Arena(S, "arena", ARW)

    def vcol(l, v):
        o = (l * NV + v) * 8
        return vecs[:, o:o + 8]

    V_GPRE_M, V_GPRE_F, V_HGN, V_CW0, V_CB, V_MGN, V_SKIP = 0, 1, 2, 3, 7, 8, 9

    op("pool", lambda e: e.memset(idf[:], 0.0), writes=["idf"])
    op("pool", lambda e: e.affine_select(out=idf[:], in_=idf[:], pattern=[[-1, 128]], compare_op=ALU.not_equal,
                                         fill=1.0, base=0, channel_multiplier=1), reads=["idf"], writes=["idf"])
    op("dve", lambda e: e.tensor_copy(out=idb[:], in_=idf[:]), reads=["idf"], writes=["idb"])
    op("pool", lambda e: e.memset(mask2[:], 1.0), writes=["mask2"])
    op("pool", lambda e: e.affine_select(out=mask2[:], in_=mask2[:], pattern=[[1, 128]], compare_op=ALU.is_ge,
                                         fill=0.0, base=0, channel_multiplier=-1), reads=["mask2"], writes=["mask2"])
    op("pool", lambda e: e.memset(mask2[0:64, 64:128], 0.0), reads=["mask2"], writes=["mask2"])
    op("pool", lambda e: e.tensor_scalar_mul(out=maskm[:], in0=mask2[:], scalar1=1.0 / 16.0), reads=["mask2"], writes=["maskm"])
    op("pool", lambda e: e.memset(mask01[:], 1.0), writes=["mask01"])
    op("pool", lambda e: e.memset(mask01[:].rearrange("p (c j) -> p c j", j=64)[:, :, 0:1], 0.0), reads=["mask01"], writes=["mask01"])
    op("pool", lambda e: e.memset(negm[:], 0.0), writes=["negm"])
    op("pool", lambda e: e.memset(negm[:].rearrange("p (c j) -> p c j", j=64)[:, :, 0:1], -1e30), reads=["negm"], writes=["negm"])
    op("pool", lambda e: e.memset(m01r[:], 1.0), writes=["m01r"])
    op("pool", lambda e: e.memset(m01r[:].rearrange("p (c j) -> p c j", j=64)[:, :, 0:1], 0.0), reads=["m01r"], writes=["m01r"])
    op("pool", lambda e: e.memset(ones4[:], 1.0), writes=["ones4"])
    op("pool", lambda e: e.tensor_copy(out=id4[:], in_=idf[0:4, 0:4]), reads=["idf"], writes=["id4"])
    op("pool", lambda e: e.tensor_copy(out=sel4[:], in_=bc(idf[0:4, 0:4].unsqueeze(2), [4, 4, 128])), reads=["idf"], writes=["sel4"])
    op("pool", lambda e: e.memset(onesb[:], 1.0), writes=["onesb"])
    S.dma("sp", vecs[:], vecs_d, writes=["vecs"])
    S.dma("sp", ccol[:], ccol_d, writes=["ccol"])
    op("act", lambda e: e.activation(out=cact[:], in_=ccol[:], func=AF.Silu), reads=["ccol"], writes=["cact"])
    op("dve", lambda e: e.tensor_copy(out=cbc[:], in_=bc(cact[:].unsqueeze(2), [128, 8, 128])), reads=["cact"], writes=["cbc"])
    lbraw = vecs[:, DEPTH * NV * 8:DEPTH * NV * 8 + DEPTH * 8].rearrange("p (l c) -> p l c", c=8)
    lbe = S.sb("lbe", [128, DEPTH, 8], F32)
    lbs = S.sb("lbs", [128, 8], F32)
    op("act", lambda e: e.activation(out=lbe[:], in_=lbraw, func=AF.Exp), reads=["vecs"], writes=["lbe"])
    op("dve", lambda e: e.tensor_add(out=lbs[:], in0=lbe[:, 0, :], in1=lbe[:, 1, :]), reads=["lbe"], writes=["lbs"])
    op("dve", lambda e: e.tensor_add(out=lbs[:], in0=lbs[:], in1=lbe[:, 2, :]), reads=["lbe", "lbs"], writes=["lbs"])
    op("dve", lambda e: e.tensor_add(out=lbs[:], in0=lbs[:], in1=lbe[:, 3, :]), reads=["lbe", "lbs"], writes=["lbs"])
    op("dve", lambda e: e.reciprocal(out=lbs[:], in_=lbs[:]), reads=["lbs"], writes=["lbs"])
    op("dve", lambda e: e.tensor_mul(out=lbe[:], in0=lbe[:], in1=bc(lbs[:].unsqueeze(1), [128, DEPTH, 8])), reads=["lbe", "lbs"], writes=["lbe"])
    op("dve", lambda e: e.memset(lbc[:, 0, :], 0.0), writes=["lbc"])
    for l in range(1, DEPTH):
        op("dve", lambda e, l=l: e.tensor_add(out=lbc[:, l, :], in0=lbc[:, l - 1, :], in1=lbe[:, l, :]), reads=["lbe", "lbc"], writes=["lbc"])
    op("dve", lambda e: e.tensor_scalar(out=oml[:], in0=lbc[:], scalar1=-1.0, scalar2=1.0, op0=ALU.mult, op1=ALU.add), reads=["lbc"], writes=["oml"])
    op("dve", lambda e: e.tensor_scalar_mul(out=noml[:], in0=oml[:], scalar1=-1.0), reads=["oml"], writes=["noml"])

    def conv_kxn(dst, src, ngroups, depname):
        v = src.rearrange("(kc p) (g c) -> g p kc c", p=128, c=256)
        for g in range(ngroups):
            S.dma("pool", dst[g], v[g], writes=[depname])

    def conv_layer(l):
        conv_kxn(win_s[l], w_in_d[l], 32, "win%d" % l)
        conv_kxn(wpa_s[l], wpa_d[l], 4, "wpa%d" % l)
        conv_kxn(wpb_s[l], wpb_d[l], 4, "wpb%d" % l)
        conv_kxn(wo_s[l], wo_d[l], 4, "wo%d" % l)
        if not do_ffn:
            return
        j = l // 2
        if l % 2 == 0:
            v1 = f1_d[j].rearrange("(kc p) (g c) -> g p kc c", p=128, c=256)
            v3 = f3_d[j].rearrange("(kc p) (g c) -> g p kc c", p=128, c=256)
            for g in range(11):
                S.dma("pool", f13_s[j, g, :, 0], v1[g], writes=["f13_%d" % l])
                S.dma("pool", f13_s[j, g, :, 1], v3[g], writes=["f13_%d" % l])
            v2 = f2_d[j].rearrange("(fc p) (h c) -> h p fc c", p=128, c=512)
            for h in range(2):
                S.dma("pool", f2_s[j, h], v2[h], writes=["f2_%d" % l])
        else:
            for ex in range(8):
                v1 = m1_d[j, ex].rearrange("(kc p) (g c) -> g p kc c", p=128, c=256)
                v3 = m3_d[j, ex].rearrange("(kc p) (g c) -> g p kc c", p=128, c=256)
                for g in range(14):
                    S.dma("pool", m13_s[j, ex, g, :, 0], v1[g], writes=["m13_%d_%d" % (l, ex)])
                    S.dma("pool", m13_s[j, ex, g, :, 1], v3[g], writes=["m13_%d_%d" % (l, ex)])
                v2 = m2_d[j, ex].rearrange("(fc p) (h c) -> h p fc c", p=128, c=512)
                for h in range(2):
                    S.dma("pool", m2_s[j, ex, h], v2[h], writes=["m2_%d_%d" % (l, ex)])

    for l in range(n_layers):
        conv_layer(l)

    def rstd_from_ss(rs, ss, n, dep_ss, dep_rs):
        op("dve", lambda e: e.tensor_scalar(out=rs, in0=ss, scalar1=1.0 / n, scalar2=EPS, op0=ALU.mult, op1=ALU.add), reads=[dep_ss], writes=[dep_rs])
        op("act", lambda e: e.activation(out=rs, in_=rs, func=AF.Ln), reads=[dep_rs], writes=[dep_rs])
        op("act", lambda e: e.activation(out=rs, in_=rs, func=AF.Exp, scale=-0.5), reads=[dep_rs], writes=[dep_rs])

    xkeys = {}
    last_x_dma = {}

    def xsrc(l, first):
        return x_in if (l == 0 and first) else out_d

    def layer_prologue(l):
        AR.reset()
        S.barrier()
        wad = [AR.alloc([128, 8, 512], BF16) for _ in range(2)]
        bad = AR.alloc([1, 6 * D], BF16)
        gpb = [AR.alloc([128, D], F32) for _ in range(2)]
        tmpd = AR.alloc([128, 4, 128], F32)
        S.dma("pool", bad, b_ada_d[l:l + 1, :], writes=["bad"])
        S.dma("sp", gpb[0], gpm_d[l].partition_broadcast(128), writes=["gpb0"])
        S.dma("sp", gpb[1], gpf_d[l].partition_broadcast(128), writes=["gpb1"])
        S.dma("pool", bdt[:].rearrange("p a b c -> p (a b c)"), bd_d[l], writes=["bdt"])
        S.dma("pool", wgt[:].rearrange("p a b -> p (a b)"), wgate_d[l], writes=["wgt"])
        S.dma("sp", bgt[:], bgate_d[l].partition_broadcast(128), writes=["bgt"])
        if l % 2 == 1:
            S.dma("sp", rtt[:].rearrange("p a b -> p (a b)"), rt_d[l // 2], writes=["rtt"])
        wv = w_ada_d[l].rearrange("(kc p) (g c) -> g p kc c", p=128, c=512)
        for g in range(12):
            w = wad[g % 2]
            wn = "wad%d" % (g % 2)
            S.dma("pool", w, wv[g], writes=[wn], key="pl3_%d" % (g % 2))
            pb = PB[g % 2]
            pn = ["P%da" % (g % 2), "P%db" % (g % 2)]
            for kc in range(8):
                op("pe", lambda e, w=w, kc=kc, pb=pb: e.matmul(pb[:, :], lhsT=cbc[:, kc, :], rhs=w[:, kc, :], start=(kc == 0), stop=False),
                   reads=[wn, "cbc"], writes=pn)
            op("pe", lambda e, g=g, pb=pb: e.matmul(pb[:, :], lhsT=onesb[0:1, :], rhs=bad[0:1, g * 512:(g + 1) * 512], start=False, stop=True),
               reads=["bad", "onesb"], writes=pn)
            which = g // 2
            half = g % 2
            if which in (2, 5):
                dst = ggm if which == 2 else ggf
                dn = "ggm" if which == 2 else "ggf"
                gp = gpb[0] if which == 2 else gpb[1]
                gn = "gpb0" if which == 2 else "gpb1"
                op("dve", lambda e, pb=pb, dst=dst, gp=gp, half=half: e.tensor_mul(out=dst[:, half * 512:(half + 1) * 512], in0=pb[:, :], in1=gp[:, half * 512:(half + 1) * 512]),
                   reads=pn + [gn], writes=[dn])
            else:
                dst = {0: hbi_m, 1: hsc_m, 3: hbi_f, 4: hsc_f}[which]
                dn = {0: "hbi_m", 1: "hsc_m", 3: "hbi_f", 4: "hsc_f"}[which]
                op("dve", lambda e, pb=pb: e.tensor_mul(out=tmpd, in0=pb[:, :].rearrange("p (c j) -> p c j", j=128), in1=bc(idf[:].unsqueeze(1), [128, 4, 128])),
                   reads=pn + ["idf"], writes=["tmpd"])
                op("dve", lambda e, dst=dst, half=half: e.tensor_reduce(out=dst[:, half * 4:(half + 1) * 4], in_=tmpd, axis=AX.X, op=ALU.add),
                   reads=["tmpd"], writes=[dn])
        for (hs, hn, vi) in ((hsc_m, "hsc_m", V_GPRE_M), (hsc_f, "hsc_f", V_GPRE_F)):
            op("dve", lambda e, hs=hs, vi=vi: e.scalar_tensor_tensor(out=hs[:], in0=hs[:], scalar=1.0, in1=vcol(l, vi), op0=ALU.add, op1=ALU.mult),
               reads=[hn, "vecs"], writes=[hn])
        op("pool", lambda e: e.memset(hst[:], 0.0), writes=["hst"])
        op("pool", lambda e: e.memset(Cst[:], 0.0), writes=["Cst"])
        op("pool", lambda e: e.memset(Cbf[:], 0.0), writes=["Cbf"])
        op("pool", lambda e: e.memset(mprev[:], 0.0), writes=["mprev"])

    def load_norm_transpose(l, first, t0, nsub, hsc, hbi, hscn, hbin, xt, xn, hT, junk, ss, rs, hTf=None, xnn="xn", hTfn="hTf", junkn="junk"):
        src = xsrc(l, first)
        op("dve", lambda e: e.memset(ss, 0.0), writes=["ss"])
        for s in range(nsub):
            blk = (t0 + s * 128) // TT
            S.dma("sp", xt[:, s, :], src[t0 + s * 128:t0 + (s + 1) * 128, :], reads=["xrow%d" % blk], writes=["xt%d" % s], key="xl%d" % s)
            op("act", lambda e, s=s: e.activation(out=junk, in_=xt[:, s, :], func=AF.Square, accum_out=ss[:, s:s + 1]),
               reads=["xt%d" % s], writes=[junkn, "ss"])
        rstd_from_ss(rs, ss, D, "ss", "rs")
        dt = F32 if hTf is not None else BF16
        for s in range(nsub):
            op("dve", lambda e, s=s: e.tensor_scalar(out=xn[:, s, :], in0=xt[:, s, :], scalar1=rs[:, s:s + 1], scalar2=None, op0=ALU.mult),
               reads=["xt%d" % s, "rs"], writes=[xnn])
        idt = idf if hTf is not None else idb
        n = nsub * 128
        for dc in range(8):
            if hTf is not None:
                pt = PB[6 + dc % 2][:, 0:n]
                pn = ["P6a", "P6b"] if dc % 2 == 0 else P7N
            else:
                pt, pn = p7((dc % 2) * 2, n)
            for s in range(nsub):
                op("pe", lambda e, s=s, dc=dc, pt=pt: e.transpose(out=pt[:, s * 128:(s + 1) * 128], in_=xn[:, s, dc * 128:(dc + 1) * 128], identity=idt[:]),
                   reads=[xnn, "idb", "idf"], writes=pn)
            tgt = hTf if hTf is not None else hT
            tgn = hTfn if hTf is not None else "hT"
            op("act", lambda e, dc=dc, pt=pt, tgt=tgt: e.activation(out=tgt[:, dc, :], in_=pt, func=AF.Identity, scale=hsc[:, dc:dc + 1], bias=hbi[:, dc:dc + 1]),
               reads=pn + [hscn, hbin], writes=[tgn])
        if hTf is not None:
            op("dve", lambda e: e.tensor_copy(out=hT, in_=hTf), reads=[hTfn], writes=["hT"])

    def residual_update(l, t0, s, ypsum_list, ypn, xt, ysb, tmpx, junk, ssy, rsy, gg, ggn, yacc=None, ysbn="ysb", tmpxn="tmpx", junkn="junk"):
        if yacc is None:
            for (pa, c0, ncol) in ypsum_list:
                op("act", lambda e, pa=pa, c0=c0, ncol=ncol: e.copy(out=ysb[:, c0:c0 + ncol], in_=pa), reads=ypn, writes=[ysbn])
            ysrc, ysn = ysb, ysbn
        else:
            ysrc, ysn = yacc, "yacc"
        op("dve", lambda e: e.memset(ssy[:, 0:1], 0.0), writes=["ssy"])
        op("act", lambda e: e.activation(out=junk, in_=ysrc, func=AF.Square, accum_out=ssy[:, 0:1]), reads=[ysn], writes=[junkn, "ssy"])
        rstd_from_ss(rsy[:, 0:1], ssy[:, 0:1], D, "ssy", "rsy")
        op("dve", lambda e: e.scalar_tensor_tensor(out=tmpx, in0=ysrc, scalar=rsy[:, 0:1], in1=gg[:], op0=ALU.mult, op1=ALU.mult),
           reads=[ysn, "rsy", ggn], writes=[tmpxn])
        op("dve", lambda e: e.tensor_add(out=xt[:, s, :], in0=xt[:, s, :], in1=tmpx), reads=[tmpxn, "xt%d" % s], writes=["xt%d" % s])
        blk = (t0 + s * 128) // TT
        d = S.dma("sp", out_d[t0 + s * 128:t0 + (s + 1) * 128, :], xt[:, s, :], reads=["xt%d" % s], writes=["xrow%d" % blk], key="xs%d" % s)
        last_x_dma["xs%d" % s] = d

    def mixer(l):
        AR.reset()
        S.barrier()
        A = AR.alloc
        xt = A([128, 2, D]); xn = A([128, 2, D], BF16); hT = A([128, 8, TT], BF16)
        ss = A([128, 2]); rs = A([128, 2]); ssy = A([128, 2]); rsy = A([128, 2])
        NWG = 3
        wg = [A([128, 8, 256], BF16) for _ in range(NWG)]
        qs = A([128, 8, TT], BF16)
        T1 = A([128, 8, TT]); T2 = A([128, 8, TT]); T3 = A([128, 8, TT]); T4 = A([128, 8, TT])
        QEO = A([128, 8, 2, 2, 128], BF16)
        KT = A([128, 8, TT], BF16)
        Ktok = A([128, 8, 2, 128], BF16)
        vtok = A([128, 2, D], BF16)
        sga = A([128, 8, TT], BF16)
        yaT = A([128, 8, TT], BF16)
        E1 = A([128, 8, 4]); E2 = A([128, 8, 4]); E3 = A([128, 8, 4]); dE = A([128, 8, 4])
        STb8 = A([128, 8, 128], BF16)
        STb = A([128, 2, 128], BF16)
        ssq = A([128, 8]); rsq = A([128, 8])
        onb = A([128, D], BF16)
        xm = A([128, 8, 3 + TT])
        xcT = A([128, 8, TT], BF16); xmT = A([128, 8, TT], BF16)
        qT = A([128, 8, TT], BF16); kT = A([128, 8, TT], BF16); vT = A([128, 8, TT], BF16)
        ktok = A([128, 2, D], BF16)
        vext = A([128, 2, 4, 257], BF16)
        sob = A([128, 8, TT], BF16); sgA = A([128, 8, TT], BF16); sgB = A([128, 8, TT], BF16)
        DT = A([128, 2, 128]); DTm = A([128, 2, 128])
        kw = A([128, 2, 256], BF16)
        tnum = A([128, 257])
        hnb = A([128, D], BF16)
        junk = hnb
        ybT = A([128, 8, TT], BF16)
        mixT = A([128, 8, TT], BF16)
        gpre = A([128, 8]); gli = A([128, 4]); glf = A([128, 4])
        rows = A([4, 8, 128])
        gcol = A([128, 2, 16])
        decb = A([128, 2, 2, 4])
        dexp = A([4, 4, 2])
        sm = A([128, 32])
        acc = T1; o_sb = T2.rearrange("p a b -> p (a b)")[:, 0:D]; sqb = T3.rearrange("p a b -> p (a b)")[:, 0:D]
        tot = T4.rearrange("p a b -> p (a b)")[:, 0:4 * 257].rearrange("p (a b) -> p a b", b=257)
        sqm = T3.rearrange("p a b -> p (a b)")[:, 0:D].rearrange("p (a b) -> p a b", b=256)
        ysb = T1.rearrange("p a b -> p (a b)")[:, 0:D]
        tmpx = T2.rearrange("p a b -> p (a b)")[:, 0:D]
        m1 = T3.rearrange("p a b -> p (a b)")[:, 0:2 * TT].rearrange("p (a b) -> p a b", b=TT)
        m2 = T4.rearrange("p a b -> p (a b)")[:, 0:2 * TT].rearrange("p (a b) -> p a b", b=TT)
        clb = T1.rearrange("p a b -> p (a b)")[:, 0:1024].rearrange("p (h t) -> p h t", t=128)
        tkv8 = T1.rearrange("p a b -> p (a b)")[:, 1024:2048].rearrange("p (h t) -> p h t", t=128)
        hsb8 = T3.rearrange("p a b -> p (a b)")[:, 0:1024].bitcast(BF16).rearrange("p (e h t) -> p e h t", e=2, t=128)

        print("mixer arena words", AR.off)
        op("pool", lambda e: e.memset(xm[:, :, 0:3], 0.0), writes=["xm"])
        op("pool", lambda e: e.memset(QEO, 0.0), writes=["QEO"])
        op("pool", lambda e: e.memset(vext[:, :, :, 256:257], 1.0), writes=["vext"])

        NPP = 6
        pp_names = ["P%d" % i for i in range(NPP)]

        def pp_view(i):
            return PB[i][:, 0:256]

        state = {"pp": 0, "wg": 0}

        def next_pp():
            i = state["pp"] % NPP
            state["pp"] += 1
            return pp_view(i), [pp_names[i]]

        def load_wg(src_ap, depname):
            i = state["wg"] % NWG
            state["wg"] += 1
            S.dma("sp", wg[i], src_ap, reads=[depname], writes=["wg%d" % i], key="wg%d" % i)
            return wg[i], "wg%d" % i

        for ti in range(n_mix_tiles):
            t0 = ti * TT
            load_norm_transpose(l, True, t0, 2, hsc_m, hbi_m, "hsc_m", "hbi_m", xt, xn, hT, junk, ss, rs, junkn="hnb")
            def fm_chunk(w, wn, half, rhs_t, rhs_n, evac):
                pv, pn = next_pp()
                for kc in range(8):
                    op("pe", lambda e, kc=kc, pv=pv: e.matmul(pv, lhsT=w[:, kc, half * 128:(half + 1) * 128], rhs=rhs_t[:, kc, :], start=(kc == 0), stop=(kc == 7)),
                       reads=[wn, rhs_n], writes=pn)
                evac(pv, pn)

            for grp in (0, 1, 2, 3, 12, 13, 14, 15, 4, 5, 6, 7, 20, 21, 22, 23, 24, 25, 26, 27, 28, 29, 30, 31, 16, 17, 18, 19, 8, 9, 10, 11):
                w, wn = load_wg(win_s[l, grp], "win%d" % l)
                if 8 <= grp < 12:
                    for s in range(2):
                        pv, pn = next_pp()
                        for kc in range(8):
                            op("pe", lambda e, kc=kc, pv=pv, s=s, w=w: e.matmul(pv, lhsT=hT[:, kc, s * 128:(s + 1) * 128], rhs=w[:, kc, :], start=(kc == 0), stop=(kc == 7)),
                               reads=[wn, "hT"], writes=pn)
                        c0 = (grp - 8) * 256
                        op("dve", lambda e, pv=pv, s=s, c0=c0: e.tensor_copy(out=vtok[:, s, c0:c0 + 256], in_=pv), reads=pn, writes=["vtok"])
                    continue
                for half in range(2):
                    m = grp * 2 + half
                    kind, hd = m // 8, m % 8
                    if kind == 0:
                        ev = lambda pv, pn, hd=hd: op("act", lambda e: e.activation(out=qs[:, hd, :], in_=pv, func=AF.Silu), reads=pn, writes=["qs"])
                    elif kind == 1:
                        ev = lambda pv, pn, hd=hd: op("act", lambda e: e.activation(out=T1[:, hd, :], in_=pv, func=AF.Sigmoid), reads=pn, writes=["T1"])
                    elif kind == 3:
                        ev = lambda pv, pn, hd=hd: op("act", lambda e: e.activation(out=sga[:, hd, :], in_=pv, func=AF.Silu), reads=pn, writes=["sga"])
                    elif kind == 4:
                        ev = lambda pv, pn, hd=hd: op("dve", lambda e: e.tensor_copy(out=xm[:, hd, 3:3 + TT], in_=pv), reads=pn, writes=["xm"])
                    elif kind == 5:
                        ev = lambda pv, pn, hd=hd: op("act", lambda e: e.activation(out=sob[:, hd, :], in_=pv, func=AF.Sigmoid), reads=pn, writes=["sob"])
                    elif kind == 6:
                        ev = lambda pv, pn, hd=hd: op("act", lambda e: e.activation(out=sgA[:, hd, :], in_=pv, func=AF.Sigmoid), reads=pn, writes=["sgA"])
                    else:
                        ev = lambda pv, pn, hd=hd: op("act", lambda e: e.activation(out=sgB[:, hd, :], in_=pv, func=AF.Sigmoid), reads=pn, writes=["sgB"])
                    fm_chunk(w, wn, half, hT, "hT", ev)

            for hd in range(8):
                op("dve", lambda e, hd=hd: e.tensor_scalar(out=T4[:, hd, :], in0=T1[:, hd, :], scalar1=noml[:, l, hd:hd + 1], scalar2=oml[:, l, hd:hd + 1], op0=ALU.mult, op1=ALU.add),
                   reads=["T1", "noml", "oml"], writes=["T4"])
            for hd in range(8):
                op("act", lambda e, hd=hd: e.activation(out=T2[:, hd, :], in_=T1[:, hd, :], func=AF.Ln, scale=oml[:, l, hd:hd + 1], bias=lbc[:, l, hd:hd + 1]),
                   reads=["T1", "oml", "lbc"], writes=["T2"])
            for hd in range(8):
                op("dve", lambda e, hd=hd: e.tensor_tensor_scan(out=T3[:, hd, :], data0=mask01[:], data1=T2[:, hd, :], initial=0.0, op0=ALU.mult, op1=ALU.add),
                   reads=["T2", "mask01"], writes=["T3"])
            b4 = T3.rearrange("p h (c j) -> p h c j", j=64)
            op("dve", lambda e: e.tensor_sub(out=dE, in0=b4[:, :, :, 63], in1=b4[:, :, :, 31]), reads=["T3"], writes=["dE"])
            op("act", lambda e: e.activation(out=E1, in_=b4[:, :, :, 63], func=AF.Exp), reads=["T3"], writes=["E1"])
            op("act", lambda e: e.activation(out=E2, in_=dE, func=AF.Exp), reads=["dE"], writes=["E2"])
            op("act", lambda e: e.activation(out=E3, in_=b4[:, :, :, 31], func=AF.Exp), reads=["T3"], writes=["E3"])
            op("dve", lambda e: e.tensor_sub(out=T2.rearrange("p h (c j) -> p h c j", j=64), in0=b4, in1=bc(b4[:, :, :, 31:32], [128, 8, 4, 64])),
               reads=["T3", "T2"], writes=["T2"])
            op("act", lambda e: e.activation(out=T1, in_=T2, func=AF.Exp), reads=["T2", "T1"], writes=["T1"])
            op("act", lambda e: e.activation(out=T3, in_=T2, func=AF.Exp, scale=-1.0), reads=["T2", "E1", "E3", "dE"], writes=["T3"])
            for hd in range(8):
                qo = QEO[:, hd].rearrange("p a b c -> p (a b c)")
                for pr in range(2):
                    for eo in range(2):
                        c = pr * 2 + eo
                        o = pr * 256 + eo * 192
                        op("dve", lambda e, hd=hd, c=c, o=o, qo=qo: e.tensor_mul(out=qo[:, o:o + 64], in0=qs[:, hd, c * 64:(c + 1) * 64], in1=T1[:, hd, c * 64:(c + 1) * 64]),
                           reads=["qs", "T1"], writes=["QEO"])
            op("dve", lambda e: e.tensor_mul(out=KT, in0=T4, in1=T3), reads=["T4", "T3"], writes=["KT"])
            for hd in range(8):
                pt, pn = p7(2 + hd % 2, 256)
                for pr in range(2):
                    op("pe", lambda e, hd=hd, pr=pr, pt=pt: e.transpose(out=pt[:, pr * 128:(pr + 1) * 128], in_=KT[:, hd, pr * 128:(pr + 1) * 128], identity=idb[:]),
                       reads=["KT", "idb"], writes=pn)
                op("act", lambda e, hd=hd, pt=pt: e.copy(out=Ktok[:, hd].rearrange("p a b -> p (a b)"), in_=pt), reads=pn, writes=["Ktok"])
            for dc in range(8):
                op("dve", lambda e, dc=dc: e.tensor_scalar(out=acc[:, dc, :], in0=xm[:, dc, 3:3 + TT], scalar1=vcol(l, V_CW0 + 3)[:, dc:dc + 1], scalar2=vcol(l, V_CB)[:, dc:dc + 1], op0=ALU.mult, op1=ALU.add),
                   reads=["xm", "vecs", "T1"], writes=["T1"])
                for j in range(3):
                    op("dve", lambda e, dc=dc, j=j: e.scalar_tensor_tensor(out=acc[:, dc, :], in0=xm[:, dc, j:j + TT], scalar=vcol(l, V_CW0 + j)[:, dc:dc + 1], in1=acc[:, dc, :], op0=ALU.mult, op1=ALU.add),
                       reads=["xm", "vecs", "T1"], writes=["T1"])
            op("act", lambda e: e.activation(out=xcT, in_=acc, func=AF.Silu), reads=["T1"], writes=["xcT"])
            op("act", lambda e: e.copy(out=xmT, in_=xm[:, :, 3:3 + TT]), reads=["xm"], writes=["xmT"])
            op("pool", lambda e: e.tensor_copy(out=xm[:, :, 0:3], in_=xm[:, :, TT:TT + 3]), reads=["xm"], writes=["xm"])
            for (mi, srcT, srcn, dstT, dstn) in ((0, xcT, "xcT", qT, "qT"), (1, xcT, "xcT", kT, "kT"), (2, xmT, "xmT", vT, "vT")):
                for dc in range(8):
                    pv, pn = next_pp()
                    op("pe", lambda e, mi=mi, dc=dc, pv=pv, srcT=srcT: e.matmul(pv, lhsT=bdt[:, mi, dc, :], rhs=srcT[:, dc, :], start=True, stop=True),
                       reads=["bdt", srcn], writes=pn)
                    eng = "act" if dc % 2 == 0 else "dve"
                    if eng == "act":
                        op("act", lambda e, dc=dc, pv=pv, dstT=dstT: e.copy(out=dstT[:, dc, :], in_=pv), reads=pn, writes=[dstn])
                    else:
                        op("dve", lambda e, dc=dc, pv=pv, dstT=dstT: e.tensor_copy(out=dstT[:, dc, :], in_=pv), reads=pn, writes=[dstn])
            for pr in range(2):
                for (mi, srcT, srcn) in ((1, xcT, "xcT"), (2, xmT, "xmT")):
                    for hb in range(2):
                        pbk = PB[2]
                        pn = ["P2a", "P2b", "P2c", "P2d"]
                        for j in range(4):
                            dc = hb * 4 + j
                            op("pe", lambda e, mi=mi, dc=dc, j=j, srcT=srcT, pr=pr: e.matmul(pbk[:, j * 128:(j + 1) * 128], lhsT=srcT[:, dc, pr * 128:(pr + 1) * 128], rhs=bdt[:, mi, dc, :], start=True, stop=True),
                               reads=["bdt", srcn], writes=pn)
                        if mi == 1:
                            op("act", lambda e, pr=pr, hb=hb: e.copy(out=ktok[:, pr, hb * 512:(hb + 1) * 512], in_=pbk[:, :]), reads=pn, writes=["ktok"])
                        else:
                            op("dve", lambda e, pr=pr, hb=hb: e.tensor_copy(out=vext[:, pr, hb * 2:hb * 2 + 2, 0:256], in_=pbk[:, :].rearrange("p (a b) -> p a b", b=256)), reads=pn, writes=["vext"])
            for pr in range(2):
                pg = PB[5][:, 256:264]
                pgn = ["P5c"]
                i = 0
                for (srcT, srcn) in ((qT, "qT"), (kT, "kT"), (vT, "vT")):
                    for dc in range(8):
                        op("pe", lambda e, srcT=srcT, dc=dc, i=i, pr=pr: e.matmul(pg, lhsT=srcT[:, dc, pr * 128:(pr + 1) * 128], rhs=wgt[:, i, :], start=(i == 0), stop=(i == 23)),
                           reads=[srcn, "wgt"], writes=pgn)
                        i += 1
                op("dve", lambda e: e.tensor_add(out=gpre, in0=pg, in1=bgt[:]), reads=pgn + ["bgt"], writes=["gpre"])
                op("act", lambda e: e.activation(out=glf, in_=gpre[:, 4:8], func=AF.Exp, scale=-1.0), reads=["gpre"], writes=["glf"])
                op("act", lambda e: e.activation(out=glf, in_=glf, func=AF.Ln, bias=1.0), reads=["glf"], writes=["glf"])
                op("dve", lambda e: e.tensor_scalar_mul(out=glf, in0=glf, scalar1=-1.0), reads=["glf"], writes=["glf"])
                op("dve", lambda e: e.tensor_copy(out=gli, in_=gpre[:, 0:4]), reads=["gpre"], writes=["gli"])
                prw = PB[5][0:4, 384:512]
                prn = ["P5d"]
                op("pe", lambda e: e.matmul(prw, lhsT=gli, rhs=idf[:], start=True, stop=True), reads=["gli", "idf"], writes=prn)
                op("dve", lambda e: e.tensor_copy(out=rows[:, 0, :], in_=prw), reads=prn, writes=["rows"])
                op("pe", lambda e: e.matmul(prw, lhsT=glf, rhs=idf[:], start=True, stop=True), reads=["glf", "idf"], writes=prn)
                op("dve", lambda e: e.tensor_copy(out=rows[:, 1, :], in_=prw), reads=prn, writes=["rows"])
                op("dve", lambda e: e.tensor_tensor_scan(out=rows[:, 2, :], data0=m01r[:], data1=rows[:, 1, :], initial=0.0, op0=ALU.mult, op1=ALU.add), reads=["rows", "m01r"], writes=["rows"])
                op("dve", lambda e: e.tensor_sub(out=rows[:, 3, :], in0=rows[:, 0, :], in1=rows[:, 2, :]), reads=["rows"], writes=["rows"])
                op("dve", lambda e: e.tensor_tensor_scan(out=rows[:, 4, :], data0=negm[:], data1=rows[:, 3, :], initial=-1e30, op0=ALU.add, op1=ALU.max), reads=["rows", "negm"], writes=["rows"])
                for eo in range(2):
                    cs = slice(eo * 64, (eo + 1) * 64)
                    op("dve", lambda e, cs=cs: e.tensor_scalar(out=rows[:, 5, cs], in0=rows[:, 4, cs], scalar1=mprev[:, 0:1], scalar2=None, op0=ALU.max), reads=["rows", "mprev"], writes=["rows"])
                    op("dve", lambda e, cs=cs: e.tensor_scalar(out=rows[:, 6, cs], in0=rows[:, 5, cs], scalar1=mprev[:, 0:1], scalar2=-1.0, op0=ALU.subtract, op1=ALU.mult), reads=["rows", "mprev"], writes=["rows"])
                    last = eo * 64 + 63
                    op("dve", lambda e, cs=cs, last=last: e.tensor_scalar(out=rows[:, 7, cs], in0=rows[:, 3, cs], scalar1=rows[:, 5, last:last + 1], scalar2=None, op0=ALU.subtract), reads=["rows"], writes=["rows"])
                    op("dve", lambda e, last=last: e.tensor_add(out=mprev[:, 0:1], in0=rows[:, 2, last:last + 1], in1=rows[:, 5, last:last + 1]), reads=["rows", "mprev"], writes=["mprev"])
                op("dve", lambda e: e.scalar_tensor_tensor(out=rows[:, 1, :], in0=rows[:, 2, :], scalar=-1.0, in1=rows[:, 5, :], op0=ALU.mult, op1=ALU.subtract), reads=["rows"], writes=["rows"])
                op("act", lambda e: e.activation(out=rows[:, 6, :], in_=rows[:, 6, :], func=AF.Exp), reads=["rows"], writes=["rows"])
                op("act", lambda e: e.activation(out=rows[:, 1, :], in_=rows[:, 1, :], func=AF.Exp), reads=["rows"], writes=["rows"])
                op("act", lambda e: e.activation(out=rows[:, 7, :], in_=rows[:, 7, :], func=AF.Exp, bias=float(-np.log(16.0))), reads=["rows"], writes=["rows"])
                pcl = PB[5][:, 264:280]
                for qi, ri in enumerate((3, 6, 1, 7)):
                    op("pe", lambda e, qi=qi, ri=ri: e.matmul(pcl[:, qi * 4:(qi + 1) * 4], lhsT=rows[:, ri, :], rhs=id4[:], start=True, stop=True), reads=["rows", "id4"], writes=pgn)
                op("dve", lambda e, pr=pr: e.tensor_copy(out=gcol[:, pr, :], in_=pcl), reads=pgn, writes=["gcol"])
                wl = rows[:, 6, :].rearrange("p (c j) -> p c j", j=64)[:, :, 63]
                op("dve", lambda e, wl=wl: e.tensor_mul(out=dexp, in0=bc(wl.unsqueeze(1), [4, 4, 2]), in1=bc(id4[:].unsqueeze(2), [4, 4, 2])), reads=["rows", "id4"], writes=["dexp"])
                pdc = PB[5][:, 280:288]
                op("pe", lambda e: e.matmul(pdc, lhsT=ones4[:], rhs=dexp.rearrange("p a b -> p (a b)"), start=True, stop=True), reads=["dexp", "ones4"], writes=pgn)
                op("dve", lambda e, pr=pr: e.tensor_copy(out=decb[:, pr].rearrange("p e h -> p h e"), in_=pdc.rearrange("p (h e) -> p h e", e=2)), reads=pgn, writes=["decb"])
                for hd in range(8):
                    pv = PB[hd // 4][:, (hd % 4) * 128:(hd % 4) * 128 + 128]
                    pn = ["P%d" % (hd // 4)]
                    op("pe", lambda e, hd=hd, pv=pv: e.matmul(pv, lhsT=KT[:, hd, pr * 128:(pr + 1) * 128], rhs=QEO[:, hd, pr, 0, :], start=True, stop=False), reads=["KT", "QEO"], writes=pn)
                    op("pe", lambda e, hd=hd, pv=pv: e.matmul(pv, lhsT=KT[:, hd, pr * 128:(pr + 1) * 128], rhs=QEO[:, hd, pr, 1, :], start=False, stop=True), reads=["KT", "QEO"], writes=pn)
                for hf in range(2):
                    op("dve", lambda e, hf=hf: e.tensor_scalar(out=clb[:, hf * 4:(hf + 1) * 4, :], in0=PB[hf][:, :].rearrange("p (h t) -> p h t", t=128), scalar1=1e30, scalar2=-1e30, op0=ALU.min, op1=ALU.max),
                       reads=["P%d" % hf], writes=["clb%d" % hf, "T1"])
                op("dve", lambda e: e.tensor_mul(out=STb8, in0=clb, in1=bc(mask2[:].unsqueeze(1), [128, 8, 128])), reads=["clb0", "clb1", "mask2"], writes=["STb8"])
                for eo in range(2):
                    c = pr * 2 + eo
                    op("dve", lambda e, eo=eo, c=c: e.tensor_mul(out=hsb8[:, eo], in0=hst[:], in1=bc(E3[:, :, c:c + 1], [128, 8, 128])), reads=["hst", "E3"], writes=["hsb8_%d" % eo, "T3"])
                    for hd in range(8):
                        pkv = PB[5 + hd // 4][:, (hd % 4) * 128:(hd % 4) * 128 + 128]
                        op("pe", lambda e, hd=hd, eo=eo, pkv=pkv: e.matmul(pkv, lhsT=Ktok[eo * 64:(eo + 1) * 64, hd, pr, :], rhs=vtok[eo * 64:(eo + 1) * 64, pr, hd * 128:(hd + 1) * 128], start=True, stop=True),
                           reads=["Ktok", "vtok"], writes=["P%d" % (5 + hd // 4)])
                    for hf in range(2):
                        op("dve", lambda e, hf=hf, c=c: e.tensor_mul(out=tkv8[:, hf * 4:(hf + 1) * 4, :], in0=PB[5 + hf][:, :].rearrange("p (h t) -> p h t", t=128), in1=bc(E2[:, hf * 4:(hf + 1) * 4, c:c + 1], [128, 4, 128])),
                           reads=["P%d" % (5 + hf), "E2"], writes=["tkv8_%d" % hf, "T1"])
                    op("dve", lambda e, c=c: e.tensor_mul(out=hst[:], in0=hst[:], in1=bc(E1[:, :, c:c + 1], [128, 8, 128])), reads=["hst", "E1"], writes=["hst"])
                    op("dve", lambda e: e.tensor_add(out=hst[:], in0=hst[:], in1=tkv8), reads=["hst", "tkv8_0", "tkv8_1"], writes=["hst"])
                for hd in range(8):
                    po = PB[3 + hd // 4][:, (hd % 4) * 128:(hd % 4) * 128 + 128]
                    pon = ["P3" if hd < 4 else "P4"]
                    op("pe", lambda e, hd=hd, po=po: e.matmul(po, lhsT=QEO[:, hd, pr, 0, :], rhs=hsb8[:, 0, hd, :], start=True, stop=False), reads=["QEO", "hsb8_0"], writes=pon)
                    op("pe", lambda e, hd=hd, po=po: e.matmul(po, lhsT=QEO[:, hd, pr, 1, :], rhs=hsb8[:, 1, hd, :], start=False, stop=False), reads=["QEO", "hsb8_1"], writes=pon)
                    op("pe", lambda e, hd=hd, po=po: e.matmul(po, lhsT=STb8[:, hd, :], rhs=vtok[:, pr, hd * 128:(hd + 1) * 128], start=False, stop=True), reads=["STb8", "vtok"], writes=pon)
                op("act", lambda e: e.copy(out=o_sb[:, 0:512], in_=PB[3][:, :]), reads=["P3"], writes=["T2"])
                op("act", lambda e: e.copy(out=o_sb[:, 512:1024], in_=PB[4][:, :]), reads=["P4"], writes=["T2"])
                op("act", lambda e: e.activation(out=sqb, in_=o_sb, func=AF.Square), reads=["T2"], writes=["T3"])
                op("dve", lambda e: e.tensor_reduce(out=ssq, in_=sqb.rearrange("p (h v) -> p h v", v=128), axis=AX.X, op=ALU.add), reads=["T3"], writes=["ssq"])
                rstd_from_ss(rsq, ssq, 128, "ssq", "rsq")
                op("dve", lambda e: e.tensor_mul(out=onb.rearrange("p (h v) -> p h v", v=128), in0=o_sb.rearrange("p (h v) -> p h v", v=128), in1=bc(rsq.unsqueeze(2), [128, 8, 128])),
                   reads=["T2", "rsq"], writes=["onb"])
                for hd in range(8):
                    pt, pn = p7((hd % 2) * 2, 128)
                    op("pe", lambda e, hd=hd, pt=pt: e.transpose(out=pt, in_=onb[:, hd * 128:(hd + 1) * 128], identity=idb[:]), reads=["onb", "idb"], writes=pn)
                    op("dve", lambda e, hd=hd, pt=pt: e.scalar_tensor_tensor(out=yaT[:, hd, pr * 128:(pr + 1) * 128], in0=pt, scalar=vcol(l, V_HGN)[:, hd:hd + 1], in1=sga[:, hd, pr * 128:(pr + 1) * 128], op0=ALU.mult, op1=ALU.mult),
                       reads=pn + ["vecs", "sga"], writes=["yaT"])
                for h in range(4):
                    psc = PB[5][:, 0:128]
                    pM = PB[2][:, 0:128]
                    op("pe", lambda e, h=h: e.matmul(psc, lhsT=kT[:, 2 * h, pr * 128:(pr + 1) * 128], rhs=qT[:, 2 * h, pr * 128:(pr + 1) * 128], start=True, stop=False), reads=["kT", "qT"], writes=["P5a"])
                    op("pe", lambda e, h=h: e.matmul(psc, lhsT=kT[:, 2 * h + 1, pr * 128:(pr + 1) * 128], rhs=qT[:, 2 * h + 1, pr * 128:(pr + 1) * 128], start=False, stop=True), reads=["kT", "qT"], writes=["P5a"])
                    op("pe", lambda e, h=h: e.matmul(pM, lhsT=sel4[:, h, :], rhs=rows[:, 5, :], start=True, stop=True), reads=["sel4", "rows"], writes=["P2"])
                    op("act", lambda e, h=h: e.activation(out=DT[:, h % 2, :], in_=pM, func=AF.Exp, scale=-1.0, bias=gcol[:, pr, h:h + 1]), reads=["P2", "gcol"], writes=["DT%d" % (h % 2)])
                    op("dve", lambda e, h=h: e.tensor_mul(out=DTm[:, h % 2, :], in0=DT[:, h % 2, :], in1=maskm[:]), reads=["DT%d" % (h % 2), "maskm"], writes=["DTm%d" % (h % 2)])
                    stn = "STb%d" % (h % 2)
                    op("dve", lambda e, h=h: e.tensor_mul(out=STb[:, h % 2, :], in0=psc, in1=DTm[:, h % 2, :]), reads=["P5a", "DTm%d" % (h % 2)], writes=[stn])
                    pnum = PB[6][:, 0:257]
                    op("pe", lambda e, h=h: e.matmul(pnum, lhsT=STb[:, h % 2, :], rhs=vext[:, pr, h, :], start=True, stop=True), reads=[stn, "vext"], writes=["P6a", "P6b"])
                    op("act", lambda e: e.copy(out=tnum, in_=pnum), reads=["P6a", "P6b"], writes=["tnum"])
                    op("dve", lambda e, h=h: e.tensor_scalar(out=kw[:, h % 2, :], in0=ktok[:, pr, h * 256:(h + 1) * 256], scalar1=gcol[:, pr, 12 + h:13 + h], scalar2=None, op0=ALU.mult),
                       reads=["ktok", "gcol"], writes=["kw%d" % (h % 2)])
                    cn = "C%d" % h
                    cbn = "Cb%d" % h
                    for eo in range(2):
                        pint = PB[eo][:, 0:257]
                        pintn = ["P%da" % eo, "P%db" % eo]
                        rsl = slice(eo * 64, (eo + 1) * 64)
                        for j in range(2):
                            op("pe", lambda e, h=h, j=j, pint=pint: e.matmul(pint, lhsT=qT[:, 2 * h + j, pr * 128:(pr + 1) * 128], rhs=Cbf[:, h, j, :], start=(j == 0), stop=(j == 1)), reads=["qT", cbn], writes=pintn)
                        op("dve", lambda e, h=h, rsl=rsl, pint=pint: e.scalar_tensor_tensor(out=tot[rsl, h, :], in0=pint[rsl, :], scalar=gcol[rsl, pr, 4 + h:5 + h], in1=tnum[rsl, :], op0=ALU.mult, op1=ALU.add),
                           reads=pintn + ["gcol", "tnum"], writes=["T4"])
                        for j in range(2):
                            pC = PB[3 + j][:, 0:257]
                            pCn = ["P3" if j == 0 else "P4"]
                            op("pe", lambda e, h=h, j=j, rsl=rsl, pC=pC: e.matmul(pC, lhsT=kw[rsl, h % 2, j * 128:(j + 1) * 128], rhs=vext[rsl, pr, h, :], start=True, stop=True), reads=["kw%d" % (h % 2), "vext"], writes=pCn)
                            op("dve", lambda e, h=h, j=j, eo=eo, pC=pC: e.scalar_tensor_tensor(out=Cst[:, h, j, :], in0=Cst[:, h, j, :], scalar=decb[:, pr, eo, h:h + 1], in1=pC, op0=ALU.mult, op1=ALU.add),
                               reads=pCn + ["decb", cn], writes=[cn])
                            op("act", lambda e, h=h, j=j: e.copy(out=Cbf[:, h, j, :], in_=Cst[:, h, j, :]), reads=[cn], writes=[cbn])
                den = tot[:, :, 256]
                op("dve", lambda e: e.tensor_scalar_mul(out=sm[:, 16:20], in0=den, scalar1=-1.0), reads=["T4"], writes=["sm"])
                op("dve", lambda e: e.tensor_max(out=sm[:, 4:8], in0=sm[:, 16:20], in1=den), reads=["T4", "sm"], writes=["sm"])
                op("dve", lambda e: e.tensor_max(out=sm[:, 4:8], in0=sm[:, 4:8], in1=gcol[:, pr, 8:12]), reads=["sm", "gcol"], writes=["sm"])
                op("dve", lambda e: e.reciprocal(out=sm[:, 4:8], in_=sm[:, 4:8]), reads=["sm"], writes=["sm"])
                op("dve", lambda e: e.tensor_reduce(out=sm[:, 8:12], in_=tot[:, :, 0:256], axis=AX.X, op=ALU.add), reads=["T4"], writes=["sm"])
                op("act", lambda e: e.activation(out=sqm, in_=tot[:, :, 0:256], func=AF.Square), reads=["T4"], writes=["T3"])
                op("dve", lambda e: e.tensor_reduce(out=sm[:, 12:16], in_=sqm, axis=AX.X, op=ALU.add), reads=["T3"], writes=["sm"])
                op("dve", lambda e: e.tensor_scalar_mul(out=sm[:, 8:12], in0=sm[:, 8:12], scalar1=1.0 / 256), reads=["sm"], writes=["sm"])
                op("dve", lambda e: e.tensor_mul(out=sm[:, 16:20], in0=sm[:, 8:12], in1=sm[:, 8:12]), reads=["sm"], writes=["sm"])
                op("dve", lambda e: e.scalar_tensor_tensor(out=sm[:, 12:16], in0=sm[:, 12:16], scalar=1.0 / 256, in1=sm[:, 16:20], op0=ALU.mult, op1=ALU.subtract), reads=["sm"], writes=["sm"])
                op("dve", lambda e: e.tensor_mul(out=sm[:, 16:20], in0=sm[:, 4:8], in1=sm[:, 4:8]), reads=["sm"], writes=["sm"])
                op("dve", lambda e: e.tensor_mul(out=sm[:, 12:16], in0=sm[:, 12:16], in1=sm[:, 16:20]), reads=["sm"], writes=["sm"])
                op("dve", lambda e: e.tensor_scalar(out=sm[:, 12:16], in0=sm[:, 12:16], scalar1=0.0, scalar2=EPS, op0=ALU.max, op1=ALU.add), reads=["sm"], writes=["sm"])
                op("act", lambda e: e.activation(out=sm[:, 12:16], in_=sm[:, 12:16], func=AF.Ln), reads=["sm"], writes=["sm"])
                op("act", lambda e: e.activation(out=sm[:, 12:16], in_=sm[:, 12:16], func=AF.Exp, scale=-0.5), reads=["sm"], writes=["sm"])
                op("dve", lambda e: e.tensor_mul(out=sm[:, 20:24], in0=sm[:, 4:8], in1=sm[:, 12:16]), reads=["sm"], writes=["sm"])
                op("dve", lambda e: e.scalar_tensor_tensor(out=sm[:, 24:28], in0=sm[:, 8:12], scalar=-1.0, in1=sm[:, 20:24], op0=ALU.mult, op1=ALU.mult), reads=["sm"], writes=["sm"])
                for h in range(4):
                    op("dve", lambda e, h=h: e.tensor_scalar(out=hnb[:, h * 256:(h + 1) * 256], in0=tot[:, h, 0:256], scalar1=sm[:, 20 + h:21 + h], scalar2=sm[:, 24 + h:25 + h], op0=ALU.mult, op1=ALU.add),
                       reads=["T4", "sm"], writes=["hnb"])
                for dc in range(8):
                    pt, pn = p7((dc % 2) * 2 + 1, 128)
                    op("pe", lambda e, dc=dc, pt=pt: e.transpose(out=pt, in_=hnb[:, dc * 128:(dc + 1) * 128], identity=idb[:]), reads=["hnb", "idb"], writes=pn)
                    cs = slice(pr * 128, (pr + 1) * 128)
                    op("act", lambda e, dc=dc, cs=cs: e.activation(out=ybT[:, dc, cs], in_=xcT[:, dc, cs], func=AF.Copy, scale=vcol(l, V_SKIP)[:, dc:dc + 1]),
                       reads=["vecs", "xcT"], writes=["ybT"])
                    op("dve", lambda e, dc=dc, pt=pt, cs=cs: e.scalar_tensor_tensor(out=ybT[:, dc, cs], in0=pt, scalar=vcol(l, V_MGN)[:, dc:dc + 1], in1=ybT[:, dc, cs], op0=ALU.mult, op1=ALU.add),
                       reads=pn + ["vecs", "ybT"], writes=["ybT"])
                    op("dve", lambda e, dc=dc, cs=cs: e.tensor_mul(out=ybT[:, dc, cs], in0=ybT[:, dc, cs], in1=sob[:, dc, cs]), reads=["ybT", "sob"], writes=["ybT"])
            for g in range(4):
                wa, wan = load_wg(wpa_s[l, g], "wpa%d" % l)
                wb, wbn = load_wg(wpb_s[l, g], "wpb%d" % l)
                for half in range(2):
                    m = g * 2 + half
                    i = m % 2
                    fm_chunk(wa, wan, half, yaT, "yaT", lambda pv, pn, m=m, i=i: op("dve", lambda e: e.tensor_mul(out=m1[:, i, :], in0=pv, in1=sgA[:, m, :]), reads=pn + ["sgA"], writes=["T3"]))
                    fm_chunk(wb, wbn, half, ybT, "ybT", lambda pv, pn, m=m, i=i: op("dve", lambda e: e.tensor_mul(out=m2[:, i, :], in0=pv, in1=sgB[:, m, :]), reads=pn + ["sgB"], writes=["T4"]))
                    op("dve", lambda e, m=m, i=i: e.tensor_add(out=mixT[:, m, :], in0=m1[:, i, :], in1=m2[:, i, :]), reads=["T3", "T4"], writes=["mixT"])
            for s in range(2):
                ybanks = (PB[3], PB[4]) if s == 0 else (PB[5], PB[6])
                ybn = ["P3", "P4"] if s == 0 else ["P5a", "P5b", "P5c", "P5d", "P6a", "P6b"]
                for g in range(4):
                    w, wn = load_wg(wo_s[l, g], "wo%d" % l)
                    yv = ybanks[g // 2][:, (g % 2) * 256:(g % 2) * 256 + 256]
                    for kc in range(8):
                        op("pe", lambda e, kc=kc, yv=yv, w=w, s=s: e.matmul(yv, lhsT=mixT[:, kc, s * 128:(s + 1) * 128], rhs=w[:, kc, :], start=(kc == 0), stop=(kc == 7)),
                           reads=["mixT", wn], writes=ybn)
                residual_update(l, t0, s, [(ybanks[0][:, :], 0, 512), (ybanks[1][:, :], 512, 512)], ybn, xt, ysb, tmpx, junk, ssy, rsy, ggm, "ggm", ysbn="T1", tmpxn="T2", junkn="hnb")
            if DEBUG and l == 0 and ti == 0:
                for (nm, tl, dn) in (("hT", hT, "hT"), ("yaT", yaT, "yaT"), ("ybT", ybT, "ybT"), ("mixT", mixT, "mixT"), ("qT", qT, "qT"), ("kT", kT, "kT"), ("vT", vT, "vT"), ("xcT", xcT, "xcT"), ("KT", KT, "KT"), ("sga", sga, "sga")):
                    dd = nc.dram_tensor("dbg_" + nm, [128, 8, TT], BF16, kind="ExternalOutput").ap()
                    last_x_dma["dbg_" + nm] = S.dma("sp", dd, tl, reads=[dn], key="dbg_" + nm)
                dd = nc.dram_tensor("dbg_gcol", [128, 2, 16], F32, kind="ExternalOutput").ap()
                last_x_dma["dbg_gcol"] = S.dma("sp", dd, gcol, reads=["gcol"], key="dbg_gcol")
                dd = nc.dram_tensor("dbg_tot", [128, 4, 257], F32, kind="ExternalOutput").ap()
                last_x_dma["dbg_tot"] = S.dma("sp", dd, tot, reads=["T4"], key="dbg_tot")

    def ffn(l):
        AR.reset()
        S.barrier()
        A = AR.alloc
        moe = (l % 2 == 1)
        j = l // 2
        NG = 14 if moe else 11
        NFC = 2 * NG
        NE = 8 if moe else 1
        xt = A([128, 4, D])
        hT = A([128, 8, TF], BF16)
        junk = A([128, D], BF16); ss = A([128, 4]); rs = A([128, 4]); ssy = A([128, 2]); rsy = A([128, 2])
        aT = A([128, NFC, TF], BF16)
        w2t = [A([128, NFC, 512], BF16) for _ in range(2)]
        NW13 = 2
        w13 = [A([128, 2, 8, 256], BF16) for _ in range(NW13)]
        if moe:
            xn = w2t[0].rearrange("p a b -> p (a b)")[:, 0:8192].bitcast(F32).rearrange("p (a b) -> p a b", b=D)
            hTf = w2t[1].rearrange("p a b -> p (a b)")[:, 0:8192].bitcast(F32).rearrange("p (a b) -> p a b", b=TF)
            xnn, hTfn = "w2_0", "w2_1"
        else:
            xn = A([128, 4, D], BF16)
            hTf = None
            xnn, hTfn = "xn", "hTf"
        yacc = A([128, 4, D])
        gt = A([128, TF])
        tmpx = A([128, D])
        lg = A([128, 4, 8]); gates = A([128, 4, 8]); gm = A([128, 4, 8])
        mx1 = A([128, 4]); mx2 = A([128, 4]); den = A([128, 4])
        print("ffn arena words", AR.off)
        cnt = {"w13": 0, "w2": 0, "g": 0, "y": 0}
        for ti in range(n_ffn_tiles):
            t0 = ti * TF
            load_norm_transpose(l, False, t0, 4, hsc_f, hbi_f, "hsc_f", "hbi_f", xt, xn, hT, junk, ss, rs, hTf=hTf, xnn=xnn, hTfn=hTfn)
            if moe:
                for s in range(4):
                    pl = PB[5][:, 256 + s * 8:256 + s * 8 + 8]
                    for kc in range(8):
                        op("pe", lambda e, s=s, kc=kc, pl=pl: e.matmul(pl, lhsT=hTf[:, kc, s * 128:(s + 1) * 128], rhs=rtt[:, kc, :], start=(kc == 0), stop=(kc == 7)), reads=[hTfn, "rtt"], writes=["P5c"])
                op("dve", lambda e: e.tensor_copy(out=lg, in_=PB[5][:, 256:288].rearrange("p (s e) -> p s e", e=8)), reads=["P5c"], writes=["lg"])
                op("dve", lambda e: e.tensor_reduce(out=mx1, in_=lg, axis=AX.X, op=ALU.max), reads=["lg"], writes=["mx1"])
                op("dve", lambda e: e.tensor_tensor(out=gm, in0=lg, in1=bc(mx1.unsqueeze(2), [128, 4, 8]), op=ALU.is_equal), reads=["lg", "mx1"], writes=["gm"])
                op("dve", lambda e: e.scalar_tensor_tensor(out=gm, in0=gm, scalar=-1e30, in1=lg, op0=ALU.mult, op1=ALU.add), reads=["gm", "lg"], writes=["gm"])
                op("dve", lambda e: e.tensor_reduce(out=mx2, in_=gm, axis=AX.X, op=ALU.max), reads=["gm"], writes=["mx2"])
                op("dve", lambda e: e.tensor_tensor(out=gm, in0=lg, in1=bc(mx2.unsqueeze(2), [128, 4, 8]), op=ALU.is_ge), reads=["lg", "mx2", "gm"], writes=["gm"])
                op("dve", lambda e: e.tensor_sub(out=lg, in0=lg, in1=bc(mx1.unsqueeze(2), [128, 4, 8])), reads=["lg", "mx1"], writes=["lg"])
                op("act", lambda e: e.activation(out=lg, in_=lg, func=AF.Exp), reads=["lg"], writes=["lg"])
                op("dve", lambda e: e.tensor_sub(out=den, in0=mx2, in1=mx1), reads=["mx1", "mx2"], writes=["den"])
                op("act", lambda e: e.activation(out=den, in_=den, func=AF.Exp), reads=["den"], writes=["den"])
                op("dve", lambda e: e.tensor_scalar_add(out=den, in0=den, scalar1=1.0), reads=["den"], writes=["den"])
                op("dve", lambda e: e.reciprocal(out=den, in_=den), reads=["den"], writes=["den"])
                op("dve", lambda e: e.tensor_mul(out=gates, in0=lg, in1=gm), reads=["lg", "gm"], writes=["gates"])
                op("dve", lambda e: e.tensor_mul(out=gates, in0=gates, in1=bc(den.unsqueeze(2), [128, 4, 8])), reads=["gates", "den"], writes=["gates"])
            for ex in range(NE):
                for g in range(NG):
                    i = cnt["w13"] % NW13
                    cnt["w13"] += 1
                    wn = "w13_%d" % i
                    if moe:
                        S.dma("sp", w13[i], m13_s[j, ex, g], reads=["m13_%d_%d" % (l, ex)], writes=[wn], key=wn)
                    else:
                        S.dma("sp", w13[i], f13_s[j, g], reads=["f13_%d" % l], writes=[wn], key=wn)
                    for half in range(2):
                        fc = g * 2 + half
                        gi = cnt["g"] % 2
                        cnt["g"] += 1
                        pg_, pu_ = PB[gi], PB[2 + gi]
                        pgn = ["P%da" % gi, "P%db" % gi]
                        pun = ["P2a", "P2b", "P2c", "P2d"] if gi == 0 else ["P3"]
                        for kc in range(8):
                            op("pe", lambda e, kc=kc, half=half, w=w13[i], pg_=pg_: e.matmul(pg_[:, :], lhsT=w[:, 0, kc, half * 128:(half + 1) * 128], rhs=hT[:, kc, :], start=(kc == 0), stop=(kc == 7)), reads=[wn, "hT"], writes=pgn)
                        for kc in range(8):
                            op("pe", lambda e, kc=kc, half=half, w=w13[i], pu_=pu_: e.matmul(pu_[:, :], lhsT=w[:, 1, kc, half * 128:(half + 1) * 128], rhs=hT[:, kc, :], start=(kc == 0), stop=(kc == 7)), reads=[wn, "hT"], writes=pun)
                        op("act", lambda e, pg_=pg_: e.activation(out=gt, in_=pg_[:, :], func=AF.Silu), reads=pgn, writes=["gt"])
                        op("dve", lambda e, fc=fc, pu_=pu_: e.tensor_mul(out=aT[:, fc, :], in0=pu_[:, :], in1=gt), reads=pun + ["gt"], writes=["aT"])
                for half in range(2):
                    wi = cnt["w2"] % 2
                    cnt["w2"] += 1
                    w2n = "w2_%d" % wi
                    if moe:
                        S.dma("sp", w2t[wi], m2_s[j, ex, half], reads=["m2_%d_%d" % (l, ex)], writes=[w2n], key=w2n)
                    else:
                        S.dma("sp", w2t[wi], f2_s[j, half], reads=["f2_%d" % l], writes=[w2n], key=w2n)
                    for s in range(4):
                        yi = cnt["y"] % 2
                        cnt["y"] += 1
                        py = PB[4 + yi]
                        pyn = ["P4"] if yi == 0 else ["P5a", "P5b", "P5c", "P5d"]
                        for fc in range(NFC):
                            op("pe", lambda e, fc=fc, s=s, py=py, w=w2t[wi]: e.matmul(py[:, :], lhsT=aT[:, fc, s * 128:(s + 1) * 128], rhs=w[:, fc, :], start=(fc == 0), stop=(fc == NFC - 1)), reads=["aT", w2n], writes=pyn)
                        ya = yacc[:, s, half * 512:(half + 1) * 512]
                        if not moe:
                            op("act", lambda e, py=py, ya=ya: e.copy(out=ya, in_=py[:, :]), reads=pyn, writes=["yacc"])
                        elif ex == 0:
                            op("dve", lambda e, py=py, ya=ya, s=s, ex=ex: e.tensor_scalar(out=ya, in0=py[:, :], scalar1=gates[:, s, ex:ex + 1], scalar2=None, op0=ALU.mult), reads=pyn + ["gates"], writes=["yacc"])
                        else:
                            op("dve", lambda e, py=py, ya=ya, s=s, ex=ex: e.scalar_tensor_tensor(out=ya, in0=py[:, :], scalar=gates[:, s, ex:ex + 1], in1=ya, op0=ALU.mult, op1=ALU.add), reads=pyn + ["gates", "yacc"], writes=["yacc"])
            for s in range(4):
                residual_update(l, t0, s, None, None, xt, None, tmpx, junk, ssy, rsy, ggf, "ggf", yacc=yacc[:, s, :])

    for l in range(n_layers):
        layer_prologue(l)
        mixer(l)
        if do_ffn:
            ffn(l)
    if S.limit is not None:
        S.emit(final_waits=[S.ops[e][-1] for e in S.ENGS if S.ops[e]])
    else:
        S.emit(final_waits=list(last_x_dma.values()))
    S.close()
    return nc


def prep_inputs(inputs):
    f = lambda a: np.ascontiguousarray(np.asarray(a, dtype=np.float32))
    col = lambda v: f(v).reshape(8, 128).T
    L = DEPTH
    cols = []
    for l in range(L):
        vs = [inputs["g_pre_mix"][l], inputs["g_pre_ffn"][l], inputs["hgrn_gnorm"][l],
              inputs["mlstm_conv_w"][l][0], inputs["mlstm_conv_w"][l][1], inputs["mlstm_conv_w"][l][2], inputs["mlstm_conv_w"][l][3],
              inputs["mlstm_conv_b"][l], inputs["mlstm_gnorm"][l], inputs["mlstm_skip"][l]]
        for v in vs:
            cols.append(col(v))
    for l in range(L):
        cols.append(col(inputs["hgrn_lb"][l]))
    vecs = f(np.concatenate(cols, axis=1))
    bd = np.zeros((L, 128, 3, 8, 128), np.float32)
    for mi, nm in enumerate(("mlstm_wq", "mlstm_wk", "mlstm_wv")):
        w = f(inputs[nm]).reshape(L, 8, 32, 4, 4)
        for n in range(32):
            bd[:, 4 * n:4 * n + 4, mi, :, 4 * n:4 * n + 4] = w[:, :, n].transpose(0, 2, 1, 3)
    bd = f(bd.reshape(L, 128, 3 * 8 * 128))
    wg = np.concatenate([f(inputs["mlstm_w_ig"]), f(inputs["mlstm_w_fg"])], axis=2)
    wg = f(wg.reshape(L, 24, 128, 8).transpose(0, 2, 1, 3).reshape(L, 128, 192))
    bg = f(np.concatenate([f(inputs["mlstm_b_ig"]), f(inputs["mlstm_b_fg"])], axis=1))
    rt = f(f(inputs["moe_router"]).reshape(2, 8, 128, 8).transpose(0, 2, 1, 3).reshape(2, 128, 64))
    shared = {
        "vecs": vecs, "w_ada": f(inputs["w_ada"]), "b_ada": f(inputs["b_ada"]),
        "g_post_mix": f(inputs["g_post_mix"]), "g_post_ffn": f(inputs["g_post_ffn"]),
        "w_in": f(inputs["w_in"]), "bd": bd, "wgate": wg, "bgate": bg,
        "w_proj_a": f(inputs["w_proj_a"]), "w_proj_b": f(inputs["w_proj_b"]), "w_out": f(inputs["w_out"]),
        "ffn_w1": f(inputs["ffn_w1"]), "ffn_w3": f(inputs["ffn_w3"]), "ffn_w2": f(inputs["ffn_w2"]),
        "router": rt, "moe_w1": f(inputs["moe_w1"]), "moe_w3": f(inputs["moe_w3"]), "moe_w2": f(inputs["moe_w2"]),
    }
    x = f(inputs["x"])
    c = f(inputs["c"])
    maps = []
    for b in range(x.shape[0]):
        m = dict(shared)
        m["x"] = x[b]
        m["ccol"] = f(c[b].reshape(8, 128).T)
        maps.append(m)
    return maps


_NC_CACHE = {}


def kernel(**inputs):
    maps = prep_inputs(inputs)
    if "nc" not in _NC_CACHE:
        _NC_CACHE["nc"] = build_program()
    nc = _NC_CACHE["nc"]
    res = run_bass_kernel_spmd(nc, maps, core_ids=list(range(NCORES)))
    return np.stack([np.asarray(r["out"], dtype=np.float32) for r in res.results], axis=0)
```

```python
import contextlib
import types
import numpy as np
import concourse.bass as bass
import concourse.mybir as mybir
from concourse.bass_utils import run_bass_kernel_spmd

F32 = mybir.dt.float32
BF16 = mybir.dt.bfloat16
AF = mybir.ActivationFunctionType
ALU = mybir.AluOpType
AX = mybir.AxisListType

SEM_CAP = 30000
D = 1024
SEQ = 4096
DEPTH = 4
NCORES = 8
TT = 256
TF = 512
EPS = 1e-6
NV = 10
DEBUG = False


class Dep:
    __slots__ = ("name", "last_w", "readers")

    def __init__(self, name):
        self.name = name
        self.last_w = None
        self.readers = []


class Op:
    __slots__ = ("eng", "fn", "deps", "is_dma", "key", "needs_inc", "sem", "val")

    def __init__(self, eng, fn, is_dma=False, key=None):
        self.eng = eng
        self.fn = fn
        self.deps = []
        self.is_dma = is_dma
        self.key = key
        self.needs_inc = False
        self.sem = None
        self.val = 0


class Sched:
    ENGS = ("pe", "act", "dve", "pool", "sp")

    def __init__(self, nc):
        self.nc = nc
        self.ops = {e: [] for e in self.ENGS}
        self.all_ops = []
        self.deps = {}
        self.stack = contextlib.ExitStack()
        self.fence = []
        self.passed = {e: True for e in self.ENGS}

    def sb(self, name, shape, dtype=F32):
        return self.stack.enter_context(self.nc.sbuf_tensor("sb_" + name, list(shape), dtype))

    def ps(self, name, shape, dtype=F32):
        return self.stack.enter_context(self.nc.psum_tensor("ps_" + name, list(shape), dtype))

    def _D(self, x):
        d = self.deps.get(x)
        if d is None:
            d = self.deps[x] = Dep(x)
        return d

    def barrier(self):
        self.fence = [self.ops[e][-1] for e in self.ENGS if self.ops[e]]
        self.passed = {e: False for e in self.ENGS}

    limit = None

    def _record(self, op, reads, writes):
        if self.limit is not None and len(self.all_ops) >= self.limit:
            return op
        rr, ww = [], []
        for r in reads:
            if len(r) >= 2 and r[0] == "P" and r[1].isdigit():
                ww.append(r[:2])
            else:
                rr.append(r)
        for w in writes:
            if len(w) >= 2 and w[0] == "P" and w[1].isdigit():
                ww.append(w[:2])
            else:
                ww.append(w)
        reads, writes = rr, list(dict.fromkeys(ww))
        deps = []
        if not self.passed[op.eng]:
            deps.extend(self.fence)
            self.passed[op.eng] = True
        for r in reads:
            r = self._D(r)
            if r.last_w is not None:
                deps.append(r.last_w)
        for w in writes:
            w = self._D(w)
            if w.last_w is not None:
                deps.append(w.last_w)
            deps.extend(w.readers)
        seen = set()
        for d in deps:
            if d is op or id(d) in seen:
                continue
            seen.add(id(d))
            op.deps.append(d)
        for r in reads:
            self._D(r).readers.append(op)
        for w in writes:
            w = self._D(w)
            w.last_w = op
            w.readers = []
        self.ops[op.eng].append(op)
        self.all_ops.append(op)
        return op

    @staticmethod
    def _freeze(fn):
        if fn.__closure__ is None:
            return fn
        cells = []
        for c in fn.__closure__:
            try:
                cells.append(types.CellType(c.cell_contents))
            except ValueError:
                cells.append(c)
        return types.FunctionType(fn.__code__, fn.__globals__, fn.__name__, fn.__defaults__, tuple(cells))

    defer_to = None

    def op(self, eng, fn, reads=(), writes=()):
        if self.defer_to is not None:
            self.defer_to.append((eng, self._freeze(fn), list(reads), list(writes)))
            return None
        return self._record(Op(eng, self._freeze(fn)), reads, writes)

    def drain(self, q, k):
        for _ in range(min(k, len(q))):
            eng, fn, reads, writes = q.pop(0)
            self._record(Op(eng, fn), reads, writes)

    def dma(self, eng, out, in_, reads=(), writes=(), key=None, **kw):
        if key is None:
            key = writes[0] if writes else reads[0]
        fn = lambda e: e.dma_start(out=out, in_=in_, **kw)
        return self._record(Op(eng, fn, is_dma=True, key=key), reads, writes)

    def emit(self, final_waits=()):
        nc = self.nc
        for op in self.all_ops:
            for d in op.deps:
                if d.eng == "pe" and op.eng == "pe" and not d.is_dma:
                    continue
                d.needs_inc = True
        for op in final_waits:
            op.needs_inc = True
        print("ops per engine", {e: len(self.ops[e]) for e in self.ENGS})
        sems = {}

        def get_sem(name):
            s = sems.get(name)
            if s is None:
                s = sems[name] = self.stack.enter_context(nc.semaphore(name))
            return s

        cnt = {}
        ccnt = {e: 0 for e in self.ENGS}
        for op in self.all_ops:
            if op.is_dma:
                k = cnt.get(op.key, 0) + 1
                cnt[op.key] = k
                per = SEM_CAP // 16
                ep, v = divmod(k - 1, per)
                op.sem = get_sem("d_%s_%d" % (op.key, ep))
                op.val = (v + 1) * 16
            elif op.needs_inc:
                c = ccnt[op.eng]
                ep, v = divmod(c, SEM_CAP)
                op.sem = get_sem("e_%s_%d" % (op.eng, ep))
                op.val = v + 1
                ccnt[op.eng] = c + 1
        engmap = {"pe": "tensor", "act": "scalar", "dve": "vector", "pool": "gpsimd", "sp": "sync"}

        def run(engname, eng):
            known = {}
            for op in self.ops[engname]:
                for d in op.deps:
                    if d.eng == "pe" and engname == "pe" and not d.is_dma:
                        continue
                    sid = d.sem.name
                    if known.get(sid, 0) >= d.val:
                        continue
                    eng.wait_ge(d.sem, d.val)
                    known[sid] = d.val
                ins = op.fn(eng)
                if op.is_dma:
                    ins.then_inc(op.sem, 16)
                elif op.needs_inc:
                    ins.then_inc(op.sem, 1)
            if engname == "sp":
                for op in final_waits:
                    eng.wait_ge(op.sem, op.val)

        with nc.Block() as block:
            for engname in self.ENGS:
                getattr(block, engmap[engname])(lambda eng, _n=engname: run(_n, eng))

    def close(self):
        self.stack.close()


class Arena:
    def __init__(self, S, name, words):
        self.t = S.sb(name, [128, words], F32)
        self.words = words
        self.off = 0

    def reset(self):
        self.off = 0

    def alloc(self, shape, dtype=F32):
        n = int(np.prod(shape[1:]))
        w = n if dtype == F32 else (n + 1) // 2
        assert self.off + w <= self.words, ("arena overflow", self.off, w, self.words)
        v = self.t[0:shape[0], self.off:self.off + w]
        self.off += w
        if dtype != F32:
            v = v.bitcast(dtype)[:, 0:n]
        if len(shape) == 3:
            v = v.rearrange("p (a b) -> p a b", b=shape[2])
        elif len(shape) == 4:
            v = v.rearrange("p (a b c) -> p a b c", b=shape[2], c=shape[3])
        elif len(shape) == 5:
            v = v.rearrange("p (a b c d) -> p a b c d", b=shape[2], c=shape[3], d=shape[4])
        return v


def bc(ap, shape):
    return ap.to_broadcast(list(shape))


def build_program(n_layers=DEPTH, do_ffn=True, n_mix_tiles=SEQ // TT, n_ffn_tiles=SEQ // TF):
    nc = bass.Bass("TRN2", target_bir_lowering=False)
    di = lambda name, shape, dt=F32: nc.dram_tensor(name, list(shape), dt, kind="ExternalInput").ap()
    x_in = di("x", [SEQ, D])
    ccol_d = di("ccol", [128, 8])
    vecs_d = di("vecs", [128, DEPTH * NV * 8 + DEPTH * 8])
    w_ada_d = di("w_ada", [DEPTH, D, 6 * D])
    b_ada_d = di("b_ada", [DEPTH, 6 * D])
    gpm_d = di("g_post_mix", [DEPTH, D])
    gpf_d = di("g_post_ffn", [DEPTH, D])
    w_in_d = di("w_in", [DEPTH, D, 8 * D])
    bd_d = di("bd", [DEPTH, 128, 3 * 8 * 128])
    wgate_d = di("wgate", [DEPTH, 128, 24 * 8])
    bgate_d = di("bgate", [DEPTH, 8])
    wpa_d = di("w_proj_a", [DEPTH, D, D])
    wpb_d = di("w_proj_b", [DEPTH, D, D])
    wo_d = di("w_out", [DEPTH, D, D])
    if not do_ffn:
        di = lambda name, shape, dt=F32: nc.dram_tensor(name, [1, 1], dt, kind="ExternalInput").ap()
    f1_d = di("ffn_w1", [2, D, 2816])
    f3_d = di("ffn_w3", [2, D, 2816])
    f2_d = di("ffn_w2", [2, 2816, D])
    rt_d = di("router", [2, 128, 64])
    m1_d = di("moe_w1", [2, 8, D, 3584])
    m3_d = di("moe_w3", [2, 8, D, 3584])
    m2_d = di("moe_w2", [2, 8, 3584, D])
    out_d = nc.dram_tensor("out", [SEQ, D], F32, kind="ExternalOutput").ap()
    sc = lambda name, shape: nc.dram_tensor(name, list(shape), BF16).ap()
    win_s = sc("win_s", [DEPTH, 32, 128, 8, 256])
    wpa_s = sc("wpa_s", [DEPTH, 4, 128, 8, 256])
    wpb_s = sc("wpb_s", [DEPTH, 4, 128, 8, 256])
    wo_s = sc("wo_s", [DEPTH, 4, 128, 8, 256])
    f13_s = sc("f13_s", [2, 11, 128, 2, 8, 256])
    f2_s = sc("f2_s", [2, 2, 128, 22, 512])
    m13_s = sc("m13_s", [2, 8, 14, 128, 2, 8, 256])
    m2_s = sc("m2_s", [2, 8, 2, 128, 28, 512])

    S = Sched(nc)
    op = S.op
    idf = S.sb("idf", [128, 128], F32)
    idb = S.sb("idb", [128, 128], BF16)
    mask2 = S.sb("mask2", [128, 128], F32)
    maskm = S.sb("maskm", [128, 128], F32)
    mask01 = S.sb("mask01", [128, TT], F32)
    negm = S.sb("negm", [4, 128], F32)
    m01r = S.sb("m01r", [4, 128], F32)
    ones4 = S.sb("ones4", [4, 128], F32)
    sel4 = S.sb("sel4", [4, 4, 128], F32)
    id4 = S.sb("id4", [4, 4], F32)
    onesb = S.sb("onesb", [1, 128], BF16)
    vecs = S.sb("vecs", [128, DEPTH * NV * 8 + DEPTH * 8], F32)
    lbc = S.sb("lbc", [128, DEPTH, 8], F32)
    oml = S.sb("oml", [128, DEPTH, 8], F32)
    noml = S.sb("noml", [128, DEPTH, 8], F32)
    cbc = S.sb("cbc", [128, 8, 128], BF16)
    ccol = S.sb("ccol", [128, 8], F32)
    cact = S.sb("cact", [128, 8], F32)
    hsc_m = S.sb("hsc_m", [128, 8], F32)
    hbi_m = S.sb("hbi_m", [128, 8], F32)
    hsc_f = S.sb("hsc_f", [128, 8], F32)
    hbi_f = S.sb("hbi_f", [128, 8], F32)
    ggm = S.sb("ggm", [128, D], F32)
    ggf = S.sb("ggf", [128, D], F32)
    bdt = S.sb("bdt", [128, 3, 8, 128], BF16)
    wgt = S.sb("wgt", [128, 24, 8], BF16)
    bgt = S.sb("bgt", [128, 8], F32)
    rtt = S.sb("rtt", [128, 8, 8], F32)
    hst = S.sb("hst", [128, 8, 128], F32)
    hstb = S.sb("hstb", [128, 2, 2, 128], BF16)
    Cst = S.sb("Cst", [128, 4, 2, 257], F32)
    Cbf = S.sb("Cbf", [128, 4, 2, 257], BF16)
    mprev = S.sb("mprev", [4, 1], F32)
    PB = [S.ps("pb%d" % i, [128, 512], F32) for i in range(8)]
    P7b = PB[7][:, :].bitcast(BF16)
    P7N = ["P7a", "P7b", "P7c", "P7d"]

    def p7(q, n):
        nq = (n + 255) // 256
        return P7b[:, q * 256:q * 256 + n], P7N[q:q + nq]
    ARW = 42100
    AR = Arena(S, "arena", ARW)

    def vcol(l, v):
        o = (l * NV + v) * 8
        return vecs[:, o:o + 8]

    V_GPRE_M, V_GPRE_F, V_HGN, V_CW0, V_CB, V_MGN, V_SKIP = 0, 1, 2, 3, 7, 8, 9

    op("pool", lambda e: e.memset(idf[:], 0.0), writes=["idf"])
    op("pool", lambda e: e.affine_select(out=idf[:], in_=idf[:], pattern=[[-1, 128]], compare_op=ALU.not_equal,
                                         fill=1.0, base=0, channel_multiplier=1), reads=["idf"], writes=["idf"])
    op("dve", lambda e: e.tensor_copy(out=idb[:], in_=idf[:]), reads=["idf"], writes=["idb"])
    op("pool", lambda e: e.memset(mask2[:], 1.0), writes=["mask2"])
    op("pool", lambda e: e.affine_select(out=mask2[:], in_=mask2[:], pattern=[[1, 128]], compare_op=ALU.is_ge,
                                         fill=0.0, base=0, channel_multiplier=-1), reads=["mask2"], writes=["mask2"])
    op("pool", lambda e: e.memset(mask2[0:64, 64:128], 0.0), reads=["mask2"], writes=["mask2"])
    op("pool", lambda e: e.tensor_scalar_mul(out=maskm[:], in0=mask2[:], scalar1=1.0 / 16.0), reads=["mask2"], writes=["maskm"])
    op("pool", lambda e: e.memset(mask01[:], 1.0), writes=["mask01"])
    op("pool", lambda e: e.memset(mask01[:].rearrange("p (c j) -> p c j", j=64)[:, :, 0:1], 0.0), reads=["mask01"], writes=["mask01"])
    op("pool", lambda e: e.memset(negm[:], 0.0), writes=["negm"])
    op("pool", lambda e: e.memset(negm[:].rearrange("p (c j) -> p c j", j=64)[:, :, 0:1], -1e30), reads=["negm"], writes=["negm"])
    op("pool", lambda e: e.memset(m01r[:], 1.0), writes=["m01r"])
    op("pool", lambda e: e.memset(m01r[:].rearrange("p (c j) -> p c j", j=64)[:, :, 0:1], 0.0), reads=["m01r"], writes=["m01r"])
    op("pool", lambda e: e.memset(ones4[:], 1.0), writes=["ones4"])
    op("pool", lambda e: e.tensor_copy(out=id4[:], in_=idf[0:4, 0:4]), reads=["idf"], writes=["id4"])
    op("pool", lambda e: e.tensor_copy(out=sel4[:], in_=bc(idf[0:4, 0:4].unsqueeze(2), [4, 4, 128])), reads=["idf"], writes=["sel4"])
    op("pool", lambda e: e.memset(onesb[:], 1.0), writes=["onesb"])
    S.dma("sp", vecs[:], vecs_d, writes=["vecs"])
    S.dma("sp", ccol[:], ccol_d, writes=["ccol"])
    op("act", lambda e: e.activation(out=cact[:], in_=ccol[:], func=AF.Silu), reads=["ccol"], writes=["cact"])
    op("dve", lambda e: e.tensor_copy(out=cbc[:], in_=bc(cact[:].unsqueeze(2), [128, 8, 128])), reads=["cact"], writes=["cbc"])
    lbraw = vecs[:, DEPTH * NV * 8:DEPTH * NV * 8 + DEPTH * 8].rearrange("p (l c) -> p l c", c=8)
    lbe = S.sb("lbe", [128, DEPTH, 8], F32)
    lbs = S.sb("lbs", [128, 8], F32)
    op("act", lambda e: e.activation(out=lbe[:], in_=lbraw, func=AF.Exp), reads=["vecs"], writes=["lbe"])
    op("dve", lambda e: e.tensor_add(out=lbs[:], in0=lbe[:, 0, :], in1=lbe[:, 1, :]), reads=["lbe"], writes=["lbs"])
    op("dve", lambda e: e.tensor_add(out=lbs[:], in0=lbs[:], in1=lbe[:, 2, :]), reads=["lbe", "lbs"], writes=["lbs"])
    op("dve", lambda e: e.tensor_add(out=lbs[:], in0=lbs[:], in1=lbe[:, 3, :]), reads=["lbe", "lbs"], writes=["lbs"])
    op("dve", lambda e: e.reciprocal(out=lbs[:], in_=lbs[:]), reads=["lbs"], writes=["lbs"])
    op("dve", lambda e: e.tensor_mul(out=lbe[:], in0=lbe[:], in1=bc(lbs[:].unsqueeze(1), [128, DEPTH, 8])), reads=["lbe", "lbs"], writes=["lbe"])
    op("dve", lambda e: e.memset(lbc[:, 0, :], 0.0), writes=["lbc"])
    for l in range(1, DEPTH):
        op("dve", lambda e, l=l: e.tensor_add(out=lbc[:, l, :], in0=lbc[:, l - 1, :], in1=lbe[:, l, :]), reads=["lbe", "lbc"], writes=["lbc"])
    op("dve", lambda e: e.tensor_scalar(out=oml[:], in0=lbc[:], scalar1=-1.0, scalar2=1.0, op0=ALU.mult, op1=ALU.add), reads=["lbc"], writes=["oml"])
    op("dve", lambda e: e.tensor_scalar_mul(out=noml[:], in0=oml[:], scalar1=-1.0), reads=["oml"], writes=["noml"])

    def conv_kxn(dst, src, ngroups, depname):
        v = src.rearrange("(kc p) (g c) -> g p kc c", p=128, c=256)
        for g in range(ngroups):
            S.dma("pool", dst[g], v[g], writes=[depname])

    def conv_layer(l):
        conv_kxn(win_s[l], w_in_d[l], 32, "win%d" % l)
        conv_kxn(wpa_s[l], wpa_d[l], 4, "wpa%d" % l)
        conv_kxn(wpb_s[l], wpb_d[l], 4, "wpb%d" % l)
        conv_kxn(wo_s[l], wo_d[l], 4, "wo%d" % l)
        if not do_ffn:
            return
        j = l // 2
        if l % 2 == 0:
            v1 = f1_d[j].rearrange("(kc p) (g c) -> g p kc c", p=128, c=256)
            v3 = f3_d[j].rearrange("(kc p) (g c) -> g p kc c", p=128, c=256)
            for g in range(11):
                S.dma("pool", f13_s[j, g, :, 0], v1[g], writes=["f13_%d" % l])
                S.dma("pool", f13_s[j, g, :, 1], v3[g], writes=["f13_%d" % l])
            v2 = f2_d[j].rearrange("(fc p) (h c) -> h p fc c", p=128, c=512)
            for h in range(2):
                S.dma("pool", f2_s[j, h], v2[h], writes=["f2_%d" % l])
        else:
            for ex in range(8):
                v1 = m1_d[j, ex].rearrange("(kc p) (g c) -> g p kc c", p=128, c=256)
                v3 = m3_d[j, ex].rearrange("(kc p) (g c) -> g p kc c", p=128, c=256)
                for g in range(14):
                    S.dma("pool", m13_s[j, ex, g, :, 0], v1[g], writes=["m13_%d_%d" % (l, ex)])
                    S.dma("pool", m13_s[j, ex, g, :, 1], v3[g], writes=["m13_%d_%d" % (l, ex)])
                v2 = m2_d[j, ex].rearrange("(fc p) (h c) -> h p fc c", p=128, c=512)
                for h in range(2):
                    S.dma("pool", m2_s[j, ex, h], v2[h], writes=["m2_%d_%d" % (l, ex)])

    for l in range(n_layers):
        conv_layer(l)

    def rstd_from_ss(rs, ss, n, dep_ss, dep_rs):
        op("dve", lambda e: e.tensor_scalar(out=rs, in0=ss, scalar1=1.0 / n, scalar2=EPS, op0=ALU.mult, op1=ALU.add), reads=[dep_ss], writes=[dep_rs])
        op("act", lambda e: e.activation(out=rs, in_=rs, func=AF.Ln), reads=[dep_rs], writes=[dep_rs])
        op("act", lambda e: e.activation(out=rs, in_=rs, func=AF.Exp, scale=-0.5), reads=[dep_rs], writes=[dep_rs])

    xkeys = {}
    last_x_dma = {}

    def xsrc(l, first):
        return x_in if (l == 0 and first) else out_d

    def layer_prologue(l):
        AR.reset()
        S.barrier()
        wad = [AR.alloc([128, 8, 512], BF16) for _ in range(2)]
        bad = AR.alloc([1, 6 * D], BF16)
        gpb = [AR.alloc([128, D], F32) for _ in range(2)]
        tmpd = AR.alloc([128, 4, 128], F32)
        S.dma("pool", bad, b_ada_d[l:l + 1, :], writes=["bad"])
        S.dma("sp", gpb[0], gpm_d[l].partition_broadcast(128), writes=["gpb0"])
        S.dma("sp", gpb[1], gpf_d[l].partition_broadcast(128), writes=["gpb1"])
        S.dma("pool", bdt[:].rearrange("p a b c -> p (a b c)"), bd_d[l], writes=["bdt"])
        S.dma("pool", wgt[:].rearrange("p a b -> p (a b)"), wgate_d[l], writes=["wgt"])
        S.dma("sp", bgt[:], bgate_d[l].partition_broadcast(128), writes=["bgt"])
        if l % 2 == 1:
            S.dma("sp", rtt[:].rearrange("p a b -> p (a b)"), rt_d[l // 2], writes=["rtt"])
        wv = w_ada_d[l].rearrange("(kc p) (g c) -> g p kc c", p=128, c=512)
        for g in range(12):
            w = wad[g % 2]
            wn = "wad%d" % (g % 2)
            S.dma("pool", w, wv[g], writes=[wn], key="pl3_%d" % (g % 2))
            pb = PB[g % 2]
            pn = ["P%da" % (g % 2), "P%db" % (g % 2)]
            for kc in range(8):
                op("pe", lambda e, w=w, kc=kc, pb=pb: e.matmul(pb[:, :], lhsT=cbc[:, kc, :], rhs=w[:, kc, :], start=(kc == 0), stop=False),
                   reads=[wn, "cbc"], writes=pn)
            op("pe", lambda e, g=g, pb=pb: e.matmul(pb[:, :], lhsT=onesb[0:1, :], rhs=bad[0:1, g * 512:(g + 1) * 512], start=False, stop=True),
               reads=["bad", "onesb"], writes=pn)
            which = g // 2
            half = g % 2
            if which in (2, 5):
                dst = ggm if which == 2 else ggf
                dn = "ggm" if which == 2 else "ggf"
                gp = gpb[0] if which == 2 else gpb[1]
                gn = "gpb0" if which == 2 else "gpb1"
                op("dve", lambda e, pb=pb, dst=dst, gp=gp, half=half: e.tensor_mul(out=dst[:, half * 512:(half + 1) * 512], in0=pb[:, :], in1=gp[:, half * 512:(half + 1) * 512]),
                   reads=pn + [gn], writes=[dn])
            else:
                dst = {0: hbi_m, 1: hsc_m, 3: hbi_f, 4: hsc_f}[which]
                dn = {0: "hbi_m", 1: "hsc_m", 3: "hbi_f", 4: "hsc_f"}[which]
                op("dve", lambda e, pb=pb: e.tensor_mul(out=tmpd, in0=pb[:, :].rearrange("p (c j) -> p c j", j=128), in1=bc(idf[:].unsqueeze(1), [128, 4, 128])),
                   reads=pn + ["idf"], writes=["tmpd"])
                op("dve", lambda e, dst=dst, half=half: e.tensor_reduce(out=dst[:, half * 4:(half + 1) * 4], in_=tmpd, axis=AX.X, op=ALU.add),
                   reads=["tmpd"], writes=[dn])
        for (hs, hn, vi) in ((hsc_m, "hsc_m", V_GPRE_M), (hsc_f, "hsc_f", V_GPRE_F)):
            op("dve", lambda e, hs=hs, vi=vi: e.scalar_tensor_tensor(out=hs[:], in0=hs[:], scalar=1.0, in1=vcol(l, vi), op0=ALU.add, op1=ALU.mult),
               reads=[hn, "vecs"], writes=[hn])
        op("pool", lambda e: e.memset(hst[:], 0.0), writes=["hst"])
        op("pool", lambda e: e.memset(Cst[:], 0.0), writes=["Cst"])
        op("pool", lambda e: e.memset(Cbf[:], 0.0), writes=["Cbf"])
        op("pool", lambda e: e.memset(mprev[:], 0.0), writes=["mprev"])

    def load_norm_transpose(l, first, t0, nsub, hsc, hbi, hscn, hbin, xt, xn, hT, junk, ss, rs, hTf=None, xnn="xn", hTfn="hTf", junkn="junk"):
        src = xsrc(l, first)
        op("dve", lambda e: e.memset(ss, 0.0), writes=["ss"])
        for s in range(nsub):
            blk = (t0 + s * 128) // TT
            S.dma("sp", xt[:, s, :], src[t0 + s * 128:t0 + (s + 1) * 128, :], reads=["xrow%d" % blk], writes=["xt%d" % s], key="xl%d" % s)
            op("act", lambda e, s=s: e.activation(out=junk, in_=xt[:, s, :], func=AF.Square, accum_out=ss[:, s:s + 1]),
               reads=["xt%d" % s], writes=[junkn, "ss"])
        rstd_from_ss(rs, ss, D, "ss", "rs")
        dt = F32 if hTf is not None else BF16
        for s in range(nsub):
            op("dve", lambda e, s=s: e.tensor_scalar(out=xn[:, s, :], in0=xt[:, s, :], scalar1=rs[:, s:s + 1], scalar2=None, op0=ALU.mult),
               reads=["xt%d" % s, "rs"], writes=[xnn])
        idt = idf if hTf is not None else idb
        n = nsub * 128
        for dc in range(8):
            if hTf is not None:
                pt = PB[6 + dc % 2][:, 0:n]
                pn = ["P6a", "P6b"] if dc % 2 == 0 else P7N
            else:
                pt, pn = p7((dc % 2) * 2, n)
            for s in range(nsub):
                op("pe", lambda e, s=s, dc=dc, pt=pt: e.transpose(out=pt[:, s * 128:(s + 1) * 128], in_=xn[:, s, dc * 128:(dc + 1) * 128], identity=idt[:]),
                   reads=[xnn, "idb", "idf"], writes=pn)
            tgt = hTf if hTf is not None else hT
            tgn = hTfn if hTf is not None else "hT"
            op("act", lambda e, dc=dc, pt=pt, tgt=tgt: e.activation(out=tgt[:, dc, :], in_=pt, func=AF.Identity, scale=hsc[:, dc:dc + 1], bias=hbi[:, dc:dc + 1]),
               reads=pn + [hscn, hbin], writes=[tgn])
        if hTf is not None:
            op("dve", lambda e: e.tensor_copy(out=hT, in_=hTf), reads=[hTfn], writes=["hT"])

    def residual_update(l, t0, s, ypsum_list, ypn, xt, ysb, tmpx, junk, ssy, rsy, gg, ggn, yacc=None, ysbn="ysb", tmpxn="tmpx", junkn="junk"):
        if yacc is None:
            for (pa, c0, ncol) in ypsum_list:
                op("act", lambda e, pa=pa, c0=c0, ncol=ncol: e.copy(out=ysb[:, c0:c0 + ncol], in_=pa), reads=ypn, writes=[ysbn])
            ysrc, ysn = ysb, ysbn
        else:
            ysrc, ysn = yacc, "yacc"
        op("dve", lambda e: e.memset(ssy[:, 0:1], 0.0), writes=["ssy"])
        op("act", lambda e: e.activation(out=junk, in_=ysrc, func=AF.Square, accum_out=ssy[:, 0:1]), reads=[ysn], writes=[junkn, "ssy"])
        rstd_from_ss(rsy[:, 0:1], ssy[:, 0:1], D, "ssy", "rsy")
        op("dve", lambda e: e.scalar_tensor_tensor(out=tmpx, in0=ysrc, scalar=rsy[:, 0:1], in1=gg[:], op0=ALU.mult, op1=ALU.mult),
           reads=[ysn, "rsy", ggn], writes=[tmpxn])
        op("dve", lambda e: e.tensor_add(out=xt[:, s, :], in0=xt[:, s, :], in1=tmpx), reads=[tmpxn, "xt%d" % s], writes=["xt%d" % s])
        blk = (t0 + s * 128) // TT
        d = S.dma("sp", out_d[t0 + s * 128:t0 + (s + 1) * 128, :], xt[:, s, :], reads=["xt%d" % s], writes=["xrow%d" % blk], key="xs%d" % s)
        last_x_dma["xs%d" % s] = d

    def mixer(l):
        AR.reset()
        S.barrier()
        A = AR.alloc
        xt = A([128, 2, D]); xn = A([128, 2, D], BF16); hT = A([128, 8, TT], BF16)
        ss = A([128, 2]); rs = A([128, 2]); ssy = A([128, 2]); rsy = A([128, 2])
        NWG = 3
        wg = [A([128, 8, 256], BF16) for _ in range(NWG)]
        qs = A([128, 8, TT], BF16)
        T1 = A([128, 8, TT]); T2 = A([128, 8, TT]); T3 = A([128, 8, TT]); T4 = A([128, 8, TT])
        QEO = A([128, 8, 2, 2, 128], BF16)
        KT = A([128, 8, TT], BF16)
        Ktok = A([128, 8, 2, 128], BF16)
        vtok = A([128, 2, D], BF16)
        sga = A([128, 8, TT], BF16)
        yaT = A([128, 8, TT], BF16)
        E1 = A([128, 8, 4]); E2 = A([128, 8, 4]); E3 = A([128, 8, 4]); dE = A([128, 8, 4])
        STb8 = A([128, 8, 128], BF16)
        STb = A([128, 2, 128], BF16)
        ssq = A([128, 8]); rsq = A([128, 8])
        onb = A([128, D], BF16)
        xm = A([128, 8, 3 + TT])
        xcT = A([128, 8, TT], BF16); xmT = A([128, 8, TT], BF16)
        qT = A([128, 8, TT], BF16); kT = A([128, 8, TT], BF16); vT = A([128, 8, TT], BF16)
        ktok = A([128, 2, D], BF16)
        vext = A([128, 2, 4, 257], BF16)
        sob = A([128, 8, TT], BF16); sgA = A([128, 8, TT], BF16); sgB = A([128, 8, TT], BF16)
        DT = A([128, 2, 128]); DTm = A([128, 2, 128])
        kw = A([128, 2, 256], BF16)
        tnum = A([128, 257])
        hnb = A([128, D], BF16)
        junk = hnb
        ybT = A([128, 8, TT], BF16)
        mixT = A([128, 8, TT], BF16)
        gpre = A([128, 8]); gli = A([128, 4]); glf = A([128, 4])
        rows = A([4, 8, 128])
        gcol = A([128, 2, 16])
        decb = A([128, 2, 2, 4])
        dexp = A([4, 4, 2])
        sm = A([128, 32])
        acc = T1; o_sb = T2.rearrange("p a b -> p (a b)")[:, 0:D]; sqb = T3.rearrange("p a b -> p (a b)")[:, 0:D]
        tot = T4.rearrange("p a b -> p (a b)")[:, 0:4 * 257].rearrange("p (a b) -> p a b", b=257)
        sqm = T3.rearrange("p a b -> p (a b)")[:, 0:D].rearrange("p (a b) -> p a b", b=256)
        ysb = T1.rearrange("p a b -> p (a b)")[:, 0:D]
        tmpx = T2.rearrange("p a b -> p (a b)")[:, 0:D]
        m1 = T3.rearrange("p a b -> p (a b)")[:, 0:2 * TT].rearrange("p (a b) -> p a b", b=TT)
        m2 = T4.rearrange("p a b -> p (a b)")[:, 0:2 * TT].rearrange("p (a b) -> p a b", b=TT)
        clb = T1.rearrange("p a b -> p (a b)")[:, 0:1024].rearrange("p (h t) -> p h t", t=128)
        tkv8 = T1.rearrange("p a b -> p (a b)")[:, 1024:2048].rearrange("p (h t) -> p h t", t=128)
        hsb8 = T3.rearrange("p a b -> p (a b)")[:, 0:1024].bitcast(BF16).rearrange("p (e h t) -> p e h t", e=2, t=128)

        print("mixer arena words", AR.off)
        op("pool", lambda e: e.memset(xm[:, :, 0:3], 0.0), writes=["xm"])
        op("pool", lambda e: e.memset(QEO, 0.0), writes=["QEO"])
        op("pool", lambda e: e.memset(vext[:, :, :, 256:257], 1.0), writes=["vext"])

        NPP = 6
        pp_names = ["P%d" % i for i in range(NPP)]

        def pp_view(i):
            return PB[i][:, 0:256]

        state = {"pp": 0, "wg": 0}

        def next_pp():
            i = state["pp"] % NPP
            state["pp"] += 1
            return pp_view(i), [pp_names[i]]

        def load_wg(src_ap, depname):
            i = state["wg"] % NWG
            state["wg"] += 1
            S.dma("sp", wg[i], src_ap, reads=[depname], writes=["wg%d" % i], key="wg%d" % i)
            return wg[i], "wg%d" % i

        for ti in range(n_mix_tiles):
            t0 = ti * TT
            load_norm_transpose(l, True, t0, 2, hsc_m, hbi_m, "hsc_m", "hbi_m", xt, xn, hT, junk, ss, rs, junkn="hnb")
            def fm_chunk(w, wn, half, rhs_t, rhs_n, evac):
                pv, pn = next_pp()
                for kc in range(8):
                    op("pe", lambda e, kc=kc, pv=pv: e.matmul(pv, lhsT=w[:, kc, half * 128:(half + 1) * 128], rhs=rhs_t[:, kc, :], start=(kc == 0), stop=(kc == 7)),
                       reads=[wn, rhs_n], writes=pn)
                evac(pv, pn)

            QA, QB = [], []
            S.defer_to = QA
            for hd in range(8):
                op("dve", lambda e, hd=hd: e.tensor_scalar(out=T4[:, hd, :], in0=T1[:, hd, :], scalar1=noml[:, l, hd:hd + 1], scalar2=oml[:, l, hd:hd + 1], op0=ALU.mult, op1=ALU.add),
                   reads=["T1", "noml", "oml"], writes=["T4"])
            for hd in range(8):
                op("act", lambda e, hd=hd: e.activation(out=T2[:, hd, :], in_=T1[:, hd, :], func=AF.Ln, scale=oml[:, l, hd:hd + 1], bias=lbc[:, l, hd:hd + 1]),
                   reads=["T1", "oml", "lbc"], writes=["T2"])
            for hd in range(8):
                op("dve", lambda e, hd=hd: e.tensor_tensor_scan(out=T3[:, hd, :], data0=mask01[:], data1=T2[:, hd, :], initial=0.0, op0=ALU.mult, op1=ALU.add),
                   reads=["T2", "mask01"], writes=["T3"])
            b4 = T3.rearrange("p h (c j) -> p h c j", j=64)
            op("dve", lambda e: e.tensor_sub(out=dE, in0=b4[:, :, :, 63], in1=b4[:, :, :, 31]), reads=["T3"], writes=["dE"])
            op("act", lambda e: e.activation(out=E1, in_=b4[:, :, :, 63], func=AF.Exp), reads=["T3"], writes=["E1"])
            op("act", lambda e: e.activation(out=E2, in_=dE, func=AF.Exp), reads=["dE"], writes=["E2"])
            op("act", lambda e: e.activation(out=E3, in_=b4[:, :, :, 31], func=AF.Exp), reads=["T3"], writes=["E3"])
            op("dve", lambda e: e.tensor_sub(out=T2.rearrange("p h (c j) -> p h c j", j=64), in0=b4, in1=bc(b4[:, :, :, 31:32], [128, 8, 4, 64])),
               reads=["T3", "T2"], writes=["T2"])
            op("act", lambda e: e.activation(out=T1, in_=T2, func=AF.Exp), reads=["T2", "T1"], writes=["T1"])
            op("act", lambda e: e.activation(out=T3, in_=T2, func=AF.Exp, scale=-1.0), reads=["T2", "E1", "E3", "dE"], writes=["T3"])
            for hd in range(8):
                qo = QEO[:, hd].rearrange("p a b c -> p (a b c)")
                for pr in range(2):
                    for eo in range(2):
                        c = pr * 2 + eo
                        o = pr * 256 + eo * 192
                        op("dve", lambda e, hd=hd, c=c, o=o, qo=qo: e.tensor_mul(out=qo[:, o:o + 64], in0=qs[:, hd, c * 64:(c + 1) * 64], in1=T1[:, hd, c * 64:(c + 1) * 64]),
                           reads=["qs", "T1"], writes=["QEO"])
            op("dve", lambda e: e.tensor_mul(out=KT, in0=T4, in1=T3), reads=["T4", "T3"], writes=["KT"])
            S.defer_to = QB
            for dc in range(8):
                op("dve", lambda e, dc=dc: e.tensor_scalar(out=acc[:, dc, :], in0=xm[:, dc, 3:3 + TT], scalar1=vcol(l, V_CW0 + 3)[:, dc:dc + 1], scalar2=vcol(l, V_CB)[:, dc:dc + 1], op0=ALU.mult, op1=ALU.add),
                   reads=["xm", "vecs", "T1"], writes=["T1"])
                for j in range(3):
                    op("dve", lambda e, dc=dc, j=j: e.scalar_tensor_tensor(out=acc[:, dc, :], in0=xm[:, dc, j:j + TT], scalar=vcol(l, V_CW0 + j)[:, dc:dc + 1], in1=acc[:, dc, :], op0=ALU.mult, op1=ALU.add),
                       reads=["xm", "vecs", "T1"], writes=["T1"])
            op("act", lambda e: e.activation(out=xcT, in_=acc, func=AF.Silu), reads=["T1"], writes=["xcT"])
            op("act", lambda e: e.copy(out=xmT, in_=xm[:, :, 3:3 + TT]), reads=["xm"], writes=["xmT"])
            op("pool", lambda e: e.tensor_copy(out=xm[:, :, 0:3], in_=xm[:, :, TT:TT + 3]), reads=["xm"], writes=["xm"])
            S.defer_to = None
            for gi, grp in enumerate((0, 1, 2, 3, 12, 13, 14, 15, 4, 5, 6, 7, 16, 17, 18, 19, 8, 9, 10, 11, 20, 21, 22, 23, 24, 25, 26, 27, 28, 29, 30, 31)):
                if gi >= 12:
                    S.drain(QA, 8 if gi < 20 else len(QA))
                if gi >= 20:
                    S.drain(QB, 5)
                w, wn = load_wg(win_s[l, grp], "win%d" % l)
                if 8 <= grp < 12:
                    for s in range(2):
                        pv, pn = next_pp()
                        for kc in range(8):
                            op("pe", lambda e, kc=kc, pv=pv, s=s, w=w: e.matmul(pv, lhsT=hT[:, kc, s * 128:(s + 1) * 128], rhs=w[:, kc, :], start=(kc == 0), stop=(kc == 7)),
                               reads=[wn, "hT"], writes=pn)
                        c0 = (grp - 8) * 256
                        op("dve", lambda e, pv=pv, s=s, c0=c0: e.tensor_copy(out=vtok[:, s, c0:c0 + 256], in_=pv), reads=pn, writes=["vtok"])
                    continue
                for half in range(2):
                    m = grp * 2 + half
                    kind, hd = m // 8, m % 8
                    if kind == 0:
                        ev = lambda pv, pn, hd=hd: op("act", lambda e: e.activation(out=qs[:, hd, :], in_=pv, func=AF.Silu), reads=pn, writes=["qs"])
                    elif kind == 1:
                        ev = lambda pv, pn, hd=hd: op("act", lambda e: e.activation(out=T1[:, hd, :], in_=pv, func=AF.Sigmoid), reads=pn, writes=["T1"])
                    elif kind == 3:
                        ev = lambda pv, pn, hd=hd: op("act", lambda e: e.activation(out=sga[:, hd, :], in_=pv, func=AF.Silu), reads=pn, writes=["sga"])
                    elif kind == 4:
                        ev = lambda pv, pn, hd=hd: op("dve", lambda e: e.tensor_copy(out=xm[:, hd, 3:3 + TT], in_=pv), reads=pn, writes=["xm"])
                    elif kind == 5:
                        ev = lambda pv, pn, hd=hd: op("act", lambda e: e.activation(out=sob[:, hd, :], in_=pv, func=AF.Sigmoid), reads=pn, writes=["sob"])
                    elif kind == 6:
                        ev = lambda pv, pn, hd=hd: op("act", lambda e: e.activation(out=sgA[:, hd, :], in_=pv, func=AF.Sigmoid), reads=pn, writes=["sgA"])
                    else:
                        ev = lambda pv, pn, hd=hd: op("act", lambda e: e.activation(out=sgB[:, hd, :], in_=pv, func=AF.Sigmoid), reads=pn, writes=["sgB"])
                    fm_chunk(w, wn, half, hT, "hT", ev)

            S.drain(QA, len(QA))
            S.drain(QB, len(QB))
            for hd in range(8):
                pt, pn = p7(2 + hd % 2, 256)
                for pr in range(2):
                    op("pe", lambda e, hd=hd, pr=pr, pt=pt: e.transpose(out=pt[:, pr * 128:(pr + 1) * 128], in_=KT[:, hd, pr * 128:(pr + 1) * 128], identity=idb[:]),
                       reads=["KT", "idb"], writes=pn)
                op("act", lambda e, hd=hd, pt=pt: e.copy(out=Ktok[:, hd].rearrange("p a b -> p (a b)"), in_=pt), reads=pn, writes=["Ktok"])
            for (mi, srcT, srcn, dstT, dstn) in ((0, xcT, "xcT", qT, "qT"), (1, xcT, "xcT", kT, "kT"), (2, xmT, "xmT", vT, "vT")):
                for dc in range(8):
                    pv, pn = next_pp()
                    op("pe", lambda e, mi=mi, dc=dc, pv=pv, srcT=srcT: e.matmul(pv, lhsT=bdt[:, mi, dc, :], rhs=srcT[:, dc, :], start=True, stop=True),
                       reads=["bdt", srcn], writes=pn)
                    eng = "act" if dc % 2 == 0 else "dve"
                    if eng == "act":
                        op("act", lambda e, dc=dc, pv=pv, dstT=dstT: e.copy(out=dstT[:, dc, :], in_=pv), reads=pn, writes=[dstn])
                    else:
                        op("dve", lambda e, dc=dc, pv=pv, dstT=dstT: e.tensor_copy(out=dstT[:, dc, :], in_=pv), reads=pn, writes=[dstn])
            for pr in range(2):
                for (mi, srcT, srcn) in ((1, xcT, "xcT"), (2, xmT, "xmT")):
                    for hb in range(2):
                        pbk = PB[2]
                        pn = ["P2a", "P2b", "P2c", "P2d"]
                        for j in range(4):
                            dc = hb * 4 + j
                            op("pe", lambda e, mi=mi, dc=dc, j=j, srcT=srcT, pr=pr: e.matmul(pbk[:, j * 128:(j + 1) * 128], lhsT=srcT[:, dc, pr * 128:(pr + 1) * 128], rhs=bdt[:, mi, dc, :], start=True, stop=True),
                               reads=["bdt", srcn], writes=pn)
                        if mi == 1:
                            op("act", lambda e, pr=pr, hb=hb: e.copy(out=ktok[:, pr, hb * 512:(hb + 1) * 512], in_=pbk[:, :]), reads=pn, writes=["ktok"])
                        else:
                            op("dve", lambda e, pr=pr, hb=hb: e.tensor_copy(out=vext[:, pr, hb * 2:hb * 2 + 2, 0:256], in_=pbk[:, :].rearrange("p (a b) -> p a b", b=256)), reads=pn, writes=["vext"])
            for pr in range(2):
                pg = PB[5][:, 256:264]
                pgn = ["P5c"]
                i = 0
                for (srcT, srcn) in ((qT, "qT"), (kT, "kT"), (vT, "vT")):
                    for dc in range(8):
                        op("pe", lambda e, srcT=srcT, dc=dc, i=i, pr=pr: e.matmul(pg, lhsT=srcT[:, dc, pr * 128:(pr + 1) * 128], rhs=wgt[:, i, :], start=(i == 0), stop=(i == 23)),
                           reads=[srcn, "wgt"], writes=pgn)
                        i += 1
                op("dve", lambda e: e.tensor_add(out=gpre, in0=pg, in1=bgt[:]), reads=pgn + ["bgt"], writes=["gpre"])
                op("act", lambda e: e.activation(out=glf, in_=gpre[:, 4:8], func=AF.Exp, scale=-1.0), reads=["gpre"], writes=["glf"])
                op("act", lambda e: e.activation(out=glf, in_=glf, func=AF.Ln, bias=1.0), reads=["glf"], writes=["glf"])
                op("dve", lambda e: e.tensor_scalar_mul(out=glf, in0=glf, scalar1=-1.0), reads=["glf"], writes=["glf"])
                op("dve", lambda e: e.tensor_copy(out=gli, in_=gpre[:, 0:4]), reads=["gpre"], writes=["gli"])
                prw = PB[5][0:4, 384:512]
                prn = ["P5d"]
                op("pe", lambda e: e.matmul(prw, lhsT=gli, rhs=idf[:], start=True, stop=True), reads=["gli", "idf"], writes=prn)
                op("dve", lambda e: e.tensor_copy(out=rows[:, 0, :], in_=prw), reads=prn, writes=["rows"])
                op("pe", lambda e: e.matmul(prw, lhsT=glf, rhs=idf[:], start=True, stop=True), reads=["glf", "idf"], writes=prn)
                op("dve", lambda e: e.tensor_copy(out=rows[:, 1, :], in_=prw), reads=prn, writes=["rows"])
                op("dve", lambda e: e.tensor_tensor_scan(out=rows[:, 2, :], data0=m01r[:], data1=rows[:, 1, :], initial=0.0, op0=ALU.mult, op1=ALU.add), reads=["rows", "m01r"], writes=["rows"])
                op("dve", lambda e: e.tensor_sub(out=rows[:, 3, :], in0=rows[:, 0, :], in1=rows[:, 2, :]), reads=["rows"], writes=["rows"])
                op("dve", lambda e: e.tensor_tensor_scan(out=rows[:, 4, :], data0=negm[:], data1=rows[:, 3, :], initial=-1e30, op0=ALU.add, op1=ALU.max), reads=["rows", "negm"], writes=["rows"])
                for eo in range(2):
                    cs = slice(eo * 64, (eo + 1) * 64)
                    op("dve", lambda e, cs=cs: e.tensor_scalar(out=rows[:, 5, cs], in0=rows[:, 4, cs], scalar1=mprev[:, 0:1], scalar2=None, op0=ALU.max), reads=["rows", "mprev"], writes=["rows"])
                    op("dve", lambda e, cs=cs: e.tensor_scalar(out=rows[:, 6, cs], in0=rows[:, 5, cs], scalar1=mprev[:, 0:1], scalar2=-1.0, op0=ALU.subtract, op1=ALU.mult), reads=["rows", "mprev"], writes=["rows"])
                    last = eo * 64 + 63
                    op("dve", lambda e, cs=cs, last=last: e.tensor_scalar(out=rows[:, 7, cs], in0=rows[:, 3, cs], scalar1=rows[:, 5, last:last + 1], scalar2=None, op0=ALU.subtract), reads=["rows"], writes=["rows"])
                    op("dve", lambda e, last=last: e.tensor_add(out=mprev[:, 0:1], in0=rows[:, 2, last:last + 1], in1=rows[:, 5, last:last + 1]), reads=["rows", "mprev"], writes=["mprev"])
                op("dve", lambda e: e.scalar_tensor_tensor(out=rows[:, 1, :], in0=rows[:, 2, :], scalar=-1.0, in1=rows[:, 5, :], op0=ALU.mult, op1=ALU.subtract), reads=["rows"], writes=["rows"])
                op("act", lambda e: e.activation(out=rows[:, 6, :], in_=rows[:, 6, :], func=AF.Exp), reads=["rows"], writes=["rows"])
                op("act", lambda e: e.activation(out=rows[:, 1, :], in_=rows[:, 1, :], func=AF.Exp), reads=["rows"], writes=["rows"])
                op("act", lambda e: e.activation(out=rows[:, 7, :], in_=rows[:, 7, :], func=AF.Exp, bias=float(-np.log(16.0))), reads=["rows"], writes=["rows"])
                pcl = PB[5][:, 264:280]
                for qi, ri in enumerate((3, 6, 1, 7)):
                    op("pe", lambda e, qi=qi, ri=ri: e.matmul(pcl[:, qi * 4:(qi + 1) * 4], lhsT=rows[:, ri, :], rhs=id4[:], start=True, stop=True), reads=["rows", "id4"], writes=pgn)
                op("dve", lambda e, pr=pr: e.tensor_copy(out=gcol[:, pr, :], in_=pcl), reads=pgn, writes=["gcol"])
                wl = rows[:, 6, :].rearrange("p (c j) -> p c j", j=64)[:, :, 63]
                op("dve", lambda e, wl=wl: e.tensor_mul(out=dexp, in0=bc(wl.unsqueeze(1), [4, 4, 2]), in1=bc(id4[:].unsqueeze(2), [4, 4, 2])), reads=["rows", "id4"], writes=["dexp"])
                pdc = PB[5][:, 280:288]
                op("pe", lambda e: e.matmul(pdc, lhsT=ones4[:], rhs=dexp.rearrange("p a b -> p (a b)"), start=True, stop=True), reads=["dexp", "ones4"], writes=pgn)
                op("dve", lambda e, pr=pr: e.tensor_copy(out=decb[:, pr].rearrange("p e h -> p h e"), in_=pdc.rearrange("p (h e) -> p h e", e=2)), reads=pgn, writes=["decb"])
                for hd in range(8):
                    pv = PB[hd // 4][:, (hd % 4) * 128:(hd % 4) * 128 + 128]
                    pn = ["P%d" % (hd // 4)]
                    op("pe", lambda e, hd=hd, pv=pv: e.matmul(pv, lhsT=KT[:, hd, pr * 128:(pr + 1) * 128], rhs=QEO[:, hd, pr, 0, :], start=True, stop=False), reads=["KT", "QEO"], writes=pn)
                    op("pe", lambda e, hd=hd, pv=pv: e.matmul(pv, lhsT=KT[:, hd, pr * 128:(pr + 1) * 128], rhs=QEO[:, hd, pr, 1, :], start=False, stop=True), reads=["KT", "QEO"], writes=pn)
                for hf in range(2):
                    op("dve", lambda e, hf=hf: e.tensor_scalar(out=clb[:, hf * 4:(hf + 1) * 4, :], in0=PB[hf][:, :].rearrange("p (h t) -> p h t", t=128), scalar1=1e30, scalar2=-1e30, op0=ALU.min, op1=ALU.max),
                       reads=["P%d" % hf], writes=["clb%d" % hf, "T1"])
                op("dve", lambda e: e.tensor_mul(out=STb8, in0=clb, in1=bc(mask2[:].unsqueeze(1), [128, 8, 128])), reads=["clb0", "clb1", "mask2"], writes=["STb8"])
                for eo in range(2):
                    c = pr * 2 + eo
                    op("dve", lambda e, eo=eo, c=c: e.tensor_mul(out=hsb8[:, eo], in0=hst[:], in1=bc(E3[:, :, c:c + 1], [128, 8, 128])), reads=["hst", "E3"], writes=["hsb8_%d" % eo, "T3"])
                    for hd in range(8):
                        pkv = PB[5 + hd // 4][:, (hd % 4) * 128:(hd % 4) * 128 + 128]
                        op("pe", lambda e, hd=hd, eo=eo, pkv=pkv: e.matmul(pkv, lhsT=Ktok[eo * 64:(eo + 1) * 64, hd, pr, :], rhs=vtok[eo * 64:(eo + 1) * 64, pr, hd * 128:(hd + 1) * 128], start=True, stop=True),
                           reads=["Ktok", "vtok"], writes=["P%d" % (5 + hd // 4)])
                    for hf in range(2):
                        op("dve", lambda e, hf=hf, c=c: e.tensor_mul(out=tkv8[:, hf * 4:(hf + 1) * 4, :], in0=PB[5 + hf][:, :].rearrange("p (h t) -> p h t", t=128), in1=bc(E2[:, hf * 4:(hf + 1) * 4, c:c + 1], [128, 4, 128])),
                           reads=["P%d" % (5 + hf), "E2"], writes=["tkv8_%d" % hf, "T1"])
                    op("dve", lambda e, c=c: e.tensor_mul(out=hst[:], in0=hst[:], in1=bc(E1[:, :, c:c + 1], [128, 8, 128])), reads=["hst", "E1"], writes=["hst"])
                    op("dve", lambda e: e.tensor_add(out=hst[:], in0=hst[:], in1=tkv8), reads=["hst", "tkv8_0", "tkv8_1"], writes=["hst"])
                for hd in range(8):
                    po = PB[3 + hd // 4][:, (hd % 4) * 128:(hd % 4) * 128 + 128]
                    pon = ["P3" if hd < 4 else "P4"]
                    op("pe", lambda e, hd=hd, po=po: e.matmul(po, lhsT=QEO[:, hd, pr, 0, :], rhs=hsb8[:, 0, hd, :], start=True, stop=False), reads=["QEO", "hsb8_0"], writes=pon)
                    op("pe", lambda e, hd=hd, po=po: e.matmul(po, lhsT=QEO[:, hd, pr, 1, :], rhs=hsb8[:, 1, hd, :], start=False, stop=False), reads=["QEO", "hsb8_1"], writes=pon)
                    op("pe", lambda e, hd=hd, po=po: e.matmul(po, lhsT=STb8[:, hd, :], rhs=vtok[:, pr, hd * 128:(hd + 1) * 128], start=False, stop=True), reads=["STb8", "vtok"], writes=pon)
                op("act", lambda e: e.copy(out=o_sb[:, 0:512], in_=PB[3][:, :]), reads=["P3"], writes=["T2"])
                op("act", lambda e: e.copy(out=o_sb[:, 512:1024], in_=PB[4][:, :]), reads=["P4"], writes=["T2"])
                op("act", lambda e: e.activation(out=sqb, in_=o_sb, func=AF.Square), reads=["T2"], writes=["T3"])
                op("dve", lambda e: e.tensor_reduce(out=ssq, in_=sqb.rearrange("p (h v) -> p h v", v=128), axis=AX.X, op=ALU.add), reads=["T3"], writes=["ssq"])
                rstd_from_ss(rsq, ssq, 128, "ssq", "rsq")
                op("dve", lambda e: e.tensor_mul(out=onb.rearrange("p (h v) -> p h v", v=128), in0=o_sb.rearrange("p (h v) -> p h v", v=128), in1=bc(rsq.unsqueeze(2), [128, 8, 128])),
                   reads=["T2", "rsq"], writes=["onb"])
                for hd in range(8):
                    pt, pn = p7((hd % 2) * 2, 128)
                    op("pe", lambda e, hd=hd, pt=pt: e.transpose(out=pt, in_=onb[:, hd * 128:(hd + 1) * 128], identity=idb[:]), reads=["onb", "idb"], writes=pn)
                    op("dve", lambda e, hd=hd, pt=pt: e.scalar_tensor_tensor(out=yaT[:, hd, pr * 128:(pr + 1) * 128], in0=pt, scalar=vcol(l, V_HGN)[:, hd:hd + 1], in1=sga[:, hd, pr * 128:(pr + 1) * 128], op0=ALU.mult, op1=ALU.mult),
                       reads=pn + ["vecs", "sga"], writes=["yaT"])
                for h in range(4):
                    psc = PB[5][:, 0:128]
                    pM = PB[2][:, 0:128]
                    op("pe", lambda e, h=h: e.matmul(psc, lhsT=kT[:, 2 * h, pr * 128:(pr + 1) * 128], rhs=qT[:, 2 * h, pr * 128:(pr + 1) * 128], start=True, stop=False), reads=["kT", "qT"], writes=["P5a"])
                    op("pe", lambda e, h=h: e.matmul(psc, lhsT=kT[:, 2 * h + 1, pr * 128:(pr + 1) * 128], rhs=qT[:, 2 * h + 1, pr * 128:(pr + 1) * 128], start=False, stop=True), reads=["kT", "qT"], writes=["P5a"])
                    op("pe", lambda e, h=h: e.matmul(pM, lhsT=sel4[:, h, :], rhs=rows[:, 5, :], start=True, stop=True), reads=["sel4", "rows"], writes=["P2"])
                    op("act", lambda e, h=h: e.activation(out=DT[:, h % 2, :], in_=pM, func=AF.Exp, scale=-1.0, bias=gcol[:, pr, h:h + 1]), reads=["P2", "gcol"], writes=["DT%d" % (h % 2)])
                    op("dve", lambda e, h=h: e.tensor_mul(out=DTm[:, h % 2, :], in0=DT[:, h % 2, :], in1=maskm[:]), reads=["DT%d" % (h % 2), "maskm"], writes=["DTm%d" % (h % 2)])
                    stn = "STb%d" % (h % 2)
                    op("dve", lambda e, h=h: e.tensor_mul(out=STb[:, h % 2, :], in0=psc, in1=DTm[:, h % 2, :]), reads=["P5a", "DTm%d" % (h % 2)], writes=[stn])
                    pnum = PB[6][:, 0:257]
                    op("pe", lambda e, h=h: e.matmul(pnum, lhsT=STb[:, h % 2, :], rhs=vext[:, pr, h, :], start=True, stop=True), reads=[stn, "vext"], writes=["P6a", "P6b"])
                    op("act", lambda e: e.copy(out=tnum, in_=pnum), reads=["P6a", "P6b"], writes=["tnum"])
                    op("dve", lambda e, h=h: e.tensor_scalar(out=kw[:, h % 2, :], in0=ktok[:, pr, h * 256:(h + 1) * 256], scalar1=gcol[:, pr, 12 + h:13 + h], scalar2=None, op0=ALU.mult),
                       reads=["ktok", "gcol"], writes=["kw%d" % (h % 2)])
                    cn = "C%d" % h
                    cbn = "Cb%d" % h
                    for eo in range(2):
                        pint = PB[eo][:, 0:257]
                        pintn = ["P%da" % eo, "P%db" % eo]
                        rsl = slice(eo * 64, (eo + 1) * 64)
                        for j in range(2):
                            op("pe", lambda e, h=h, j=j, pint=pint: e.matmul(pint, lhsT=qT[:, 2 * h + j, pr * 128:(pr + 1) * 128], rhs=Cbf[:, h, j, :], start=(j == 0), stop=(j == 1)), reads=["qT", cbn], writes=pintn)
                        op("dve", lambda e, h=h, rsl=rsl, pint=pint: e.scalar_tensor_tensor(out=tot[rsl, h, :], in0=pint[rsl, :], scalar=gcol[rsl, pr, 4 + h:5 + h], in1=tnum[rsl, :], op0=ALU.mult, op1=ALU.add),
                           reads=pintn + ["gcol", "tnum"], writes=["T4"])
                        for j in range(2):
                            pC = PB[3 + j][:, 0:257]
                            pCn = ["P3" if j == 0 else "P4"]
                            op("pe", lambda e, h=h, j=j, rsl=rsl, pC=pC: e.matmul(pC, lhsT=kw[rsl, h % 2, j * 128:(j + 1) * 128], rhs=vext[rsl, pr, h, :], start=True, stop=True), reads=["kw%d" % (h % 2), "vext"], writes=pCn)
                            op("dve", lambda e, h=h, j=j, eo=eo, pC=pC: e.scalar_tensor_tensor(out=Cst[:, h, j, :], in0=Cst[:, h, j, :], scalar=decb[:, pr, eo, h:h + 1], in1=pC, op0=ALU.mult, op1=ALU.add),
                               reads=pCn + ["decb", cn], writes=[cn])
                            op("act", lambda e, h=h, j=j: e.copy(out=Cbf[:, h, j, :], in_=Cst[:, h, j, :]), reads=[cn], writes=[cbn])
                den = tot[:, :, 256]
                op("dve", lambda e: e.tensor_scalar_mul(out=sm[:, 16:20], in0=den, scalar1=-1.0), reads=["T4"], writes=["sm"])
                op("dve", lambda e: e.tensor_max(out=sm[:, 4:8], in0=sm[:, 16:20], in1=den), reads=["T4", "sm"], writes=["sm"])
                op("dve", lambda e: e.tensor_max(out=sm[:, 4:8], in0=sm[:, 4:8], in1=gcol[:, pr, 8:12]), reads=["sm", "gcol"], writes=["sm"])
                op("dve", lambda e: e.reciprocal(out=sm[:, 4:8], in_=sm[:, 4:8]), reads=["sm"], writes=["sm"])
                op("dve", lambda e: e.tensor_reduce(out=sm[:, 8:12], in_=tot[:, :, 0:256], axis=AX.X, op=ALU.add), reads=["T4"], writes=["sm"])
                op("act", lambda e: e.activation(out=sqm, in_=tot[:, :, 0:256], func=AF.Square), reads=["T4"], writes=["T3"])
                op("dve", lambda e: e.tensor_reduce(out=sm[:, 12:16], in_=sqm, axis=AX.X, op=ALU.add), reads=["T3"], writes=["sm"])
                op("dve", lambda e: e.tensor_scalar_mul(out=sm[:, 8:12], in0=sm[:, 8:12], scalar1=1.0 / 256), reads=["sm"], writes=["sm"])
                op("dve", lambda e: e.tensor_mul(out=sm[:, 16:20], in0=sm[:, 8:12], in1=sm[:, 8:12]), reads=["sm"], writes=["sm"])
                op("dve", lambda e: e.scalar_tensor_tensor(out=sm[:, 12:16], in0=sm[:, 12:16], scalar=1.0 / 256, in1=sm[:, 16:20], op0=ALU.mult, op1=ALU.subtract), reads=["sm"], writes=["sm"])
                op("dve", lambda e: e.tensor_mul(out=sm[:, 16:20], in0=sm[:, 4:8], in1=sm[:, 4:8]), reads=["sm"], writes=["sm"])
                op("dve", lambda e: e.tensor_mul(out=sm[:, 12:16], in0=sm[:, 12:16], in1=sm[:, 16:20]), reads=["sm"], writes=["sm"])
                op("dve", lambda e: e.tensor_scalar(out=sm[:, 12:16], in0=sm[:, 12:16], scalar1=0.0, scalar2=EPS, op0=ALU.max, op1=ALU.add), reads=["sm"], writes=["sm"])
                op("act", lambda e: e.activation(out=sm[:, 12:16], in_=sm[:, 12:16], func=AF.Ln), reads=["sm"], writes=["sm"])
                op("act", lambda e: e.activation(out=sm[:, 12:16], in_=sm[:, 12:16], func=AF.Exp, scale=-0.5), reads=["sm"], writes=["sm"])
                op("dve", lambda e: e.tensor_mul(out=sm[:, 20:24], in0=sm[:, 4:8], in1=sm[:, 12:16]), reads=["sm"], writes=["sm"])
                op("dve", lambda e: e.scalar_tensor_tensor(out=sm[:, 24:28], in0=sm[:, 8:12], scalar=-1.0, in1=sm[:, 20:24], op0=ALU.mult, op1=ALU.mult), reads=["sm"], writes=["sm"])
                for h in range(4):
                    op("dve", lambda e, h=h: e.tensor_scalar(out=hnb[:, h * 256:(h + 1) * 256], in0=tot[:, h, 0:256], scalar1=sm[:, 20 + h:21 + h], scalar2=sm[:, 24 + h:25 + h], op0=ALU.mult, op1=ALU.add),
                       reads=["T4", "sm"], writes=["hnb"])
                for dc in range(8):
                    pt, pn = p7((dc % 2) * 2 + 1, 128)
                    op("pe", lambda e, dc=dc, pt=pt: e.transpose(out=pt, in_=hnb[:, dc * 128:(dc + 1) * 128], identity=idb[:]), reads=["hnb", "idb"], writes=pn)
                    cs = slice(pr * 128, (pr + 1) * 128)
                    op("act", lambda e, dc=dc, cs=cs: e.activation(out=ybT[:, dc, cs], in_=xcT[:, dc, cs], func=AF.Copy, scale=vcol(l, V_SKIP)[:, dc:dc + 1]),
                       reads=["vecs", "xcT"], writes=["ybT"])
                    op("dve", lambda e, dc=dc, pt=pt, cs=cs: e.scalar_tensor_tensor(out=ybT[:, dc, cs], in0=pt, scalar=vcol(l, V_MGN)[:, dc:dc + 1], in1=ybT[:, dc, cs], op0=ALU.mult, op1=ALU.add),
                       reads=pn + ["vecs", "ybT"], writes=["ybT"])
                    op("dve", lambda e, dc=dc, cs=cs: e.tensor_mul(out=ybT[:, dc, cs], in0=ybT[:, dc, cs], in1=sob[:, dc, cs]), reads=["ybT", "sob"], writes=["ybT"])
            for g in range(4):
                wa, wan = load_wg(wpa_s[l, g], "wpa%d" % l)
                wb, wbn = load_wg(wpb_s[l, g], "wpb%d" % l)
                for half in range(2):
                    m = g * 2 + half
                    i = m % 2
                    fm_chunk(wa, wan, half, yaT, "yaT", lambda pv, pn, m=m, i=i: op("dve", lambda e: e.tensor_mul(out=m1[:, i, :], in0=pv, in1=sgA[:, m, :]), reads=pn + ["sgA"], writes=["T3"]))
                    fm_chunk(wb, wbn, half, ybT, "ybT", lambda pv, pn, m=m, i=i: op("dve", lambda e: e.tensor_mul(out=m2[:, i, :], in0=pv, in1=sgB[:, m, :]), reads=pn + ["sgB"], writes=["T4"]))
                    op("dve", lambda e, m=m, i=i: e.tensor_add(out=mixT[:, m, :], in0=m1[:, i, :], in1=m2[:, i, :]), reads=["T3", "T4"], writes=["mixT"])
            for s in range(2):
                ybanks = (PB[3], PB[4]) if s == 0 else (PB[5], PB[6])
                ybn = ["P3", "P4"] if s == 0 else ["P5a", "P5b", "P5c", "P5d", "P6a", "P6b"]
                for g in range(4):
                    w, wn = load_wg(wo_s[l, g], "wo%d" % l)
                    yv = ybanks[g // 2][:, (g % 2) * 256:(g % 2) * 256 + 256]
                    for kc in range(8):
                        op("pe", lambda e, kc=kc, yv=yv, w=w, s=s: e.matmul(yv, lhsT=mixT[:, kc, s * 128:(s + 1) * 128], rhs=w[:, kc, :], start=(kc == 0), stop=(kc == 7)),
                           reads=["mixT", wn], writes=ybn)
                residual_update(l, t0, s, [(ybanks[0][:, :], 0, 512), (ybanks[1][:, :], 512, 512)], ybn, xt, ysb, tmpx, junk, ssy, rsy, ggm, "ggm", ysbn="T1", tmpxn="T2", junkn="hnb")
            if DEBUG and l == 0 and ti == 0:
                for (nm, tl, dn) in (("hT", hT, "hT"), ("yaT", yaT, "yaT"), ("ybT", ybT, "ybT"), ("mixT", mixT, "mixT"), ("qT", qT, "qT"), ("kT", kT, "kT"), ("vT", vT, "vT"), ("xcT", xcT, "xcT"), ("KT", KT, "KT"), ("sga", sga, "sga")):
                    dd = nc.dram_tensor("dbg_" + nm, [128, 8, TT], BF16, kind="ExternalOutput").ap()
                    last_x_dma["dbg_" + nm] = S.dma("sp", dd, tl, reads=[dn], key="dbg_" + nm)
                dd = nc.dram_tensor("dbg_gcol", [128, 2, 16], F32, kind="ExternalOutput").ap()
                last_x_dma["dbg_gcol"] = S.dma("sp", dd, gcol, reads=["gcol"], key="dbg_gcol")
                dd = nc.dram_tensor("dbg_tot", [128, 4, 257], F32, kind="ExternalOutput").ap()
                last_x_dma["dbg_tot"] = S.dma("sp", dd, tot, reads=["T4"], key="dbg_tot")

    def ffn(l):
        AR.reset()
        S.barrier()
        A = AR.alloc
        moe = (l % 2 == 1)
        j = l // 2
        NG = 14 if moe else 11
        NFC = 2 * NG
        NE = 8 if moe else 1
        xt = A([128, 4, D])
        hT = A([128, 8, TF], BF16)
        junk = A([128, D], BF16); ss = A([128, 4]); rs = A([128, 4]); ssy = A([128, 2]); rsy = A([128, 2])
        aT = A([128, NFC, TF], BF16)
        w2t = [A([128, NFC, 512], BF16) for _ in range(2)]
        NW13 = 2
        w13 = [A([128, 2, 8, 256], BF16) for _ in range(NW13)]
        if moe:
            xn = w2t[0].rearrange("p a b -> p (a b)")[:, 0:8192].bitcast(F32).rearrange("p (a b) -> p a b", b=D)
            hTf = w2t[1].rearrange("p a b -> p (a b)")[:, 0:8192].bitcast(F32).rearrange("p (a b) -> p a b", b=TF)
            xnn, hTfn = "w2_0", "w2_1"
        else:
            xn = A([128, 4, D], BF16)
            hTf = None
            xnn, hTfn = "xn", "hTf"
        yacc = A([128, 4, D])
        gt = A([128, TF])
        tmpx = A([128, D])
        lg = A([128, 4, 8]); gates = A([128, 4, 8]); gm = A([128, 4, 8])
        mx1 = A([128, 4]); mx2 = A([128, 4]); den = A([128, 4])
        print("ffn arena words", AR.off)
        cnt = {"w13": 0, "w2": 0, "g": 0, "y": 0}
        for ti in range(n_ffn_tiles):
            t0 = ti * TF
            load_norm_transpose(l, False, t0, 4, hsc_f, hbi_f, "hsc_f", "hbi_f", xt, xn, hT, junk, ss, rs, hTf=hTf, xnn=xnn, hTfn=hTfn)
            if moe:
                for s in range(4):
                    pl = PB[5][:, 256 + s * 8:256 + s * 8 + 8]
                    for kc in range(8):
                        op("pe", lambda e, s=s, kc=kc, pl=pl: e.matmul(pl, lhsT=hTf[:, kc, s * 128:(s + 1) * 128], rhs=rtt[:, kc, :], start=(kc == 0), stop=(kc == 7)), reads=[hTfn, "rtt"], writes=["P5c"])
                op("dve", lambda e: e.tensor_copy(out=lg, in_=PB[5][:, 256:288].rearrange("p (s e) -> p s e", e=8)), reads=["P5c"], writes=["lg"])
                op("dve", lambda e: e.tensor_reduce(out=mx1, in_=lg, axis=AX.X, op=ALU.max), reads=["lg"], writes=["mx1"])
                op("dve", lambda e: e.tensor_tensor(out=gm, in0=lg, in1=bc(mx1.unsqueeze(2), [128, 4, 8]), op=ALU.is_equal), reads=["lg", "mx1"], writes=["gm"])
                op("dve", lambda e: e.scalar_tensor_tensor(out=gm, in0=gm, scalar=-1e30, in1=lg, op0=ALU.mult, op1=ALU.add), reads=["gm", "lg"], writes=["gm"])
                op("dve", lambda e: e.tensor_reduce(out=mx2, in_=gm, axis=AX.X, op=ALU.max), reads=["gm"], writes=["mx2"])
                op("dve", lambda e: e.tensor_tensor(out=gm, in0=lg, in1=bc(mx2.unsqueeze(2), [128, 4, 8]), op=ALU.is_ge), reads=["lg", "mx2", "gm"], writes=["gm"])
                op("dve", lambda e: e.tensor_sub(out=lg, in0=lg, in1=bc(mx1.unsqueeze(2), [128, 4, 8])), reads=["lg", "mx1"], writes=["lg"])
                op("act", lambda e: e.activation(out=lg, in_=lg, func=AF.Exp), reads=["lg"], writes=["lg"])
                op("dve", lambda e: e.tensor_sub(out=den, in0=mx2, in1=mx1), reads=["mx1", "mx2"], writes=["den"])
                op("act", lambda e: e.activation(out=den, in_=den, func=AF.Exp), reads=["den"], writes=["den"])
                op("dve", lambda e: e.tensor_scalar_add(out=den, in0=den, scalar1=1.0), reads=["den"], writes=["den"])
                op("dve", lambda e: e.reciprocal(out=den, in_=den), reads=["den"], writes=["den"])
                op("dve", lambda e: e.tensor_mul(out=gates, in0=lg, in1=gm), reads=["lg", "gm"], writes=["gates"])
                op("dve", lambda e: e.tensor_mul(out=gates, in0=gates, in1=bc(den.unsqueeze(2), [128, 4, 8])), reads=["gates", "den"], writes=["gates"])
            for ex in range(NE):
                for g in range(NG):
                    i = cnt["w13"] % NW13
                    cnt["w13"] += 1
                    wn = "w13_%d" % i
                    if moe:
                        S.dma("sp", w13[i], m13_s[j, ex, g], reads=["m13_%d_%d" % (l, ex)], writes=[wn], key=wn)
                    else:
                        S.dma("sp", w13[i], f13_s[j, g], reads=["f13_%d" % l], writes=[wn], key=wn)
                    for half in range(2):
                        fc = g * 2 + half
                        gi = cnt["g"] % 2
                        cnt["g"] += 1
                        pg_, pu_ = PB[gi], PB[2 + gi]
                        pgn = ["P%da" % gi, "P%db" % gi]
                        pun = ["P2a", "P2b", "P2c", "P2d"] if gi == 0 else ["P3"]
                        for kc in range(8):
                            op("pe", lambda e, kc=kc, half=half, w=w13[i], pg_=pg_: e.matmul(pg_[:, :], lhsT=w[:, 0, kc, half * 128:(half + 1) * 128], rhs=hT[:, kc, :], start=(kc == 0), stop=(kc == 7)), reads=[wn, "hT"], writes=pgn)
                        for kc in range(8):
                            op("pe", lambda e, kc=kc, half=half, w=w13[i], pu_=pu_: e.matmul(pu_[:, :], lhsT=w[:, 1, kc, half * 128:(half + 1) * 128], rhs=hT[:, kc, :], start=(kc == 0), stop=(kc == 7)), reads=[wn, "hT"], writes=pun)
                        op("act", lambda e, pg_=pg_: e.activation(out=gt, in_=pg_[:, :], func=AF.Silu), reads=pgn, writes=["gt"])
                        op("dve", lambda e, fc=fc, pu_=pu_: e.tensor_mul(out=aT[:, fc, :], in0=pu_[:, :], in1=gt), reads=pun + ["gt"], writes=["aT"])
                for half in range(2):
                    wi = cnt["w2"] % 2
                    cnt["w2"] += 1
                    w2n = "w2_%d" % wi
                    if moe:
                        S.dma("sp", w2t[wi], m2_s[j, ex, half], reads=["m2_%d_%d" % (l, ex)], writes=[w2n], key=w2n)
                    else:
                        S.dma("sp", w2t[wi], f2_s[j, half], reads=["f2_%d" % l], writes=[w2n], key=w2n)
                    for s in range(4):
                        yi = cnt["y"] % 2
                        cnt["y"] += 1
                        py = PB[4 + yi]
                        pyn = ["P4"] if yi == 0 else ["P5a", "P5b", "P5c", "P5d"]
                        for fc in range(NFC):
                            op("pe", lambda e, fc=fc, s=s, py=py, w=w2t[wi]: e.matmul(py[:, :], lhsT=aT[:, fc, s * 128:(s + 1) * 128], rhs=w[:, fc, :], start=(fc == 0), stop=(fc == NFC - 1)), reads=["aT", w2n], writes=pyn)
                        ya = yacc[:, s, half * 512:(half + 1) * 512]
                        if not moe:
                            op("act", lambda e, py=py, ya=ya: e.copy(out=ya, in_=py[:, :]), reads=pyn, writes=["yacc"])
                        elif ex == 0:
                            op("dve", lambda e, py=py, ya=ya, s=s, ex=ex: e.tensor_scalar(out=ya, in0=py[:, :], scalar1=gates[:, s, ex:ex + 1], scalar2=None, op0=ALU.mult), reads=pyn + ["gates"], writes=["yacc"])
                        else:
                            op("dve", lambda e, py=py, ya=ya, s=s, ex=ex: e.scalar_tensor_tensor(out=ya, in0=py[:, :], scalar=gates[:, s, ex:ex + 1], in1=ya, op0=ALU.mult, op1=ALU.add), reads=pyn + ["gates", "yacc"], writes=["yacc"])
            for s in range(4):
                residual_update(l, t0, s, None, None, xt, None, tmpx, junk, ssy, rsy, ggf, "ggf", yacc=yacc[:, s, :])

    for l in range(n_layers):
        layer_prologue(l)
        mixer(l)
        if do_ffn:
            ffn(l)
    if S.limit is not None:
        S.emit(final_waits=[S.ops[e][-1] for e in S.ENGS if S.ops[e]])
    else:
        S.emit(final_waits=list(last_x_dma.values()))
    S.close()
    return nc


def prep_inputs(inputs):
    f = lambda a: np.ascontiguousarray(np.asarray(a, dtype=np.float32))
    col = lambda v: f(v).reshape(8, 128).T
    L = DEPTH
    cols = []
    for l in range(L):
        vs = [inputs["g_pre_mix"][l], inputs["g_pre_ffn"][l], inputs["hgrn_gnorm"][l],
              inputs["mlstm_conv_w"][l][0], inputs["mlstm_conv_w"][l][1], inputs["mlstm_conv_w"][l][2], inputs["mlstm_conv_w"][l][3],
              inputs["mlstm_conv_b"][l], inputs["mlstm_gnorm"][l], inputs["mlstm_skip"][l]]
        for v in vs:
            cols.append(col(v))
    for l in range(L):
        cols.append(col(inputs["hgrn_lb"][l]))
    vecs = f(np.concatenate(cols, axis=1))
    bd = np.zeros((L, 128, 3, 8, 128), np.float32)
    for mi, nm in enumerate(("mlstm_wq", "mlstm_wk", "mlstm_wv")):
        w = f(inputs[nm]).reshape(L, 8, 32, 4, 4)
        for n in range(32):
            bd[:, 4 * n:4 * n + 4, mi, :, 4 * n:4 * n + 4] = w[:, :, n].transpose(0, 2, 1, 3)
    bd = f(bd.reshape(L, 128, 3 * 8 * 128))
    wg = np.concatenate([f(inputs["mlstm_w_ig"]), f(inputs["mlstm_w_fg"])], axis=2)
    wg = f(wg.reshape(L, 24, 128, 8).transpose(0, 2, 1, 3).reshape(L, 128, 192))
    bg = f(np.concatenate([f(inputs["mlstm_b_ig"]), f(inputs["mlstm_b_fg"])], axis=1))
    rt = f(f(inputs["moe_router"]).reshape(2, 8, 128, 8).transpose(0, 2, 1, 3).reshape(2, 128, 64))
    shared = {
        "vecs": vecs, "w_ada": f(inputs["w_ada"]), "b_ada": f(inputs["b_ada"]),
        "g_post_mix": f(inputs["g_post_mix"]), "g_post_ffn": f(inputs["g_post_ffn"]),
        "w_in": f(inputs["w_in"]), "bd": bd, "wgate": wg, "bgate": bg,
        "w_proj_a": f(inputs["w_proj_a"]), "w_proj_b": f(inputs["w_proj_b"]), "w_out": f(inputs["w_out"]),
        "ffn_w1": f(inputs["ffn_w1"]), "ffn_w3": f(inputs["ffn_w3"]), "ffn_w2": f(inputs["ffn_w2"]),
        "router": rt, "moe_w1": f(inputs["moe_w1"]), "moe_w3": f(inputs["moe_w3"]), "moe_w2": f(inputs["moe_w2"]),
    }
    x = f(inputs["x"])
    c = f(inputs["c"])
    maps = []
    for b in range(x.shape[0]):
        m = dict(shared)
        m["x"] = x[b]
        m["ccol"] = f(c[b].reshape(8, 128).T)
        maps.append(m)
    return maps


_NC_CACHE = {}


def kernel(**inputs):
    maps = prep_inputs(inputs)
    if "nc" not in _NC_CACHE:
        _NC_CACHE["nc"] = build_program()
    nc = _NC_CACHE["nc"]
    res = run_bass_kernel_spmd(nc, maps, core_ids=list(range(NCORES)))
    return np.stack([np.asarray(r["out"], dtype=np.float32) for r in res.results], axis=0)
```

```python
import contextlib
import types
import numpy as np
import concourse.bass as bass
import concourse.mybir as mybir
from concourse.bass_utils import run_bass_kernel_spmd

F32 = mybir.dt.float32
BF16 = mybir.dt.bfloat16
AF = mybir.ActivationFunctionType
ALU = mybir.AluOpType
AX = mybir.AxisListType

SEM_CAP = 30000
D = 1024
SEQ = 4096
DEPTH = 4
NCORES = 8
TT = 256
TF = 512
EPS = 1e-6
NV = 10
DEBUG = False


class Dep:
    __slots__ = ("name", "last_w", "readers")

    def __init__(self, name):
        self.name = name
        self.last_w = None
        self.readers = []


class Op:
    __slots__ = ("eng", "fn", "deps", "is_dma", "key", "needs_inc", "sem", "val")

    def __init__(self, eng, fn, is_dma=False, key=None):
        self.eng = eng
        self.fn = fn
        self.deps = []
        self.is_dma = is_dma
        self.key = key
        self.needs_inc = False
        self.sem = None
        self.val = 0


class Sched:
    ENGS = ("pe", "act", "dve", "pool", "sp")

    def __init__(self, nc):
        self.nc = nc
        self.ops = {e: [] for e in self.ENGS}
        self.all_ops = []
        self.deps = {}
        self.stack = contextlib.ExitStack()
        self.fence = []
        self.passed = {e: True for e in self.ENGS}

    def sb(self, name, shape, dtype=F32):
        return self.stack.enter_context(self.nc.sbuf_tensor("sb_" + name, list(shape), dtype))

    def ps(self, name, shape, dtype=F32):
        return self.stack.enter_context(self.nc.psum_tensor("ps_" + name, list(shape), dtype))

    def _D(self, x):
        d = self.deps.get(x)
        if d is None:
            d = self.deps[x] = Dep(x)
        return d

    def barrier(self):
        self.fence = [self.ops[e][-1] for e in self.ENGS if self.ops[e]]
        self.passed = {e: False for e in self.ENGS}

    limit = None

    def _record(self, op, reads, writes):
        if self.limit is not None and len(self.all_ops) >= self.limit:
            return op
        rr, ww = [], []
        for r in reads:
            if len(r) >= 2 and r[0] == "P" and r[1].isdigit():
                ww.append(r[:2])
            else:
                rr.append(r)
        for w in writes:
            if len(w) >= 2 and w[0] == "P" and w[1].isdigit():
                ww.append(w[:2])
            else:
                ww.append(w)
        reads, writes = rr, list(dict.fromkeys(ww))
        deps = []
        if not self.passed[op.eng]:
            deps.extend(self.fence)
            self.passed[op.eng] = True
        for r in reads:
            r = self._D(r)
            if r.last_w is not None:
                deps.append(r.last_w)
        for w in writes:
            w = self._D(w)
            if w.last_w is not None:
                deps.append(w.last_w)
            deps.extend(w.readers)
        seen = set()
        for d in deps:
            if d is op or id(d) in seen:
                continue
            seen.add(id(d))
            op.deps.append(d)
        for r in reads:
            self._D(r).readers.append(op)
        for w in writes:
            w = self._D(w)
            w.last_w = op
            w.readers = []
        self.ops[op.eng].append(op)
        self.all_ops.append(op)
        return op

    @staticmethod
    def _freeze(fn):
        if fn.__closure__ is None:
            return fn
        cells = []
        for c in fn.__closure__:
            try:
                cells.append(types.CellType(c.cell_contents))
            except ValueError:
                cells.append(c)
        return types.FunctionType(fn.__code__, fn.__globals__, fn.__name__, fn.__defaults__, tuple(cells))

    defer_to = None

    def op(self, eng, fn, reads=(), writes=()):
        if self.defer_to is not None:
            self.defer_to.append((eng, self._freeze(fn), list(reads), list(writes)))
            return None
        return self._record(Op(eng, self._freeze(fn)), reads, writes)

    def drain(self, q, k):
        for _ in range(min(k, len(q))):
            eng, fn, reads, writes = q.pop(0)
            self._record(Op(eng, fn), reads, writes)

    def dma(self, eng, out, in_, reads=(), writes=(), key=None, **kw):
        if key is None:
            key = writes[0] if writes else reads[0]
        fn = lambda e: e.dma_start(out=out, in_=in_, **kw)
        return self._record(Op(eng, fn, is_dma=True, key=key), reads, writes)

    def emit(self, final_waits=()):
        nc = self.nc
        for op in self.all_ops:
            for d in op.deps:
                if d.eng == "pe" and op.eng == "pe" and not d.is_dma:
                    continue
                d.needs_inc = True
        for op in final_waits:
            op.needs_inc = True
        print("ops per engine", {e: len(self.ops[e]) for e in self.ENGS})
        sems = {}

        def get_sem(name):
            s = sems.get(name)
            if s is None:
                s = sems[name] = self.stack.enter_context(nc.semaphore(name))
            return s

        cnt = {}
        ccnt = {e: 0 for e in self.ENGS}
        for op in self.all_ops:
            if op.is_dma:
                k = cnt.get(op.key, 0) + 1
                cnt[op.key] = k
                per = SEM_CAP // 16
                ep, v = divmod(k - 1, per)
                op.sem = get_sem("d_%s_%d" % (op.key, ep))
                op.val = (v + 1) * 16
            elif op.needs_inc:
                c = ccnt[op.eng]
                ep, v = divmod(c, SEM_CAP)
                op.sem = get_sem("e_%s_%d" % (op.eng, ep))
                op.val = v + 1
                ccnt[op.eng] = c + 1
        engmap = {"pe": "tensor", "act": "scalar", "dve": "vector", "pool": "gpsimd", "sp": "sync"}

        def run(engname, eng):
            known = {}
            for op in self.ops[engname]:
                for d in op.deps:
                    if d.eng == "pe" and engname == "pe" and not d.is_dma:
                        continue
                    sid = d.sem.name
                    if known.get(sid, 0) >= d.val:
                        continue
                    eng.wait_ge(d.sem, d.val)
                    known[sid] = d.val
                ins = op.fn(eng)
                if op.is_dma:
                    ins.then_inc(op.sem, 16)
                elif op.needs_inc:
                    ins.then_inc(op.sem, 1)
            if engname == "sp":
                for op in final_waits:
                    eng.wait_ge(op.sem, op.val)

        with nc.Block() as block:
            for engname in self.ENGS:
                getattr(block, engmap[engname])(lambda eng, _n=engname: run(_n, eng))

    def close(self):
        self.stack.close()


class Arena:
    def __init__(self, S, name, words):
        self.t = S.sb(name, [128, words], F32)
        self.words = words
        self.off = 0

    def reset(self):
        self.off = 0

    def alloc(self, shape, dtype=F32):
        n = int(np.prod(shape[1:]))
        w = n if dtype == F32 else (n + 1) // 2
        assert self.off + w <= self.words, ("arena overflow", self.off, w, self.words)
        v = self.t[0:shape[0], self.off:self.off + w]
        self.off += w
        if dtype != F32:
            v = v.bitcast(dtype)[:, 0:n]
        if len(shape) == 3:
            v = v.rearrange("p (a b) -> p a b", b=shape[2])
        elif len(shape) == 4:
            v = v.rearrange("p (a b c) -> p a b c", b=shape[2], c=shape[3])
        elif len(shape) == 5:
            v = v.rearrange("p (a b c d) -> p a b c d", b=shape[2], c=shape[3], d=shape[4])
        return v


def bc(ap, shape):
    return ap.to_broadcast(list(shape))


def build_program(n_layers=DEPTH, do_ffn=True, n_mix_tiles=SEQ // TT, n_ffn_tiles=SEQ // TF):
    nc = bass.Bass("TRN2", target_bir_lowering=False)
    di = lambda name, shape, dt=F32: nc.dram_tensor(name, list(shape), dt, kind="ExternalInput").ap()
    x_in = di("x", [SEQ, D])
    ccol_d = di("ccol", [128, 8])
    vecs_d = di("vecs", [128, DEPTH * NV * 8 + DEPTH * 8])
    w_ada_d = di("w_ada", [DEPTH, D, 6 * D])
    b_ada_d = di("b_ada", [DEPTH, 6 * D])
    gpm_d = di("g_post_mix", [DEPTH, D])
    gpf_d = di("g_post_ffn", [DEPTH, D])
    w_in_d = di("w_in", [DEPTH, D, 8 * D])
    bd_d = di("bd", [DEPTH, 128, 3 * 8 * 128])
    wgate_d = di("wgate", [DEPTH, 128, 24 * 8])
    bgate_d = di("bgate", [DEPTH, 8])
    wpa_d = di("w_proj_a", [DEPTH, D, D])
    wpb_d = di("w_proj_b", [DEPTH, D, D])
    wo_d = di("w_out", [DEPTH, D, D])
    if not do_ffn:
        di = lambda name, shape, dt=F32: nc.dram_tensor(name, [1, 1], dt, kind="ExternalInput").ap()
    f1_d = di("ffn_w1", [2, D, 2816])
    f3_d = di("ffn_w3", [2, D, 2816])
    f2_d = di("ffn_w2", [2, 2816, D])
    rt_d = di("router", [2, 128, 64])
    m1_d = di("moe_w1", [2, 8, D, 3584])
    m3_d = di("moe_w3", [2, 8, D, 3584])
    m2_d = di("moe_w2", [2, 8, 3584, D])
    out_d = nc.dram_tensor("out", [SEQ, D], F32, kind="ExternalOutput").ap()
    sc = lambda name, shape: nc.dram_tensor(name, list(shape), BF16).ap()
    win_s = sc("win_s", [DEPTH, 32, 128, 8, 256])
    wpa_s = sc("wpa_s", [DEPTH, 4, 128, 8, 256])
    wpb_s = sc("wpb_s", [DEPTH, 4, 128, 8, 256])
    wo_s = sc("wo_s", [DEPTH, 4, 128, 8, 256])
    f13_s = sc("f13_s", [2, 11, 128, 2, 8, 256])
    f2_s = sc("f2_s", [2, 2, 128, 22, 512])
    m13_s = sc("m13_s", [2, 8, 14, 128, 2, 8, 256])
    m2_s = sc("m2_s", [2, 8, 2, 128, 28, 512])

    S = Sched(nc)
    op = S.op
    idf = S.sb("idf", [128, 128], F32)
    idb = S.sb("idb", [128, 128], BF16)
    mask2 = S.sb("mask2", [128, 128], F32)
    maskm = S.sb("maskm", [128, 128], F32)
    mask01 = S.sb("mask01", [128, TT], F32)
    negm = S.sb("negm", [4, 128], F32)
    m01r = S.sb("m01r", [4, 128], F32)
    ones4 = S.sb("ones4", [4, 128], F32)
    sel4 = S.sb("sel4", [4, 4, 128], F32)
    id4 = S.sb("id4", [4, 4], F32)
    onesb = S.sb("onesb", [1, 128], BF16)
    vecs = S.sb("vecs", [128, DEPTH * NV * 8 + DEPTH * 8], F32)
    lbc = S.sb("lbc", [128, DEPTH, 8], F32)
    oml = S.sb("oml", [128, DEPTH, 8], F32)
    noml = S.sb("noml", [128, DEPTH, 8], F32)
    cbc = S.sb("cbc", [128, 8, 128], BF16)
    ccol = S.sb("ccol", [128, 8], F32)
    cact = S.sb("cact", [128, 8], F32)
    hsc_m = S.sb("hsc_m", [128, 8], F32)
    hbi_m = S.sb("hbi_m", [128, 8], F32)
    hsc_f = S.sb("hsc_f", [128, 8], F32)
    hbi_f = S.sb("hbi_f", [128, 8], F32)
    ggm = S.sb("ggm", [128, D], F32)
    ggf = S.sb("ggf", [128, D], F32)
    bdt = S.sb("bdt", [128, 3, 8, 128], BF16)
    wgt = S.sb("wgt", [128, 24, 8], BF16)
    bgt = S.sb("bgt", [128, 8], F32)
    rtt = S.sb("rtt", [128, 8, 8], F32)
    hst = S.sb("hst", [128, 8, 128], F32)
    Cst = S.sb("Cst", [128, 4, 2, 257], F32)
    Cbf = S.sb("Cbf", [128, 4, 2, 257], BF16)
    mprev = S.sb("mprev", [4, 1], F32)
    PB = [S.ps("pb%d" % i, [128, 512], F32) for i in range(8)]
    P7b = PB[7][:, :].bitcast(BF16)
    P7N = ["P7a", "P7b", "P7c", "P7d"]

    def p7(q, n):
        nq = (n + 255) // 256
        return P7b[:, q * 256:q * 256 + n], P7N[q:q + nq]
    ARW = 42340
    AR = Arena(S, "arena", ARW)

    def vcol(l, v):
        o = (l * NV + v) * 8
        return vecs[:, o:o + 8]

    V_GPRE_M, V_GPRE_F, V_HGN, V_CW0, V_CB, V_MGN, V_SKIP = 0, 1, 2, 3, 7, 8, 9

    op("pool", lambda e: e.memset(idf[:], 0.0), writes=["idf"])
    op("pool", lambda e: e.affine_select(out=idf[:], in_=idf[:], pattern=[[-1, 128]], compare_op=ALU.not_equal,
                                         fill=1.0, base=0, channel_multiplier=1), reads=["idf"], writes=["idf"])
    op("dve", lambda e: e.tensor_copy(out=idb[:], in_=idf[:]), reads=["idf"], writes=["idb"])
    op("pool", lambda e: e.memset(mask2[:], 1.0), writes=["mask2"])
    op("pool", lambda e: e.affine_select(out=mask2[:], in_=mask2[:], pattern=[[1, 128]], compare_op=ALU.is_ge,
                                         fill=0.0, base=0, channel_multiplier=-1), reads=["mask2"], writes=["mask2"])
    op("pool", lambda e: e.memset(mask2[0:64, 64:128], 0.0), reads=["mask2"], writes=["mask2"])
    op("pool", lambda e: e.tensor_scalar_mul(out=maskm[:], in0=mask2[:], scalar1=1.0 / 16.0), reads=["mask2"], writes=["maskm"])
    op("pool", lambda e: e.memset(mask01[:], 1.0), writes=["mask01"])
    op("pool", lambda e: e.memset(mask01[:].rearrange("p (c j) -> p c j", j=64)[:, :, 0:1], 0.0), reads=["mask01"], writes=["mask01"])
    op("pool", lambda e: e.memset(negm[:], 0.0), writes=["negm"])
    op("pool", lambda e: e.memset(negm[:].rearrange("p (c j) -> p c j", j=64)[:, :, 0:1], -1e30), reads=["negm"], writes=["negm"])
    op("pool", lambda e: e.memset(m01r[:], 1.0), writes=["m01r"])
    op("pool", lambda e: e.memset(m01r[:].rearrange("p (c j) -> p c j", j=64)[:, :, 0:1], 0.0), reads=["m01r"], writes=["m01r"])
    op("pool", lambda e: e.memset(ones4[:], 1.0), writes=["ones4"])
    op("pool", lambda e: e.tensor_copy(out=id4[:], in_=idf[0:4, 0:4]), reads=["idf"], writes=["id4"])
    op("pool", lambda e: e.tensor_copy(out=sel4[:], in_=bc(idf[0:4, 0:4].unsqueeze(2), [4, 4, 128])), reads=["idf"], writes=["sel4"])
    op("pool", lambda e: e.memset(onesb[:], 1.0), writes=["onesb"])
    S.dma("sp", vecs[:], vecs_d, writes=["vecs"])
    S.dma("sp", ccol[:], ccol_d, writes=["ccol"])
    op("act", lambda e: e.activation(out=cact[:], in_=ccol[:], func=AF.Silu), reads=["ccol"], writes=["cact"])
    op("dve", lambda e: e.tensor_copy(out=cbc[:], in_=bc(cact[:].unsqueeze(2), [128, 8, 128])), reads=["cact"], writes=["cbc"])
    lbraw = vecs[:, DEPTH * NV * 8:DEPTH * NV * 8 + DEPTH * 8].rearrange("p (l c) -> p l c", c=8)
    lbe = S.sb("lbe", [128, DEPTH, 8], F32)
    lbs = S.sb("lbs", [128, 8], F32)
    op("act", lambda e: e.activation(out=lbe[:], in_=lbraw, func=AF.Exp), reads=["vecs"], writes=["lbe"])
    op("dve", lambda e: e.tensor_add(out=lbs[:], in0=lbe[:, 0, :], in1=lbe[:, 1, :]), reads=["lbe"], writes=["lbs"])
    op("dve", lambda e: e.tensor_add(out=lbs[:], in0=lbs[:], in1=lbe[:, 2, :]), reads=["lbe", "lbs"], writes=["lbs"])
    op("dve", lambda e: e.tensor_add(out=lbs[:], in0=lbs[:], in1=lbe[:, 3, :]), reads=["lbe", "lbs"], writes=["lbs"])
    op("dve", lambda e: e.reciprocal(out=lbs[:], in_=lbs[:]), reads=["lbs"], writes=["lbs"])
    op("dve", lambda e: e.tensor_mul(out=lbe[:], in0=lbe[:], in1=bc(lbs[:].unsqueeze(1), [128, DEPTH, 8])), reads=["lbe", "lbs"], writes=["lbe"])
    op("dve", lambda e: e.memset(lbc[:, 0, :], 0.0), writes=["lbc"])
    for l in range(1, DEPTH):
        op("dve", lambda e, l=l: e.tensor_add(out=lbc[:, l, :], in0=lbc[:, l - 1, :], in1=lbe[:, l, :]), reads=["lbe", "lbc"], writes=["lbc"])
    op("dve", lambda e: e.tensor_scalar(out=oml[:], in0=lbc[:], scalar1=-1.0, scalar2=1.0, op0=ALU.mult, op1=ALU.add), reads=["lbc"], writes=["oml"])
    op("dve", lambda e: e.tensor_scalar_mul(out=noml[:], in0=oml[:], scalar1=-1.0), reads=["oml"], writes=["noml"])

    def conv_kxn(dst, src, ngroups, depname):
        v = src.rearrange("(kc p) (g c) -> g p kc c", p=128, c=256)
        for g in range(ngroups):
            S.dma("pool", dst[g], v[g], writes=[depname])

    def conv_layer(l):
        conv_kxn(win_s[l], w_in_d[l], 32, "win%d" % l)
        conv_kxn(wpa_s[l], wpa_d[l], 4, "wpa%d" % l)
        conv_kxn(wpb_s[l], wpb_d[l], 4, "wpb%d" % l)
        conv_kxn(wo_s[l], wo_d[l], 4, "wo%d" % l)
        if not do_ffn:
            return
        j = l // 2
        if l % 2 == 0:
            v1 = f1_d[j].rearrange("(kc p) (g c) -> g p kc c", p=128, c=256)
            v3 = f3_d[j].rearrange("(kc p) (g c) -> g p kc c", p=128, c=256)
            for g in range(11):
                S.dma("pool", f13_s[j, g, :, 0], v1[g], writes=["f13_%d" % l])
                S.dma("pool", f13_s[j, g, :, 1], v3[g], writes=["f13_%d" % l])
            v2 = f2_d[j].rearrange("(fc p) (h c) -> h p fc c", p=128, c=512)
            for h in range(2):
                S.dma("pool", f2_s[j, h], v2[h], writes=["f2_%d" % l])
        else:
            for ex in range(8):
                v1 = m1_d[j, ex].rearrange("(kc p) (g c) -> g p kc c", p=128, c=256)
                v3 = m3_d[j, ex].rearrange("(kc p) (g c) -> g p kc c", p=128, c=256)
                for g in range(14):
                    S.dma("pool", m13_s[j, ex, g, :, 0], v1[g], writes=["m13_%d_%d" % (l, ex)])
                    S.dma("pool", m13_s[j, ex, g, :, 1], v3[g], writes=["m13_%d_%d" % (l, ex)])
                v2 = m2_d[j, ex].rearrange("(fc p) (h c) -> h p fc c", p=128, c=512)
                for h in range(2):
                    S.dma("pool", m2_s[j, ex, h], v2[h], writes=["m2_%d_%d" % (l, ex)])

    for l in range(n_layers):
        conv_layer(l)

    def rstd_from_ss(rs, ss, n, dep_ss, dep_rs):
        op("dve", lambda e: e.tensor_scalar(out=rs, in0=ss, scalar1=1.0 / n, scalar2=EPS, op0=ALU.mult, op1=ALU.add), reads=[dep_ss], writes=[dep_rs])
        op("act", lambda e: e.activation(out=rs, in_=rs, func=AF.Ln), reads=[dep_rs], writes=[dep_rs])
        op("act", lambda e: e.activation(out=rs, in_=rs, func=AF.Exp, scale=-0.5), reads=[dep_rs], writes=[dep_rs])

    xkeys = {}
    last_x_dma = {}

    def xsrc(l, first):
        return x_in if (l == 0 and first) else out_d

    def layer_prologue(l):
        AR.reset()
        S.barrier()
        wad = [AR.alloc([128, 8, 512], BF16) for _ in range(2)]
        bad = AR.alloc([1, 6 * D], BF16)
        gpb = [AR.alloc([128, D], F32) for _ in range(2)]
        tmpd = AR.alloc([128, 4, 128], F32)
        S.dma("pool", bad, b_ada_d[l:l + 1, :], writes=["bad"])
        S.dma("sp", gpb[0], gpm_d[l].partition_broadcast(128), writes=["gpb0"])
        S.dma("sp", gpb[1], gpf_d[l].partition_broadcast(128), writes=["gpb1"])
        S.dma("pool", bdt[:].rearrange("p a b c -> p (a b c)"), bd_d[l], writes=["bdt"])
        S.dma("pool", wgt[:].rearrange("p a b -> p (a b)"), wgate_d[l], writes=["wgt"])
        S.dma("sp", bgt[:], bgate_d[l].partition_broadcast(128), writes=["bgt"])
        if l % 2 == 1:
            S.dma("sp", rtt[:].rearrange("p a b -> p (a b)"), rt_d[l // 2], writes=["rtt"])
        wv = w_ada_d[l].rearrange("(kc p) (g c) -> g p kc c", p=128, c=512)
        for g in range(12):
            w = wad[g % 2]
            wn = "wad%d" % (g % 2)
            S.dma("pool", w, wv[g], writes=[wn], key="pl3_%d" % (g % 2))
            pb = PB[g % 2]
            pn = ["P%da" % (g % 2), "P%db" % (g % 2)]
            for kc in range(8):
                op("pe", lambda e, w=w, kc=kc, pb=pb: e.matmul(pb[:, :], lhsT=cbc[:, kc, :], rhs=w[:, kc, :], start=(kc == 0), stop=False),
                   reads=[wn, "cbc"], writes=pn)
            op("pe", lambda e, g=g, pb=pb: e.matmul(pb[:, :], lhsT=onesb[0:1, :], rhs=bad[0:1, g * 512:(g + 1) * 512], start=False, stop=True),
               reads=["bad", "onesb"], writes=pn)
            which = g // 2
            half = g % 2
            if which in (2, 5):
                dst = ggm if which == 2 else ggf
                dn = "ggm" if which == 2 else "ggf"
                gp = gpb[0] if which == 2 else gpb[1]
                gn = "gpb0" if which == 2 else "gpb1"
                op("dve", lambda e, pb=pb, dst=dst, gp=gp, half=half: e.tensor_mul(out=dst[:, half * 512:(half + 1) * 512], in0=pb[:, :], in1=gp[:, half * 512:(half + 1) * 512]),
                   reads=pn + [gn], writes=[dn])
            else:
                dst = {0: hbi_m, 1: hsc_m, 3: hbi_f, 4: hsc_f}[which]
                dn = {0: "hbi_m", 1: "hsc_m", 3: "hbi_f", 4: "hsc_f"}[which]
                op("dve", lambda e, pb=pb: e.tensor_mul(out=tmpd, in0=pb[:, :].rearrange("p (c j) -> p c j", j=128), in1=bc(idf[:].unsqueeze(1), [128, 4, 128])),
                   reads=pn + ["idf"], writes=["tmpd"])
                op("dve", lambda e, dst=dst, half=half: e.tensor_reduce(out=dst[:, half * 4:(half + 1) * 4], in_=tmpd, axis=AX.X, op=ALU.add),
                   reads=["tmpd"], writes=[dn])
        for (hs, hn, vi) in ((hsc_m, "hsc_m", V_GPRE_M), (hsc_f, "hsc_f", V_GPRE_F)):
            op("dve", lambda e, hs=hs, vi=vi: e.scalar_tensor_tensor(out=hs[:], in0=hs[:], scalar=1.0, in1=vcol(l, vi), op0=ALU.add, op1=ALU.mult),
               reads=[hn, "vecs"], writes=[hn])
        op("pool", lambda e: e.memset(hst[:], 0.0), writes=["hst"])
        op("pool", lambda e: e.memset(Cst[:], 0.0), writes=["Cst"])
        op("pool", lambda e: e.memset(Cbf[:], 0.0), writes=["Cbf"])
        op("pool", lambda e: e.memset(mprev[:], 0.0), writes=["mprev"])

    def load_norm_transpose(l, first, t0, nsub, hsc, hbi, hscn, hbin, xt, xn, hT, junk, ss, rs, hTf=None, xnn="xn", hTfn="hTf", junkn="junk"):
        src = xsrc(l, first)
        op("dve", lambda e: e.memset(ss, 0.0), writes=["ss"])
        for s in range(nsub):
            blk = (t0 + s * 128) // TT
            S.dma("sp", xt[:, s, :], src[t0 + s * 128:t0 + (s + 1) * 128, :], reads=["xrow%d" % blk], writes=["xt%d" % s], key="xl%d" % s)
            op("act", lambda e, s=s: e.activation(out=junk, in_=xt[:, s, :], func=AF.Square, accum_out=ss[:, s:s + 1]),
               reads=["xt%d" % s], writes=[junkn, "ss"])
        rstd_from_ss(rs, ss, D, "ss", "rs")
        dt = F32 if hTf is not None else BF16
        for s in range(nsub):
            op("dve", lambda e, s=s: e.tensor_scalar(out=xn[:, s, :], in0=xt[:, s, :], scalar1=rs[:, s:s + 1], scalar2=None, op0=ALU.mult),
               reads=["xt%d" % s, "rs"], writes=[xnn])
        idt = idf if hTf is not None else idb
        n = nsub * 128
        for dc in range(8):
            if hTf is not None:
                pt = PB[6 + dc % 2][:, 0:n]
                pn = ["P6a", "P6b"] if dc % 2 == 0 else P7N
            else:
                pt, pn = p7((dc % 2) * 2, n)
            for s in range(nsub):
                op("pe", lambda e, s=s, dc=dc, pt=pt: e.transpose(out=pt[:, s * 128:(s + 1) * 128], in_=xn[:, s, dc * 128:(dc + 1) * 128], identity=idt[:]),
                   reads=[xnn, "idb", "idf"], writes=pn)
            tgt = hTf if hTf is not None else hT
            tgn = hTfn if hTf is not None else "hT"
            op("act", lambda e, dc=dc, pt=pt, tgt=tgt: e.activation(out=tgt[:, dc, :], in_=pt, func=AF.Identity, scale=hsc[:, dc:dc + 1], bias=hbi[:, dc:dc + 1]),
               reads=pn + [hscn, hbin], writes=[tgn])
        if hTf is not None:
            op("dve", lambda e: e.tensor_copy(out=hT, in_=hTf), reads=[hTfn], writes=["hT"])

    def residual_update(l, t0, s, ypsum_list, ypn, xt, ysb, tmpx, junk, ssy, rsy, gg, ggn, yacc=None, ysbn="ysb", tmpxn="tmpx", junkn="junk"):
        if yacc is None:
            for (pa, c0, ncol) in ypsum_list:
                op("act", lambda e, pa=pa, c0=c0, ncol=ncol: e.copy(out=ysb[:, c0:c0 + ncol], in_=pa), reads=ypn, writes=[ysbn])
            ysrc, ysn = ysb, ysbn
        else:
            ysrc, ysn = yacc, "yacc"
        op("dve", lambda e: e.memset(ssy[:, 0:1], 0.0), writes=["ssy"])
        op("act", lambda e: e.activation(out=junk, in_=ysrc, func=AF.Square, accum_out=ssy[:, 0:1]), reads=[ysn], writes=[junkn, "ssy"])
        rstd_from_ss(rsy[:, 0:1], ssy[:, 0:1], D, "ssy", "rsy")
        op("dve", lambda e: e.scalar_tensor_tensor(out=tmpx, in0=ysrc, scalar=rsy[:, 0:1], in1=gg[:], op0=ALU.mult, op1=ALU.mult),
           reads=[ysn, "rsy", ggn], writes=[tmpxn])
        op("dve", lambda e: e.tensor_add(out=xt[:, s, :], in0=xt[:, s, :], in1=tmpx), reads=[tmpxn, "xt%d" % s], writes=["xt%d" % s])
        blk = (t0 + s * 128) // TT
        d = S.dma("sp", out_d[t0 + s * 128:t0 + (s + 1) * 128, :], xt[:, s, :], reads=["xt%d" % s], writes=["xrow%d" % blk], key="xs%d" % s)
        last_x_dma["xs%d" % s] = d

    def mixer(l):
        AR.reset()
        S.barrier()
        A = AR.alloc
        xt = A([128, 2, D]); xn = A([128, 2, D], BF16); hT = A([128, 8, TT], BF16)
        ss = A([128, 2]); rs = A([128, 2]); ssy = A([128, 2]); rsy = A([128, 2])
        NWG = 4
        wg = [A([128, 8, 256], BF16) for _ in range(NWG)]
        qs = A([128, 8, TT], BF16)
        T1 = A([128, 8, TT]); T2 = A([128, 8, TT]); T3 = A([128, 8, TT]); T4 = A([128, 8, TT])
        QEO = A([128, 8, 2, 2, 128], BF16)
        KT = A([128, 8, TT], BF16)
        Ktok = A([128, 8, 2, 128], BF16)
        vtok = A([128, 2, D], BF16)
        sga = A([128, 8, TT], BF16)
        yaT = A([128, 8, TT], BF16)
        E1 = A([128, 8, 4]); E2 = A([128, 8, 4]); E3 = A([128, 8, 4]); dE = A([128, 8, 4])
        STb8 = A([128, 8, 128], BF16)
        STb = A([128, 2, 128], BF16)
        ssq = A([128, 8]); rsq = A([128, 8])
        xm = A([128, 8, 3 + TT])
        xcT = A([128, 8, TT], BF16); xmT = A([128, 8, TT], BF16)
        qT = A([128, 8, TT], BF16); kT = A([128, 8, TT], BF16); vT = A([128, 8, TT], BF16)
        ktok = A([128, 2, D], BF16)
        vext = A([128, 2, 4, 257], BF16)
        sob = A([128, 8, TT], BF16); sgA = A([128, 8, TT], BF16); sgB = A([128, 8, TT], BF16)
        DT = A([128, 2, 128]); DTm = DT
        kw = A([128, 2, 256], BF16)
        tnum = A([128, 257])
        hnb = A([128, D], BF16)
        junk = hnb
        onb = hnb
        ybT = A([128, 8, TT], BF16)
        mixT = A([128, 8, TT], BF16)
        gpre = A([128, 8]); gli = A([128, 4]); glf = A([128, 4])
        rows = A([4, 8, 128])
        gcol = A([128, 2, 16])
        decb = A([128, 2, 2, 4])
        dexp = A([4, 4, 2])
        sm = A([128, 32])
        acc = T1; o_sb = T2.rearrange("p a b -> p (a b)")[:, 0:D]; sqb = T3.rearrange("p a b -> p (a b)")[:, 0:D]
        tot = T4.rearrange("p a b -> p (a b)")[:, 0:4 * 257].rearrange("p (a b) -> p a b", b=257)
        sqm = T3.rearrange("p a b -> p (a b)")[:, 0:D].rearrange("p (a b) -> p a b", b=256)
        ysb = T1.rearrange("p a b -> p (a b)")[:, 0:D]
        tmpx = T2.rearrange("p a b -> p (a b)")[:, 0:D]
        m1 = T3.rearrange("p a b -> p (a b)")[:, 0:2 * TT].rearrange("p (a b) -> p a b", b=TT)
        m2 = T4.rearrange("p a b -> p (a b)")[:, 0:2 * TT].rearrange("p (a b) -> p a b", b=TT)
        clb = T1.rearrange("p a b -> p (a b)")[:, 0:1024].rearrange("p (h t) -> p h t", t=128)
        tkv8 = T1.rearrange("p a b -> p (a b)")[:, 1024:2048].rearrange("p (h t) -> p h t", t=128)
        hsb8 = T3.rearrange("p a b -> p (a b)")[:, 0:1024].bitcast(BF16).rearrange("p (e h t) -> p e h t", e=2, t=128)

        print("mixer arena words", AR.off)
        op("pool", lambda e: e.memset(xm[:, :, 0:3], 0.0), writes=["xm"])
        op("pool", lambda e: e.memset(QEO, 0.0), writes=["QEO"])
        op("pool", lambda e: e.memset(vext[:, :, :, 256:257], 1.0), writes=["vext"])

        NPP = 6
        pp_names = ["P%d" % i for i in range(NPP)]

        def pp_view(i):
            return PB[i][:, 0:256]

        state = {"pp": 0, "wg": 0}

        def next_pp():
            i = state["pp"] % NPP
            state["pp"] += 1
            return pp_view(i), [pp_names[i]]

        def load_wg(src_ap, depname):
            i = state["wg"] % NWG
            state["wg"] += 1
            S.dma("sp", wg[i], src_ap, reads=[depname], writes=["wg%d" % i], key="wg%d" % i)
            return wg[i], "wg%d" % i

        for ti in range(n_mix_tiles):
            t0 = ti * TT
            load_norm_transpose(l, True, t0, 2, hsc_m, hbi_m, "hsc_m", "hbi_m", xt, xn, hT, junk, ss, rs, junkn="hnb")
            def fm_chunk(w, wn, half, rhs_t, rhs_n, evac):
                pv, pn = next_pp()
                for kc in range(8):
                    op("pe", lambda e, kc=kc, pv=pv: e.matmul(pv, lhsT=w[:, kc, half * 128:(half + 1) * 128], rhs=rhs_t[:, kc, :], start=(kc == 0), stop=(kc == 7)),
                       reads=[wn, rhs_n], writes=pn)
                evac(pv, pn)

            QA, QB = [], []
            S.defer_to = QA
            for hd in range(8):
                op("dve", lambda e, hd=hd: e.tensor_scalar(out=T4[:, hd, :], in0=T1[:, hd, :], scalar1=noml[:, l, hd:hd + 1], scalar2=oml[:, l, hd:hd + 1], op0=ALU.mult, op1=ALU.add),
                   reads=["T1", "noml", "oml"], writes=["T4"])
            for hd in range(8):
                op("act", lambda e, hd=hd: e.activation(out=T2[:, hd, :], in_=T1[:, hd, :], func=AF.Ln, scale=oml[:, l, hd:hd + 1], bias=lbc[:, l, hd:hd + 1]),
                   reads=["T1", "oml", "lbc"], writes=["T2"])
            for hd in range(8):
                op("dve", lambda e, hd=hd: e.tensor_tensor_scan(out=T3[:, hd, :], data0=mask01[:], data1=T2[:, hd, :], initial=0.0, op0=ALU.mult, op1=ALU.add),
                   reads=["T2", "mask01"], writes=["T3"])
            b4 = T3.rearrange("p h (c j) -> p h c j", j=64)
            op("dve", lambda e: e.tensor_sub(out=dE, in0=b4[:, :, :, 63], in1=b4[:, :, :, 31]), reads=["T3"], writes=["dE"])
            op("act", lambda e: e.activation(out=E1, in_=b4[:, :, :, 63], func=AF.Exp), reads=["T3"], writes=["E1"])
            op("act", lambda e: e.activation(out=E2, in_=dE, func=AF.Exp), reads=["dE"], writes=["E2"])
            op("act", lambda e: e.activation(out=E3, in_=b4[:, :, :, 31], func=AF.Exp), reads=["T3"], writes=["E3"])
            op("dve", lambda e: e.tensor_sub(out=T2.rearrange("p h (c j) -> p h c j", j=64), in0=b4, in1=bc(b4[:, :, :, 31:32], [128, 8, 4, 64])),
               reads=["T3", "T2"], writes=["T2"])
            op("act", lambda e: e.activation(out=T1, in_=T2, func=AF.Exp), reads=["T2", "T1"], writes=["T1"])
            op("act", lambda e: e.activation(out=T3, in_=T2, func=AF.Exp, scale=-1.0), reads=["T2", "E1", "E3", "dE"], writes=["T3"])
            for hd in range(8):
                qo = QEO[:, hd].rearrange("p a b c -> p (a b c)")
                for pr in range(2):
                    for eo in range(2):
                        c = pr * 2 + eo
                        o = pr * 256 + eo * 192
                        op("dve", lambda e, hd=hd, c=c, o=o, qo=qo: e.tensor_mul(out=qo[:, o:o + 64], in0=qs[:, hd, c * 64:(c + 1) * 64], in1=T1[:, hd, c * 64:(c + 1) * 64]),
                           reads=["qs", "T1"], writes=["QEO"])
            op("dve", lambda e: e.tensor_mul(out=KT, in0=T4, in1=T3), reads=["T4", "T3"], writes=["KT"])
            S.defer_to = QB
            for dc in range(8):
                op("dve", lambda e, dc=dc: e.tensor_scalar(out=acc[:, dc, :], in0=xm[:, dc, 3:3 + TT], scalar1=vcol(l, V_CW0 + 3)[:, dc:dc + 1], scalar2=vcol(l, V_CB)[:, dc:dc + 1], op0=ALU.mult, op1=ALU.add),
                   reads=["xm", "vecs", "T1"], writes=["T1"])
                for j in range(3):
                    op("dve", lambda e, dc=dc, j=j: e.scalar_tensor_tensor(out=acc[:, dc, :], in0=xm[:, dc, j:j + TT], scalar=vcol(l, V_CW0 + j)[:, dc:dc + 1], in1=acc[:, dc, :], op0=ALU.mult, op1=ALU.add),
                       reads=["xm", "vecs", "T1"], writes=["T1"])
            op("act", lambda e: e.activation(out=xcT, in_=acc, func=AF.Silu), reads=["T1"], writes=["xcT"])
            op("act", lambda e: e.copy(out=xmT, in_=xm[:, :, 3:3 + TT]), reads=["xm"], writes=["xmT"])
            op("pool", lambda e: e.tensor_copy(out=xm[:, :, 0:3], in_=xm[:, :, TT:TT + 3]), reads=["xm"], writes=["xm"])
            S.defer_to = None
            for gi, grp in enumerate((0, 1, 2, 3, 12, 13, 14, 15, 4, 5, 6, 7, 16, 17, 18, 19, 8, 9, 10, 11, 20, 21, 22, 23, 24, 25, 26, 27, 28, 29, 30, 31)):
                if gi >= 12:
                    S.drain(QA, 8 if gi < 20 else len(QA))
                if gi >= 20:
                    S.drain(QB, 5)
                w, wn = load_wg(win_s[l, grp], "win%d" % l)
                if 8 <= grp < 12:
                    for s in range(2):
                        pv, pn = next_pp()
                        for kc in range(8):
                            op("pe", lambda e, kc=kc, pv=pv, s=s, w=w: e.matmul(pv, lhsT=hT[:, kc, s * 128:(s + 1) * 128], rhs=w[:, kc, :], start=(kc == 0), stop=(kc == 7)),
                               reads=[wn, "hT"], writes=pn)
                        c0 = (grp - 8) * 256
                        op("dve", lambda e, pv=pv, s=s, c0=c0: e.tensor_copy(out=vtok[:, s, c0:c0 + 256], in_=pv), reads=pn, writes=["vtok"])
                    continue
                for half in range(2):
                    m = grp * 2 + half
                    kind, hd = m // 8, m % 8
                    if kind == 0:
                        ev = lambda pv, pn, hd=hd: op("act", lambda e: e.activation(out=qs[:, hd, :], in_=pv, func=AF.Silu), reads=pn, writes=["qs"])
                    elif kind == 1:
                        ev = lambda pv, pn, hd=hd: op("act", lambda e: e.activation(out=T1[:, hd, :], in_=pv, func=AF.Sigmoid), reads=pn, writes=["T1"])
                    elif kind == 3:
                        ev = lambda pv, pn, hd=hd: op("act", lambda e: e.activation(out=sga[:, hd, :], in_=pv, func=AF.Silu), reads=pn, writes=["sga"])
                    elif kind == 4:
                        ev = lambda pv, pn, hd=hd: op("dve", lambda e: e.tensor_copy(out=xm[:, hd, 3:3 + TT], in_=pv), reads=pn, writes=["xm"])
                    elif kind == 5:
                        ev = lambda pv, pn, hd=hd: op("act", lambda e: e.activation(out=sob[:, hd, :], in_=pv, func=AF.Sigmoid), reads=pn, writes=["sob"])
                    elif kind == 6:
                        ev = lambda pv, pn, hd=hd: op("act", lambda e: e.activation(out=sgA[:, hd, :], in_=pv, func=AF.Sigmoid), reads=pn, writes=["sgA"])
                    else:
                        ev = lambda pv, pn, hd=hd: op("act", lambda e: e.activation(out=sgB[:, hd, :], in_=pv, func=AF.Sigmoid), reads=pn, writes=["sgB"])
                    fm_chunk(w, wn, half, hT, "hT", ev)

            S.drain(QA, len(QA))
            S.drain(QB, len(QB))
            for hd in range(8):
                pt, pn = p7(2 + hd % 2, 256)
                for pr in range(2):
                    op("pe", lambda e, hd=hd, pr=pr, pt=pt: e.transpose(out=pt[:, pr * 128:(pr + 1) * 128], in_=KT[:, hd, pr * 128:(pr + 1) * 128], identity=idb[:]),
                       reads=["KT", "idb"], writes=pn)
                op("act", lambda e, hd=hd, pt=pt: e.copy(out=Ktok[:, hd].rearrange("p a b -> p (a b)"), in_=pt), reads=pn, writes=["Ktok"])
            for (mi, srcT, srcn, dstT, dstn) in ((0, xcT, "xcT", qT, "qT"), (1, xcT, "xcT", kT, "kT"), (2, xmT, "xmT", vT, "vT")):
                for dc in range(8):
                    pv, pn = next_pp()
                    op("pe", lambda e, mi=mi, dc=dc, pv=pv, srcT=srcT: e.matmul(pv, lhsT=bdt[:, mi, dc, :], rhs=srcT[:, dc, :], start=True, stop=True),
                       reads=["bdt", srcn], writes=pn)
                    eng = "act" if dc % 2 == 0 else "dve"
                    if eng == "act":
                        op("act", lambda e, dc=dc, pv=pv, dstT=dstT: e.copy(out=dstT[:, dc, :], in_=pv), reads=pn, writes=[dstn])
                    else:
                        op("dve", lambda e, dc=dc, pv=pv, dstT=dstT: e.tensor_copy(out=dstT[:, dc, :], in_=pv), reads=pn, writes=[dstn])
            for pr in range(2):
                for (mi, srcT, srcn) in ((1, xcT, "xcT"), (2, xmT, "xmT")):
                    for hb in range(2):
                        pbk = PB[2]
                        pn = ["P2a", "P2b", "P2c", "P2d"]
                        for j in range(4):
                            dc = hb * 4 + j
                            op("pe", lambda e, mi=mi, dc=dc, j=j, srcT=srcT, pr=pr: e.matmul(pbk[:, j * 128:(j + 1) * 128], lhsT=srcT[:, dc, pr * 128:(pr + 1) * 128], rhs=bdt[:, mi, dc, :], start=True, stop=True),
                               reads=["bdt", srcn], writes=pn)
                        if mi == 1:
                            op("act", lambda e, pr=pr, hb=hb: e.copy(out=ktok[:, pr, hb * 512:(hb + 1) * 512], in_=pbk[:, :]), reads=pn, writes=["ktok"])
                        else:
                            op("dve", lambda e, pr=pr, hb=hb: e.tensor_copy(out=vext[:, pr, hb * 2:hb * 2 + 2, 0:256], in_=pbk[:, :].rearrange("p (a b) -> p a b", b=256)), reads=pn, writes=["vext"])
            for pr in range(2):
                pg = PB[5][:, 256:264]
                pgn = ["P5c"]
                i = 0
                for (srcT, srcn) in ((qT, "qT"), (kT, "kT"), (vT, "vT")):
                    for dc in range(8):
                        op("pe", lambda e, srcT=srcT, dc=dc, i=i, pr=pr: e.matmul(pg, lhsT=srcT[:, dc, pr * 128:(pr + 1) * 128], rhs=wgt[:, i, :], start=(i == 0), stop=(i == 23)),
                           reads=[srcn, "wgt"], writes=pgn)
                        i += 1
                op("dve", lambda e: e.tensor_add(out=gpre, in0=pg, in1=bgt[:]), reads=pgn + ["bgt"], writes=["gpre"])
                op("act", lambda e: e.activation(out=glf, in_=gpre[:, 4:8], func=AF.Exp, scale=-1.0), reads=["gpre"], writes=["glf"])
                op("act", lambda e: e.activation(out=glf, in_=glf, func=AF.Ln, bias=1.0), reads=["glf"], writes=["glf"])
                op("dve", lambda e: e.tensor_scalar_mul(out=glf, in0=glf, scalar1=-1.0), reads=["glf"], writes=["glf"])
                op("dve", lambda e: e.tensor_copy(out=gli, in_=gpre[:, 0:4]), reads=["gpre"], writes=["gli"])
                prw = PB[5][0:4, 384:512]
                prn = ["P5d"]
                op("pe", lambda e: e.matmul(prw, lhsT=gli, rhs=idf[:], start=True, stop=True), reads=["gli", "idf"], writes=prn)
                op("dve", lambda e: e.tensor_copy(out=rows[:, 0, :], in_=prw), reads=prn, writes=["rows"])
                op("pe", lambda e: e.matmul(prw, lhsT=glf, rhs=idf[:], start=True, stop=True), reads=["glf", "idf"], writes=prn)
                op("dve", lambda e: e.tensor_copy(out=rows[:, 1, :], in_=prw), reads=prn, writes=["rows"])
                op("dve", lambda e: e.tensor_tensor_scan(out=rows[:, 2, :], data0=m01r[:], data1=rows[:, 1, :], initial=0.0, op0=ALU.mult, op1=ALU.add), reads=["rows", "m01r"], writes=["rows"])
                op("dve", lambda e: e.tensor_sub(out=rows[:, 3, :], in0=rows[:, 0, :], in1=rows[:, 2, :]), reads=["rows"], writes=["rows"])
                op("dve", lambda e: e.tensor_tensor_scan(out=rows[:, 4, :], data0=negm[:], data1=rows[:, 3, :], initial=-1e30, op0=ALU.add, op1=ALU.max), reads=["rows", "negm"], writes=["rows"])
                for eo in range(2):
                    cs = slice(eo * 64, (eo + 1) * 64)
                    op("dve", lambda e, cs=cs: e.tensor_scalar(out=rows[:, 5, cs], in0=rows[:, 4, cs], scalar1=mprev[:, 0:1], scalar2=None, op0=ALU.max), reads=["rows", "mprev"], writes=["rows"])
                    op("dve", lambda e, cs=cs: e.tensor_scalar(out=rows[:, 6, cs], in0=rows[:, 5, cs], scalar1=mprev[:, 0:1], scalar2=-1.0, op0=ALU.subtract, op1=ALU.mult), reads=["rows", "mprev"], writes=["rows"])
                    last = eo * 64 + 63
                    op("dve", lambda e, cs=cs, last=last: e.tensor_scalar(out=rows[:, 7, cs], in0=rows[:, 3, cs], scalar1=rows[:, 5, last:last + 1], scalar2=None, op0=ALU.subtract), reads=["rows"], writes=["rows"])
                    op("dve", lambda e, last=last: e.tensor_add(out=mprev[:, 0:1], in0=rows[:, 2, last:last + 1], in1=rows[:, 5, last:last + 1]), reads=["rows", "mprev"], writes=["mprev"])
                op("dve", lambda e: e.scalar_tensor_tensor(out=rows[:, 1, :], in0=rows[:, 2, :], scalar=-1.0, in1=rows[:, 5, :], op0=ALU.mult, op1=ALU.subtract), reads=["rows"], writes=["rows"])
                op("act", lambda e: e.activation(out=rows[:, 6, :], in_=rows[:, 6, :], func=AF.Exp), reads=["rows"], writes=["rows"])
                op("act", lambda e: e.activation(out=rows[:, 1, :], in_=rows[:, 1, :], func=AF.Exp), reads=["rows"], writes=["rows"])
                op("act", lambda e: e.activation(out=rows[:, 7, :], in_=rows[:, 7, :], func=AF.Exp, bias=float(-np.log(16.0))), reads=["rows"], writes=["rows"])
                pcl = PB[5][:, 264:280]
                for qi, ri in enumerate((3, 6, 1, 7)):
                    op("pe", lambda e, qi=qi, ri=ri: e.matmul(pcl[:, qi * 4:(qi + 1) * 4], lhsT=rows[:, ri, :], rhs=id4[:], start=True, stop=True), reads=["rows", "id4"], writes=pgn)
                op("dve", lambda e, pr=pr: e.tensor_copy(out=gcol[:, pr, :], in_=pcl), reads=pgn, writes=["gcol"])
                wl = rows[:, 6, :].rearrange("p (c j) -> p c j", j=64)[:, :, 63]
                op("dve", lambda e, wl=wl: e.tensor_mul(out=dexp, in0=bc(wl.unsqueeze(1), [4, 4, 2]), in1=bc(id4[:].unsqueeze(2), [4, 4, 2])), reads=["rows", "id4"], writes=["dexp"])
                pdc = PB[5][:, 280:288]
                op("pe", lambda e: e.matmul(pdc, lhsT=ones4[:], rhs=dexp.rearrange("p a b -> p (a b)"), start=True, stop=True), reads=["dexp", "ones4"], writes=pgn)
                op("dve", lambda e, pr=pr: e.tensor_copy(out=decb[:, pr].rearrange("p e h -> p h e"), in_=pdc.rearrange("p (h e) -> p h e", e=2)), reads=pgn, writes=["decb"])
                for hd in range(8):
                    pv = PB[hd // 4][:, (hd % 4) * 128:(hd % 4) * 128 + 128]
                    pn = ["P%d" % (hd // 4)]
                    op("pe", lambda e, hd=hd, pv=pv: e.matmul(pv, lhsT=KT[:, hd, pr * 128:(pr + 1) * 128], rhs=QEO[:, hd, pr, 0, :], start=True, stop=False), reads=["KT", "QEO"], writes=pn)
                    op("pe", lambda e, hd=hd, pv=pv: e.matmul(pv, lhsT=KT[:, hd, pr * 128:(pr + 1) * 128], rhs=QEO[:, hd, pr, 1, :], start=False, stop=True), reads=["KT", "QEO"], writes=pn)
                for hf in range(2):
                    op("dve", lambda e, hf=hf: e.tensor_scalar(out=clb[:, hf * 4:(hf + 1) * 4, :], in0=PB[hf][:, :].rearrange("p (h t) -> p h t", t=128), scalar1=1e30, scalar2=-1e30, op0=ALU.min, op1=ALU.max),
                       reads=["P%d" % hf], writes=["clb%d" % hf, "T1"])
                op("dve", lambda e: e.tensor_mul(out=STb8, in0=clb, in1=bc(mask2[:].unsqueeze(1), [128, 8, 128])), reads=["clb0", "clb1", "mask2"], writes=["STb8"])
                for eo in range(2):
                    c = pr * 2 + eo
                    op("dve", lambda e, eo=eo, c=c: e.tensor_mul(out=hsb8[:, eo], in0=hst[:], in1=bc(E3[:, :, c:c + 1], [128, 8, 128])), reads=["hst", "E3"], writes=["hsb8_%d" % eo, "T3"])
                    for hd in range(8):
                        pkv = PB[5 + hd // 4][:, (hd % 4) * 128:(hd % 4) * 128 + 128]
                        op("pe", lambda e, hd=hd, eo=eo, pkv=pkv: e.matmul(pkv, lhsT=Ktok[eo * 64:(eo + 1) * 64, hd, pr, :], rhs=vtok[eo * 64:(eo + 1) * 64, pr, hd * 128:(hd + 1) * 128], start=True, stop=True),
                           reads=["Ktok", "vtok"], writes=["P%d" % (5 + hd // 4)])
                    for hf in range(2):
                        op("dve", lambda e, hf=hf, c=c: e.tensor_mul(out=tkv8[:, hf * 4:(hf + 1) * 4, :], in0=PB[5 + hf][:, :].rearrange("p (h t) -> p h t", t=128), in1=bc(E2[:, hf * 4:(hf + 1) * 4, c:c + 1], [128, 4, 128])),
                           reads=["P%d" % (5 + hf), "E2"], writes=["tkv8_%d" % hf, "T1"])
                    op("dve", lambda e, c=c: e.tensor_mul(out=hst[:], in0=hst[:], in1=bc(E1[:, :, c:c + 1], [128, 8, 128])), reads=["hst", "E1"], writes=["hst"])
                    op("dve", lambda e: e.tensor_add(out=hst[:], in0=hst[:], in1=tkv8), reads=["hst", "tkv8_0", "tkv8_1"], writes=["hst"])
                for hd in range(8):
                    po = PB[3 + hd // 4][:, (hd % 4) * 128:(hd % 4) * 128 + 128]
                    pon = ["P3" if hd < 4 else "P4"]
                    op("pe", lambda e, hd=hd, po=po: e.matmul(po, lhsT=QEO[:, hd, pr, 0, :], rhs=hsb8[:, 0, hd, :], start=True, stop=False), reads=["QEO", "hsb8_0"], writes=pon)
                    op("pe", lambda e, hd=hd, po=po: e.matmul(po, lhsT=QEO[:, hd, pr, 1, :], rhs=hsb8[:, 1, hd, :], start=False, stop=False), reads=["QEO", "hsb8_1"], writes=pon)
                    op("pe", lambda e, hd=hd, po=po: e.matmul(po, lhsT=STb8[:, hd, :], rhs=vtok[:, pr, hd * 128:(hd + 1) * 128], start=False, stop=True), reads=["STb8", "vtok"], writes=pon)
                op("act", lambda e: e.copy(out=o_sb[:, 0:512], in_=PB[3][:, :]), reads=["P3"], writes=["T2"])
                op("act", lambda e: e.copy(out=o_sb[:, 512:1024], in_=PB[4][:, :]), reads=["P4"], writes=["T2"])
                op("act", lambda e: e.activation(out=sqb, in_=o_sb, func=AF.Square), reads=["T2"], writes=["T3"])
                op("dve", lambda e: e.tensor_reduce(out=ssq, in_=sqb.rearrange("p (h v) -> p h v", v=128), axis=AX.X, op=ALU.add), reads=["T3"], writes=["ssq"])
                rstd_from_ss(rsq, ssq, 128, "ssq", "rsq")
                op("dve", lambda e: e.tensor_mul(out=onb.rearrange("p (h v) -> p h v", v=128), in0=o_sb.rearrange("p (h v) -> p h v", v=128), in1=bc(rsq.unsqueeze(2), [128, 8, 128])),
                   reads=["T2", "rsq"], writes=["hnb"])
                for hd in range(8):
                    pt, pn = p7((hd % 2) * 2, 128)
                    op("pe", lambda e, hd=hd, pt=pt: e.transpose(out=pt, in_=onb[:, hd * 128:(hd + 1) * 128], identity=idb[:]), reads=["hnb", "idb"], writes=pn)
                    op("dve", lambda e, hd=hd, pt=pt: e.scalar_tensor_tensor(out=yaT[:, hd, pr * 128:(pr + 1) * 128], in0=pt, scalar=vcol(l, V_HGN)[:, hd:hd + 1], in1=sga[:, hd, pr * 128:(pr + 1) * 128], op0=ALU.mult, op1=ALU.mult),
                       reads=pn + ["vecs", "sga"], writes=["yaT"])
                for h in range(4):
                    psc = PB[5][:, 0:128]
                    pM = PB[2][:, 0:128]
                    op("pe", lambda e, h=h: e.matmul(psc, lhsT=kT[:, 2 * h, pr * 128:(pr + 1) * 128], rhs=qT[:, 2 * h, pr * 128:(pr + 1) * 128], start=True, stop=False), reads=["kT", "qT"], writes=["P5a"])
                    op("pe", lambda e, h=h: e.matmul(psc, lhsT=kT[:, 2 * h + 1, pr * 128:(pr + 1) * 128], rhs=qT[:, 2 * h + 1, pr * 128:(pr + 1) * 128], start=False, stop=True), reads=["kT", "qT"], writes=["P5a"])
                    op("pe", lambda e, h=h: e.matmul(pM, lhsT=sel4[:, h, :], rhs=rows[:, 5, :], start=True, stop=True), reads=["sel4", "rows"], writes=["P2"])
                    op("act", lambda e, h=h: e.activation(out=DT[:, h % 2, :], in_=pM, func=AF.Exp, scale=-1.0, bias=gcol[:, pr, h:h + 1]), reads=["P2", "gcol"], writes=["DT%d" % (h % 2)])
                    op("dve", lambda e, h=h: e.tensor_mul(out=DTm[:, h % 2, :], in0=DT[:, h % 2, :], in1=maskm[:]), reads=["DT%d" % (h % 2), "maskm"], writes=["DT%d" % (h % 2)])
                    stn = "STb%d" % (h % 2)
                    op("dve", lambda e, h=h: e.tensor_mul(out=STb[:, h % 2, :], in0=psc, in1=DTm[:, h % 2, :]), reads=["P5a", "DT%d" % (h % 2)], writes=[stn])
                    pnum = PB[6][:, 0:257]
                    op("pe", lambda e, h=h: e.matmul(pnum, lhsT=STb[:, h % 2, :], rhs=vext[:, pr, h, :], start=True, stop=True), reads=[stn, "vext"], writes=["P6a", "P6b"])
                    op("act", lambda e: e.copy(out=tnum, in_=pnum), reads=["P6a", "P6b"], writes=["tnum"])
                    op("dve", lambda e, h=h: e.tensor_scalar(out=kw[:, h % 2, :], in0=ktok[:, pr, h * 256:(h + 1) * 256], scalar1=gcol[:, pr, 12 + h:13 + h], scalar2=None, op0=ALU.mult),
                       reads=["ktok", "gcol"], writes=["kw%d" % (h % 2)])
                    cn = "C%d" % h
                    cbn = "Cb%d" % h
                    for eo in range(2):
                        pint = PB[eo][:, 0:257]
                        pintn = ["P%da" % eo, "P%db" % eo]
                        rsl = slice(eo * 64, (eo + 1) * 64)
                        for j in range(2):
                            op("pe", lambda e, h=h, j=j, pint=pint: e.matmul(pint, lhsT=qT[:, 2 * h + j, pr * 128:(pr + 1) * 128], rhs=Cbf[:, h, j, :], start=(j == 0), stop=(j == 1)), reads=["qT", cbn], writes=pintn)
                        op("dve", lambda e, h=h, rsl=rsl, pint=pint: e.scalar_tensor_tensor(out=tot[rsl, h, :], in0=pint[rsl, :], scalar=gcol[rsl, pr, 4 + h:5 + h], in1=tnum[rsl, :], op0=ALU.mult, op1=ALU.add),
                           reads=pintn + ["gcol", "tnum"], writes=["T4"])
                        for j in range(2):
                            pC = PB[3 + j][:, 0:257]
                            pCn = ["P3" if j == 0 else "P4"]
                            op("pe", lambda e, h=h, j=j, rsl=rsl, pC=pC: e.matmul(pC, lhsT=kw[rsl, h % 2, j * 128:(j + 1) * 128], rhs=vext[rsl, pr, h, :], start=True, stop=True), reads=["kw%d" % (h % 2), "vext"], writes=pCn)
                            op("dve", lambda e, h=h, j=j, eo=eo, pC=pC: e.scalar_tensor_tensor(out=Cst[:, h, j, :], in0=Cst[:, h, j, :], scalar=decb[:, pr, eo, h:h + 1], in1=pC, op0=ALU.mult, op1=ALU.add),
                               reads=pCn + ["decb", cn], writes=[cn])
                            op("act", lambda e, h=h, j=j: e.copy(out=Cbf[:, h, j, :], in_=Cst[:, h, j, :]), reads=[cn], writes=[cbn])
                den = tot[:, :, 256]
                op("dve", lambda e: e.tensor_scalar_mul(out=sm[:, 16:20], in0=den, scalar1=-1.0), reads=["T4"], writes=["sm"])
                op("dve", lambda e: e.tensor_max(out=sm[:, 4:8], in0=sm[:, 16:20], in1=den), reads=["T4", "sm"], writes=["sm"])
                op("dve", lambda e: e.tensor_max(out=sm[:, 4:8], in0=sm[:, 4:8], in1=gcol[:, pr, 8:12]), reads=["sm", "gcol"], writes=["sm"])
                op("dve", lambda e: e.reciprocal(out=sm[:, 4:8], in_=sm[:, 4:8]), reads=["sm"], writes=["sm"])
                op("dve", lambda e: e.tensor_reduce(out=sm[:, 8:12], in_=tot[:, :, 0:256], axis=AX.X, op=ALU.add), reads=["T4"], writes=["sm"])
                op("act", lambda e: e.activation(out=sqm, in_=tot[:, :, 0:256], func=AF.Square), reads=["T4"], writes=["T3"])
                op("dve", lambda e: e.tensor_reduce(out=sm[:, 12:16], in_=sqm, axis=AX.X, op=ALU.add), reads=["T3"], writes=["sm"])
                op("dve", lambda e: e.tensor_scalar_mul(out=sm[:, 8:12], in0=sm[:, 8:12], scalar1=1.0 / 256), reads=["sm"], writes=["sm"])
                op("dve", lambda e: e.tensor_mul(out=sm[:, 16:20], in0=sm[:, 8:12], in1=sm[:, 8:12]), reads=["sm"], writes=["sm"])
                op("dve", lambda e: e.scalar_tensor_tensor(out=sm[:, 12:16], in0=sm[:, 12:16], scalar=1.0 / 256, in1=sm[:, 16:20], op0=ALU.mult, op1=ALU.subtract), reads=["sm"], writes=["sm"])
                op("dve", lambda e: e.tensor_mul(out=sm[:, 16:20], in0=sm[:, 4:8], in1=sm[:, 4:8]), reads=["sm"], writes=["sm"])
                op("dve", lambda e: e.tensor_mul(out=sm[:, 12:16], in0=sm[:, 12:16], in1=sm[:, 16:20]), reads=["sm"], writes=["sm"])
                op("dve", lambda e: e.tensor_scalar(out=sm[:, 12:16], in0=sm[:, 12:16], scalar1=0.0, scalar2=EPS, op0=ALU.max, op1=ALU.add), reads=["sm"], writes=["sm"])
                op("act", lambda e: e.activation(out=sm[:, 12:16], in_=sm[:, 12:16], func=AF.Ln), reads=["sm"], writes=["sm"])
                op("act", lambda e: e.activation(out=sm[:, 12:16], in_=sm[:, 12:16], func=AF.Exp, scale=-0.5), reads=["sm"], writes=["sm"])
                op("dve", lambda e: e.tensor_mul(out=sm[:, 20:24], in0=sm[:, 4:8], in1=sm[:, 12:16]), reads=["sm"], writes=["sm"])
                op("dve", lambda e: e.scalar_tensor_tensor(out=sm[:, 24:28], in0=sm[:, 8:12], scalar=-1.0, in1=sm[:, 20:24], op0=ALU.mult, op1=ALU.mult), reads=["sm"], writes=["sm"])
                for h in range(4):
                    op("dve", lambda e, h=h: e.tensor_scalar(out=hnb[:, h * 256:(h + 1) * 256], in0=tot[:, h, 0:256], scalar1=sm[:, 20 + h:21 + h], scalar2=sm[:, 24 + h:25 + h], op0=ALU.mult, op1=ALU.add),
                       reads=["T4", "sm"], writes=["hnb"])
                for dc in range(8):
                    pt, pn = p7((dc % 2) * 2 + 1, 128)
                    op("pe", lambda e, dc=dc, pt=pt: e.transpose(out=pt, in_=hnb[:, dc * 128:(dc + 1) * 128], identity=idb[:]), reads=["hnb", "idb"], writes=pn)
                    cs = slice(pr * 128, (pr + 1) * 128)
                    op("act", lambda e, dc=dc, cs=cs: e.activation(out=ybT[:, dc, cs], in_=xcT[:, dc, cs], func=AF.Copy, scale=vcol(l, V_SKIP)[:, dc:dc + 1]),
                       reads=["vecs", "xcT"], writes=["ybT"])
                    op("dve", lambda e, dc=dc, pt=pt, cs=cs: e.scalar_tensor_tensor(out=ybT[:, dc, cs], in0=pt, scalar=vcol(l, V_MGN)[:, dc:dc + 1], in1=ybT[:, dc, cs], op0=ALU.mult, op1=ALU.add),
                       reads=pn + ["vecs", "ybT"], writes=["ybT"])
                    op("dve", lambda e, dc=dc, cs=cs: e.tensor_mul(out=ybT[:, dc, cs], in0=ybT[:, dc, cs], in1=sob[:, dc, cs]), reads=["ybT", "sob"], writes=["ybT"])
            for g in range(4):
                wa, wan = load_wg(wpa_s[l, g], "wpa%d" % l)
                wb, wbn = load_wg(wpb_s[l, g], "wpb%d" % l)
                for half in range(2):
                    m = g * 2 + half
                    i = m % 2
                    fm_chunk(wa, wan, half, yaT, "yaT", lambda pv, pn, m=m, i=i: op("dve", lambda e: e.tensor_mul(out=m1[:, i, :], in0=pv, in1=sgA[:, m, :]), reads=pn + ["sgA"], writes=["T3"]))
                    fm_chunk(wb, wbn, half, ybT, "ybT", lambda pv, pn, m=m, i=i: op("dve", lambda e: e.tensor_mul(out=m2[:, i, :], in0=pv, in1=sgB[:, m, :]), reads=pn + ["sgB"], writes=["T4"]))
                    op("dve", lambda e, m=m, i=i: e.tensor_add(out=mixT[:, m, :], in0=m1[:, i, :], in1=m2[:, i, :]), reads=["T3", "T4"], writes=["mixT"])
            for s in range(2):
                ybanks = (PB[3], PB[4]) if s == 0 else (PB[5], PB[6])
                ybn = ["P3", "P4"] if s == 0 else ["P5a", "P5b", "P5c", "P5d", "P6a", "P6b"]
                for g in range(4):
                    w, wn = load_wg(wo_s[l, g], "wo%d" % l)
                    yv = ybanks[g // 2][:, (g % 2) * 256:(g % 2) * 256 + 256]
                    for kc in range(8):
                        op("pe", lambda e, kc=kc, yv=yv, w=w, s=s: e.matmul(yv, lhsT=mixT[:, kc, s * 128:(s + 1) * 128], rhs=w[:, kc, :], start=(kc == 0), stop=(kc == 7)),
                           reads=["mixT", wn], writes=ybn)
                residual_update(l, t0, s, [(ybanks[0][:, :], 0, 512), (ybanks[1][:, :], 512, 512)], ybn, xt, ysb, tmpx, junk, ssy, rsy, ggm, "ggm", ysbn="T1", tmpxn="T2", junkn="hnb")
            if DEBUG and l == 0 and ti == 0:
                for (nm, tl, dn) in (("hT", hT, "hT"), ("yaT", yaT, "yaT"), ("ybT", ybT, "ybT"), ("mixT", mixT, "mixT"), ("qT", qT, "qT"), ("kT", kT, "kT"), ("vT", vT, "vT"), ("xcT", xcT, "xcT"), ("KT", KT, "KT"), ("sga", sga, "sga")):
                    dd = nc.dram_tensor("dbg_" + nm, [128, 8, TT], BF16, kind="ExternalOutput").ap()
                    last_x_dma["dbg_" + nm] = S.dma("sp", dd, tl, reads=[dn], key="dbg_" + nm)
                dd = nc.dram_tensor("dbg_gcol", [128, 2, 16], F32, kind="ExternalOutput").ap()
                last_x_dma["dbg_gcol"] = S.dma("sp", dd, gcol, reads=["gcol"], key="dbg_gcol")
                dd = nc.dram_tensor("dbg_tot", [128, 4, 257], F32, kind="ExternalOutput").ap()
                last_x_dma["dbg_tot"] = S.dma("sp", dd, tot, reads=["T4"], key="dbg_tot")

    def ffn(l):
        AR.reset()
        S.barrier()
        A = AR.alloc
        moe = (l % 2 == 1)
        j = l // 2
        NG = 14 if moe else 11
        NFC = 2 * NG
        NE = 8 if moe else 1
        xt = A([128, 4, D])
        hT = A([128, 8, TF], BF16)
        junk = A([128, D], BF16); ss = A([128, 4]); rs = A([128, 4]); ssy = A([128, 2]); rsy = A([128, 2])
        aT = A([128, NFC, TF], BF16)
        w2t = [A([128, NFC, 512], BF16) for _ in range(2)]
        NW13 = 3 if moe else 4
        w13 = [A([128, 2, 8, 256], BF16) for _ in range(NW13)]
        if moe:
            xn = w2t[0].rearrange("p a b -> p (a b)")[:, 0:8192].bitcast(F32).rearrange("p (a b) -> p a b", b=D)
            hTf = w2t[1].rearrange("p a b -> p (a b)")[:, 0:8192].bitcast(F32).rearrange("p (a b) -> p a b", b=TF)
            xnn, hTfn = "w2_0", "w2_1"
        else:
            xn = A([128, 4, D], BF16)
            hTf = None
            xnn, hTfn = "xn", "hTf"
        yacc = A([128, 4, D])
        gt2 = [A([128, TF]) for _ in range(2)]
        tmpx = A([128, D])
        lg = A([128, 4, 8]); gates = A([128, 4, 8]); gm = A([128, 4, 8])
        mx1 = A([128, 4]); mx2 = A([128, 4]); den = A([128, 4])
        print("ffn arena words", AR.off)
        cnt = {"w13": 0, "w2": 0, "g": 0, "y": 0}
        for ti in range(n_ffn_tiles):
            t0 = ti * TF
            load_norm_transpose(l, False, t0, 4, hsc_f, hbi_f, "hsc_f", "hbi_f", xt, xn, hT, junk, ss, rs, hTf=hTf, xnn=xnn, hTfn=hTfn)
            if moe:
                for s in range(4):
                    pl = PB[5][:, 256 + s * 8:256 + s * 8 + 8]
                    for kc in range(8):
                        op("pe", lambda e, s=s, kc=kc, pl=pl: e.matmul(pl, lhsT=hTf[:, kc, s * 128:(s + 1) * 128], rhs=rtt[:, kc, :], start=(kc == 0), stop=(kc == 7)), reads=[hTfn, "rtt"], writes=["P5c"])
                op("dve", lambda e: e.tensor_copy(out=lg, in_=PB[5][:, 256:288].rearrange("p (s e) -> p s e", e=8)), reads=["P5c"], writes=["lg"])
                op("dve", lambda e: e.tensor_reduce(out=mx1, in_=lg, axis=AX.X, op=ALU.max), reads=["lg"], writes=["mx1"])
                op("dve", lambda e: e.tensor_tensor(out=gm, in0=lg, in1=bc(mx1.unsqueeze(2), [128, 4, 8]), op=ALU.is_equal), reads=["lg", "mx1"], writes=["gm"])
                op("dve", lambda e: e.scalar_tensor_tensor(out=gm, in0=gm, scalar=-1e30, in1=lg, op0=ALU.mult, op1=ALU.add), reads=["gm", "lg"], writes=["gm"])
                op("dve", lambda e: e.tensor_reduce(out=mx2, in_=gm, axis=AX.X, op=ALU.max), reads=["gm"], writes=["mx2"])
                op("dve", lambda e: e.tensor_tensor(out=gm, in0=lg, in1=bc(mx2.unsqueeze(2), [128, 4, 8]), op=ALU.is_ge), reads=["lg", "mx2", "gm"], writes=["gm"])
                op("dve", lambda e: e.tensor_sub(out=lg, in0=lg, in1=bc(mx1.unsqueeze(2), [128, 4, 8])), reads=["lg", "mx1"], writes=["lg"])
                op("act", lambda e: e.activation(out=lg, in_=lg, func=AF.Exp), reads=["lg"], writes=["lg"])
                op("dve", lambda e: e.tensor_sub(out=den, in0=mx2, in1=mx1), reads=["mx1", "mx2"], writes=["den"])
                op("act", lambda e: e.activation(out=den, in_=den, func=AF.Exp), reads=["den"], writes=["den"])
                op("dve", lambda e: e.tensor_scalar_add(out=den, in0=den, scalar1=1.0), reads=["den"], writes=["den"])
                op("dve", lambda e: e.reciprocal(out=den, in_=den), reads=["den"], writes=["den"])
                op("dve", lambda e: e.tensor_mul(out=gates, in0=lg, in1=gm), reads=["lg", "gm"], writes=["gates"])
                op("dve", lambda e: e.tensor_mul(out=gates, in0=gates, in1=bc(den.unsqueeze(2), [128, 4, 8])), reads=["gates", "den"], writes=["gates"])
            for ex in range(NE):
                for g in range(NG):
                    i = cnt["w13"] % NW13
                    cnt["w13"] += 1
                    wn = "w13_%d" % i
                    if moe:
                        S.dma("sp", w13[i], m13_s[j, ex, g], reads=["m13_%d_%d" % (l, ex)], writes=[wn], key=wn)
                    else:
                        S.dma("sp", w13[i], f13_s[j, g], reads=["f13_%d" % l], writes=[wn], key=wn)
                    for half in range(2):
                        fc = g * 2 + half
                        gi = cnt["g"] % 2
                        cnt["g"] += 1
                        pg_, pu_ = PB[gi], PB[2 + gi]
                        pgn = ["P%da" % gi, "P%db" % gi]
                        pun = ["P2a", "P2b", "P2c", "P2d"] if gi == 0 else ["P3"]
                        for kc in range(8):
                            op("pe", lambda e, kc=kc, half=half, w=w13[i], pg_=pg_: e.matmul(pg_[:, :], lhsT=w[:, 0, kc, half * 128:(half + 1) * 128], rhs=hT[:, kc, :], start=(kc == 0), stop=(kc == 7)), reads=[wn, "hT"], writes=pgn)
                        for kc in range(8):
                            op("pe", lambda e, kc=kc, half=half, w=w13[i], pu_=pu_: e.matmul(pu_[:, :], lhsT=w[:, 1, kc, half * 128:(half + 1) * 128], rhs=hT[:, kc, :], start=(kc == 0), stop=(kc == 7)), reads=[wn, "hT"], writes=pun)
                        gt = gt2[gi]
                        op("act", lambda e, pg_=pg_, gt=gt: e.activation(out=gt, in_=pg_[:, :], func=AF.Silu), reads=pgn, writes=["gt%d" % gi])
                        op("dve", lambda e, fc=fc, pu_=pu_, gt=gt: e.tensor_mul(out=aT[:, fc, :], in0=pu_[:, :], in1=gt), reads=pun + ["gt%d" % gi], writes=["aT"])
                for half in range(2):
                    wi = cnt["w2"] % 2
                    cnt["w2"] += 1
                    w2n = "w2_%d" % wi
                    if moe:
                        S.dma("sp", w2t[wi], m2_s[j, ex, half], reads=["m2_%d_%d" % (l, ex)], writes=[w2n], key=w2n)
                    else:
                        S.dma("sp", w2t[wi], f2_s[j, half], reads=["f2_%d" % l], writes=[w2n], key=w2n)
                    for s in range(4):
                        yi = cnt["y"] % 2
                        cnt["y"] += 1
                        py = PB[4 + yi]
                        pyn = ["P4"] if yi == 0 else ["P5a", "P5b", "P5c", "P5d"]
                        for fc in range(NFC):
                            op("pe", lambda e, fc=fc, s=s, py=py, w=w2t[wi]: e.matmul(py[:, :], lhsT=aT[:, fc, s * 128:(s + 1) * 128], rhs=w[:, fc, :], start=(fc == 0), stop=(fc == NFC - 1)), reads=["aT", w2n], writes=pyn)
                        ya = yacc[:, s, half * 512:(half + 1) * 512]
                        if not moe:
                            op("act", lambda e, py=py, ya=ya: e.copy(out=ya, in_=py[:, :]), reads=pyn, writes=["yacc"])
                        elif ex == 0:
                            op("dve", lambda e, py=py, ya=ya, s=s, ex=ex: e.tensor_scalar(out=ya, in0=py[:, :], scalar1=gates[:, s, ex:ex + 1], scalar2=None, op0=ALU.mult), reads=pyn + ["gates"], writes=["yacc"])
                        else:
                            op("dve", lambda e, py=py, ya=ya, s=s, ex=ex: e.scalar_tensor_tensor(out=ya, in0=py[:, :], scalar=gates[:, s, ex:ex + 1], in1=ya, op0=ALU.mult, op1=ALU.add), reads=pyn + ["gates", "yacc"], writes=["yacc"])
            for s in range(4):
                residual_update(l, t0, s, None, None, xt, None, tmpx, junk, ssy, rsy, ggf, "ggf", yacc=yacc[:, s, :])

    for l in range(n_layers):
        layer_prologue(l)
        mixer(l)
        if do_ffn:
            ffn(l)
    if S.limit is not None:
        S.emit(final_waits=[S.ops[e][-1] for e in S.ENGS if S.ops[e]])
    else:
        S.emit(final_waits=list(last_x_dma.values()))
    S.close()
    return nc


def prep_inputs(inputs):
    f = lambda a: np.ascontiguousarray(np.asarray(a, dtype=np.float32))
    col = lambda v: f(v).reshape(8, 128).T
    L = DEPTH
    cols = []
    for l in range(L):
        vs = [inputs["g_pre_mix"][l], inputs["g_pre_ffn"][l], inputs["hgrn_gnorm"][l],
              inputs["mlstm_conv_w"][l][0], inputs["mlstm_conv_w"][l][1], inputs["mlstm_conv_w"][l][2], inputs["mlstm_conv_w"][l][3],
              inputs["mlstm_conv_b"][l], inputs["mlstm_gnorm"][l], inputs["mlstm_skip"][l]]
        for v in vs:
            cols.append(col(v))
    for l in range(L):
        cols.append(col(inputs["hgrn_lb"][l]))
    vecs = f(np.concatenate(cols, axis=1))
    bd = np.zeros((L, 128, 3, 8, 128), np.float32)
    for mi, nm in enumerate(("mlstm_wq", "mlstm_wk", "mlstm_wv")):
        w = f(inputs[nm]).reshape(L, 8, 32, 4, 4)
        for n in range(32):
            bd[:, 4 * n:4 * n + 4, mi, :, 4 * n:4 * n + 4] = w[:, :, n].transpose(0, 2, 1, 3)
    bd = f(bd.reshape(L, 128, 3 * 8 * 128))
    wg = np.concatenate([f(inputs["mlstm_w_ig"]), f(inputs["mlstm_w_fg"])], axis=2)
    wg = f(wg.reshape(L, 24, 128, 8).transpose(0, 2, 1, 3).reshape(L, 128, 192))
    bg = f(np.concatenate([f(inputs["mlstm_b_ig"]), f(inputs["mlstm_b_fg"])], axis=1))
    rt = f(f(inputs["moe_router"]).reshape(2, 8, 128, 8).transpose(0, 2, 1, 3).reshape(2, 128, 64))
    shared = {
        "vecs": vecs, "w_ada": f(inputs["w_ada"]), "b_ada": f(inputs["b_ada"]),
        "g_post_mix": f(inputs["g_post_mix"]), "g_post_ffn": f(inputs["g_post_ffn"]),
        "w_in": f(inputs["w_in"]), "bd": bd, "wgate": wg, "bgate": bg,
        "w_proj_a": f(inputs["w_proj_a"]), "w_proj_b": f(inputs["w_proj_b"]), "w_out": f(inputs["w_out"]),
        "ffn_w1": f(inputs["ffn_w1"]), "ffn_w3": f(inputs["ffn_w3"]), "ffn_w2": f(inputs["ffn_w2"]),
        "router": rt, "moe_w1": f(inputs["moe_w1"]), "moe_w3": f(inputs["moe_w3"]), "moe_w2": f(inputs["moe_w2"]),
    }
    x = f(inputs["x"])
    c = f(inputs["c"])
    maps = []
    for b in range(x.shape[0]):
        m = dict(shared)
        m["x"] = x[b]
        m["ccol"] = f(c[b].reshape(8, 128).T)
        maps.append(m)
    return maps


_NC_CACHE = {}


def kernel(**inputs):
    maps = prep_inputs(inputs)
    if "nc" not in _NC_CACHE:
        _NC_CACHE["nc"] = build_program()
    nc = _NC_CACHE["nc"]
    res = run_bass_kernel_spmd(nc, maps, core_ids=list(range(NCORES)))
    return np.stack([np.asarray(r["out"], dtype=np.float32) for r in res.results], axis=0)
```
